# Optimizing a Trainium2 kernel written in Bass

```python
import math
import jax
import jax.numpy as jnp
from jax import lax
import numpy as np


D_MODEL = 1024
BATCH = 16
SEQ = 2048
DEPTH = 2

GRID_W = 64
CTX_LEN = 256
N_MIXERS = 2
NORM_EPS = 1e-6

RW_HEAD = 64
RW_HEADS = D_MODEL // RW_HEAD
DECAY_LORA = 64
AAA_LORA = 64
GATE_LORA = 128
LNX_EPS = 64e-5

DA_HEADS = 8
DA_HEAD = D_MODEL // (2 * DA_HEADS)
QBLOCK = 128
ROPE_BASE = 10000.0
ROPE_FREQS = DA_HEAD // 4

N_GROUPS = 4
EXPERTS_PER_GROUP = 8
TOP_K_IN_GROUP = 2
D_EXPERT = D_MODEL // 4

kernel_name = 'hybrid_rwkv7_diffattn_hmoe_prefix_dit'


def rmsnorm(x, g, eps=NORM_EPS):
    xf = x.astype(jnp.float32)
    y = xf * lax.rsqrt(jnp.mean(xf * xf, axis=-1, keepdims=True) + eps)
    return y.astype(x.dtype) * g


def modulate(x, g, shift, scale):
    return rmsnorm(x, g) * (1 + scale) + shift


def centred_delta(h):
    hp = jnp.pad(h, ((0, 0), (1, 1), (0, 0)))
    return 0.5 * (hp[:, :-2] + hp[:, 2:]) - h


def split_heads(z):
    return z.reshape(z.shape[:-1] + (RW_HEADS, RW_HEAD))


def rwkv_features(h, mu, w_rkv, w0, w1, w2, a0, a1, a2, g1, g2, k_k, k_a):
    dx = centred_delta(h)
    mix = lambda j: h + dx * mu[j]
    r, k, v = jnp.einsum('jbtd,jde->jbte', jnp.stack([mix(0), mix(1), mix(2)]), w_rkv)
    xw, xa, xg = mix(3), mix(4), mix(5)
    w_lora = jnp.einsum('zbtr,zre->zbte', jnp.tanh(jnp.einsum('btd,zdr->zbtr', xw, w1)), w2)
    decay = jnp.exp(-jnp.exp(-jax.nn.softplus(-(w0[:, None, None, :] + w_lora)) - 0.5))
    a = jax.nn.sigmoid(a0[:, None, None, :]
                       + jnp.einsum('zbtr,zre->zbte', jnp.einsum('btd,zdr->zbtr', xa, a1), a2))
    g = jax.nn.sigmoid(xg @ g1) @ g2
    kkf = split_heads(k * k_k).astype(jnp.float32)
    kk = (kkf * lax.rsqrt(jnp.sum(kkf * kkf, axis=-1, keepdims=True) + 1e-12)).astype(k.dtype)
    k_dir = k[None] * (1 + (a - 1) * k_a)
    return (split_heads(r), split_heads(decay), split_heads(k_dir), split_heads(v), kk,
            split_heads(a), g)


def dir_time_major(z):
    z = jnp.stack([z[0], jnp.flip(z[1], axis=1)])
    return jnp.moveaxis(z, 2, 0).astype(jnp.float32)


def both_dirs(z):
    return dir_time_major(jnp.stack([z, z]))


def rwkv_scan(S0, r, decay, k_dir, v, kk, a):
    xs = (both_dirs(r), dir_time_major(decay), dir_time_major(k_dir), both_dirs(v),
          both_dirs(kk), dir_time_major(a))

    def step(S, inp):
        r_t, w_t, k_t, v_t, kk_t, a_t = inp
        sa = jnp.einsum('zbhij,zbhj->zbhi', S, -kk_t)
        S = (S * w_t[..., None, :] + sa[..., None] * (kk_t * a_t)[..., None, :]
             + v_t[..., None] * k_t[..., None, :])
        return S, jnp.einsum('zbhij,zbhj->zbhi', S, r_t)

    S_final, y = lax.scan(step, S0, xs)
    y = jnp.moveaxis(y, 0, 2)
    return S_final, y[0] + jnp.flip(y[1], axis=1)


def rwkv_output(y, r, k_dir, v, g, r_k, lnx_g, lnx_b, w_o):
    B, T, H, N = y.shape
    mean = jnp.mean(y, axis=-1, keepdims=True)
    var = jnp.mean(jnp.square(y - mean), axis=-1, keepdims=True)
    yn = ((y - mean) * lax.rsqrt(var + LNX_EPS)).reshape(B, T, H * N).astype(v.dtype) * lnx_g + lnx_b
    bonus = jnp.sum(r[None] * k_dir * r_k, axis=(0, 4))[..., None] * v
    return ((yn + bonus.reshape(B, T, H * N)) * g) @ w_o


def rwkv_mixer(hc, hl, need_ctx, mu, w_rkv, w0, w1, w2, a0, a1, a2, g1, g2, k_k, k_a, r_k,
               lnx_g, lnx_b, w_o):
    rc, wc, kc, vc, kkc, ac, gc = rwkv_features(hc, mu, w_rkv, w0, w1, w2, a0, a1, a2, g1, g2, k_k, k_a)
    rl, wl, kl, vl, kkl, al, gl = rwkv_features(hl, mu, w_rkv, w0, w1, w2, a0, a1, a2, g1, g2, k_k, k_a)
    S0 = jnp.zeros((2, hl.shape[0], RW_HEADS, RW_HEAD, RW_HEAD), jnp.float32)
    S_ctx, yc = rwkv_scan(S0, rc, wc, kc, vc, kkc, ac)
    _, yl = rwkv_scan(S_ctx, rl, wl, kl, vl, kkl, al)
    ol = rwkv_output(yl, rl, kl, vl, gl, r_k, lnx_g, lnx_b, w_o)
    oc = rwkv_output(yc, rc, kc, vc, gc, r_k, lnx_g, lnx_b, w_o) if need_ctx else None
    return oc, ol


def axial_rope_tables(L):
    rows = L // GRID_W
    row = jnp.repeat(jnp.arange(rows), GRID_W)
    col = jnp.tile(jnp.arange(GRID_W), rows)
    inv = ROPE_BASE ** (-jnp.arange(ROPE_FREQS, dtype=jnp.float32) / ROPE_FREQS)
    ang = jnp.stack([row, col], axis=-1).astype(jnp.float32)[:, :, None] * inv
    return jnp.cos(ang), jnp.sin(ang)


def apply_axial_rope(x, cos, sin):
    xs = x.reshape(x.shape[:-1] + (2, 2, ROPE_FREQS))
    x1, x2 = xs[..., 0, :], xs[..., 1, :]
    c = cos.astype(x.dtype)[:, None, None]
    s = sin.astype(x.dtype)[:, None, None]
    out = jnp.stack([x1 * c - x2 * s, x1 * s + x2 * c], axis=-2)
    return out.reshape(x.shape)


def diff_attend(q, k, v, lam):
    s = jnp.einsum('bqhmd,bkhmd->bhmqk', q, k).astype(jnp.float32) * (DA_HEAD ** -0.5)
    p = jax.nn.softmax(s, axis=-1)
    attn = (p[:, :, 0] - lam * p[:, :, 1]).astype(v.dtype)
    return jnp.einsum('bhqk,bkhe->bqhe', attn, v)


def diff_mixer(hc, hl, need_ctx, lam_init, w_qkv, q_norm_g, k_norm_g, lam_q1, lam_k1, lam_q2,
               lam_k2, subln_g, w_o):
    def project(h):
        B, T, _ = h.shape
        q, k, v = jnp.split(h @ w_qkv, 3, axis=-1)
        q = rmsnorm(q.reshape(B, T, DA_HEADS, 2, DA_HEAD), q_norm_g)
        k = rmsnorm(k.reshape(B, T, DA_HEADS, 2, DA_HEAD), k_norm_g)
        return q, k, v.reshape(B, T, DA_HEADS, 2 * DA_HEAD)

    def finish(o):
        B, T = o.shape[:2]
        return (rmsnorm(o, subln_g) * (1 - lam_init)).reshape(B, T, D_MODEL) @ w_o

    f32 = jnp.float32
    lam = (jnp.exp(jnp.sum(lam_q1.astype(f32) * lam_k1.astype(f32)))
           - jnp.exp(jnp.sum(lam_q2.astype(f32) * lam_k2.astype(f32))) + lam_init)
    qc, kc, vc = project(hc)
    ql, kl, vl = project(hl)
    B, L = hl.shape[:2]
    cos, sin = axial_rope_tables(L)
    ql = apply_axial_rope(ql, cos, sin)
    kl = apply_axial_rope(kl, cos, sin)
    k_all = jnp.concatenate([kc, kl], axis=1)
    v_all = jnp.concatenate([vc, vl], axis=1)
    nb = L // QBLOCK
    qb = jnp.moveaxis(ql.reshape(B, nb, QBLOCK, DA_HEADS, 2, DA_HEAD), 1, 0)
    ob = lax.map(lambda q: diff_attend(q, k_all, v_all, lam), qb)
    ol = finish(jnp.moveaxis(ob, 0, 1).reshape(B, L, DA_HEADS, 2 * DA_HEAD))
    oc = finish(diff_attend(qc, kc, vc, lam)) if need_ctx else None
    return oc, ol


def hier_moe(h, router_g, router_g_b, router_e, router_e_b, w_gate, w_up, w_down):
    pg = jax.nn.softmax((h @ router_g + router_g_b).astype(jnp.float32), axis=-1)
    gi = jnp.argmax(pg, axis=-1)
    gw = jnp.take_along_axis(pg, gi[:, None], axis=-1)
    el = (jnp.einsum('nd,gde->nge', h, router_e) + router_e_b).astype(jnp.float32)
    el = jnp.take_along_axis(el, gi[:, None, None], axis=1)[:, 0]
    tv, ti = lax.top_k(jax.nn.softmax(el, axis=-1), TOP_K_IN_GROUP)
    w_comb = gw * tv / jnp.sum(tv, axis=-1, keepdims=True)
    eid = gi[:, None] * EXPERTS_PER_GROUP + ti
    gates = jnp.sum(jax.nn.one_hot(eid, N_GROUPS * EXPERTS_PER_GROUP, dtype=jnp.float32)
                    * w_comb[..., None], axis=1)
    gates = gates.reshape(-1, N_GROUPS, EXPERTS_PER_GROUP).astype(h.dtype)
    y = jnp.zeros_like(h)
    for g in range(N_GROUPS):
        hid = (jax.nn.silu(jnp.einsum('nd,edf->nef', h, w_gate[g]))
               * jnp.einsum('nd,edf->nef', h, w_up[g]))
        y = y + jnp.einsum('nef,efd->nd', hid * gates[:, g, :, None], w_down[g])
    return y


def setup_inputs(seed: int = 0) -> dict:
    key = jax.random.key(seed)
    ks = iter(jax.random.split(key, 64))
    f32 = jnp.float32
    D = D_MODEL
    sd = D ** -0.5
    nr = (DEPTH + 1) // 2
    nd = DEPTH // 2

    def nrm(shape, scale=1.0):
        return scale * jax.random.normal(next(ks), shape, f32)

    def near(shape, base=1.0):
        return base + 0.05 * jax.random.normal(next(ks), shape, f32)

    G, E, F = N_GROUPS, EXPERTS_PER_GROUP, D_EXPERT
    return {
        'x': nrm((BATCH, SEQ, D)),
        'c': nrm((BATCH, D)),
        'ctx': nrm((BATCH, CTX_LEN, D)),
        'c_ctx': nrm((D,)),
        'norm1_g': near((DEPTH, D)),
        'norm2_g': near((DEPTH, D)),
        'w_mod': nrm((DEPTH, D, 6 * D), 0.5 * sd),
        'b_mod': nrm((DEPTH, 6 * D), 0.02),
        'rw_mu': jax.random.uniform(next(ks), (nr, 6, D), f32),
        'rw_w_rkv': nrm((nr, 3, D, D), sd),
        'rw_w0': jax.random.uniform(next(ks), (nr, 2, D), f32, -6.0, -0.5),
        'rw_w1': nrm((nr, 2, D, DECAY_LORA), sd),
        'rw_w2': nrm((nr, 2, DECAY_LORA, D), 0.1 * DECAY_LORA ** -0.5),
        'rw_a0': nrm((nr, 2, D), 0.1),
        'rw_a1': nrm((nr, 2, D, AAA_LORA), sd),
        'rw_a2': nrm((nr, 2, AAA_LORA, D), 0.1 * AAA_LORA ** -0.5),
        'rw_g1': nrm((nr, D, GATE_LORA), sd),
        'rw_g2': nrm((nr, GATE_LORA, D), GATE_LORA ** -0.5),
        'rw_k_k': near((nr, D), 0.85),
        'rw_k_a': near((nr, D)),
        'rw_r_k': nrm((nr, RW_HEADS, RW_HEAD), 0.1),
        'rw_lnx_g': near((nr, D)),
        'rw_lnx_b': nrm((nr, D), 0.02),
        'rw_w_o': nrm((nr, D, D), sd),
        'da_w_qkv': nrm((nd, D, 3 * D), sd),
        'da_q_norm_g': near((nd, DA_HEAD)),
        'da_k_norm_g': near((nd, DA_HEAD)),
        'da_lam_q1': nrm((nd, DA_HEAD), 0.1),
        'da_lam_k1': nrm((nd, DA_HEAD), 0.1),
        'da_lam_q2': nrm((nd, DA_HEAD), 0.1),
        'da_lam_k2': nrm((nd, DA_HEAD), 0.1),
        'da_subln_g': near((nd, 2 * DA_HEAD)),
        'da_w_o': nrm((nd, D, D), sd),
        'moe_router_g': nrm((DEPTH, D, G), sd),
        'moe_router_g_b': nrm((DEPTH, G), 0.01),
        'moe_router_e': nrm((DEPTH, G, D, E), sd),
        'moe_router_e_b': nrm((DEPTH, G, E), 0.01),
        'moe_w_gate': nrm((DEPTH, G, E, D, F), sd),
        'moe_w_up': nrm((DEPTH, G, E, D, F), sd),
        'moe_w_down': nrm((DEPTH, G, E, F, D), F ** -0.5),
    }


def reference(x, c, ctx, c_ctx, norm1_g, norm2_g, w_mod, b_mod,
              rw_mu, rw_w_rkv, rw_w0, rw_w1, rw_w2, rw_a0, rw_a1, rw_a2, rw_g1, rw_g2,
              rw_k_k, rw_k_a, rw_r_k, rw_lnx_g, rw_lnx_b, rw_w_o,
              da_w_qkv, da_q_norm_g, da_k_norm_g, da_lam_q1, da_lam_k1, da_lam_q2, da_lam_k2,
              da_subln_g, da_w_o,
              moe_router_g, moe_router_g_b, moe_router_e, moe_router_e_b,
              moe_w_gate, moe_w_up, moe_w_down):
    B, L, D = x.shape
    n_ctx = ctx.shape[1]
    for i in range(DEPTH):
        need_ctx = i < DEPTH - 1
        j = i // N_MIXERS
        mod = jnp.split((jax.nn.silu(c) @ w_mod[i] + b_mod[i])[:, None, :], 6, axis=-1)
        mod_c = jnp.split(jax.nn.silu(c_ctx) @ w_mod[i] + b_mod[i], 6, axis=-1)
        hl = modulate(x, norm1_g[i], mod[0], mod[1])
        hc = modulate(ctx, norm1_g[i], mod_c[0], mod_c[1])
        if i % N_MIXERS == 0:
            oc, ol = rwkv_mixer(hc, hl, need_ctx, rw_mu[j], rw_w_rkv[j], rw_w0[j], rw_w1[j], rw_w2[j],
                                rw_a0[j], rw_a1[j], rw_a2[j], rw_g1[j], rw_g2[j], rw_k_k[j],
                                rw_k_a[j], rw_r_k[j], rw_lnx_g[j], rw_lnx_b[j], rw_w_o[j])
        else:
            lam_init = 0.8 - 0.6 * math.exp(-0.3 * i)
            oc, ol = diff_mixer(hc, hl, need_ctx, lam_init, da_w_qkv[j], da_q_norm_g[j],
                                da_k_norm_g[j], da_lam_q1[j], da_lam_k1[j], da_lam_q2[j],
                                da_lam_k2[j], da_subln_g[j], da_w_o[j])
        x = x + mod[2] * ol
        hl = modulate(x, norm2_g[i], mod[3], mod[4])
        moe_args = (moe_router_g[i], moe_router_g_b[i], moe_router_e[i], moe_router_e_b[i],
                    moe_w_gate[i], moe_w_up[i], moe_w_down[i])
        if need_ctx:
            ctx = ctx + mod_c[2] * oc
            hc = modulate(ctx, norm2_g[i], mod_c[3], mod_c[4])
            f = hier_moe(jnp.concatenate([hc.reshape(-1, D), hl.reshape(-1, D)], axis=0), *moe_args)
            ctx = ctx + mod_c[5] * f[:B * n_ctx].reshape(ctx.shape)
            x = x + mod[5] * f[B * n_ctx:].reshape(x.shape)
        else:
            x = x + mod[5] * hier_moe(hl.reshape(-1, D), *moe_args).reshape(x.shape)
    return x
```

```python
import numpy as np
import concourse.bass as bass
import concourse.mybir as mybir

F32 = mybir.dt.float32
BF16 = mybir.dt.bfloat16
I32 = mybir.dt.int32
U32 = mybir.dt.uint32
AF = mybir.ActivationFunctionType
ALU = mybir.AluOpType
AX = mybir.AxisListType

ENGS = ("pe", "dve", "act", "pool", "sp")
SEM_LIMIT = 30000
N_DMA_SEMS = 24


class V:
    __slots__ = ("ap", "toks", "excl")

    def __init__(self, ap, toks, excl=False):
        self.ap = ap
        self.toks = toks
        self.excl = excl


class Tl:
    def __init__(self, S, t, name, excl=False, toks=None):
        self.S = S
        self.t = t
        self.name = name
        self.excl = excl
        self.toks = toks

    def _tk(self, key):
        if self.toks is not None:
            return list(self.toks)
        return [(self.name, None if self.excl else key)]

    def __getitem__(self, idx):
        return V(self.t[idx], self._tk(None), self.excl)

    def k(self, key, idx=None):
        ap = self.t[idx] if idx is not None else None
        return V(ap, self._tk(key), self.excl)

    def v(self, ap, key=None):
        return V(ap, self._tk(key), self.excl)


class Sched:
    def __init__(self, nc, same_engine_sync=True):
        self.nc = nc
        self.q = {e: [] for e in ENGS}
        self.cnt = {e: 0 for e in ENGS}
        self.semi = {e: 0 for e in ENGS}
        self.nsem = {e: 1 for e in ENGS}
        self.state = {}
        self.waited = {e: {} for e in ENGS}
        self.same = same_engine_sync
        self.dma_i = 0
        self.dma_cnt = [0] * N_DMA_SEMS
        self.dma_last = [None] * N_DMA_SEMS
        self.ctx = []
        self.n_instr = 0
        self.out_deps = []

    def sbuf(self, name, shape, dtype=F32):
        cm = self.nc.sbuf_tensor(name, list(shape), dtype)
        t = cm.__enter__()
        self.ctx.append(cm)
        return Tl(self, t, name)

    def psum(self, name, shape, dtype=F32):
        cm = self.nc.psum_tensor(name, list(shape), dtype)
        t = cm.__enter__()
        self.ctx.append(cm)
        return Tl(self, t, name, excl=True)

    def dram(self, name, shape, dtype=F32, kind="Internal"):
        t = self.nc.dram_tensor(name, list(shape), dtype, kind=kind)
        return Tl(self, t.ap(), name)

    def push_scope(self):
        self.scopes = getattr(self, "scopes", [])
        self.scopes.append(len(self.ctx))

    def pop_scope(self):
        self.barrier()
        n = self.scopes.pop()
        while len(self.ctx) > n:
            self.ctx.pop().__exit__(None, None, None)

    def barrier(self):
        deps = []
        for e in ENGS:
            if self.cnt[e] > 0:
                deps.append(((e, self.semi[e]), self.cnt[e], e))
        for i in range(N_DMA_SEMS):
            if self.dma_last[i] is not None:
                deps.append(self.dma_last[i])
        for e in ENGS:
            d = [x for x in deps if x[2] != e]
            w = self._waits(e, d)
            if w:
                self.q[e].append(("wait", None, w, None))

    def _deps(self, reads, writes):
        deps = []
        for v in reads:
            for tok in v.toks:
                st = self.state.get(tok)
                if st and st[0] is not None:
                    deps.append(st[0])
        for v in writes:
            for tok in v.toks:
                st = self.state.get(tok)
                if st:
                    if st[0] is not None:
                        deps.append(st[0])
                    deps.extend(st[1])
        return deps

    def _update(self, reads, writes, me, real_writes=None):
        for v in reads:
            for tok in v.toks:
                st = self.state.setdefault(tok, [None, [], None])
                st[1].append(me)
                if len(st[1]) > 64:
                    best = {}
                    for d in st[1]:
                        if d[0] not in best or best[d[0]][1] < d[1]:
                            best[d[0]] = d
                    st[1] = list(best.values())
        rw = writes if real_writes is None else real_writes
        rwt = set()
        for v in rw:
            rwt.update(v.toks)
        for v in writes:
            for tok in v.toks:
                old = self.state.get(tok)
                lrw = me if tok in rwt else (old[2] if old else None)
                self.state[tok] = [me, [], lrw]

    def _waits(self, eng, deps, raw_toks_same=None):
        need = {}
        for d in deps:
            semkey, val, deng, is_raw = d[0], d[1], d[2], True
            if deng == eng and not self.same:
                continue
            w = self.waited[eng].get(semkey, 0)
            if val > w and val > need.get(semkey, 0):
                need[semkey] = val
        for sk, val in need.items():
            self.waited[eng][sk] = val
        return list(need.items())

    def op(self, eng, fn, reads=(), writes=(), same_ok=False):
        reads = list(reads)
        real_writes = list(writes)
        writes = real_writes + [v for v in reads if v.excl]
        deps = self._deps(reads, writes)
        if same_ok or eng == "pe":
            deps = [d for d in deps if d[2] != eng]
        elif self.same:
            rd = []
            for v in reads:
                for tok in v.toks:
                    st = self.state.get(tok)
                    if st and st[2] is not None and st[2][2] == eng:
                        rd.append(st[2])
            deps = [d for d in deps if d[2] != eng] + rd
        waits = self._waits(eng, deps)
        if self.cnt[eng] >= SEM_LIMIT:
            self.semi[eng] += 1
            self.nsem[eng] = max(self.nsem[eng], self.semi[eng] + 1)
            self.cnt[eng] = 0
        self.cnt[eng] += 1
        me = ((eng, self.semi[eng]), self.cnt[eng], eng)
        self.q[eng].append(("op", fn, waits, me[0]))
        self._update(reads, writes, me, real_writes)
        self.n_instr += 1
        return me

    def dma(self, queue, out, in_, **kw):
        reads, writes = [in_], [out]
        deps = self._deps(reads, writes)
        i = self.dma_i % N_DMA_SEMS
        self.dma_i += 1
        if self.dma_last[i] is not None:
            deps.append(self.dma_last[i])
        waits = self._waits(queue, deps)
        oa, ia = out.ap, in_.ap
        parts = [(oa, ia)]
        try:
            osh, ish = list(oa.shape), list(ia.shape)
            nbytes = 1
            for d_ in osh:
                nbytes *= int(d_)
            nbytes *= 2 if oa.tensor.dtype == BF16 else 4
            bc = any(int(st_) == 0 for st_, _n in list(ia.ap)[:1])
            if osh[0] == 128 and ish[0] == 128 and not bc and nbytes >= 256 * 1024:
                nsp = 8 if nbytes >= 1024 * 1024 else 4
                step = 128 // nsp
                parts = [(oa[k * step:(k + 1) * step], ia[k * step:(k + 1) * step]) for k in range(nsp)]
        except Exception as ex:
            self.split_err = getattr(self, "split_err", 0) + 1
            self.split_ex = repr(ex)
            parts = [(oa, ia)]
        for k, (o_, a_) in enumerate(parts):
            self.dma_cnt[i] += 16
            self.q[queue].append(("dma", (o_, a_, kw), waits if k == 0 else [], ("dma", i)))
            self.n_instr += 1
        me = (("dma", i), self.dma_cnt[i], "dma")
        self.dma_last[i] = me
        self._update(reads, writes, me)
        return me

    def emit(self, final_deps=None):
        nc = self.nc
        sems = {}
        cms = []

        def mk(name):
            cm = nc.semaphore(name)
            s = cm.__enter__()
            cms.append(cm)
            return s
        for e in ENGS:
            for i in range(self.nsem[e]):
                sems[(e, i)] = mk(f"s_{e}_{i}")
        for i in range(N_DMA_SEMS):
            sems[("dma", i)] = mk(f"s_dma_{i}")
        engobj = {"pe": "tensor", "dve": "vector", "act": "scalar", "pool": "gpsimd", "sp": "sync"}
        if final_deps:
            w = self._waits("sp", final_deps)
            self.q["sp"].append(("wait", None, w, None))
        with nc.Block() as block:
            for e in ENGS:
                items = self.q[e]
                if not items:
                    continue

                def body(eng, items=items):
                    for kind, fn, waits, semkey in items:
                        for sk, val in waits:
                            eng.wait_ge(sems[sk], val)
                        if kind == "op":
                            ins = fn(eng)
                            ins.then_inc(sems[semkey], 1)
                        elif kind == "dma":
                            oa, ia, kw = fn
                            eng.dma_start(out=oa, in_=ia, **kw).then_inc(sems[semkey], 16)
                getattr(block, engobj[e])(body)
        for cm in reversed(cms):
            cm.__exit__(None, None, None)
        for cm in reversed(self.ctx):
            cm.__exit__(None, None, None)

    def mm(self, out, lhsT, rhs, start=True, stop=True, **kw):
        return self.op("pe", lambda e: e.matmul(out.ap, lhsT.ap, rhs.ap, start=start, stop=stop, **kw),
                       reads=[lhsT, rhs] + ([] if start else [out]), writes=[out])

    def tr(self, out, in_, ident):
        return self.op("pe", lambda e: e.transpose(out.ap, in_.ap, ident.ap),
                       reads=[in_, ident], writes=[out])

    def act(self, out, in_, func, bias=None, scale=None, accum_out=None, eng="act"):
        reads = [in_]
        kw = {}
        if isinstance(bias, V):
            reads.append(bias)
            kw["bias"] = bias.ap
        elif bias is not None:
            kw["bias"] = bias
        if isinstance(scale, V):
            reads.append(scale)
            kw["scale"] = scale.ap
        elif scale is not None:
            kw["scale"] = scale
        writes = [out]
        if accum_out is not None:
            writes.append(accum_out)
            kw["accum_out"] = accum_out.ap
        return self.op("act", lambda e: e.activation(out.ap, in_.ap, func, **kw), reads=reads, writes=writes)

    def tt(self, eng, out, in0, in1, op):
        return self.op(eng, lambda e: e.tensor_tensor(out.ap, in0.ap, in1.ap, op), reads=[in0, in1], writes=[out])

    def ts(self, eng, out, in0, s1, s2=None, op0=ALU.mult, op1=None, accum_out=None):
        reads = [in0]
        a1 = s1.ap if isinstance(s1, V) else s1
        a2 = s2.ap if isinstance(s2, V) else s2
        if isinstance(s1, V):
            reads.append(s1)
        if isinstance(s2, V):
            reads.append(s2)
        writes = [out]
        kw = {}
        if op1 is not None:
            kw["op1"] = op1
        if accum_out is not None:
            kw["accum_out"] = accum_out.ap
            writes.append(accum_out)
        return self.op(eng, lambda e: e.tensor_scalar(out.ap, in0.ap, a1, a2, op0, **kw), reads=reads, writes=writes)

    def stt(self, out, in0, scalar, in1, op0, op1, eng="dve"):
        reads = [in0, in1]
        a = scalar.ap if isinstance(scalar, V) else scalar
        if isinstance(scalar, V):
            reads.append(scalar)
        return self.op(eng, lambda e: e.scalar_tensor_tensor(out.ap, in0.ap, a, in1.ap, op0, op1), reads=reads, writes=[out])

    def copy(self, eng, out, in_):
        if eng == "act":
            return self.op("act", lambda e: e.copy(out.ap, in_.ap), reads=[in_], writes=[out])
        return self.op(eng, lambda e: e.tensor_copy(out.ap, in_.ap), reads=[in_], writes=[out])

    def memset(self, eng, out, val):
        return self.op(eng, lambda e: e.memset(out.ap, val), reads=[], writes=[out])

    def reduce(self, out, in_, op=ALU.add, axis=AX.X, eng="dve"):
        return self.op(eng, lambda e: e.tensor_reduce(out.ap, in_.ap, axis, op), reads=[in_], writes=[out])

    def recip(self, out, in_):
        return self.op("dve", lambda e: e.reciprocal(out.ap, in_.ap), reads=[in_], writes=[out])
D = 1024
NB = 2
LAT = 2048
CTX = 256
TOK = CTX + LAT
NT = TOK // 128
EPS = 1e-6
DECAY_C = -0.6065306597126334
LNX_EPS = 64e-5
NGRP = 4
NEXP = 32
DFF = 256

PV_N1 = (0, 16)
PV_N2 = (16, 32)
PV_MU = 32
PV_BMOD = 80
PV_ROWS = 176


def host_consts():
    c = {}
    c["ident_f"] = np.eye(128, dtype=np.float32)
    c["ones_f"] = np.ones((128, 128), dtype=np.float32)
    idx = np.arange(128)
    cs, ct = idx[:, None] // 64, idx[None, :] // 64
    same = (cs == ct)
    s_, t_ = idx[:, None], idx[None, :]
    maskA = np.zeros((2, 128, 256), np.float32)
    maskAT = np.zeros((2, 128, 128), np.float32)
    tri = np.zeros((2, 128, 384), np.float32)
    for z in range(2):
        prec = same & ((s_ < t_) if z == 0 else (s_ > t_))
        preceq = same & ((s_ <= t_) if z == 0 else (s_ >= t_))
        succ = same & ((s_ > t_) if z == 0 else (s_ < t_))
        maskA[z, :, 0:128] = prec
        maskA[z, :, 128:256] = preceq
        maskAT[z] = prec.T
        tri[z, :, 0:128] = preceq * DECAY_C
        tri[z, :, 128:256] = prec * DECAY_C
        tri[z, :, 256:384] = succ * DECAY_C
    c["maskA"] = maskA
    c["maskAT"] = maskAT
    c["tri"] = tri
    ind = np.zeros((128, 2), np.float32)
    ind[0:64, 0] = DECAY_C
    ind[64:128, 1] = DECAY_C
    c["ind"] = ind
    rows = LAT // 64
    row = np.repeat(np.arange(rows), 64)
    col = np.tile(np.arange(64), rows)
    inv = (10000.0 ** (-np.arange(16, dtype=np.float32) / 16)).astype(np.float32)
    ang = np.stack([row, col], axis=-1).astype(np.float32)[:, :, None] * inv
    c["rope_cos"] = np.cos(ang).astype(np.float32).reshape(LAT, 32)
    c["rope_sin"] = np.sin(ang).astype(np.float32).reshape(LAT, 32)
    sel = np.zeros((32, 32, 128), np.float32)
    for e in range(32):
        sel[e, e, :] = 1.0
    c["sel"] = sel.reshape(32, 32 * 128)
    return c


class Ctx:
    pass


def std_psum(S, G, tag):
    G.psA = S.psum(f"psA{tag}", [128, 1024], F32)
    nm = G.psA.name
    G.psA.toks = [(nm, 0), (nm, 1)]
    G.psA_h = [Tl(S, G.psA.t[:, i * 512:(i + 1) * 512].rearrange("p (a b) -> p a b", b=128), nm, excl=True,
                  toks=[(nm, i)]) for i in range(2)]
    G.psY = S.psum(f"psY{tag}", [128, 1024], F32)
    G.psT = S.psum(f"psT{tag}", [128, 1024], BF16)
    G.psH = [S.psum(f"psH{i}{tag}", [128, 4, 128], F32) for i in range(3)]


def setup_common(S, G, need=("rwkv", "da", "moe")):
    def I(name, shape, dt=F32):
        grp = name.split('_')[0]
        if grp in ('rw', 'da', 'moe') and {'rw': 'rwkv', 'da': 'da', 'moe': 'moe'}[grp] not in need:
            return None
        return S.dram(name, shape, dt, kind="ExternalInput")
    G.x = I("x", [NB, LAT, D])
    G.ctx = I("ctx", [NB, CTX, D])
    G.c3 = I("c3", [3, D])
    G.pvec = I("pvec", [PV_ROWS, 128])
    G.w_mod = I("w_mod", [2, D, 6 * D])
    G.b_mod = I("b_mod", [2, 6 * D])
    G.ident_f_d = I("ident_f", [128, 128])
    G.ones_f_d = I("ones_f", [128, 128])
    G.maskA_d = I("maskA", [2, 128, 256])
    G.maskAT_d = I("maskAT", [2, 128, 128])
    G.tri_d = I("tri", [2, 128, 384])
    G.ind_d = I("ind", [128, 2])
    G.rope_cos_d = I("rope_cos", [LAT, 32])
    G.rope_sin_d = I("rope_sin", [LAT, 32])
    G.sel_d = I("sel", [32, 32 * 128])
    G.rw_w_rkv = I("rw_w_rkv", [3, D, D])
    G.rw_w0 = I("rw_w0", [2, D])
    G.rw_w1 = I("rw_w1", [2, D, 64])
    G.rw_w2 = I("rw_w2", [2, 64, D])
    G.rw_a0 = I("rw_a0", [2, D])
    G.rw_a1 = I("rw_a1", [2, D, 64])
    G.rw_a2 = I("rw_a2", [2, 64, D])
    G.rw_g1 = I("rw_g1", [D, 128])
    G.rw_g2 = I("rw_g2", [128, D])
    G.rw_k_k = I("rw_k_k", [1, D])
    G.rw_k_a = I("rw_k_a", [1, D])
    G.rw_r_k = I("rw_r_k", [1, D])
    G.rw_lnx_g = I("rw_lnx_g", [1, D])
    G.rw_lnx_b = I("rw_lnx_b", [1, D])
    G.rw_w_o = I("rw_w_o", [D, D])
    G.da_w_qkv = I("da_w_qkv", [D, 3 * D])
    G.da_q_norm_g = I("da_q_norm_g", [1, 64])
    G.da_k_norm_g = I("da_k_norm_g", [1, 64])
    G.da_lam = I("da_lam", [4, 64])
    G.da_subln_g = I("da_subln_g", [1, 128])
    G.da_w_o = I("da_w_o", [D, D])
    G.moe_router = I("moe_router", [2, D, 36])
    G.moe_router_b = I("moe_router_b", [2, 1, 36])
    G.moe_w_gate = I("moe_w_gate", [2, NEXP, D, DFF])
    G.moe_w_up = I("moe_w_up", [2, NEXP, D, DFF])
    G.moe_w_down = I("moe_w_down", [2, NEXP, DFF, D])

    G.ident_f = S.sbuf("ident_f_s", [128, 128], F32)
    G.ident_b = S.sbuf("ident_b_s", [128, 128], BF16)
    G.ones_f = S.sbuf("ones_f_s", [128, 128], F32)
    S.dma("sp", G.ident_f[:], G.ident_f_d[:])
    S.dma("pool", G.ident_b[:], G.ident_f_d[:])
    S.dma("sp", G.ones_f[:], G.ones_f_d[:])
    G.pfm = S.sbuf("pfm", [128, PV_ROWS], F32)
    S.push_scope()
    std_psum(S, G, "c")
    pv = S.sbuf("pv_ld", [128, 2, 128], F32)
    S.memset("dve", pv[:], 0.0)
    S.dma("sp", pv[:, 0, :], G.pvec[0:128, :])
    S.dma("sp", pv[0:PV_ROWS - 128, 1, :], G.pvec[128:PV_ROWS, :])
    S.tr(G.psA[:, 0:128], pv[:, 0, :], G.ident_f[:])
    S.tr(G.psA[:, 128:256], pv[:, 1, :], G.ident_f[:])
    S.copy("dve", G.pfm[:], G.psA[:, 0:PV_ROWS])
    S.pop_scope()
    G.modT = S.sbuf("modT", [128, 48, 4], F32)
    G.A1 = S.sbuf("A1", [128, 3, 8], F32)
    G.A2 = S.sbuf("A2", [128, 3, 8], F32)
    G.gates_d = S.dram("gates_d", [2, 3, D], F32)


def mod_phase(S, G, li):
    S.push_scope()
    std_psum(S, G, f"m{li}")
    crow = S.sbuf(f"crow{li}", [4, D], F32)
    sc = S.sbuf(f"sc{li}", [4, D], F32)
    scT = S.sbuf(f"scT{li}", [128, 8, 4], F32)
    brow = S.sbuf(f"brow{li}", [1, 6 * D], F32)
    grow = S.sbuf(f"grow{li}", [4, 512], F32)
    wblk = [S.sbuf(f"wblk{i}_{li}", [128, 8, 512], F32) for i in range(2)]
    S.memset("dve", crow[:], 0.0)
    S.dma("sp", crow[0:3, :], G.c3[:])
    S.dma("sp", brow[:], G.b_mod[li:li + 1, :])
    S.act(sc[:], crow[:], AF.Silu)
    for kc in range(8):
        S.tr(G.psA[:, kc * 4:(kc + 1) * 4], sc[0:4, kc * 128:(kc + 1) * 128], G.ident_f[0:4, 0:4])
    S.copy("dve", scT[:], G.psA.v(G.psA.t[:, 0:32].rearrange("p (a b) -> p a b", b=4)))
    wsrc = G.w_mod.t[li].rearrange("(kc p) n -> p kc n", p=128)
    gate_blocks = {4: (0, 0), 5: (0, 1), 10: (1, 0), 11: (1, 1)}
    for blk in range(12):
        wb = wblk[blk % 2]
        S.dma("sp", wb[:], G.w_mod.v(wsrc[:, :, blk * 512:(blk + 1) * 512]))
        if blk in gate_blocks:
            which, half = gate_blocks[blk]
            ps = G.psY[0:4, 0:512]
            for kc in range(8):
                S.mm(ps, scT[:, kc, :], wb[:, kc, :], start=(kc == 0), stop=False)
            S.mm(ps, G.ones_f[0:1, 0:4], brow[0:1, blk * 512:(blk + 1) * 512], start=False, stop=True)
            S.copy("dve", grow[:], ps)
            S.dma("sp", G.gates_d.v(G.gates_d.t[which, :, half * 512:(half + 1) * 512]), grow[0:3, :])
        else:
            for ec in range(4):
                ch = blk * 4 + ec
                ps = G.psA[:, ch * 4:(ch + 1) * 4]
                for kc in range(8):
                    S.mm(ps, wb[:, kc, ec * 128:(ec + 1) * 128], scT[:, kc, :], start=(kc == 0), stop=(kc == 7))
                S.ts("dve", G.modT[:, ch, :], ps, G.pfm[:, PV_BMOD + li * 48 + ch:PV_BMOD + li * 48 + ch + 1], None, op0=ALU.add)
    for r in range(3):
        for (A, sc0, gofs) in ((G.A1, 8, PV_N1[0] + li * 8), (G.A2, 32, PV_N2[0] + li * 8)):
            S.ts("dve", A[:, r, :], G.modT[:, sc0:sc0 + 8, r], 1.0, None, op0=ALU.add)
            S.tt("dve", A[:, r, :], A[:, r, :], G.pfm[:, gofs:gofs + 8], ALU.mult)
    S.pop_scope()


def norm_tile_to_fm(S, G, xt, r, A, shift_ch0, out_fm, wk, fp32_out=None):
    st = wk["st"]
    S.act(wk["junk"][:], xt, AF.Square, accum_out=st[:, 0:1])
    S.ts("dve", st[:, 1:2], st[:, 0:1], 1.0 / D, EPS, op0=ALU.mult, op1=ALU.add)
    S.act(st[:, 2:3], st[:, 1:2], AF.Sqrt)
    S.recip(st[:, 3:4], st[:, 2:3])
    if fp32_out is None:
        xn = wk["xn"]
        S.ts("dve", xn[:], xt, st[:, 3:4], None, op0=ALU.mult)
        for kc in range(8):
            S.tr(G.psT[:, kc * 128:(kc + 1) * 128], xn[:, kc * 128:(kc + 1) * 128], G.ident_b[:])
        src = G.psT.v(G.psT.t[:, :].rearrange("p (a b) -> p a b", b=128))
    else:
        xn = wk["xn32"]
        S.ts("dve", xn[:], xt, st[:, 3:4], None, op0=ALU.mult)
        for kc in range(8):
            S.tr(G.psA[:, kc * 128:(kc + 1) * 128], xn[:, kc * 128:(kc + 1) * 128], G.ident_f[:])
        src = G.psA.v(G.psA.t[:, :].rearrange("p (a b) -> p a b", b=128))
    Abc = A.v(A.t[:, r, :].unsqueeze(2).to_broadcast([128, 8, 128]))
    shbc = G.modT.v(G.modT.t[:, shift_ch0:shift_ch0 + 8, r].unsqueeze(2).to_broadcast([128, 8, 128]))
    tmp = wk["fm32"]
    S.tt("dve", tmp[:], src, Abc, ALU.mult)
    if fp32_out is not None:
        S.tt("pool", fp32_out, tmp[:], shbc, ALU.add)
        S.copy("act", out_fm, fp32_out)
    else:
        S.tt("pool", out_fm, tmp[:], shbc, ALU.add)


def wcast_phase(S, G, need):
    items = []
    G.wbf = {}

    def add(name, src3, n):
        dst = S.dram("wbf_" + name, [128, n], BF16)
        G.wbf[name] = dst
        off = 0
        a, b = src3.shape[1], src3.shape[2]
        rows = max(1, 2048 // b)
        if b > 2048:
            for i in range(a):
                for c0 in range(0, b, 2048):
                    c1 = min(b, c0 + 2048)
                    items.append((src3[:, i:i + 1, c0:c1], dst, i * b + c0, c1 - c0, (1, c1 - c0)))
        else:
            for i in range(0, a, rows):
                i1 = min(a, i + rows)
                items.append((src3[:, i:i1, :], dst, i * b, (i1 - i) * b, (i1 - i, b)))

    if "rwkv" in need:
        for j, nm in enumerate(("Wr", "Wk", "Wv")):
            add(nm, G.rw_w_rkv.t[j].rearrange("(kc p) n -> p kc n", p=128), 8 * D)
        add("rwWo", G.rw_w_o.t.rearrange("(kc p) n -> p kc n", p=128), 8 * D)
    if "da" in need:
        add("Wqkv", G.da_w_qkv.t.rearrange("(kc p) n -> p kc n", p=128), 8 * 3 * D)
        add("daWo", G.da_w_o.t.rearrange("(kc p) n -> p kc n", p=128), 8 * D)
    if "moe" in need:
        for li in range(2):
            for e in range(NEXP):
                add(f"g{li}_{e}", G.moe_w_gate.t[li, e].rearrange("(kc p) n -> p kc n", p=128), 8 * DFF)
                add(f"u{li}_{e}", G.moe_w_up.t[li, e].rearrange("(kc p) n -> p kc n", p=128), 8 * DFF)
                add(f"d{li}_{e}", G.moe_w_down.t[li, e].rearrange("(fc p) n -> p fc n", p=128), 2 * D)
    S.push_scope()
    NBUF = 4
    stg = [S.sbuf(f"wc_stg{i}", [128, 2048], F32) for i in range(NBUF)]
    ob = [S.sbuf(f"wc_ob{i}", [128, 2048], BF16) for i in range(NBUF)]
    engs = ["dve", "pool", "dve"]

    def load(i):
        src3, dst, off, n, (a, b) = items[i]
        t = stg[i % NBUF]
        S.dma("sp", t.v(t.t[:, 0:n].rearrange("p (a b) -> p a b", b=b)), V(src3, [("wsrc", None)]))

    for i in range(min(NBUF - 1, len(items))):
        load(i)
    for i in range(len(items)):
        if i + NBUF - 1 < len(items):
            load(i + NBUF - 1)
        src3, dst, off, n, _ = items[i]
        S.copy(engs[i % 3], ob[i % NBUF][:, 0:n], stg[i % NBUF][:, 0:n])
        S.dma("act", dst.v(dst.t[:, off:off + n]), ob[i % NBUF][:, 0:n])
    S.pop_scope()

def rwkv_phase(S, G, x1_d, dbg=None, nb=NB, nt0=NT, do_dir=3, nt1=NT, nheads=16, fl=99):
    li = 0
    H = 16
    yf_d = S.dram("yf_d", [NB, NT, 128, 1040], F32)
    cache_d = S.dram("cache_d", [NB, NT, 128, 6 * D], BF16)
    sg1_d = S.dram("sg1_d", [NB, NT, 128, D], F32)

    def load_bc(name, src, dt=BF16, n=D, q="pool"):
        t = S.sbuf(name, [128, n], dt)
        S.dma(q, t[:], src.v(src.t[0:1, :].partition_broadcast(128)))
        return t

    S.push_scope()
    std_psum(S, G, "r")
    maskA = S.sbuf("maskA_s", [128, 2, 256], BF16)
    maskAT = S.sbuf("maskAT_s", [128, 2, 128], BF16)
    tri = S.sbuf("tri_s", [128, 2, 384], F32)
    ind = S.sbuf("ind_s", [128, 2], F32)
    for z in range(2):
        S.dma("pool", maskA[:, z, :], G.maskA_d.v(G.maskA_d.t[z]))
        S.dma("pool", maskAT[:, z, :], G.maskAT_d.v(G.maskAT_d.t[z]))
        S.dma("sp", tri[:, z, :], G.tri_d.v(G.tri_d.t[z]))
    S.dma("sp", ind[:], G.ind_d[:])
    k_a_bc = load_bc("k_a_bc", G.rw_k_a)
    r_k_bc = load_bc("r_k_bc", G.rw_r_k)
    scr1 = S.sbuf("scr1", [128, D], F32)
    scr2 = S.sbuf("scr2", [128, D], F32)
    sg_sb = S.sbuf("sg_sb", [128, D], F32)
    kdir_sb = S.sbuf("kdir_sb", [128, D], BF16)
    b_sb = S.sbuf("b_sb", [128, D], BF16)
    tm = [S.sbuf(f"tm{i}", [128, D], BF16) for i in range(2)]
    R19 = S.sbuf("R19", [128, H, 128], BF16)
    Bh = S.sbuf("Bh", [128, D], BF16)
    Kh = S.sbuf("Kh", [128, D], BF16)
    arT = S.sbuf("arT", [128, 8, 2, 128], BF16)
    btT = S.sbuf("btT", [128, 8, 128], BF16)
    ktT = S.sbuf("ktT", [128, 8, 128], BF16)
    gC = S.sbuf("gC", [128, 8, 2], F32)
    bon = S.sbuf("bon", [128, 2, 16], F32)
    NSET = 4
    M1 = [S.sbuf(f"M1_{i}", [128, 256], BF16) for i in range(NSET)]
    M2 = [S.sbuf(f"M2_{i}", [128, 256], BF16) for i in range(NSET)]
    MabT = [S.sbuf(f"MabT_{i}", [128, 128], BF16) for i in range(NSET)]
    Pb = [[S.sbuf(f"Pb_{i}_{j}", [128, 128], BF16) for j in range(2)] for i in range(NSET)]
    PTb = [[S.sbuf(f"PTb_{i}_{j}", [128, 128], BF16) for j in range(2)] for i in range(NSET)]
    Tb = [S.sbuf(f"Tb_{i}", [128, 128], BF16) for i in range(NSET)]
    WP = [S.sbuf(f"WP_{i}", [128, 128], BF16) for i in range(NSET)]
    G_all = S.sbuf("G_all", [128, 8, 128], BF16)
    Y0_all = S.sbuf("Y0_all", [128, H, 64], BF16)
    D_all = S.sbuf("D_all", [128, 8, 2, 128], BF16)
    E_all = S.sbuf("E_all", [128, 8, 2, 128], BF16)
    Sb = S.sbuf("Sb", [128, 8, 128], BF16)
    S.memset("pool", D_all[:], 0.0)
    S.memset("pool", E_all[:], 0.0)
    yfw = S.sbuf("yfw", [128, 1040], F32)
    banks = list(G.psH) + list(G.psA_h)
    NBK = len(banks)
    bank_ctr = [0]
    slot_ctr = [0] * NBK

    def slot(n=1):
        bk = bank_ctr[0] % NBK
        bank_ctr[0] += 1
        if n == 2:
            i = ((slot_ctr[bk] + 1) // 2 * 2) % 4
            slot_ctr[bk] = i + 2
        else:
            i = slot_ctr[bk] % 4
            slot_ctr[bk] = i + 1
        return (bk, i)

    def psl(s, p0=0, p1=128, c0=0, c1=128, n=1):
        bk, i = s
        t = banks[bk]
        if n == 2:
            return t.v(t.t[p0:p1, i:i + 2, :].rearrange("p a b -> p (a b)")[:, c0:c1])
        return t.v(t.t[p0:p1, i, c0:c1])

    def transposes_to(src_tm, dst_view):
        for ec in range(8):
            S.tr(G.psT[:, ec * 128:(ec + 1) * 128], src_tm[:, ec * 128:(ec + 1) * 128], G.ident_b[:])
        S.copy("act", dst_view, G.psT.v(G.psT.t[:, :].rearrange("p (a b) -> p a b", b=128)))

    def dir_part(z, r_v, k_v, v_v, kk_v, a_v, chunk_order):
        zsl = slice(z, z + 1)
        S.stt(scr1[:], a_v, -1.0, k_a_bc[:], ALU.add, ALU.mult)
        S.stt(kdir_sb[:], scr1[:], 1.0, k_v, ALU.add, ALU.mult)
        S.tt("pool", b_sb[:], kk_v, a_v, ALU.mult)
        S.tt("pool", scr1[:], r_v, kdir_sb[:], ALU.mult)
        S.tt("pool", scr1[:], scr1[:], r_k_bc[:], ALU.mult)
        S.reduce(bon[:, z, :], scr1.v(scr1.t[:, :].rearrange("p (h n) -> p h n", n=64)))
        def cum(which):
            for n in range(2):
                S.mm(G.psA[:, n * 512:(n + 1) * 512], tri[:, z, which * 128:(which + 1) * 128], sg_sb[:, n * 512:(n + 1) * 512])
        cum(0)
        S.act(scr2[:], G.psA[:], AF.Exp)
        S.tt("dve", tm[0][:], r_v, scr2[:], ALU.mult)
        transposes_to(tm[0], arT.v(arT.t[:, :, 1, :]))
        S.act(scr2[:], G.psA[:], AF.Exp, scale=-1.0)
        S.tt("dve", tm[1][:], b_sb[:], scr2[:], ALU.mult)
        transposes_to(tm[1], btT[:])
        S.tt("dve", tm[0][:], kdir_sb[:], scr2[:], ALU.mult)
        transposes_to(tm[0], ktT[:])
        cum(1)
        S.act(scr2[:], G.psA[:], AF.Exp)
        S.stt(tm[1][:], kk_v, -1.0, scr2[:], ALU.mult, ALU.mult)
        S.copy("pool", V(R19.t[:, :, 64:128], [("R19", h) for h in range(H)]),
               tm[1].v(tm[1].t[:, :].rearrange("p (h n) -> p h n", n=64)))
        transposes_to(tm[1], arT.v(arT.t[:, :, 0, :]))
        cum(2)
        S.act(scr2[:], G.psA[:], AF.Exp)
        S.tt("dve", Bh[:], b_sb[:], scr2[:], ALU.mult)
        S.tt("pool", Kh[:], kdir_sb[:], scr2[:], ALU.mult)
        sg_ = slot()
        for ec in range(8):
            S.mm(psl(sg_, c0=ec * 2, c1=ec * 2 + 2), sg_sb[:, ec * 128:(ec + 1) * 128], ind[:])
        S.act(gC[:], banks[sg_[0]].v(banks[sg_[0]].t[:, sg_[1], 0:16].rearrange("p (a b) -> p a b", b=2)), AF.Exp)

        if do_dir < 2:
            return
        def head_gen(h):
            ec, po = h // 2, (h % 2) * 64
            hc = slice(h * 64, (h + 1) * 64)
            pr = slice(po, po + 64)
            i2 = h % NSET
            bt_h = btT[pr, ec, :]
            kt_h = ktT[pr, ec, :]
            ar_h = arT.v(arT.t[pr, ec, :, :].rearrange("p a b -> p (a b)"))
            at_h = arT[pr, ec, 0, :]
            rt_h = arT[pr, ec, 1, :]
            s1 = slot(2)
            S.mm(psl(s1, n=2, c1=256), bt_h, ar_h)
            S.tt("dve", M1[i2][:], psl(s1, n=2, c1=256), maskA[:, z, :], ALU.mult)
            s3 = slot()
            S.mm(psl(s3), at_h, bt_h)
            S.tt("dve", MabT[i2][:], psl(s3), maskAT[:, z, :], ALU.mult)
            s2 = slot(2)
            S.mm(psl(s2, n=2, c1=256), kt_h, ar_h)
            S.tt("dve", M2[i2][:], psl(s2, n=2, c1=256), maskA[:, z, :], ALU.mult)
            T = Tb[i2]
            S.tt("pool", T[:], M1[i2][:, 0:128], G.ident_b[:], ALU.add)
            P, PT = M1[i2][:, 0:128], MabT[i2][:]
            yield
            for kstep in range(1, 6):
                if kstep < 5:
                    sa = slot()
                    S.mm(psl(sa), PT, P)
                    P2 = Pb[i2][kstep % 2]
                    S.copy("act", P2[:], psl(sa))
                sb_ = slot()
                S.mm(psl(sb_), P, PT)
                P2T = PTb[i2][kstep % 2]
                S.copy("act", P2T[:], psl(sb_))
                if kstep == 1:
                    sx = slot()
                    S.mm(psl(sx, c1=64), M2[i2][:, 0:128], v_v_slice(v_v, hc))
                    S.copy("act", R19.k(h, (slice(None), h, slice(0, 64))), psl(sx, c1=64))
                yield
                sc_ = slot()
                S.mm(psl(sc_), P2T[:], T[:])
                S.tt("dve", T[:], T[:], psl(sc_), ALU.add)
                if kstep < 5:
                    P, PT = P2[:], P2T[:]
            yield
            sw = slot()
            S.mm(psl(sw), T[:], R19.k(h, (slice(None), h, slice(None))))
            S.copy("act", WP[i2][:], psl(sw))
            yield
            sg2 = slot()
            S.mm(psl(sg2, p0=po, p1=po + 64), WP[i2][:, 64:128], M1[i2][:, 128:256])
            S.tt("dve", G_all.k(h, (pr, ec, slice(None))), psl(sg2, p0=po, p1=po + 64), rt_h, ALU.add)
            sy = slot()
            S.mm(psl(sy, c1=64), M1[i2][:, 128:256], WP[i2][:, 0:64], start=True, stop=False)
            S.mm(psl(sy, c1=64), M2[i2][:, 128:256], v_v_slice(v_v, hc), start=False, stop=True)
            S.copy("act", Y0_all.k(h, (slice(None), h, slice(None))), psl(sy, c1=64))
            sds = [slot(), slot()]
            for c in range(2):
                cr = slice(c * 64, (c + 1) * 64)
                S.mm(psl(sds[c], p0=po, p1=po + 64, c1=64), WP[i2][cr, 64:128], Bh[cr, hc])
            for c in range(2):
                S.stt(D_all.k(h, (pr, ec, c, slice(po, po + 64))), G.ident_f[pr, po:po + 64], gC[pr, ec, c:c + 1],
                      psl(sds[c], p0=po, p1=po + 64, c1=64), ALU.mult, ALU.add)
            ses = [slot(), slot()]
            for c in range(2):
                cr = slice(c * 64, (c + 1) * 64)
                S.mm(psl(ses[c], p0=po, p1=po + 64, c1=64), Bh[cr, hc], WP[i2][cr, 0:64], start=True, stop=False)
                S.mm(psl(ses[c], p0=po, p1=po + 64, c1=64), Kh[cr, hc], v_v_slice(v_v, hc, cr), start=False, stop=True)
            for c in range(2):
                S.copy("act", E_all.k(h, (pr, ec, c, slice(po, po + 64))), psl(ses[c], p0=po, p1=po + 64, c1=64))

        pending = list(range(nheads))
        active = []
        rnd, last_admit = 0, -99
        while pending or active:
            if pending and len(active) <= NSET - 2 and (rnd - last_admit >= 4 or not active):
                for _ in range(2):
                    if pending:
                        active.append(head_gen(pending.pop(0)))
                last_admit = rnd
            nxt = []
            for g in active:
                try:
                    next(g)
                    nxt.append(g)
                except StopIteration:
                    pass
            active = nxt
            rnd += 1
        if do_dir < 3:
            return
        for c in chunk_order:
            for ec in range(8):
                pair = [2 * ec, 2 * ec + 1]
                Gv = V(G_all.t[:, ec, c * 64:(c + 1) * 64], [("G_all", h) for h in pair])
                Dv = V(D_all.t[:, ec, c, :], [("D_all", h) for h in pair])
                S.mm(G.psY.v(G.psY.t[c * 64:(c + 1) * 64, ec * 128:(ec + 1) * 128]), Gv, Sb[:, ec, :])
                S.mm(G.psA.v(G.psA.t[:, ec * 128:(ec + 1) * 128]), Dv, Sb[:, ec, :])
            S.tt("dve", Sb[:], G.psA.v(G.psA.t[:, :].rearrange("p (a b) -> p a b", b=128)),
                 V(E_all.t[:, :, c, :], [("E_all", h) for h in range(H)]), ALU.add)

    def v_v_slice(v_v, hc, rows=slice(None)):
        return V(v_v.ap[rows, hc], v_v.toks)

    S.push_scope()
    Wr, Wk, Wv = [S.sbuf(n, [128, 8, D], BF16) for n in ("Wr", "Wk", "Wv")]
    for nm, W in (("Wr", Wr), ("Wk", Wk), ("Wv", Wv)):
        S.dma("sp", W.v(W.t[:, :, :].rearrange("p a b -> p (a b)")), G.wbf[nm][:])
    w1 = S.sbuf("w1", [128, 2, 8, 64], BF16)
    a1 = S.sbuf("a1", [128, 2, 8, 64], BF16)
    g1 = S.sbuf("g1", [128, 8, 128], BF16)
    w2x = S.sbuf("w2x", [65, 2, D], BF16)
    a2x = S.sbuf("a2x", [65, 2, D], BF16)
    g2 = S.sbuf("g2", [128, D], BF16)
    for z in range(2):
        S.dma("pool", w1[:, z, :, :], G.rw_w1.v(G.rw_w1.t[z].rearrange("(kc p) n -> p kc n", p=128)))
        S.dma("pool", a1[:, z, :, :], G.rw_a1.v(G.rw_a1.t[z].rearrange("(kc p) n -> p kc n", p=128)))
        S.dma("pool", w2x[0:64, z, :], G.rw_w2.v(G.rw_w2.t[z]))
        S.dma("pool", w2x[64:65, z, :], G.rw_w0.v(G.rw_w0.t[z:z + 1, :]))
        S.dma("pool", a2x[0:64, z, :], G.rw_a2.v(G.rw_a2.t[z]))
        S.dma("pool", a2x[64:65, z, :], G.rw_a0.v(G.rw_a0.t[z:z + 1, :]))
    S.dma("pool", g1[:], G.rw_g1.v(G.rw_g1.t.rearrange("(kc p) n -> p kc n", p=128)))
    S.dma("pool", g2[:], G.rw_g2[:])
    k_k_bc = load_bc("k_k_bc", G.rw_k_k)
    hTc = S.sbuf("hTc", [128, 8, CTX + 2], BF16)
    hTl = S.sbuf("hTl", [128, 8, LAT + 2], BF16)
    xin = scr2
    wk = {"junk": scr1, "st": S.sbuf("st", [128, 4], F32), "xn": tm[0],
          "fm32": S.sbuf("fm32", [128, 8, 128], F32)}
    dxt = S.sbuf("dxt", [128, 8, 128], F32)
    mix = [S.sbuf(f"mix{i}", [128, 8, 128], BF16) for i in range(2)]
    cach = S.sbuf("cach", [128, 6, D], BF16)
    a0_sb = S.sbuf("a0_sb", [128, D], BF16)
    sg1_v = yfw[:, 0:D]
    hwx = S.sbuf("hwx", [65, 2, 128], BF16)
    hax = S.sbuf("hax", [65, 2, 128], BF16)
    hgs = S.sbuf("hgs", [128, 128], BF16)
    st2 = S.sbuf("st2", [128, 3, 16], F32)
    S.memset("dve", hwx[:], 1.0)
    S.memset("dve", hax[:], 1.0)
    for hT in (hTc, hTl):
        S.memset("pool", hT[:], 0.0)

    mix_ctr = [0]

    def make_mix(hT, c0, j):
        m = mix[mix_ctr[0] % 2]
        mix_ctr[0] += 1
        mu = G.pfm.v(G.pfm.t[:, PV_MU + j * 8:PV_MU + j * 8 + 8].unsqueeze(2).to_broadcast([128, 8, 128]))
        S.tt("pool", wk["fm32"][:], dxt[:], mu, ALU.mult)
        S.tt("pool", m[:], wk["fm32"][:], hT[:, :, c0:c0 + 128], ALU.add)
        return m

    def proj_tm(ps, m, W):
        for n in range(2):
            for kc in range(8):
                S.mm(ps[:, n * 512:(n + 1) * 512], m[:, kc, :], W[:, kc, n * 512:(n + 1) * 512], start=(kc == 0), stop=(kc == 7))

    for b in range(nb):
        for ti in range(NT):
            if ti < 2:
                src, r, hT, t0 = G.ctx.v(G.ctx.t[b, ti * 128:(ti + 1) * 128, :]), 2, hTc, ti * 128
            else:
                src, r, hT, t0 = G.x.v(G.x.t[b, (ti - 2) * 128:(ti - 1) * 128, :]), b, hTl, (ti - 2) * 128
            S.dma("sp", xin[:], src)
            norm_tile_to_fm(S, G, xin[:], r, G.A1, 0, hT[:, :, t0 + 1:t0 + 129], wk)
        if dbg is not None and "hT" in dbg and b == 0:
            S.dma("sp", dbg["hT"][:], hTl[:])
        S.memset("dve", Sb[:], 0.0)
        for ti in range(nt0):
            hT, t0 = (hTc, ti * 128) if ti < 2 else (hTl, (ti - 2) * 128)
            c0 = t0 + 1
            S.tt("dve", dxt[:], hT[:, :, c0 - 1:c0 + 127], hT[:, :, c0 + 1:c0 + 129], ALU.add)
            S.stt(dxt[:], dxt[:], 0.5, hT[:, :, c0:c0 + 128], ALU.mult, ALU.subtract)
            if fl < 1:
                continue
            m = make_mix(hT, c0, 0)
            proj_tm(G.psA, m, Wr)
            S.copy("act", cach[:, 0, :], G.psA[:])
            if fl < 2:
                continue
            m = make_mix(hT, c0, 2)
            proj_tm(G.psY, m, Wv)
            S.copy("act", cach[:, 2, :], G.psY[:])
            if fl < 3:
                continue
            m = make_mix(hT, c0, 4)
            for z in range(2):
                sl_ = slot()
                for kc in range(8):
                    S.mm(psl(sl_, p1=64), a1[:, z, kc, :], m[:, kc, :], start=(kc == 0), stop=(kc == 7))
                S.copy("act", hax[0:64, z, :], psl(sl_, p1=64))
            for z in range(2):
                ps = G.psA if z == 0 else G.psY
                for n in range(2):
                    S.mm(ps[:, n * 512:(n + 1) * 512], hax[:, z, :], a2x[:, z, n * 512:(n + 1) * 512])
                S.act(a0_sb[:] if z == 0 else cach[:, 4, :], ps[:], AF.Sigmoid)
            if fl < 4:
                continue
            m = make_mix(hT, c0, 3)
            for z in range(2):
                sl_ = slot()
                for kc in range(8):
                    S.mm(psl(sl_, p1=64), w1[:, z, kc, :], m[:, kc, :], start=(kc == 0), stop=(kc == 7))
                S.act(hwx[0:64, z, :], psl(sl_, p1=64), AF.Tanh)
            for z in range(2):
                ps = G.psA if z == 0 else G.psY
                for n in range(2):
                    S.mm(ps[:, n * 512:(n + 1) * 512], hwx[:, z, :], w2x[:, z, n * 512:(n + 1) * 512])
                S.act(sg_sb[:] if z == 0 else sg1_v, ps[:], AF.Sigmoid)
            if fl < 5:
                continue
            m = make_mix(hT, c0, 5)
            sl_ = slot()
            for kc in range(8):
                S.mm(psl(sl_), g1[:, kc, :], m[:, kc, :], start=(kc == 0), stop=(kc == 7))
            S.act(hgs[:], psl(sl_), AF.Sigmoid)
            for n in range(2):
                S.mm(G.psY[:, n * 512:(n + 1) * 512], hgs[:], g2[:, n * 512:(n + 1) * 512])
            S.copy("act", cach[:, 5, :], G.psY[:])
            if fl < 6:
                continue
            m = make_mix(hT, c0, 1)
            proj_tm(G.psA, m, Wk)
            S.copy("act", cach[:, 1, :], G.psA[:])
            if fl < 6.1:
                continue
            S.tt("dve", scr1[:], G.psA[:], k_k_bc[:], ALU.mult)
            if fl < 6.2:
                continue
            S.act(scr2[:], scr1[:], AF.Square)
            S.reduce(st2[:, 0, :], scr2.v(scr2.t[:, :].rearrange("p (h n) -> p h n", n=64)))
            if fl < 6.3:
                continue
            S.ts("dve", st2[:, 1, :], st2[:, 0, :], 1e-12, None, op0=ALU.add)
            S.act(st2[:, 1, :], st2[:, 1, :], AF.Sqrt)
            S.recip(st2[:, 2, :], st2[:, 1, :])
            if fl < 6.4:
                continue
            S.tt("dve", cach.v(cach.t[:, 3, :].rearrange("p (h n) -> p h n", n=64)),
                 scr1.v(scr1.t[:, :].rearrange("p (h n) -> p h n", n=64)),
                 st2.v(st2.t[:, 2, :].unsqueeze(2).to_broadcast([128, 16, 64])), ALU.mult)
            if fl < 7:
                continue
            S.dma("sp", cache_d.v(cache_d.t[b, ti].rearrange("p (a n) -> p a n", n=D)), cach[:])
            S.dma("sp", sg1_d.v(sg1_d.t[b, ti]), sg1_v)
            if do_dir:
                dir_part(0, cach[:, 0, :], G.psA[:], cach[:, 2, :], cach[:, 3, :], a0_sb[:], (0, 1))
            S.tt("dve", yfw[:, 0:D], G.psY[:],
                 V(Y0_all.t[:, :, :].rearrange("p h n -> p (h n)"), [("Y0_all", h) for h in range(H)]), ALU.add)
            S.copy("pool", yfw[:, D:D + 16], bon[:, 0, :])
            S.dma("sp", yf_d.v(yf_d.t[b, ti]), yfw[:])
    S.pop_scope()

    S.push_scope()
    Wo = S.sbuf("Wo", [128, 8, D], BF16)
    S.dma("sp", Wo.v(Wo.t[:, :, :].rearrange("p a b -> p (a b)")), G.wbf["rwWo"][:])
    lnx_g_bc = load_bc("lnx_g_bc", G.rw_lnx_g)
    lnx_b_bc = load_bc("lnx_b_bc", G.rw_lnx_b)
    gate_bc = S.sbuf("gate_bc", [128, D], F32)
    cach = S.sbuf("cach1", [128, 6, D], BF16)
    xres = S.sbuf("xres", [128, D], F32)
    pre = S.sbuf("pre", [128, D], BF16)
    preT = S.sbuf("preT", [128, 8, 128], BF16)
    st3 = S.sbuf("st3", [128, 4, 16], F32)
    for b in range(nb):
        S.memset("dve", Sb[:], 0.0)
        order = ([1, 0] + list(range(NT - 1, 1, -1)))[:nt1]
        cur_r = None
        for ti in order:
            r = 2 if ti < 2 else b
            if r != cur_r:
                S.dma("sp", gate_bc[:], G.gates_d.v(G.gates_d.t[0, r:r + 1, :].partition_broadcast(128)))
                cur_r = r
            S.dma("sp", cach[:], cache_d.v(cache_d.t[b, ti].rearrange("p (a n) -> p a n", n=D)))
            S.dma("sp", sg_sb[:], sg1_d.v(sg1_d.t[b, ti]))
            S.dma("sp", yfw[:], yf_d.v(yf_d.t[b, ti]))
            dir_part(1, cach[:, 0, :], cach[:, 1, :], cach[:, 2, :], cach[:, 3, :], cach[:, 4, :], (1, 0))
            S.tt("dve", scr1[:], G.psY[:], V(Y0_all.t[:, :, :].rearrange("p h n -> p (h n)"), [("Y0_all", h) for h in range(H)]), ALU.add)
            S.tt("pool", scr1[:], scr1[:], yfw[:, 0:D], ALU.add)
            y3 = scr1.v(scr1.t[:, :].rearrange("p (h n) -> p h n", n=64))
            S.reduce(st3[:, 0, :], y3)
            S.ts("dve", st3[:, 0, :], st3[:, 0, :], 1.0 / 64, None, op0=ALU.mult)
            S.tt("dve", y3, y3, st3.v(st3.t[:, 0, :].unsqueeze(2).to_broadcast([128, 16, 64])), ALU.subtract)
            S.act(scr2[:], scr1[:], AF.Square)
            S.reduce(st3[:, 1, :], scr2.v(scr2.t[:, :].rearrange("p (h n) -> p h n", n=64)))
            S.ts("dve", st3[:, 1, :], st3[:, 1, :], 1.0 / 64, LNX_EPS, op0=ALU.mult, op1=ALU.add)
            S.act(st3[:, 1, :], st3[:, 1, :], AF.Sqrt)
            S.recip(st3[:, 2, :], st3[:, 1, :])
            S.tt("dve", y3, y3, st3.v(st3.t[:, 2, :].unsqueeze(2).to_broadcast([128, 16, 64])), ALU.mult)
            S.tt("pool", scr1[:], scr1[:], lnx_g_bc[:], ALU.mult)
            S.tt("pool", scr1[:], scr1[:], lnx_b_bc[:], ALU.add)
            S.tt("dve", st3[:, 3, :], bon[:, 1, :], yfw[:, D:D + 16], ALU.add)
            S.tt("dve", scr2.v(scr2.t[:, :].rearrange("p (h n) -> p h n", n=64)),
                 cach.v(cach.t[:, 2, :].rearrange("p (h n) -> p h n", n=64)),
                 st3.v(st3.t[:, 3, :].unsqueeze(2).to_broadcast([128, 16, 64])), ALU.mult)
            S.tt("pool", scr1[:], scr1[:], scr2[:], ALU.add)
            S.tt("pool", pre[:], scr1[:], cach[:, 5, :], ALU.mult)
            transposes_to(pre, preT[:])
            for n in range(2):
                for kc in range(8):
                    S.mm(G.psA[:, n * 512:(n + 1) * 512], preT[:, kc, :], Wo[:, kc, n * 512:(n + 1) * 512], start=(kc == 0), stop=(kc == 7))
            if ti < 2:
                xsrc = G.ctx.v(G.ctx.t[b, ti * 128:(ti + 1) * 128, :])
            else:
                xsrc = G.x.v(G.x.t[b, (ti - 2) * 128:(ti - 1) * 128, :])
            S.dma("sp", xres[:], xsrc)
            S.tt("dve", scr2[:], G.psA[:], gate_bc[:], ALU.mult)
            S.tt("pool", xres[:], xres[:], scr2[:], ALU.add)
            S.dma("sp", x1_d.v(x1_d.t[b, ti * 128:(ti + 1) * 128, :]), xres[:])
    S.pop_scope()
    S.pop_scope()

def moe_phase(S, G, li, xin_d, tiles, xout_fn, st_tiles, npairs=16, dbg=None):
    L = f"e{li}"
    S.push_scope()
    ysub = [S.psum(f"ysub{i}{L}", [128, 1024], F32) for i in range(2)]
    psG = [S.psum(f"psG{i}{L}", [128, 512], F32) for i in range(2)]
    psU = [S.psum(f"psU{i}{L}", [128, 512], F32) for i in range(2)]
    G.psA = ysub[0]
    STK = st_tiles * 128
    h2T = S.sbuf(f"h2T{L}", [128, 8, STK], BF16)
    y_acc = S.sbuf(f"yacc{L}", [128, st_tiles, D], F32)
    gatesT = S.sbuf(f"gatesT{L}", [32, STK], BF16)
    sel = S.sbuf(f"sel{L}", [32, 32, 128], BF16)
    S.dma("pool", sel[:], G.sel_d.v(G.sel_d.t[:, :].rearrange("p (a b) -> p a b", b=128)))
    Wrt = S.sbuf(f"Wrt{L}", [128, 8, 36], F32)
    S.dma("sp", Wrt[:], G.moe_router.v(G.moe_router.t[li].rearrange("(kc p) n -> p kc n", p=128)))
    rb = S.sbuf(f"rb{L}", [1, 36], F32)
    S.dma("sp", rb[:], G.moe_router_b.v(G.moe_router_b.t[li]))
    gate_bc = S.sbuf(f"gbc{L}", [128, 3, D], F32)
    rs_used = sorted(set(t[2] for t in tiles))
    for r in rs_used:
        S.dma("sp", gate_bc[:, r, :], G.gates_d.v(G.gates_d.t[1, r:r + 1, :].partition_broadcast(128)))
    Wg = [[S.sbuf(f"Wg{i}{e}{L}", [128, 8, DFF], BF16) for e in range(2)] for i in range(2)]
    Wu = [[S.sbuf(f"Wu{i}{e}{L}", [128, 8, DFF], BF16) for e in range(2)] for i in range(2)]
    Wd = [[S.sbuf(f"Wd{i}{e}{L}", [128, 2, D], BF16) for e in range(2)] for i in range(2)]
    NS1 = 2
    xin_s = [S.sbuf(f"xin{i}{L}", [128, D], F32) for i in range(NS1)]
    junk_s = [S.sbuf(f"junk{i}{L}", [128, D], F32) for i in range(NS1)]
    wk_s = [{"junk": junk_s[i], "st": S.sbuf(f"st{i}{L}", [128, 4], F32), "xn32": S.sbuf(f"xn32{i}{L}", [128, D], F32),
             "fm32": S.sbuf(f"fm32{i}{L}", [128, 8, 128], F32)} for i in range(NS1)]
    h32_s = [S.sbuf(f"h32{i}{L}", [128, 8, 128], F32) for i in range(NS1)]
    lg_s = [S.sbuf(f"lg{i}{L}", [128, 36], F32) for i in range(NS1)]
    sm_s = [S.sbuf(f"sm{i}{L}", [128, 64], F32) for i in range(NS1)]
    g32_s = [S.sbuf(f"g32{i}{L}", [128, 32], F32) for i in range(NS1)]
    xin3 = S.sbuf(f"xin3{L}", [128, D], F32)
    out3 = S.sbuf(f"out3{L}", [128, D], F32)
    s_sb = [S.sbuf(f"s_sb{i}{L}", [128, 256], F32) for i in range(2)]
    t_sb = [S.sbuf(f"t_sb{i}{L}", [128, 256], F32) for i in range(2)]
    hidT = [S.sbuf(f"hidT{i}{L}", [128, 256], BF16) for i in range(2)]

    def load_pair(p, buf):
        for e in range(2):
            eg = p * 2 + e
            for W, nm in ((Wg, "g"), (Wu, "u"), (Wd, "d")):
                t = W[buf][e]
                S.dma("sp", t.v(t.t[:, :, :].rearrange("p a b -> p (a b)")), G.wbf[f"{nm}{li}_{eg}"][:])

    n_super = len(tiles) // st_tiles
    assert n_super * st_tiles == len(tiles) and st_tiles % 2 == 0
    for su in range(n_super):
        stl = tiles[su * st_tiles:(su + 1) * st_tiles]
        load_pair(0, 0)
        def step1_gen(j, b, row0, r, s):
            xin, wk, h32, lg, sm, g32 = xin_s[s], wk_s[s], h32_s[s], lg_s[s], sm_s[s], g32_s[s]
            S.dma("sp", xin[:], xin_d.v(xin_d.t[b, row0:row0 + 128, :]))
            yield
            G.psA = ysub[s]
            norm_tile_to_fm(S, G, xin[:], r, G.A2, 24, h2T[:, :, j * 128:(j + 1) * 128], wk, fp32_out=h32[:])
            yield
            psr = (psG[0] if s == 0 else psU[0])[:, 0:36]
            for kc in range(8):
                S.mm(psr, h32[:, kc, :], Wrt[:, kc, :], start=(kc == 0), stop=False)
            S.mm(psr, G.ones_f[0:1, :], rb[:], start=False, stop=True)
            S.copy("dve", lg[:], psr)
            yield
            c = lambda i, n=1: sm[:, i:i + n]
            S.reduce(c(0), lg[:, 0:4], op=ALU.max)
            yield
            S.ts("dve", c(1), c(0), -1.0, None, op0=ALU.mult)
            yield
            S.ts("dve", c(4, 4), lg[:, 0:4], c(0), None, op0=ALU.is_ge)
            yield
            S.act(c(8, 4), lg[:, 0:4], AF.Exp, bias=c(1), accum_out=c(2))
            yield
            S.recip(c(3), c(2))
            yield
            S.ts("dve", c(16, 8), lg[:, 4:12], c(4), None, op0=ALU.mult)
            yield
            for g in range(1, 4):
                S.stt(c(16, 8), lg[:, 4 + 8 * g:12 + 8 * g], c(4 + g), c(16, 8), ALU.mult, ALU.add)
                yield
            S.reduce(c(12), c(16, 8), op=ALU.max)
            yield
            S.ts("dve", c(24, 8), c(16, 8), c(12), None, op0=ALU.is_ge)
            yield
            S.stt(c(32, 8), c(24, 8), -1e30, c(16, 8), ALU.mult, ALU.add)
            yield
            S.reduce(c(13), c(32, 8), op=ALU.max)
            yield
            S.ts("dve", c(40, 8), c(32, 8), c(13), None, op0=ALU.is_ge)
            yield
            S.tt("dve", c(14), c(13), c(12), ALU.subtract)
            yield
            S.act(c(15), c(14), AF.Exp)
            yield
            S.ts("dve", c(48), c(15), 1.0, None, op0=ALU.add)
            yield
            S.recip(c(49), c(48))
            yield
            S.tt("dve", c(50), c(49), c(3), ALU.mult)
            yield
            S.tt("dve", c(51), c(50), c(15), ALU.mult)
            yield
            S.ts("dve", c(52, 8), c(24, 8), c(50), None, op0=ALU.mult)
            yield
            S.stt(c(52, 8), c(40, 8), c(51), c(52, 8), ALU.mult, ALU.add)
            yield
            for g in range(4):
                S.ts("dve", g32[:, g * 8:(g + 1) * 8], c(52, 8), c(4 + g), None, op0=ALU.mult)
                yield
            pst = (psG[1] if s == 0 else psU[1])[0:32, 0:128]
            S.tr(pst, g32[:], G.ident_f[:])
            S.copy("dve", gatesT[:, j * 128:(j + 1) * 128], pst)
            if dbg is not None and "gates" in dbg and su == 0:
                S.dma("sp", dbg["gates"].v(dbg["gates"].t[j]), g32[:])

        pend1 = [step1_gen(j, b, row0, r, j % NS1) for j, (b, row0, r) in enumerate(stl)]
        act1 = []
        rnd, last1 = 0, -99
        while pend1 or act1:
            if pend1 and len(act1) < NS1 and (rnd - last1 >= 17 or not act1):
                act1.append(pend1.pop(0))
                last1 = rnd
            nxt = []
            for g_ in act1:
                try:
                    next(g_)
                    nxt.append(g_)
                except StopIteration:
                    pass
            act1 = nxt
            rnd += 1
        G.psA = ysub[0]
        items = [(p, t2, e, fc) for p in range(npairs) for t2 in range(st_tiles // 2) for e in range(2) for fc in range(2)]

        def emit_gu(idx):
            p, t2, e, fc = items[idx]
            buf, eg, ib = p % 2, p * 2 + e, idx % 2
            tok = slice(t2 * 256, (t2 + 1) * 256)
            pg, pu = psG[ib], psU[ib]
            for kc in range(8):
                S.mm(pg[:, 0:256], Wg[buf][e][:, kc, fc * 128:(fc + 1) * 128], h2T[:, kc, tok], start=(kc == 0), stop=(kc == 7))
            for kc in range(8):
                S.mm(pu[:, 0:256], Wu[buf][e][:, kc, fc * 128:(fc + 1) * 128], h2T[:, kc, tok], start=(kc == 0), stop=(kc == 7))
            S.mm(pu[:, 256:512], sel[:, eg, :], gatesT[:, tok])
            S.act(s_sb[ib][:], pg[:, 0:256], AF.Silu)
            S.tt("dve", t_sb[ib][:], s_sb[ib][:], pu[:, 0:256], ALU.mult)
            S.tt("dve", hidT[ib][:], t_sb[ib][:], pu[:, 256:512], ALU.mult)

        def emit_down(idx):
            p, t2, e, fc = items[idx]
            buf, ib, it = p % 2, idx % 2, e * 2 + fc
            for ts_ in range(2):
                for n in range(2):
                    S.mm(ysub[ts_][:, n * 512:(n + 1) * 512], hidT[ib][:, ts_ * 128:(ts_ + 1) * 128],
                         Wd[buf][e][:, fc, n * 512:(n + 1) * 512], start=(it == 0), stop=(it == 3))
            if it == 3:
                for ts_ in range(2):
                    j = t2 * 2 + ts_
                    if p == 0:
                        S.copy("dve", y_acc[:, j, :], ysub[ts_][:])
                    else:
                        S.tt("dve", y_acc[:, j, :], y_acc[:, j, :], ysub[ts_][:], ALU.add)
                    if p == npairs - 1:
                        b, row0, r = stl[j]
                        S.dma("sp", xin3[:], xin_d.v(xin_d.t[b, row0:row0 + 128, :]))
                        S.tt("pool", out3[:], y_acc[:, j, :], gate_bc[:, r, :], ALU.mult)
                        S.tt("pool", out3[:], out3[:], xin3[:], ALU.add)
                        S.dma("sp", xout_fn(b, row0), out3[:])

        for idx in range(len(items)):
            emit_gu(idx)
            if idx > 0:
                emit_down(idx - 1)
            p, t2, e, fc = items[idx]
            if t2 == 0 and e == 0 and fc == 0 and p + 1 < npairs:
                load_pair(p + 1, (p + 1) % 2)
        emit_down(len(items) - 1)
    S.pop_scope()

LAM_INIT1 = 0.8 - 0.6 * float(np.exp(-0.3 * 1))


def attn_phase(S, G, x2_d, x3_d, nb=NB, nqt=4, nh=8):
    li = 1
    NKT = NT
    S.push_scope()
    psA = S.psum("psA_a", [128, 1024], F32)
    psT = S.psum("psT_a", [128, 1024], BF16)
    psQ = [S.psum(f"psQ{i}_a", [128, 512], F32) for i in range(3)]
    psO = [S.psum(f"psO{i}_a", [128, 512], F32) for i in range(2)]
    G.psA, G.psT = psA, psT
    KT_all = S.sbuf("KT_all", [128, 8, TOK], BF16)
    QT_all = S.sbuf("QT_all", [128, 8, LAT], BF16)
    V_all = S.sbuf("V_all", [128, NKT, 8, 130], BF16)
    S.memset("pool", V_all[:], 1.0)
    gq_bc = S.sbuf("gq_bc", [128, 64], F32)
    gk_bc = S.sbuf("gk_bc", [128, 64], F32)
    sg_bc = S.sbuf("sg_bc", [128, 128], F32)
    S.dma("sp", gq_bc[:], G.da_q_norm_g.v(G.da_q_norm_g.t[0:1, :].partition_broadcast(128)))
    S.dma("sp", gk_bc[:], G.da_k_norm_g.v(G.da_k_norm_g.t[0:1, :].partition_broadcast(128)))
    S.dma("sp", sg_bc[:], G.da_subln_g.v(G.da_subln_g.t[0:1, :].partition_broadcast(128)))
    S.ts("dve", sg_bc[:], sg_bc[:], 1.0 - LAM_INIT1, None, op0=ALU.mult)
    lamv = S.sbuf("lamv", [128, 4, 64], F32)
    lsm = S.sbuf("lsm", [128, 8], F32)
    for i in range(4):
        S.dma("sp", lamv[:, i, :], G.da_lam.v(G.da_lam.t[i:i + 1, :].partition_broadcast(128)))
    S.tt("dve", lamv[:, 0, :], lamv[:, 0, :], lamv[:, 1, :], ALU.mult)
    S.tt("dve", lamv[:, 2, :], lamv[:, 2, :], lamv[:, 3, :], ALU.mult)
    S.reduce(lsm[:, 0:1], lamv[:, 0, :])
    S.reduce(lsm[:, 1:2], lamv[:, 2, :])
    S.act(lsm[:, 2:4], lsm[:, 0:2], AF.Exp)
    S.tt("dve", lsm[:, 4:5], lsm[:, 3:4], lsm[:, 2:3], ALU.subtract)
    S.ts("dve", lsm[:, 5:6], lsm[:, 4:5], -LAM_INIT1, None, op0=ALU.add)
    neglam = lsm[:, 5:6]
    junk = S.sbuf("junk_a", [128, D], F32)
    scrq = S.sbuf("scrq", [128, D], F32)
    xin = S.sbuf("xin_a", [128, D], F32)
    st = S.sbuf("st_a", [128, 4], F32)
    st16 = S.sbuf("st16_a", [128, 3, 16], F32)
    for b in range(nb):
        S.push_scope()
        Wqkv = S.sbuf(f"Wqkv{b}", [128, 8, 3 * D], BF16)
        S.dma("sp", Wqkv.v(Wqkv.t[:, :, :].rearrange("p a b -> p (a b)")), G.wbf["Wqkv"][:])
        hT_t = S.sbuf(f"hT_t{b}", [128, 8, 128], BF16)
        outq = S.sbuf(f"outq{b}", [128, D], BF16)
        wk = {"junk": junk, "st": st, "xn": S.sbuf(f"xn_a{b}", [128, D], BF16), "fm32": S.sbuf(f"fm32_a{b}", [128, 8, 128], F32)}
        cs_t = S.sbuf(f"cs_t{b}", [128, 2, 32], F32)
        tmpa = S.sbuf(f"tmpa{b}", [128, 512], F32)
        tmpb = S.sbuf(f"tmpb{b}", [128, 512], F32)

        def qk_norm(gain_bc, rope, dst_fm):
            S.act(junk[:], psA[:], AF.Square)
            S.reduce(st16[:, 0, :], junk.v(junk.t[:, :].rearrange("p (g n) -> p g n", n=64)))
            S.ts("dve", st16[:, 1, :], st16[:, 0, :], 1.0 / 64, EPS, op0=ALU.mult, op1=ALU.add)
            S.act(st16[:, 1, :], st16[:, 1, :], AF.Sqrt)
            S.recip(st16[:, 2, :], st16[:, 1, :])
            S.tt("dve", scrq.v(scrq.t[:, :].rearrange("p (g n) -> p g n", n=64)),
                 psA.v(psA.t[:, :].rearrange("p (g n) -> p g n", n=64)),
                 st16.v(st16.t[:, 2, :].unsqueeze(2).to_broadcast([128, 16, 64])), ALU.mult)
            gv = gain_bc.v(gain_bc.t[:, :].unsqueeze(1).to_broadcast([128, 16, 64]))
            if not rope:
                S.tt("pool", outq.v(outq.t[:, :].rearrange("p (g n) -> p g n", n=64)),
                     scrq.v(scrq.t[:, :].rearrange("p (g n) -> p g n", n=64)), gv, ALU.mult)
            else:
                S.tt("pool", scrq.v(scrq.t[:, :].rearrange("p (g n) -> p g n", n=64)),
                     scrq.v(scrq.t[:, :].rearrange("p (g n) -> p g n", n=64)), gv, ALU.mult)
                x5 = scrq.t[:, :].rearrange("p (g a h f) -> p g a h f", g=16, a=2, h=2)
                o5 = outq.t[:, :].rearrange("p (g a h f) -> p g a h f", g=16, a=2, h=2)
                x1, x2 = scrq.v(x5[:, :, :, 0, :]), scrq.v(x5[:, :, :, 1, :])
                o1, o2 = outq.v(o5[:, :, :, 0, :]), outq.v(o5[:, :, :, 1, :])
                cv = cs_t.v(cs_t.t[:, 0, :].rearrange("p (a f) -> p a f", a=2).unsqueeze(1).to_broadcast([128, 16, 2, 16]))
                sv = cs_t.v(cs_t.t[:, 1, :].rearrange("p (a f) -> p a f", a=2).unsqueeze(1).to_broadcast([128, 16, 2, 16]))
                ta = tmpa.v(tmpa.t[:, :].rearrange("p (g a f) -> p g a f", g=16, a=2))
                tb = tmpb.v(tmpb.t[:, :].rearrange("p (g a f) -> p g a f", g=16, a=2))
                S.tt("dve", ta, x1, cv, ALU.mult)
                S.tt("pool", tb, x2, sv, ALU.mult)
                S.tt("dve", o1, ta, tb, ALU.subtract)
                S.tt("dve", ta, x1, sv, ALU.mult)
                S.tt("pool", tb, x2, cv, ALU.mult)
                S.tt("pool", o2, ta, tb, ALU.add)
            for ec in range(8):
                S.tr(psT[:, ec * 128:(ec + 1) * 128], outq[:, ec * 128:(ec + 1) * 128], G.ident_b[:])
            S.copy("act", dst_fm, psT.v(psT.t[:, :].rearrange("p (a b) -> p a b", b=128)))

        def proj(c0):
            for n in range(2):
                for kc in range(8):
                    S.mm(psA[:, n * 512:(n + 1) * 512], hT_t[:, kc, :], Wqkv[:, kc, c0 + n * 512:c0 + (n + 1) * 512], start=(kc == 0), stop=(kc == 7))

        for ti in range(NT):
            r = 2 if ti < 2 else b
            S.dma("sp", xin[:], x2_d.v(x2_d.t[b, ti * 128:(ti + 1) * 128, :]))
            norm_tile_to_fm(S, G, xin[:], r, G.A1, 0, hT_t[:], wk)
            lat = ti >= 2
            if lat:
                t0 = (ti - 2) * 128
                S.dma("sp", cs_t[:, 0, :], G.rope_cos_d.v(G.rope_cos_d.t[t0:t0 + 128, :]))
                S.dma("sp", cs_t[:, 1, :], G.rope_sin_d.v(G.rope_sin_d.t[t0:t0 + 128, :]))
            proj(D)
            qk_norm(gk_bc, lat, KT_all[:, :, ti * 128:(ti + 1) * 128])
            proj(2 * D)
            S.copy("act", V_all[:, ti, :, 0:128], psA.v(psA.t[:, :].rearrange("p (h n) -> p h n", n=128)))
            if lat:
                proj(0)
                qk_norm(gq_bc, True, QT_all[:, :, t0:t0 + 128])
        S.pop_scope()
        S.push_scope()
        Wo = S.sbuf(f"Wo_a{b}", [128, 8, D], BF16)
        S.dma("sp", Wo.v(Wo.t[:, :, :].rearrange("p a b -> p (a b)")), G.wbf["daWo"][:])
        gate_bc = S.sbuf(f"gate_a{b}", [128, D], F32)
        S.dma("sp", gate_bc[:], G.gates_d.v(G.gates_d.t[0, b:b + 1, :].partition_broadcast(128)))
        O_all = S.sbuf(f"O_all{b}", [128, 4, D], F32)
        pT = [S.sbuf(f"pT{i}_{b}", [128, 512], BF16) for i in range(3)]
        rec = S.sbuf(f"rec{b}", [128, 8], F32)
        pre = S.sbuf(f"pre_a{b}", [128, D], BF16)
        preT = S.sbuf(f"preT_a{b}", [128, 8, 128], BF16)
        st8 = S.sbuf(f"st8_{b}", [128, 3, 8], F32)
        aitems = [(qt, h, m, kt) for qt in range(nqt) for h in range(nh) for m in range(2) for kt in range(NKT)]

        def emit_qk(i):
            qt, h, m, kt = aitems[i]
            pr = slice(m * 64, m * 64 + 64)
            ps = psQ[i % 3]
            S.mm(ps[:], KT_all[pr, h, kt * 128:(kt + 1) * 128], QT_all[pr, h, qt * 512:(qt + 1) * 512])
            S.act(pT[i % 3][:], ps[:], AF.Exp, scale=0.125)

        def emit_pv(i):
            qt, h, m, kt = aitems[i]
            for qs in range(4):
                acc = psO[qs // 2][:, (qs % 2) * 256:(qs % 2) * 256 + 129]
                S.mm(acc, pT[i % 3][:, qs * 128:(qs + 1) * 128], V_all[:, kt, h, 0:129],
                     start=(kt == 0 and qs % 2 == 0), stop=(kt == NKT - 1), skip_group_check=True)
            if kt != NKT - 1:
                return
            for qs in range(4):
                c0 = (qs % 2) * 256
                S.recip(rec[:, qs:qs + 1], psO[qs // 2][:, c0 + 128:c0 + 129])
                if m == 0:
                    S.ts("dve", O_all[:, qs, h * 128:(h + 1) * 128], psO[qs // 2][:, c0:c0 + 128], rec[:, qs:qs + 1], None, op0=ALU.mult)
                else:
                    S.tt("dve", rec[:, 4 + qs:5 + qs], rec[:, qs:qs + 1], neglam, ALU.mult)
                    S.stt(O_all[:, qs, h * 128:(h + 1) * 128], psO[qs // 2][:, c0:c0 + 128], rec[:, 4 + qs:5 + qs],
                          O_all[:, qs, h * 128:(h + 1) * 128], ALU.mult, ALU.add)
            if not (h == nh - 1 and m == 1):
                return
            for qs in range(4):
                O3 = O_all.v(O_all.t[:, qs, :].rearrange("p (h n) -> p h n", n=128))
                S.act(junk[:], O_all[:, qs, :], AF.Square)
                S.reduce(st8[:, 0, :], junk.v(junk.t[:, :].rearrange("p (h n) -> p h n", n=128)))
                S.ts("dve", st8[:, 1, :], st8[:, 0, :], 1.0 / 128, EPS, op0=ALU.mult, op1=ALU.add)
                S.act(st8[:, 1, :], st8[:, 1, :], AF.Sqrt)
                S.recip(st8[:, 2, :], st8[:, 1, :])
                S.tt("dve", O3, O3, st8.v(st8.t[:, 2, :].unsqueeze(2).to_broadcast([128, 8, 128])), ALU.mult)
                S.tt("pool", pre.v(pre.t[:, :].rearrange("p (h n) -> p h n", n=128)), O3,
                     sg_bc.v(sg_bc.t[:, :].unsqueeze(1).to_broadcast([128, 8, 128])), ALU.mult)
                for ec in range(8):
                    S.tr(psT[:, ec * 128:(ec + 1) * 128], pre[:, ec * 128:(ec + 1) * 128], G.ident_b[:])
                S.copy("act", preT[:], psT.v(psT.t[:, :].rearrange("p (a b) -> p a b", b=128)))
                for n in range(2):
                    for kc in range(8):
                        S.mm(psA[:, n * 512:(n + 1) * 512], preT[:, kc, :], Wo[:, kc, n * 512:(n + 1) * 512], start=(kc == 0), stop=(kc == 7))
                row = (qt * 4 + qs) * 128
                S.dma("sp", xin[:], x2_d.v(x2_d.t[b, CTX + row:CTX + row + 128, :]))
                S.tt("dve", scrq[:], psA[:], gate_bc[:], ALU.mult)
                S.tt("pool", xin[:], xin[:], scrq[:], ALU.add)
                S.dma("sp", x3_d.v(x3_d.t[b, row:row + 128, :]), xin[:])

        SK = 2
        for i in range(len(aitems) + SK):
            if i < len(aitems):
                emit_qk(i)
            if i >= SK:
                emit_pv(i - SK)
        S.pop_scope()
    S.pop_scope()

def build(cfg):
    nc = bass.Bass("TRN2", target_bir_lowering=False)
    S = Sched(nc)
    G = Ctx()
    setup_common(S, G, need=cfg.get("need", ("rwkv", "da", "moe")))
    outs = []
    dbg = {}
    stop = cfg.get("stop", "end")
    kind_x1 = "ExternalOutput" if stop == "rwkv" else "Internal"
    x1_d = S.dram("x1_d", [NB, TOK, D], F32, kind=kind_x1)
    if cfg.get("dbg_hT"):
        dbg["hT"] = S.dram("dbg_hT", [128, 8, LAT + 2], BF16, kind="ExternalOutput")
    wcast_phase(S, G, cfg.get("need", ("rwkv", "da", "moe")))
    if cfg.get("attn_in_ext"):
        G.x2_ext = S.dram("x2_ext", [NB, TOK, D], F32, kind="ExternalInput")
    if cfg.get("moe_in_ext"):
        G.x1_ext = S.dram("x1_ext", [NB, TOK, D], F32, kind="ExternalInput")
    if not cfg.get("skip_l0"):
        mod_phase(S, G, 0)
    if cfg.get("dbg_mod"):
        dm = S.dram("dbg_modT", [128, 48 * 4], F32, kind="ExternalOutput")
        S.dma("sp", dm[:], G.modT.v(G.modT.t[:, :, :].rearrange("p a b -> p (a b)")))
        dg = S.dram("dbg_gates", [2, 3, D], F32, kind="ExternalOutput")
        S.dma("sp", dg[:], G.gates_d[:])
    if stop == "mod":
        S.barrier()
        S.emit()
        return nc, S
    if not cfg.get("skip_rwkv") and not cfg.get("skip_l0"):
        rwkv_phase(S, G, x1_d, dbg=dbg, nb=cfg.get("nb", NB), **cfg.get("rw", {}))
    if stop == "rwkv":
        S.barrier()
        S.emit()
        return nc, S
    x2_d = S.dram("x2_d", [NB, TOK, D], F32, kind="ExternalOutput" if stop == "moe0" else "Internal")
    if not cfg.get("skip_l0"):
        moe0 = True
    else:
        moe0 = False
    tiles0 = [(b, ti * 128, 2 if ti < 2 else b) for b in range(NB) for ti in range(NT)]
    if cfg.get("dbg_gates"):
        dbg["gates"] = S.dram("dbg_gates32", [12, 128, 32], F32, kind="ExternalOutput")
    if moe0:
      moe_phase(S, G, 0, x1_d if not cfg.get("moe_in_ext") else G.x1_ext, tiles0[:cfg.get("moe_ntiles", len(tiles0))],
                lambda b, row0: x2_d.v(x2_d.t[b, row0:row0 + 128, :]), cfg.get("st0", 12), npairs=cfg.get("npairs", 16), dbg=dbg)
    if stop == "moe0":
        S.barrier()
        S.emit()
        return nc, S
    x3_d = S.dram("x3_d", [NB, LAT, D], F32, kind="ExternalOutput" if stop == "attn" else "Internal")
    mod_phase(S, G, 1)
    attn_phase(S, G, x2_d if not cfg.get("attn_in_ext") else G.x2_ext, x3_d, **cfg.get("at", {}))
    if stop == "attn":
        S.barrier()
        S.emit()
        return nc, S
    out_d = S.dram("out", [NB, LAT, D], F32, kind="ExternalOutput")
    tiles1 = [(b, ti * 128, b) for b in range(NB) for ti in range(LAT // 128)]
    moe_phase(S, G, 1, x3_d, tiles1, lambda b, row0: out_d.v(out_d.t[b, row0:row0 + 128, :]), cfg.get("st1", 8))
    S.barrier()
    S.emit()
    return nc, S


def prep_core_inputs(inputs, core, consts):
    b0 = core * NB
    f = lambda a: np.ascontiguousarray(a, dtype=np.float32)
    m = {}
    m["x"] = f(inputs["x"][b0:b0 + NB])
    m["ctx"] = f(inputs["ctx"][b0:b0 + NB])
    m["c3"] = f(np.concatenate([inputs["c"][b0:b0 + NB], inputs["c_ctx"][None, :]], axis=0))
    pv = np.concatenate([
        inputs["norm1_g"].reshape(16, 128), inputs["norm2_g"].reshape(16, 128),
        inputs["rw_mu"][0].reshape(48, 128), inputs["b_mod"].reshape(96, 128)], axis=0)
    m["pvec"] = f(pv)
    m["w_mod"] = f(inputs["w_mod"])
    m["b_mod"] = f(inputs["b_mod"])
    for k, v in consts.items():
        m[k] = v
    m["rw_w_rkv"] = f(inputs["rw_w_rkv"][0])
    for k in ("rw_w0", "rw_w1", "rw_w2", "rw_a0", "rw_a1", "rw_a2", "rw_g1", "rw_g2", "rw_w_o"):
        m[k] = f(inputs[k][0])
    for k in ("rw_k_k", "rw_k_a", "rw_lnx_g", "rw_lnx_b"):
        m[k] = f(inputs[k][0].reshape(1, D))
    m["rw_r_k"] = f(inputs["rw_r_k"][0].reshape(1, D))
    m["da_w_qkv"] = f(inputs["da_w_qkv"][0])
    m["da_q_norm_g"] = f(inputs["da_q_norm_g"][0].reshape(1, 64))
    m["da_k_norm_g"] = f(inputs["da_k_norm_g"][0].reshape(1, 64))
    m["da_lam"] = f(np.stack([inputs["da_lam_q1"][0], inputs["da_lam_k1"][0], inputs["da_lam_q2"][0], inputs["da_lam_k2"][0]]))
    m["da_subln_g"] = f(inputs["da_subln_g"][0].reshape(1, 128))
    m["da_w_o"] = f(inputs["da_w_o"][0])
    rt = np.concatenate([inputs["moe_router_g"], np.transpose(inputs["moe_router_e"], (0, 2, 1, 3)).reshape(2, D, 32)], axis=2)
    m["moe_router"] = f(rt)
    rb = np.concatenate([inputs["moe_router_g_b"], inputs["moe_router_e_b"].reshape(2, 32)], axis=1).reshape(2, 1, 36)
    m["moe_router_b"] = f(rb)
    m["moe_w_gate"] = f(inputs["moe_w_gate"]).reshape(2, NEXP, D, DFF)
    m["moe_w_up"] = f(inputs["moe_w_up"]).reshape(2, NEXP, D, DFF)
    m["moe_w_down"] = f(inputs["moe_w_down"]).reshape(2, NEXP, DFF, D)
    return m


_CACHE = {}


def kernel(**inputs):
    from concourse.bass_utils import run_bass_kernel_spmd
    n = 8
    if "nc" not in _CACHE:
        _CACHE["nc"] = build({})[0]
        _CACHE["consts"] = host_consts()
    nc = _CACHE["nc"]
    consts = _CACHE["consts"]
    inputs = {k: np.asarray(v) for k, v in inputs.items()}
    in_maps = [prep_core_inputs(inputs, c, consts) for c in range(n)]
    res = run_bass_kernel_spmd(nc, in_maps, core_ids=list(range(n)))
    out = np.concatenate([r["out"] for r in res.results], axis=0)
    return out.astype(np.float32)
```

```python
import numpy as np
import concourse.bass as bass
import concourse.mybir as mybir

F32 = mybir.dt.float32
BF16 = mybir.dt.bfloat16
I32 = mybir.dt.int32
U32 = mybir.dt.uint32
AF = mybir.ActivationFunctionType
ALU = mybir.AluOpType
AX = mybir.AxisListType

ENGS = ("pe", "dve", "act", "pool", "sp")
SEM_LIMIT = 30000
N_DMA_SEMS = 24


class V:
    __slots__ = ("ap", "toks", "excl")

    def __init__(self, ap, toks, excl=False):
        self.ap = ap
        self.toks = toks
        self.excl = excl


class Tl:
    def __init__(self, S, t, name, excl=False, toks=None):
        self.S = S
        self.t = t
        self.name = name
        self.excl = excl
        self.toks = toks

    def _tk(self, key):
        if self.toks is not None:
            return list(self.toks)
        return [(self.name, None if self.excl else key)]

    def __getitem__(self, idx):
        return V(self.t[idx], self._tk(None), self.excl)

    def k(self, key, idx=None):
        ap = self.t[idx] if idx is not None else None
        return V(ap, self._tk(key), self.excl)

    def v(self, ap, key=None):
        return V(ap, self._tk(key), self.excl)


class Sched:
    def __init__(self, nc, same_engine_sync=True):
        self.nc = nc
        self.q = {e: [] for e in ENGS}
        self.cnt = {e: 0 for e in ENGS}
        self.semi = {e: 0 for e in ENGS}
        self.nsem = {e: 1 for e in ENGS}
        self.state = {}
        self.waited = {e: {} for e in ENGS}
        self.same = same_engine_sync
        self.dma_i = 0
        self.dma_cnt = [0] * N_DMA_SEMS
        self.dma_last = [None] * N_DMA_SEMS
        self.ctx = []
        self.n_instr = 0
        self.out_deps = []

    def sbuf(self, name, shape, dtype=F32):
        cm = self.nc.sbuf_tensor(name, list(shape), dtype)
        t = cm.__enter__()
        self.ctx.append(cm)
        return Tl(self, t, name)

    def psum(self, name, shape, dtype=F32):
        cm = self.nc.psum_tensor(name, list(shape), dtype)
        t = cm.__enter__()
        self.ctx.append(cm)
        return Tl(self, t, name, excl=True)

    def dram(self, name, shape, dtype=F32, kind="Internal"):
        t = self.nc.dram_tensor(name, list(shape), dtype, kind=kind)
        return Tl(self, t.ap(), name)

    def push_scope(self):
        self.scopes = getattr(self, "scopes", [])
        self.scopes.append(len(self.ctx))

    def pop_scope(self):
        self.barrier()
        n = self.scopes.pop()
        while len(self.ctx) > n:
            self.ctx.pop().__exit__(None, None, None)

    def barrier(self):
        deps = []
        for e in ENGS:
            if self.cnt[e] > 0:
                deps.append(((e, self.semi[e]), self.cnt[e], e))
        for i in range(N_DMA_SEMS):
            if self.dma_last[i] is not None:
                deps.append(self.dma_last[i])
        for e in ENGS:
            d = [x for x in deps if x[2] != e]
            w = self._waits(e, d)
            if w:
                self.q[e].append(("wait", None, w, None))

    def _deps(self, reads, writes):
        deps = []
        for v in reads:
            for tok in v.toks:
                st = self.state.get(tok)
                if st and st[0] is not None:
                    deps.append(st[0])
        for v in writes:
            for tok in v.toks:
                st = self.state.get(tok)
                if st:
                    if st[0] is not None:
                        deps.append(st[0])
                    deps.extend(st[1])
        return deps

    def _update(self, reads, writes, me, real_writes=None):
        for v in reads:
            for tok in v.toks:
                st = self.state.setdefault(tok, [None, [], None])
                st[1].append(me)
                if len(st[1]) > 64:
                    best = {}
                    for d in st[1]:
                        if d[0] not in best or best[d[0]][1] < d[1]:
                            best[d[0]] = d
                    st[1] = list(best.values())
        rw = writes if real_writes is None else real_writes
        rwt = set()
        for v in rw:
            rwt.update(v.toks)
        for v in writes:
            for tok in v.toks:
                old = self.state.get(tok)
                lrw = me if tok in rwt else (old[2] if old else None)
                self.state[tok] = [me, [], lrw]

    def _waits(self, eng, deps, raw_toks_same=None):
        need = {}
        for d in deps:
            semkey, val, deng, is_raw = d[0], d[1], d[2], True
            if deng == eng and not self.same:
                continue
            w = self.waited[eng].get(semkey, 0)
            if val > w and val > need.get(semkey, 0):
                need[semkey] = val
        for sk, val in need.items():
            self.waited[eng][sk] = val
        return list(need.items())

    def op(self, eng, fn, reads=(), writes=(), same_ok=False):
        reads = list(reads)
        real_writes = list(writes)
        writes = real_writes + [v for v in reads if v.excl]
        deps = self._deps(reads, writes)
        if same_ok or eng == "pe":
            deps = [d for d in deps if d[2] != eng]
        elif self.same:
            rd = []
            for v in reads:
                for tok in v.toks:
                    st = self.state.get(tok)
                    if st and st[2] is not None and st[2][2] == eng:
                        rd.append(st[2])
            deps = [d for d in deps if d[2] != eng] + rd
        waits = self._waits(eng, deps)
        if self.cnt[eng] >= SEM_LIMIT:
            self.semi[eng] += 1
            self.nsem[eng] = max(self.nsem[eng], self.semi[eng] + 1)
            self.cnt[eng] = 0
        self.cnt[eng] += 1
        me = ((eng, self.semi[eng]), self.cnt[eng], eng)
        self.q[eng].append(("op", fn, waits, me[0]))
        self._update(reads, writes, me, real_writes)
        self.n_instr += 1
        return me

    def dma(self, queue, out, in_, split=0, **kw):
        reads, writes = [in_], [out]
        deps = self._deps(reads, writes)
        i = self.dma_i % N_DMA_SEMS
        self.dma_i += 1
        if self.dma_last[i] is not None:
            deps.append(self.dma_last[i])
        waits = self._waits(queue, deps)
        oa, ia = out.ap, in_.ap
        parts = [(oa, ia)]
        if split:
            step = 128 // split
            parts = [(oa[k * step:(k + 1) * step], ia[k * step:(k + 1) * step]) for k in range(split)]
        for k, (o_, a_) in enumerate(parts):
            self.dma_cnt[i] += 16
            self.q[queue].append(("dma", (o_, a_, kw), waits if k == 0 else [], ("dma", i)))
            self.n_instr += 1
        me = (("dma", i), self.dma_cnt[i], "dma")
        self.dma_last[i] = me
        self._update(reads, writes, me)
        return me

    def emit(self, final_deps=None):
        nc = self.nc
        sems = {}
        cms = []

        def mk(name):
            cm = nc.semaphore(name)
            s = cm.__enter__()
            cms.append(cm)
            return s
        for e in ENGS:
            for i in range(self.nsem[e]):
                sems[(e, i)] = mk(f"s_{e}_{i}")
        for i in range(N_DMA_SEMS):
            sems[("dma", i)] = mk(f"s_dma_{i}")
        engobj = {"pe": "tensor", "dve": "vector", "act": "scalar", "pool": "gpsimd", "sp": "sync"}
        if final_deps:
            w = self._waits("sp", final_deps)
            self.q["sp"].append(("wait", None, w, None))
        with nc.Block() as block:
            for e in ENGS:
                items = self.q[e]
                if not items:
                    continue

                def body(eng, items=items):
                    for kind, fn, waits, semkey in items:
                        for sk, val in waits:
                            eng.wait_ge(sems[sk], val)
                        if kind == "op":
                            ins = fn(eng)
                            ins.then_inc(sems[semkey], 1)
                        elif kind == "dma":
                            oa, ia, kw = fn
                            eng.dma_start(out=oa, in_=ia, **kw).then_inc(sems[semkey], 16)
                getattr(block, engobj[e])(body)
        for cm in reversed(cms):
            cm.__exit__(None, None, None)
        for cm in reversed(self.ctx):
            cm.__exit__(None, None, None)

    def mm(self, out, lhsT, rhs, start=True, stop=True, **kw):
        return self.op("pe", lambda e: e.matmul(out.ap, lhsT.ap, rhs.ap, start=start, stop=stop, **kw),
                       reads=[lhsT, rhs] + ([] if start else [out]), writes=[out])

    def tr(self, out, in_, ident):
        return self.op("pe", lambda e: e.transpose(out.ap, in_.ap, ident.ap),
                       reads=[in_, ident], writes=[out])

    def act(self, out, in_, func, bias=None, scale=None, accum_out=None, eng="act"):
        reads = [in_]
        kw = {}
        if isinstance(bias, V):
            reads.append(bias)
            kw["bias"] = bias.ap
        elif bias is not None:
            kw["bias"] = bias
        if isinstance(scale, V):
            reads.append(scale)
            kw["scale"] = scale.ap
        elif scale is not None:
            kw["scale"] = scale
        writes = [out]
        if accum_out is not None:
            writes.append(accum_out)
            kw["accum_out"] = accum_out.ap
        return self.op("act", lambda e: e.activation(out.ap, in_.ap, func, **kw), reads=reads, writes=writes)

    def tt(self, eng, out, in0, in1, op):
        return self.op(eng, lambda e: e.tensor_tensor(out.ap, in0.ap, in1.ap, op), reads=[in0, in1], writes=[out])

    def ts(self, eng, out, in0, s1, s2=None, op0=ALU.mult, op1=None, accum_out=None):
        reads = [in0]
        a1 = s1.ap if isinstance(s1, V) else s1
        a2 = s2.ap if isinstance(s2, V) else s2
        if isinstance(s1, V):
            reads.append(s1)
        if isinstance(s2, V):
            reads.append(s2)
        writes = [out]
        kw = {}
        if op1 is not None:
            kw["op1"] = op1
        if accum_out is not None:
            kw["accum_out"] = accum_out.ap
            writes.append(accum_out)
        return self.op(eng, lambda e: e.tensor_scalar(out.ap, in0.ap, a1, a2, op0, **kw), reads=reads, writes=writes)

    def stt(self, out, in0, scalar, in1, op0, op1, eng="dve"):
        reads = [in0, in1]
        a = scalar.ap if isinstance(scalar, V) else scalar
        if isinstance(scalar, V):
            reads.append(scalar)
        return self.op(eng, lambda e: e.scalar_tensor_tensor(out.ap, in0.ap, a, in1.ap, op0, op1), reads=reads, writes=[out])

    def copy(self, eng, out, in_):
        if eng == "act":
            return self.op("act", lambda e: e.copy(out.ap, in_.ap), reads=[in_], writes=[out])
        return self.op(eng, lambda e: e.tensor_copy(out.ap, in_.ap), reads=[in_], writes=[out])

    def memset(self, eng, out, val):
        return self.op(eng, lambda e: e.memset(out.ap, val), reads=[], writes=[out])

    def reduce(self, out, in_, op=ALU.add, axis=AX.X, eng="dve"):
        return self.op(eng, lambda e: e.tensor_reduce(out.ap, in_.ap, axis, op), reads=[in_], writes=[out])

    def recip(self, out, in_):
        return self.op("dve", lambda e: e.reciprocal(out.ap, in_.ap), reads=[in_], writes=[out])
D = 1024
NB = 2
LAT = 2048
CTX = 256
TOK = CTX + LAT
NT = TOK // 128
EPS = 1e-6
DECAY_C = -0.6065306597126334
LNX_EPS = 64e-5
NGRP = 4
NEXP = 32
DFF = 256

PV_N1 = (0, 16)
PV_N2 = (16, 32)
PV_MU = 32
PV_BMOD = 80
PV_ROWS = 176


def host_consts():
    c = {}
    c["ident_f"] = np.eye(128, dtype=np.float32)
    c["ones_f"] = np.ones((128, 128), dtype=np.float32)
    idx = np.arange(128)
    cs, ct = idx[:, None] // 64, idx[None, :] // 64
    same = (cs == ct)
    s_, t_ = idx[:, None], idx[None, :]
    maskA = np.zeros((2, 128, 256), np.float32)
    maskAT = np.zeros((2, 128, 128), np.float32)
    tri = np.zeros((2, 128, 384), np.float32)
    for z in range(2):
        prec = same & ((s_ < t_) if z == 0 else (s_ > t_))
        preceq = same & ((s_ <= t_) if z == 0 else (s_ >= t_))
        succ = same & ((s_ > t_) if z == 0 else (s_ < t_))
        maskA[z, :, 0:128] = prec
        maskA[z, :, 128:256] = preceq
        maskAT[z] = prec.T
        tri[z, :, 0:128] = preceq * DECAY_C
        tri[z, :, 128:256] = prec * DECAY_C
        tri[z, :, 256:384] = succ * DECAY_C
    c["maskA"] = maskA
    c["maskAT"] = maskAT
    c["tri"] = tri
    ind = np.zeros((128, 2), np.float32)
    ind[0:64, 0] = DECAY_C
    ind[64:128, 1] = DECAY_C
    c["ind"] = ind
    rows = LAT // 64
    row = np.repeat(np.arange(rows), 64)
    col = np.tile(np.arange(64), rows)
    inv = (10000.0 ** (-np.arange(16, dtype=np.float32) / 16)).astype(np.float32)
    ang = np.stack([row, col], axis=-1).astype(np.float32)[:, :, None] * inv
    c["rope_cos"] = np.cos(ang).astype(np.float32).reshape(LAT, 32)
    c["rope_sin"] = np.sin(ang).astype(np.float32).reshape(LAT, 32)
    sel = np.zeros((32, 32, 128), np.float32)
    for e in range(32):
        sel[e, e, :] = 1.0
    c["sel"] = sel.reshape(32, 32 * 128)
    return c


class Ctx:
    pass


def std_psum(S, G, tag):
    G.psA = S.psum(f"psA{tag}", [128, 1024], F32)
    nm = G.psA.name
    G.psA.toks = [(nm, 0), (nm, 1)]
    G.psA_h = [Tl(S, G.psA.t[:, i * 512:(i + 1) * 512].rearrange("p (a b) -> p a b", b=128), nm, excl=True,
                  toks=[(nm, i)]) for i in range(2)]
    G.psY = S.psum(f"psY{tag}", [128, 1024], F32)
    G.psT = S.psum(f"psT{tag}", [128, 1024], BF16)
    G.psH = [S.psum(f"psH{i}{tag}", [128, 4, 128], F32) for i in range(3)]


def setup_common(S, G, need=("rwkv", "da", "moe")):
    def I(name, shape, dt=F32):
        grp = name.split('_')[0]
        if grp in ('rw', 'da', 'moe') and {'rw': 'rwkv', 'da': 'da', 'moe': 'moe'}[grp] not in need:
            return None
        return S.dram(name, shape, dt, kind="ExternalInput")
    G.x = I("x", [NB, LAT, D])
    G.ctx = I("ctx", [NB, CTX, D])
    G.c3 = I("c3", [3, D])
    G.pvec = I("pvec", [PV_ROWS, 128])
    G.w_mod = I("w_mod", [2, D, 6 * D])
    G.b_mod = I("b_mod", [2, 6 * D])
    G.ident_f_d = I("ident_f", [128, 128])
    G.ones_f_d = I("ones_f", [128, 128])
    G.maskA_d = I("maskA", [2, 128, 256])
    G.maskAT_d = I("maskAT", [2, 128, 128])
    G.tri_d = I("tri", [2, 128, 384])
    G.ind_d = I("ind", [128, 2])
    G.rope_cos_d = I("rope_cos", [LAT, 32])
    G.rope_sin_d = I("rope_sin", [LAT, 32])
    G.sel_d = I("sel", [32, 32 * 128])
    G.rw_w_rkv = I("rw_w_rkv", [3, D, D])
    G.rw_w0 = I("rw_w0", [2, D])
    G.rw_w1 = I("rw_w1", [2, D, 64])
    G.rw_w2 = I("rw_w2", [2, 64, D])
    G.rw_a0 = I("rw_a0", [2, D])
    G.rw_a1 = I("rw_a1", [2, D, 64])
    G.rw_a2 = I("rw_a2", [2, 64, D])
    G.rw_g1 = I("rw_g1", [D, 128])
    G.rw_g2 = I("rw_g2", [128, D])
    G.rw_k_k = I("rw_k_k", [1, D])
    G.rw_k_a = I("rw_k_a", [1, D])
    G.rw_r_k = I("rw_r_k", [1, D])
    G.rw_lnx_g = I("rw_lnx_g", [1, D])
    G.rw_lnx_b = I("rw_lnx_b", [1, D])
    G.rw_w_o = I("rw_w_o", [D, D])
    G.da_w_qkv = I("da_w_qkv", [D, 3 * D])
    G.da_q_norm_g = I("da_q_norm_g", [1, 64])
    G.da_k_norm_g = I("da_k_norm_g", [1, 64])
    G.da_lam = I("da_lam", [4, 64])
    G.da_subln_g = I("da_subln_g", [1, 128])
    G.da_w_o = I("da_w_o", [D, D])
    G.moe_router = I("moe_router", [2, D, 36])
    G.moe_router_b = I("moe_router_b", [2, 1, 36])
    G.moe_w_gate = I("moe_w_gate", [2, NEXP, D, DFF])
    G.moe_w_up = I("moe_w_up", [2, NEXP, D, DFF])
    G.moe_w_down = I("moe_w_down", [2, NEXP, DFF, D])

    G.ident_f = S.sbuf("ident_f_s", [128, 128], F32)
    G.ident_b = S.sbuf("ident_b_s", [128, 128], BF16)
    G.ones_f = S.sbuf("ones_f_s", [128, 128], F32)
    S.dma("sp", G.ident_f[:], G.ident_f_d[:])
    S.dma("pool", G.ident_b[:], G.ident_f_d[:])
    S.dma("sp", G.ones_f[:], G.ones_f_d[:])
    G.pfm = S.sbuf("pfm", [128, PV_ROWS], F32)
    S.push_scope()
    std_psum(S, G, "c")
    pv = S.sbuf("pv_ld", [128, 2, 128], F32)
    S.memset("dve", pv[:], 0.0)
    S.dma("sp", pv[:, 0, :], G.pvec[0:128, :])
    S.dma("sp", pv[0:PV_ROWS - 128, 1, :], G.pvec[128:PV_ROWS, :])
    S.tr(G.psA[:, 0:128], pv[:, 0, :], G.ident_f[:])
    S.tr(G.psA[:, 128:256], pv[:, 1, :], G.ident_f[:])
    S.copy("dve", G.pfm[:], G.psA[:, 0:PV_ROWS])
    S.pop_scope()
    G.modT = S.sbuf("modT", [128, 48, 4], F32)
    G.A1 = S.sbuf("A1", [128, 3, 8], F32)
    G.A2 = S.sbuf("A2", [128, 3, 8], F32)
    G.gates_d = S.dram("gates_d", [2, 3, D], F32)


def mod_phase(S, G, li):
    S.push_scope()
    std_psum(S, G, f"m{li}")
    crow = S.sbuf(f"crow{li}", [4, D], F32)
    sc = S.sbuf(f"sc{li}", [4, D], F32)
    scT = S.sbuf(f"scT{li}", [128, 8, 4], F32)
    brow = S.sbuf(f"brow{li}", [1, 6 * D], F32)
    grow = S.sbuf(f"grow{li}", [4, 512], F32)
    wblk = [S.sbuf(f"wblk{i}_{li}", [128, 8, 512], F32) for i in range(2)]
    S.memset("dve", crow[:], 0.0)
    S.dma("sp", crow[0:3, :], G.c3[:])
    S.dma("sp", brow[:], G.b_mod[li:li + 1, :])
    S.act(sc[:], crow[:], AF.Silu)
    for kc in range(8):
        S.tr(G.psA[:, kc * 4:(kc + 1) * 4], sc[0:4, kc * 128:(kc + 1) * 128], G.ident_f[0:4, 0:4])
    S.copy("dve", scT[:], G.psA.v(G.psA.t[:, 0:32].rearrange("p (a b) -> p a b", b=4)))
    wsrc = G.w_mod.t[li].rearrange("(kc p) n -> p kc n", p=128)
    gate_blocks = {4: (0, 0), 5: (0, 1), 10: (1, 0), 11: (1, 1)}
    for blk in range(12):
        wb = wblk[blk % 2]
        S.dma("sp", wb[:], G.w_mod.v(wsrc[:, :, blk * 512:(blk + 1) * 512]), split=8)
        if blk in gate_blocks:
            which, half = gate_blocks[blk]
            ps = G.psY[0:4, 0:512]
            for kc in range(8):
                S.mm(ps, scT[:, kc, :], wb[:, kc, :], start=(kc == 0), stop=False)
            S.mm(ps, G.ones_f[0:1, 0:4], brow[0:1, blk * 512:(blk + 1) * 512], start=False, stop=True)
            S.copy("dve", grow[:], ps)
            S.dma("sp", G.gates_d.v(G.gates_d.t[which, :, half * 512:(half + 1) * 512]), grow[0:3, :])
        else:
            for ec in range(4):
                ch = blk * 4 + ec
                ps = G.psA[:, ch * 4:(ch + 1) * 4]
                for kc in range(8):
                    S.mm(ps, wb[:, kc, ec * 128:(ec + 1) * 128], scT[:, kc, :], start=(kc == 0), stop=(kc == 7))
                S.ts("dve", G.modT[:, ch, :], ps, G.pfm[:, PV_BMOD + li * 48 + ch:PV_BMOD + li * 48 + ch + 1], None, op0=ALU.add)
    for r in range(3):
        for (A, sc0, gofs) in ((G.A1, 8, PV_N1[0] + li * 8), (G.A2, 32, PV_N2[0] + li * 8)):
            S.ts("dve", A[:, r, :], G.modT[:, sc0:sc0 + 8, r], 1.0, None, op0=ALU.add)
            S.tt("dve", A[:, r, :], A[:, r, :], G.pfm[:, gofs:gofs + 8], ALU.mult)
    S.pop_scope()


def norm_tile_to_fm(S, G, xt, r, A, shift_ch0, out_fm, wk, fp32_out=None):
    st = wk["st"]
    S.act(wk["junk"][:], xt, AF.Square, accum_out=st[:, 0:1])
    S.ts("dve", st[:, 1:2], st[:, 0:1], 1.0 / D, EPS, op0=ALU.mult, op1=ALU.add)
    S.act(st[:, 2:3], st[:, 1:2], AF.Sqrt)
    S.recip(st[:, 3:4], st[:, 2:3])
    if fp32_out is None:
        xn = wk["xn"]
        S.ts("dve", xn[:], xt, st[:, 3:4], None, op0=ALU.mult)
        for kc in range(8):
            S.tr(G.psT[:, kc * 128:(kc + 1) * 128], xn[:, kc * 128:(kc + 1) * 128], G.ident_b[:])
        src = G.psT.v(G.psT.t[:, :].rearrange("p (a b) -> p a b", b=128))
    else:
        xn = wk["xn32"]
        S.ts("dve", xn[:], xt, st[:, 3:4], None, op0=ALU.mult)
        for kc in range(8):
            S.tr(G.psA[:, kc * 128:(kc + 1) * 128], xn[:, kc * 128:(kc + 1) * 128], G.ident_f[:])
        src = G.psA.v(G.psA.t[:, :].rearrange("p (a b) -> p a b", b=128))
    Abc = A.v(A.t[:, r, :].unsqueeze(2).to_broadcast([128, 8, 128]))
    shbc = G.modT.v(G.modT.t[:, shift_ch0:shift_ch0 + 8, r].unsqueeze(2).to_broadcast([128, 8, 128]))
    tmp = wk["fm32"]
    S.tt("dve", tmp[:], src, Abc, ALU.mult)
    if fp32_out is not None:
        S.tt("pool", fp32_out, tmp[:], shbc, ALU.add)
        S.copy("act", out_fm, fp32_out)
    else:
        S.tt("pool", out_fm, tmp[:], shbc, ALU.add)


def wcast_phase(S, G, need):
    items = []
    G.wbf = {}

    def add(name, src3, n):
        dst = S.dram("wbf_" + name, [128, n], BF16)
        G.wbf[name] = dst
        off = 0
        a, b = src3.shape[1], src3.shape[2]
        rows = max(1, 2048 // b)
        if b > 2048:
            for i in range(a):
                for c0 in range(0, b, 2048):
                    c1 = min(b, c0 + 2048)
                    items.append((src3[:, i:i + 1, c0:c1], dst, i * b + c0, c1 - c0, (1, c1 - c0)))
        else:
            for i in range(0, a, rows):
                i1 = min(a, i + rows)
                items.append((src3[:, i:i1, :], dst, i * b, (i1 - i) * b, (i1 - i, b)))

    if "rwkv" in need:
        for j, nm in enumerate(("Wr", "Wk", "Wv")):
            add(nm, G.rw_w_rkv.t[j].rearrange("(kc p) n -> p kc n", p=128), 8 * D)
        add("rwWo", G.rw_w_o.t.rearrange("(kc p) n -> p kc n", p=128), 8 * D)
    if "da" in need:
        add("Wqkv", G.da_w_qkv.t.rearrange("(kc p) n -> p kc n", p=128), 8 * 3 * D)
        add("daWo", G.da_w_o.t.rearrange("(kc p) n -> p kc n", p=128), 8 * D)
    if "moe" in need:
        for li in range(2):
            for e in range(NEXP):
                add(f"g{li}_{e}", G.moe_w_gate.t[li, e].rearrange("(kc p) n -> p kc n", p=128), 8 * DFF)
                add(f"u{li}_{e}", G.moe_w_up.t[li, e].rearrange("(kc p) n -> p kc n", p=128), 8 * DFF)
                add(f"d{li}_{e}", G.moe_w_down.t[li, e].rearrange("(fc p) n -> p fc n", p=128), 2 * D)
    S.push_scope()
    NBUF = 4
    stg = [S.sbuf(f"wc_stg{i}", [128, 2048], F32) for i in range(NBUF)]
    ob = [S.sbuf(f"wc_ob{i}", [128, 2048], BF16) for i in range(NBUF)]
    engs = ["dve", "pool", "dve"]

    def load(i):
        src3, dst, off, n, (a, b) = items[i]
        t = stg[i % NBUF]
        S.dma("sp", t.v(t.t[:, 0:n].rearrange("p (a b) -> p a b", b=b)), V(src3, [("wsrc", None)]))

    for i in range(min(NBUF - 1, len(items))):
        load(i)
    for i in range(len(items)):
        if i + NBUF - 1 < len(items):
            load(i + NBUF - 1)
        src3, dst, off, n, _ = items[i]
        S.copy(engs[i % 3], ob[i % NBUF][:, 0:n], stg[i % NBUF][:, 0:n])
        S.dma("act", dst.v(dst.t[:, off:off + n]), ob[i % NBUF][:, 0:n])
    S.pop_scope()

def rwkv_phase(S, G, x1_d, dbg=None, nb=NB, nt0=NT, do_dir=3, nt1=NT, nheads=16, fl=99):
    li = 0
    H = 16
    yf_d = S.dram("yf_d", [NB, NT, 128, 1040], F32)
    cache_d = S.dram("cache_d", [NB, NT, 128, 6 * D], BF16)
    sg1_d = S.dram("sg1_d", [NB, NT, 128, D], F32)

    def load_bc(name, src, dt=BF16, n=D, q="pool"):
        t = S.sbuf(name, [128, n], dt)
        S.dma(q, t[:], src.v(src.t[0:1, :].partition_broadcast(128)))
        return t

    S.push_scope()
    std_psum(S, G, "r")
    maskA = S.sbuf("maskA_s", [128, 2, 256], BF16)
    maskAT = S.sbuf("maskAT_s", [128, 2, 128], BF16)
    tri = S.sbuf("tri_s", [128, 2, 384], F32)
    ind = S.sbuf("ind_s", [128, 2], F32)
    for z in range(2):
        S.dma("pool", maskA[:, z, :], G.maskA_d.v(G.maskA_d.t[z]))
        S.dma("pool", maskAT[:, z, :], G.maskAT_d.v(G.maskAT_d.t[z]))
        S.dma("sp", tri[:, z, :], G.tri_d.v(G.tri_d.t[z]))
    S.dma("sp", ind[:], G.ind_d[:])
    k_a_bc = load_bc("k_a_bc", G.rw_k_a)
    r_k_bc = load_bc("r_k_bc", G.rw_r_k)
    scr1 = S.sbuf("scr1", [128, D], F32)
    scr2 = S.sbuf("scr2", [128, D], F32)
    sg_sb = S.sbuf("sg_sb", [128, D], F32)
    kdir_sb = S.sbuf("kdir_sb", [128, D], BF16)
    b_sb = S.sbuf("b_sb", [128, D], BF16)
    tm = [S.sbuf(f"tm{i}", [128, D], BF16) for i in range(2)]
    R19 = S.sbuf("R19", [128, H, 128], BF16)
    Bh = S.sbuf("Bh", [128, D], BF16)
    Kh = S.sbuf("Kh", [128, D], BF16)
    arT = S.sbuf("arT", [128, 8, 2, 128], BF16)
    btT = S.sbuf("btT", [128, 8, 128], BF16)
    ktT = S.sbuf("ktT", [128, 8, 128], BF16)
    gC = S.sbuf("gC", [128, 8, 2], F32)
    bon = S.sbuf("bon", [128, 2, 16], F32)
    NSET = 4
    M1 = [S.sbuf(f"M1_{i}", [128, 256], BF16) for i in range(NSET)]
    M2 = [S.sbuf(f"M2_{i}", [128, 256], BF16) for i in range(NSET)]
    MabT = [S.sbuf(f"MabT_{i}", [128, 128], BF16) for i in range(NSET)]
    Pb = [[S.sbuf(f"Pb_{i}_{j}", [128, 128], BF16) for j in range(2)] for i in range(NSET)]
    PTb = [[S.sbuf(f"PTb_{i}_{j}", [128, 128], BF16) for j in range(2)] for i in range(NSET)]
    Tb = [S.sbuf(f"Tb_{i}", [128, 128], BF16) for i in range(NSET)]
    WP = [S.sbuf(f"WP_{i}", [128, 128], BF16) for i in range(NSET)]
    G_all = S.sbuf("G_all", [128, 8, 128], BF16)
    Y0_all = S.sbuf("Y0_all", [128, H, 64], BF16)
    D_all = S.sbuf("D_all", [128, 8, 2, 128], BF16)
    E_all = S.sbuf("E_all", [128, 8, 2, 128], BF16)
    Sb = S.sbuf("Sb", [128, 8, 128], BF16)
    S.memset("pool", D_all[:], 0.0)
    S.memset("pool", E_all[:], 0.0)
    yfw = S.sbuf("yfw", [128, 1040], F32)
    banks = list(G.psH) + list(G.psA_h)
    NBK = len(banks)
    bank_ctr = [0]
    slot_ctr = [0] * NBK

    def slot(n=1):
        bk = bank_ctr[0] % NBK
        bank_ctr[0] += 1
        if n == 2:
            i = ((slot_ctr[bk] + 1) // 2 * 2) % 4
            slot_ctr[bk] = i + 2
        else:
            i = slot_ctr[bk] % 4
            slot_ctr[bk] = i + 1
        return (bk, i)

    def psl(s, p0=0, p1=128, c0=0, c1=128, n=1):
        bk, i = s
        t = banks[bk]
        if n == 2:
            return t.v(t.t[p0:p1, i:i + 2, :].rearrange("p a b -> p (a b)")[:, c0:c1])
        return t.v(t.t[p0:p1, i, c0:c1])

    def transposes_to(src_tm, dst_view):
        for ec in range(8):
            S.tr(G.psT[:, ec * 128:(ec + 1) * 128], src_tm[:, ec * 128:(ec + 1) * 128], G.ident_b[:])
        S.copy("act", dst_view, G.psT.v(G.psT.t[:, :].rearrange("p (a b) -> p a b", b=128)))

    def dir_part(z, r_v, k_v, v_v, kk_v, a_v, chunk_order):
        zsl = slice(z, z + 1)
        S.stt(scr1[:], a_v, -1.0, k_a_bc[:], ALU.add, ALU.mult)
        S.stt(kdir_sb[:], scr1[:], 1.0, k_v, ALU.add, ALU.mult)
        S.tt("pool", b_sb[:], kk_v, a_v, ALU.mult)
        S.tt("pool", scr1[:], r_v, kdir_sb[:], ALU.mult)
        S.tt("pool", scr1[:], scr1[:], r_k_bc[:], ALU.mult)
        S.reduce(bon[:, z, :], scr1.v(scr1.t[:, :].rearrange("p (h n) -> p h n", n=64)))
        def cum(which):
            for n in range(2):
                S.mm(G.psA[:, n * 512:(n + 1) * 512], tri[:, z, which * 128:(which + 1) * 128], sg_sb[:, n * 512:(n + 1) * 512])
        cum(0)
        S.act(scr2[:], G.psA[:], AF.Exp)
        S.tt("dve", tm[0][:], r_v, scr2[:], ALU.mult)
        transposes_to(tm[0], arT.v(arT.t[:, :, 1, :]))
        S.act(scr2[:], G.psA[:], AF.Exp, scale=-1.0)
        S.tt("dve", tm[1][:], b_sb[:], scr2[:], ALU.mult)
        transposes_to(tm[1], btT[:])
        S.tt("dve", tm[0][:], kdir_sb[:], scr2[:], ALU.mult)
        transposes_to(tm[0], ktT[:])
        cum(1)
        S.act(scr2[:], G.psA[:], AF.Exp)
        S.stt(tm[1][:], kk_v, -1.0, scr2[:], ALU.mult, ALU.mult)
        S.copy("pool", V(R19.t[:, :, 64:128], [("R19", h) for h in range(H)]),
               tm[1].v(tm[1].t[:, :].rearrange("p (h n) -> p h n", n=64)))
        transposes_to(tm[1], arT.v(arT.t[:, :, 0, :]))
        cum(2)
        S.act(scr2[:], G.psA[:], AF.Exp)
        S.tt("dve", Bh[:], b_sb[:], scr2[:], ALU.mult)
        S.tt("pool", Kh[:], kdir_sb[:], scr2[:], ALU.mult)
        sg_ = slot()
        for ec in range(8):
            S.mm(psl(sg_, c0=ec * 2, c1=ec * 2 + 2), sg_sb[:, ec * 128:(ec + 1) * 128], ind[:])
        S.act(gC[:], banks[sg_[0]].v(banks[sg_[0]].t[:, sg_[1], 0:16].rearrange("p (a b) -> p a b", b=2)), AF.Exp)

        if do_dir < 2:
            return
        def head_gen(h):
            ec, po = h // 2, (h % 2) * 64
            hc = slice(h * 64, (h + 1) * 64)
            pr = slice(po, po + 64)
            i2 = h % NSET
            bt_h = btT[pr, ec, :]
            kt_h = ktT[pr, ec, :]
            ar_h = arT.v(arT.t[pr, ec, :, :].rearrange("p a b -> p (a b)"))
            at_h = arT[pr, ec, 0, :]
            rt_h = arT[pr, ec, 1, :]
            s1 = slot(2)
            S.mm(psl(s1, n=2, c1=256), bt_h, ar_h)
            S.tt("dve", M1[i2][:], psl(s1, n=2, c1=256), maskA[:, z, :], ALU.mult)
            s3 = slot()
            S.mm(psl(s3), at_h, bt_h)
            S.tt("dve", MabT[i2][:], psl(s3), maskAT[:, z, :], ALU.mult)
            s2 = slot(2)
            S.mm(psl(s2, n=2, c1=256), kt_h, ar_h)
            S.tt("dve", M2[i2][:], psl(s2, n=2, c1=256), maskA[:, z, :], ALU.mult)
            T = Tb[i2]
            S.tt("pool", T[:], M1[i2][:, 0:128], G.ident_b[:], ALU.add)
            P, PT = M1[i2][:, 0:128], MabT[i2][:]
            yield
            for kstep in range(1, 6):
                if kstep < 5:
                    sa = slot()
                    S.mm(psl(sa), PT, P)
                    P2 = Pb[i2][kstep % 2]
                    S.copy("act", P2[:], psl(sa))
                sb_ = slot()
                S.mm(psl(sb_), P, PT)
                P2T = PTb[i2][kstep % 2]
                S.copy("act", P2T[:], psl(sb_))
                if kstep == 1:
                    sx = slot()
                    S.mm(psl(sx, c1=64), M2[i2][:, 0:128], v_v_slice(v_v, hc))
                    S.copy("act", R19.k(h, (slice(None), h, slice(0, 64))), psl(sx, c1=64))
                yield
                sc_ = slot()
                S.mm(psl(sc_), P2T[:], T[:])
                S.tt("dve", T[:], T[:], psl(sc_), ALU.add)
                if kstep < 5:
                    P, PT = P2[:], P2T[:]
            yield
            sw = slot()
            S.mm(psl(sw), T[:], R19.k(h, (slice(None), h, slice(None))))
            S.copy("act", WP[i2][:], psl(sw))
            yield
            sg2 = slot()
            S.mm(psl(sg2, p0=po, p1=po + 64), WP[i2][:, 64:128], M1[i2][:, 128:256])
            S.tt("dve", G_all.k(h, (pr, ec, slice(None))), psl(sg2, p0=po, p1=po + 64), rt_h, ALU.add)
            sy = slot()
            S.mm(psl(sy, c1=64), M1[i2][:, 128:256], WP[i2][:, 0:64], start=True, stop=False)
            S.mm(psl(sy, c1=64), M2[i2][:, 128:256], v_v_slice(v_v, hc), start=False, stop=True)
            S.copy("act", Y0_all.k(h, (slice(None), h, slice(None))), psl(sy, c1=64))
            sds = [slot(), slot()]
            for c in range(2):
                cr = slice(c * 64, (c + 1) * 64)
                S.mm(psl(sds[c], p0=po, p1=po + 64, c1=64), WP[i2][cr, 64:128], Bh[cr, hc])
            for c in range(2):
                S.stt(D_all.k(h, (pr, ec, c, slice(po, po + 64))), G.ident_f[pr, po:po + 64], gC[pr, ec, c:c + 1],
                      psl(sds[c], p0=po, p1=po + 64, c1=64), ALU.mult, ALU.add)
            ses = [slot(), slot()]
            for c in range(2):
                cr = slice(c * 64, (c + 1) * 64)
                S.mm(psl(ses[c], p0=po, p1=po + 64, c1=64), Bh[cr, hc], WP[i2][cr, 0:64], start=True, stop=False)
                S.mm(psl(ses[c], p0=po, p1=po + 64, c1=64), Kh[cr, hc], v_v_slice(v_v, hc, cr), start=False, stop=True)
            for c in range(2):
                S.copy("act", E_all.k(h, (pr, ec, c, slice(po, po + 64))), psl(ses[c], p0=po, p1=po + 64, c1=64))

        pending = list(range(nheads))
        active = []
        rnd, last_admit = 0, -99
        while pending or active:
            if pending and len(active) <= NSET - 2 and (rnd - last_admit >= 4 or not active):
                for _ in range(2):
                    if pending:
                        active.append(head_gen(pending.pop(0)))
                last_admit = rnd
            nxt = []
            for g in active:
                try:
                    next(g)
                    nxt.append(g)
                except StopIteration:
                    pass
            active = nxt
            rnd += 1
        if do_dir < 3:
            return
        for c in chunk_order:
            for ec in range(8):
                pair = [2 * ec, 2 * ec + 1]
                Gv = V(G_all.t[:, ec, c * 64:(c + 1) * 64], [("G_all", h) for h in pair])
                Dv = V(D_all.t[:, ec, c, :], [("D_all", h) for h in pair])
                S.mm(G.psY.v(G.psY.t[c * 64:(c + 1) * 64, ec * 128:(ec + 1) * 128]), Gv, Sb[:, ec, :])
                S.mm(G.psA.v(G.psA.t[:, ec * 128:(ec + 1) * 128]), Dv, Sb[:, ec, :])
            S.tt("dve", Sb[:], G.psA.v(G.psA.t[:, :].rearrange("p (a b) -> p a b", b=128)),
                 V(E_all.t[:, :, c, :], [("E_all", h) for h in range(H)]), ALU.add)

    def v_v_slice(v_v, hc, rows=slice(None)):
        return V(v_v.ap[rows, hc], v_v.toks)

    S.push_scope()
    Wr, Wk, Wv = [S.sbuf(n, [128, 8, D], BF16) for n in ("Wr", "Wk", "Wv")]
    for nm, W in (("Wr", Wr), ("Wk", Wk), ("Wv", Wv)):
        S.dma("sp", W.v(W.t[:, :, :].rearrange("p a b -> p (a b)")), G.wbf[nm][:])
    w1 = S.sbuf("w1", [128, 2, 8, 64], BF16)
    a1 = S.sbuf("a1", [128, 2, 8, 64], BF16)
    g1 = S.sbuf("g1", [128, 8, 128], BF16)
    w2x = S.sbuf("w2x", [65, 2, D], BF16)
    a2x = S.sbuf("a2x", [65, 2, D], BF16)
    g2 = S.sbuf("g2", [128, D], BF16)
    for z in range(2):
        S.dma("pool", w1[:, z, :, :], G.rw_w1.v(G.rw_w1.t[z].rearrange("(kc p) n -> p kc n", p=128)))
        S.dma("pool", a1[:, z, :, :], G.rw_a1.v(G.rw_a1.t[z].rearrange("(kc p) n -> p kc n", p=128)))
        S.dma("pool", w2x[0:64, z, :], G.rw_w2.v(G.rw_w2.t[z]))
        S.dma("pool", w2x[64:65, z, :], G.rw_w0.v(G.rw_w0.t[z:z + 1, :]))
        S.dma("pool", a2x[0:64, z, :], G.rw_a2.v(G.rw_a2.t[z]))
        S.dma("pool", a2x[64:65, z, :], G.rw_a0.v(G.rw_a0.t[z:z + 1, :]))
    S.dma("pool", g1[:], G.rw_g1.v(G.rw_g1.t.rearrange("(kc p) n -> p kc n", p=128)))
    S.dma("pool", g2[:], G.rw_g2[:])
    k_k_bc = load_bc("k_k_bc", G.rw_k_k)
    hTc = S.sbuf("hTc", [128, 8, CTX + 2], BF16)
    hTl = S.sbuf("hTl", [128, 8, LAT + 2], BF16)
    xin = scr2
    wk = {"junk": scr1, "st": S.sbuf("st", [128, 4], F32), "xn": tm[0],
          "fm32": S.sbuf("fm32", [128, 8, 128], F32)}
    dxt = S.sbuf("dxt", [128, 8, 128], F32)
    mix = [S.sbuf(f"mix{i}", [128, 8, 128], BF16) for i in range(2)]
    cach = S.sbuf("cach", [128, 6, D], BF16)
    a0_sb = S.sbuf("a0_sb", [128, D], BF16)
    sg1_v = yfw[:, 0:D]
    hwx = S.sbuf("hwx", [65, 2, 128], BF16)
    hax = S.sbuf("hax", [65, 2, 128], BF16)
    hgs = S.sbuf("hgs", [128, 128], BF16)
    st2 = S.sbuf("st2", [128, 3, 16], F32)
    S.memset("dve", hwx[:], 1.0)
    S.memset("dve", hax[:], 1.0)
    for hT in (hTc, hTl):
        S.memset("pool", hT[:], 0.0)

    mix_ctr = [0]

    def make_mix(hT, c0, j):
        m = mix[mix_ctr[0] % 2]
        mix_ctr[0] += 1
        mu = G.pfm.v(G.pfm.t[:, PV_MU + j * 8:PV_MU + j * 8 + 8].unsqueeze(2).to_broadcast([128, 8, 128]))
        S.tt("pool", wk["fm32"][:], dxt[:], mu, ALU.mult)
        S.tt("pool", m[:], wk["fm32"][:], hT[:, :, c0:c0 + 128], ALU.add)
        return m

    def proj_tm(ps, m, W):
        for n in range(2):
            for kc in range(8):
                S.mm(ps[:, n * 512:(n + 1) * 512], m[:, kc, :], W[:, kc, n * 512:(n + 1) * 512], start=(kc == 0), stop=(kc == 7))

    for b in range(nb):
        for ti in range(NT):
            if ti < 2:
                src, r, hT, t0 = G.ctx.v(G.ctx.t[b, ti * 128:(ti + 1) * 128, :]), 2, hTc, ti * 128
            else:
                src, r, hT, t0 = G.x.v(G.x.t[b, (ti - 2) * 128:(ti - 1) * 128, :]), b, hTl, (ti - 2) * 128
            S.dma("sp", xin[:], src, split=4)
            norm_tile_to_fm(S, G, xin[:], r, G.A1, 0, hT[:, :, t0 + 1:t0 + 129], wk)
        if dbg is not None and "hT" in dbg and b == 0:
            S.dma("sp", dbg["hT"][:], hTl[:])
        S.memset("dve", Sb[:], 0.0)
        for ti in range(nt0):
            hT, t0 = (hTc, ti * 128) if ti < 2 else (hTl, (ti - 2) * 128)
            c0 = t0 + 1
            S.tt("dve", dxt[:], hT[:, :, c0 - 1:c0 + 127], hT[:, :, c0 + 1:c0 + 129], ALU.add)
            S.stt(dxt[:], dxt[:], 0.5, hT[:, :, c0:c0 + 128], ALU.mult, ALU.subtract)
            if fl < 1:
                continue
            m = make_mix(hT, c0, 0)
            proj_tm(G.psA, m, Wr)
            S.copy("act", cach[:, 0, :], G.psA[:])
            if fl < 2:
                continue
            m = make_mix(hT, c0, 2)
            proj_tm(G.psY, m, Wv)
            S.copy("act", cach[:, 2, :], G.psY[:])
            if fl < 3:
                continue
            m = make_mix(hT, c0, 4)
            for z in range(2):
                sl_ = slot()
                for kc in range(8):
                    S.mm(psl(sl_, p1=64), a1[:, z, kc, :], m[:, kc, :], start=(kc == 0), stop=(kc == 7))
                S.copy("act", hax[0:64, z, :], psl(sl_, p1=64))
            for z in range(2):
                ps = G.psA if z == 0 else G.psY
                for n in range(2):
                    S.mm(ps[:, n * 512:(n + 1) * 512], hax[:, z, :], a2x[:, z, n * 512:(n + 1) * 512])
                S.act(a0_sb[:] if z == 0 else cach[:, 4, :], ps[:], AF.Sigmoid)
            if fl < 4:
                continue
            m = make_mix(hT, c0, 3)
            for z in range(2):
                sl_ = slot()
                for kc in range(8):
                    S.mm(psl(sl_, p1=64), w1[:, z, kc, :], m[:, kc, :], start=(kc == 0), stop=(kc == 7))
                S.act(hwx[0:64, z, :], psl(sl_, p1=64), AF.Tanh)
            for z in range(2):
                ps = G.psA if z == 0 else G.psY
                for n in range(2):
                    S.mm(ps[:, n * 512:(n + 1) * 512], hwx[:, z, :], w2x[:, z, n * 512:(n + 1) * 512])
                S.act(sg_sb[:] if z == 0 else sg1_v, ps[:], AF.Sigmoid)
            if fl < 5:
                continue
            m = make_mix(hT, c0, 5)
            sl_ = slot()
            for kc in range(8):
                S.mm(psl(sl_), g1[:, kc, :], m[:, kc, :], start=(kc == 0), stop=(kc == 7))
            S.act(hgs[:], psl(sl_), AF.Sigmoid)
            for n in range(2):
                S.mm(G.psY[:, n * 512:(n + 1) * 512], hgs[:], g2[:, n * 512:(n + 1) * 512])
            S.copy("act", cach[:, 5, :], G.psY[:])
            if fl < 6:
                continue
            m = make_mix(hT, c0, 1)
            proj_tm(G.psA, m, Wk)
            S.copy("act", cach[:, 1, :], G.psA[:])
            if fl < 6.1:
                continue
            S.tt("dve", scr1[:], G.psA[:], k_k_bc[:], ALU.mult)
            if fl < 6.2:
                continue
            S.act(scr2[:], scr1[:], AF.Square)
            S.reduce(st2[:, 0, :], scr2.v(scr2.t[:, :].rearrange("p (h n) -> p h n", n=64)))
            if fl < 6.3:
                continue
            S.ts("dve", st2[:, 1, :], st2[:, 0, :], 1e-12, None, op0=ALU.add)
            S.act(st2[:, 1, :], st2[:, 1, :], AF.Sqrt)
            S.recip(st2[:, 2, :], st2[:, 1, :])
            if fl < 6.4:
                continue
            S.tt("dve", cach.v(cach.t[:, 3, :].rearrange("p (h n) -> p h n", n=64)),
                 scr1.v(scr1.t[:, :].rearrange("p (h n) -> p h n", n=64)),
                 st2.v(st2.t[:, 2, :].unsqueeze(2).to_broadcast([128, 16, 64])), ALU.mult)
            if fl < 7:
                continue
            S.dma("sp", cache_d.v(cache_d.t[b, ti].rearrange("p (a n) -> p a n", n=D)), cach[:])
            S.dma("sp", sg1_d.v(sg1_d.t[b, ti]), sg1_v)
            if do_dir:
                dir_part(0, cach[:, 0, :], G.psA[:], cach[:, 2, :], cach[:, 3, :], a0_sb[:], (0, 1))
            S.tt("dve", yfw[:, 0:D], G.psY[:],
                 V(Y0_all.t[:, :, :].rearrange("p h n -> p (h n)"), [("Y0_all", h) for h in range(H)]), ALU.add)
            S.copy("pool", yfw[:, D:D + 16], bon[:, 0, :])
            S.dma("sp", yf_d.v(yf_d.t[b, ti]), yfw[:])
    S.pop_scope()

    S.push_scope()
    Wo = S.sbuf("Wo", [128, 8, D], BF16)
    S.dma("sp", Wo.v(Wo.t[:, :, :].rearrange("p a b -> p (a b)")), G.wbf["rwWo"][:])
    lnx_g_bc = load_bc("lnx_g_bc", G.rw_lnx_g)
    lnx_b_bc = load_bc("lnx_b_bc", G.rw_lnx_b)
    gate_bc = S.sbuf("gate_bc", [128, D], F32)
    cach = S.sbuf("cach1", [128, 6, D], BF16)
    xres = S.sbuf("xres", [128, D], F32)
    pre = S.sbuf("pre", [128, D], BF16)
    preT = S.sbuf("preT", [128, 8, 128], BF16)
    st3 = S.sbuf("st3", [128, 4, 16], F32)
    for b in range(nb):
        S.memset("dve", Sb[:], 0.0)
        order = ([1, 0] + list(range(NT - 1, 1, -1)))[:nt1]
        cur_r = None
        for ti in order:
            r = 2 if ti < 2 else b
            if r != cur_r:
                S.dma("sp", gate_bc[:], G.gates_d.v(G.gates_d.t[0, r:r + 1, :].partition_broadcast(128)))
                cur_r = r
            S.dma("sp", cach[:], cache_d.v(cache_d.t[b, ti].rearrange("p (a n) -> p a n", n=D)))
            S.dma("sp", sg_sb[:], sg1_d.v(sg1_d.t[b, ti]))
            S.dma("sp", yfw[:], yf_d.v(yf_d.t[b, ti]))
            dir_part(1, cach[:, 0, :], cach[:, 1, :], cach[:, 2, :], cach[:, 3, :], cach[:, 4, :], (1, 0))
            S.tt("dve", scr1[:], G.psY[:], V(Y0_all.t[:, :, :].rearrange("p h n -> p (h n)"), [("Y0_all", h) for h in range(H)]), ALU.add)
            S.tt("pool", scr1[:], scr1[:], yfw[:, 0:D], ALU.add)
            y3 = scr1.v(scr1.t[:, :].rearrange("p (h n) -> p h n", n=64))
            S.reduce(st3[:, 0, :], y3)
            S.ts("dve", st3[:, 0, :], st3[:, 0, :], 1.0 / 64, None, op0=ALU.mult)
            S.tt("dve", y3, y3, st3.v(st3.t[:, 0, :].unsqueeze(2).to_broadcast([128, 16, 64])), ALU.subtract)
            S.act(scr2[:], scr1[:], AF.Square)
            S.reduce(st3[:, 1, :], scr2.v(scr2.t[:, :].rearrange("p (h n) -> p h n", n=64)))
            S.ts("dve", st3[:, 1, :], st3[:, 1, :], 1.0 / 64, LNX_EPS, op0=ALU.mult, op1=ALU.add)
            S.act(st3[:, 1, :], st3[:, 1, :], AF.Sqrt)
            S.recip(st3[:, 2, :], st3[:, 1, :])
            S.tt("dve", y3, y3, st3.v(st3.t[:, 2, :].unsqueeze(2).to_broadcast([128, 16, 64])), ALU.mult)
            S.tt("pool", scr1[:], scr1[:], lnx_g_bc[:], ALU.mult)
            S.tt("pool", scr1[:], scr1[:], lnx_b_bc[:], ALU.add)
            S.tt("dve", st3[:, 3, :], bon[:, 1, :], yfw[:, D:D + 16], ALU.add)
            S.tt("dve", scr2.v(scr2.t[:, :].rearrange("p (h n) -> p h n", n=64)),
                 cach.v(cach.t[:, 2, :].rearrange("p (h n) -> p h n", n=64)),
                 st3.v(st3.t[:, 3, :].unsqueeze(2).to_broadcast([128, 16, 64])), ALU.mult)
            S.tt("pool", scr1[:], scr1[:], scr2[:], ALU.add)
            S.tt("pool", pre[:], scr1[:], cach[:, 5, :], ALU.mult)
            transposes_to(pre, preT[:])
            for n in range(2):
                for kc in range(8):
                    S.mm(G.psA[:, n * 512:(n + 1) * 512], preT[:, kc, :], Wo[:, kc, n * 512:(n + 1) * 512], start=(kc == 0), stop=(kc == 7))
            if ti < 2:
                xsrc = G.ctx.v(G.ctx.t[b, ti * 128:(ti + 1) * 128, :])
            else:
                xsrc = G.x.v(G.x.t[b, (ti - 2) * 128:(ti - 1) * 128, :])
            S.dma("sp", xres[:], xsrc)
            S.tt("dve", scr2[:], G.psA[:], gate_bc[:], ALU.mult)
            S.tt("pool", xres[:], xres[:], scr2[:], ALU.add)
            S.dma("sp", x1_d.v(x1_d.t[b, ti * 128:(ti + 1) * 128, :]), xres[:])
    S.pop_scope()
    S.pop_scope()

def moe_phase(S, G, li, xin_d, tiles, xout_fn, st_tiles, npairs=16, dbg=None):
    L = f"e{li}"
    S.push_scope()
    ysub = [S.psum(f"ysub{i}{L}", [128, 1024], F32) for i in range(2)]
    psG = [S.psum(f"psG{i}{L}", [128, 512], F32) for i in range(2)]
    psU = [S.psum(f"psU{i}{L}", [128, 512], F32) for i in range(2)]
    G.psA = ysub[0]
    STK = st_tiles * 128
    h2T = S.sbuf(f"h2T{L}", [128, 8, STK], BF16)
    y_acc = S.sbuf(f"yacc{L}", [128, st_tiles, D], F32)
    gatesT = S.sbuf(f"gatesT{L}", [32, STK], BF16)
    sel = S.sbuf(f"sel{L}", [32, 32, 128], BF16)
    S.dma("pool", sel[:], G.sel_d.v(G.sel_d.t[:, :].rearrange("p (a b) -> p a b", b=128)))
    Wrt = S.sbuf(f"Wrt{L}", [128, 8, 36], F32)
    S.dma("sp", Wrt[:], G.moe_router.v(G.moe_router.t[li].rearrange("(kc p) n -> p kc n", p=128)))
    rb = S.sbuf(f"rb{L}", [1, 36], F32)
    S.dma("sp", rb[:], G.moe_router_b.v(G.moe_router_b.t[li]))
    gate_bc = S.sbuf(f"gbc{L}", [128, 3, D], F32)
    rs_used = sorted(set(t[2] for t in tiles))
    for r in rs_used:
        S.dma("sp", gate_bc[:, r, :], G.gates_d.v(G.gates_d.t[1, r:r + 1, :].partition_broadcast(128)))
    Wg = [[S.sbuf(f"Wg{i}{e}{L}", [128, 8, DFF], BF16) for e in range(2)] for i in range(2)]
    Wu = [[S.sbuf(f"Wu{i}{e}{L}", [128, 8, DFF], BF16) for e in range(2)] for i in range(2)]
    Wd = [[S.sbuf(f"Wd{i}{e}{L}", [128, 2, D], BF16) for e in range(2)] for i in range(2)]
    NS1 = 2
    xin_s = [S.sbuf(f"xin{i}{L}", [128, D], F32) for i in range(NS1)]
    junk_s = [S.sbuf(f"junk{i}{L}", [128, D], F32) for i in range(NS1)]
    wk_s = [{"junk": junk_s[i], "st": S.sbuf(f"st{i}{L}", [128, 4], F32), "xn32": S.sbuf(f"xn32{i}{L}", [128, D], F32),
             "fm32": S.sbuf(f"fm32{i}{L}", [128, 8, 128], F32)} for i in range(NS1)]
    h32_s = [S.sbuf(f"h32{i}{L}", [128, 8, 128], F32) for i in range(NS1)]
    lg_s = [S.sbuf(f"lg{i}{L}", [128, 36], F32) for i in range(NS1)]
    sm_s = [S.sbuf(f"sm{i}{L}", [128, 64], F32) for i in range(NS1)]
    g32_s = [S.sbuf(f"g32{i}{L}", [128, 32], F32) for i in range(NS1)]
    xin3 = S.sbuf(f"xin3{L}", [128, D], F32)
    out3 = S.sbuf(f"out3{L}", [128, D], F32)
    s_sb = [S.sbuf(f"s_sb{i}{L}", [128, 256], F32) for i in range(2)]
    t_sb = [S.sbuf(f"t_sb{i}{L}", [128, 256], F32) for i in range(2)]
    hidT = [S.sbuf(f"hidT{i}{L}", [128, 256], BF16) for i in range(2)]

    def load_pair(p, buf):
        for e in range(2):
            eg = p * 2 + e
            for W, nm in ((Wg, "g"), (Wu, "u"), (Wd, "d")):
                t = W[buf][e]
                S.dma("sp", t.v(t.t[:, :, :].rearrange("p a b -> p (a b)")), G.wbf[f"{nm}{li}_{eg}"][:])

    n_super = len(tiles) // st_tiles
    assert n_super * st_tiles == len(tiles) and st_tiles % 2 == 0
    for su in range(n_super):
        stl = tiles[su * st_tiles:(su + 1) * st_tiles]
        load_pair(0, 0)
        def step1_gen(j, b, row0, r, s):
            xin, wk, h32, lg, sm, g32 = xin_s[s], wk_s[s], h32_s[s], lg_s[s], sm_s[s], g32_s[s]
            S.dma("sp", xin[:], xin_d.v(xin_d.t[b, row0:row0 + 128, :]), split=4)
            yield
            G.psA = ysub[s]
            norm_tile_to_fm(S, G, xin[:], r, G.A2, 24, h2T[:, :, j * 128:(j + 1) * 128], wk, fp32_out=h32[:])
            yield
            psr = (psG[0] if s == 0 else psU[0])[:, 0:36]
            for kc in range(8):
                S.mm(psr, h32[:, kc, :], Wrt[:, kc, :], start=(kc == 0), stop=False)
            S.mm(psr, G.ones_f[0:1, :], rb[:], start=False, stop=True)
            S.copy("dve", lg[:], psr)
            yield
            c = lambda i, n=1: sm[:, i:i + n]
            S.reduce(c(0), lg[:, 0:4], op=ALU.max)
            yield
            S.ts("dve", c(1), c(0), -1.0, None, op0=ALU.mult)
            yield
            S.ts("dve", c(4, 4), lg[:, 0:4], c(0), None, op0=ALU.is_ge)
            yield
            S.act(c(8, 4), lg[:, 0:4], AF.Exp, bias=c(1), accum_out=c(2))
            yield
            S.recip(c(3), c(2))
            yield
            S.ts("dve", c(16, 8), lg[:, 4:12], c(4), None, op0=ALU.mult)
            yield
            for g in range(1, 4):
                S.stt(c(16, 8), lg[:, 4 + 8 * g:12 + 8 * g], c(4 + g), c(16, 8), ALU.mult, ALU.add)
                yield
            S.reduce(c(12), c(16, 8), op=ALU.max)
            yield
            S.ts("dve", c(24, 8), c(16, 8), c(12), None, op0=ALU.is_ge)
            yield
            S.stt(c(32, 8), c(24, 8), -1e30, c(16, 8), ALU.mult, ALU.add)
            yield
            S.reduce(c(13), c(32, 8), op=ALU.max)
            yield
            S.ts("dve", c(40, 8), c(32, 8), c(13), None, op0=ALU.is_ge)
            yield
            S.tt("dve", c(14), c(13), c(12), ALU.subtract)
            yield
            S.act(c(15), c(14), AF.Exp)
            yield
            S.ts("dve", c(48), c(15), 1.0, None, op0=ALU.add)
            yield
            S.recip(c(49), c(48))
            yield
            S.tt("dve", c(50), c(49), c(3), ALU.mult)
            yield
            S.tt("dve", c(51), c(50), c(15), ALU.mult)
            yield
            S.ts("dve", c(52, 8), c(24, 8), c(50), None, op0=ALU.mult)
            yield
            S.stt(c(52, 8), c(40, 8), c(51), c(52, 8), ALU.mult, ALU.add)
            yield
            for g in range(4):
                S.ts("dve", g32[:, g * 8:(g + 1) * 8], c(52, 8), c(4 + g), None, op0=ALU.mult)
                yield
            pst = (psG[1] if s == 0 else psU[1])[0:32, 0:128]
            S.tr(pst, g32[:], G.ident_f[:])
            S.copy("dve", gatesT[:, j * 128:(j + 1) * 128], pst)
            if dbg is not None and "gates" in dbg and su == 0:
                S.dma("sp", dbg["gates"].v(dbg["gates"].t[j]), g32[:])

        pend1 = [step1_gen(j, b, row0, r, j % NS1) for j, (b, row0, r) in enumerate(stl)]
        act1 = []
        rnd, last1 = 0, -99
        while pend1 or act1:
            if pend1 and len(act1) < NS1 and (rnd - last1 >= 17 or not act1):
                act1.append(pend1.pop(0))
                last1 = rnd
            nxt = []
            for g_ in act1:
                try:
                    next(g_)
                    nxt.append(g_)
                except StopIteration:
                    pass
            act1 = nxt
            rnd += 1
        G.psA = ysub[0]
        items = [(p, t2, e, fc) for p in range(npairs) for t2 in range(st_tiles // 2) for e in range(2) for fc in range(2)]

        def emit_gu(idx):
            p, t2, e, fc = items[idx]
            buf, eg, ib = p % 2, p * 2 + e, idx % 2
            tok = slice(t2 * 256, (t2 + 1) * 256)
            pg, pu = psG[ib], psU[ib]
            for kc in range(8):
                S.mm(pg[:, 0:256], Wg[buf][e][:, kc, fc * 128:(fc + 1) * 128], h2T[:, kc, tok], start=(kc == 0), stop=(kc == 7))
            for kc in range(8):
                S.mm(pu[:, 0:256], Wu[buf][e][:, kc, fc * 128:(fc + 1) * 128], h2T[:, kc, tok], start=(kc == 0), stop=(kc == 7))
            S.mm(pu[:, 256:512], sel[:, eg, :], gatesT[:, tok])
            S.act(s_sb[ib][:], pg[:, 0:256], AF.Silu)
            S.tt("dve", t_sb[ib][:], s_sb[ib][:], pu[:, 0:256], ALU.mult)
            S.tt("dve", hidT[ib][:], t_sb[ib][:], pu[:, 256:512], ALU.mult)

        def emit_down(idx):
            p, t2, e, fc = items[idx]
            buf, ib, it = p % 2, idx % 2, e * 2 + fc
            for ts_ in range(2):
                for n in range(2):
                    S.mm(ysub[ts_][:, n * 512:(n + 1) * 512], hidT[ib][:, ts_ * 128:(ts_ + 1) * 128],
                         Wd[buf][e][:, fc, n * 512:(n + 1) * 512], start=(it == 0), stop=(it == 3))
            if it == 3:
                for ts_ in range(2):
                    j = t2 * 2 + ts_
                    if p == 0:
                        S.copy("dve", y_acc[:, j, :], ysub[ts_][:])
                    else:
                        S.tt("dve", y_acc[:, j, :], y_acc[:, j, :], ysub[ts_][:], ALU.add)
                    if p == npairs - 1:
                        b, row0, r = stl[j]
                        S.dma("sp", xin3[:], xin_d.v(xin_d.t[b, row0:row0 + 128, :]))
                        S.tt("pool", out3[:], y_acc[:, j, :], gate_bc[:, r, :], ALU.mult)
                        S.tt("pool", out3[:], out3[:], xin3[:], ALU.add)
                        S.dma("sp", xout_fn(b, row0), out3[:])

        for idx in range(len(items)):
            emit_gu(idx)
            if idx > 0:
                emit_down(idx - 1)
            p, t2, e, fc = items[idx]
            if t2 == 0 and e == 0 and fc == 0 and p + 1 < npairs:
                load_pair(p + 1, (p + 1) % 2)
        emit_down(len(items) - 1)
    S.pop_scope()

LAM_INIT1 = 0.8 - 0.6 * float(np.exp(-0.3 * 1))


def attn_phase(S, G, x2_d, x3_d, nb=NB, nqt=4, nh=8):
    li = 1
    NKT = NT
    S.push_scope()
    psA = S.psum("psA_a", [128, 1024], F32)
    psT = S.psum("psT_a", [128, 1024], BF16)
    psQ = [S.psum(f"psQ{i}_a", [128, 512], F32) for i in range(3)]
    psO = [S.psum(f"psO{i}_a", [128, 512], F32) for i in range(2)]
    G.psA, G.psT = psA, psT
    KT_all = S.sbuf("KT_all", [128, 8, TOK], BF16)
    QT_all = S.sbuf("QT_all", [128, 8, LAT], BF16)
    V_all = S.sbuf("V_all", [128, NKT, 8, 130], BF16)
    S.memset("pool", V_all[:], 1.0)
    gq_bc = S.sbuf("gq_bc", [128, 64], F32)
    gk_bc = S.sbuf("gk_bc", [128, 64], F32)
    sg_bc = S.sbuf("sg_bc", [128, 128], F32)
    S.dma("sp", gq_bc[:], G.da_q_norm_g.v(G.da_q_norm_g.t[0:1, :].partition_broadcast(128)))
    S.dma("sp", gk_bc[:], G.da_k_norm_g.v(G.da_k_norm_g.t[0:1, :].partition_broadcast(128)))
    S.dma("sp", sg_bc[:], G.da_subln_g.v(G.da_subln_g.t[0:1, :].partition_broadcast(128)))
    S.ts("dve", sg_bc[:], sg_bc[:], 1.0 - LAM_INIT1, None, op0=ALU.mult)
    lamv = S.sbuf("lamv", [128, 4, 64], F32)
    lsm = S.sbuf("lsm", [128, 8], F32)
    for i in range(4):
        S.dma("sp", lamv[:, i, :], G.da_lam.v(G.da_lam.t[i:i + 1, :].partition_broadcast(128)))
    S.tt("dve", lamv[:, 0, :], lamv[:, 0, :], lamv[:, 1, :], ALU.mult)
    S.tt("dve", lamv[:, 2, :], lamv[:, 2, :], lamv[:, 3, :], ALU.mult)
    S.reduce(lsm[:, 0:1], lamv[:, 0, :])
    S.reduce(lsm[:, 1:2], lamv[:, 2, :])
    S.act(lsm[:, 2:4], lsm[:, 0:2], AF.Exp)
    S.tt("dve", lsm[:, 4:5], lsm[:, 3:4], lsm[:, 2:3], ALU.subtract)
    S.ts("dve", lsm[:, 5:6], lsm[:, 4:5], -LAM_INIT1, None, op0=ALU.add)
    neglam = lsm[:, 5:6]
    junk = S.sbuf("junk_a", [128, D], F32)
    scrq = S.sbuf("scrq", [128, D], F32)
    xin = S.sbuf("xin_a", [128, D], F32)
    st = S.sbuf("st_a", [128, 4], F32)
    st16 = S.sbuf("st16_a", [128, 3, 16], F32)
    for b in range(nb):
        S.push_scope()
        Wqkv = S.sbuf(f"Wqkv{b}", [128, 8, 3 * D], BF16)
        S.dma("sp", Wqkv.v(Wqkv.t[:, :, :].rearrange("p a b -> p (a b)")), G.wbf["Wqkv"][:])
        hT_t = S.sbuf(f"hT_t{b}", [128, 8, 128], BF16)
        outq = S.sbuf(f"outq{b}", [128, D], BF16)
        wk = {"junk": junk, "st": st, "xn": S.sbuf(f"xn_a{b}", [128, D], BF16), "fm32": S.sbuf(f"fm32_a{b}", [128, 8, 128], F32)}
        cs_t = S.sbuf(f"cs_t{b}", [128, 2, 32], F32)
        tmpa = S.sbuf(f"tmpa{b}", [128, 512], F32)
        tmpb = S.sbuf(f"tmpb{b}", [128, 512], F32)

        def qk_norm(gain_bc, rope, dst_fm):
            S.act(junk[:], psA[:], AF.Square)
            S.reduce(st16[:, 0, :], junk.v(junk.t[:, :].rearrange("p (g n) -> p g n", n=64)))
            S.ts("dve", st16[:, 1, :], st16[:, 0, :], 1.0 / 64, EPS, op0=ALU.mult, op1=ALU.add)
            S.act(st16[:, 1, :], st16[:, 1, :], AF.Sqrt)
            S.recip(st16[:, 2, :], st16[:, 1, :])
            S.tt("dve", scrq.v(scrq.t[:, :].rearrange("p (g n) -> p g n", n=64)),
                 psA.v(psA.t[:, :].rearrange("p (g n) -> p g n", n=64)),
                 st16.v(st16.t[:, 2, :].unsqueeze(2).to_broadcast([128, 16, 64])), ALU.mult)
            gv = gain_bc.v(gain_bc.t[:, :].unsqueeze(1).to_broadcast([128, 16, 64]))
            if not rope:
                S.tt("pool", outq.v(outq.t[:, :].rearrange("p (g n) -> p g n", n=64)),
                     scrq.v(scrq.t[:, :].rearrange("p (g n) -> p g n", n=64)), gv, ALU.mult)
            else:
                S.tt("pool", scrq.v(scrq.t[:, :].rearrange("p (g n) -> p g n", n=64)),
                     scrq.v(scrq.t[:, :].rearrange("p (g n) -> p g n", n=64)), gv, ALU.mult)
                x5 = scrq.t[:, :].rearrange("p (g a h f) -> p g a h f", g=16, a=2, h=2)
                o5 = outq.t[:, :].rearrange("p (g a h f) -> p g a h f", g=16, a=2, h=2)
                x1, x2 = scrq.v(x5[:, :, :, 0, :]), scrq.v(x5[:, :, :, 1, :])
                o1, o2 = outq.v(o5[:, :, :, 0, :]), outq.v(o5[:, :, :, 1, :])
                cv = cs_t.v(cs_t.t[:, 0, :].rearrange("p (a f) -> p a f", a=2).unsqueeze(1).to_broadcast([128, 16, 2, 16]))
                sv = cs_t.v(cs_t.t[:, 1, :].rearrange("p (a f) -> p a f", a=2).unsqueeze(1).to_broadcast([128, 16, 2, 16]))
                ta = tmpa.v(tmpa.t[:, :].rearrange("p (g a f) -> p g a f", g=16, a=2))
                tb = tmpb.v(tmpb.t[:, :].rearrange("p (g a f) -> p g a f", g=16, a=2))
                S.tt("dve", ta, x1, cv, ALU.mult)
                S.tt("pool", tb, x2, sv, ALU.mult)
                S.tt("dve", o1, ta, tb, ALU.subtract)
                S.tt("dve", ta, x1, sv, ALU.mult)
                S.tt("pool", tb, x2, cv, ALU.mult)
                S.tt("pool", o2, ta, tb, ALU.add)
            for ec in range(8):
                S.tr(psT[:, ec * 128:(ec + 1) * 128], outq[:, ec * 128:(ec + 1) * 128], G.ident_b[:])
            S.copy("act", dst_fm, psT.v(psT.t[:, :].rearrange("p (a b) -> p a b", b=128)))

        def proj(c0):
            for n in range(2):
                for kc in range(8):
                    S.mm(psA[:, n * 512:(n + 1) * 512], hT_t[:, kc, :], Wqkv[:, kc, c0 + n * 512:c0 + (n + 1) * 512], start=(kc == 0), stop=(kc == 7))

        for ti in range(NT):
            r = 2 if ti < 2 else b
            S.dma("sp", xin[:], x2_d.v(x2_d.t[b, ti * 128:(ti + 1) * 128, :]), split=4)
            norm_tile_to_fm(S, G, xin[:], r, G.A1, 0, hT_t[:], wk)
            lat = ti >= 2
            if lat:
                t0 = (ti - 2) * 128
                S.dma("sp", cs_t[:, 0, :], G.rope_cos_d.v(G.rope_cos_d.t[t0:t0 + 128, :]))
                S.dma("sp", cs_t[:, 1, :], G.rope_sin_d.v(G.rope_sin_d.t[t0:t0 + 128, :]))
            proj(D)
            qk_norm(gk_bc, lat, KT_all[:, :, ti * 128:(ti + 1) * 128])
            proj(2 * D)
            S.copy("act", V_all[:, ti, :, 0:128], psA.v(psA.t[:, :].rearrange("p (h n) -> p h n", n=128)))
            if lat:
                proj(0)
                qk_norm(gq_bc, True, QT_all[:, :, t0:t0 + 128])
        S.pop_scope()
        S.push_scope()
        Wo = S.sbuf(f"Wo_a{b}", [128, 8, D], BF16)
        S.dma("sp", Wo.v(Wo.t[:, :, :].rearrange("p a b -> p (a b)")), G.wbf["daWo"][:])
        gate_bc = S.sbuf(f"gate_a{b}", [128, D], F32)
        S.dma("sp", gate_bc[:], G.gates_d.v(G.gates_d.t[0, b:b + 1, :].partition_broadcast(128)))
        O_all = S.sbuf(f"O_all{b}", [128, 4, D], F32)
        pT = [S.sbuf(f"pT{i}_{b}", [128, 512], BF16) for i in range(3)]
        rec = S.sbuf(f"rec{b}", [128, 8], F32)
        pre = S.sbuf(f"pre_a{b}", [128, D], BF16)
        preT = S.sbuf(f"preT_a{b}", [128, 8, 128], BF16)
        st8 = S.sbuf(f"st8_{b}", [128, 3, 8], F32)
        aitems = [(qt, h, m, kt) for qt in range(nqt) for h in range(nh) for m in range(2) for kt in range(NKT)]

        def emit_qk(i):
            qt, h, m, kt = aitems[i]
            pr = slice(m * 64, m * 64 + 64)
            ps = psQ[i % 3]
            S.mm(ps[:], KT_all[pr, h, kt * 128:(kt + 1) * 128], QT_all[pr, h, qt * 512:(qt + 1) * 512])
            S.act(pT[i % 3][:], ps[:], AF.Exp, scale=0.125)

        def emit_pv(i):
            qt, h, m, kt = aitems[i]
            for qs in range(4):
                acc = psO[qs // 2][:, (qs % 2) * 256:(qs % 2) * 256 + 129]
                S.mm(acc, pT[i % 3][:, qs * 128:(qs + 1) * 128], V_all[:, kt, h, 0:129],
                     start=(kt == 0 and qs % 2 == 0), stop=(kt == NKT - 1), skip_group_check=True)
            if kt != NKT - 1:
                return
            for qs in range(4):
                c0 = (qs % 2) * 256
                S.recip(rec[:, qs:qs + 1], psO[qs // 2][:, c0 + 128:c0 + 129])
                if m == 0:
                    S.ts("dve", O_all[:, qs, h * 128:(h + 1) * 128], psO[qs // 2][:, c0:c0 + 128], rec[:, qs:qs + 1], None, op0=ALU.mult)
                else:
                    S.tt("dve", rec[:, 4 + qs:5 + qs], rec[:, qs:qs + 1], neglam, ALU.mult)
                    S.stt(O_all[:, qs, h * 128:(h + 1) * 128], psO[qs // 2][:, c0:c0 + 128], rec[:, 4 + qs:5 + qs],
                          O_all[:, qs, h * 128:(h + 1) * 128], ALU.mult, ALU.add)
            if not (h == nh - 1 and m == 1):
                return
            for qs in range(4):
                O3 = O_all.v(O_all.t[:, qs, :].rearrange("p (h n) -> p h n", n=128))
                S.act(junk[:], O_all[:, qs, :], AF.Square)
                S.reduce(st8[:, 0, :], junk.v(junk.t[:, :].rearrange("p (h n) -> p h n", n=128)))
                S.ts("dve", st8[:, 1, :], st8[:, 0, :], 1.0 / 128, EPS, op0=ALU.mult, op1=ALU.add)
                S.act(st8[:, 1, :], st8[:, 1, :], AF.Sqrt)
                S.recip(st8[:, 2, :], st8[:, 1, :])
                S.tt("dve", O3, O3, st8.v(st8.t[:, 2, :].unsqueeze(2).to_broadcast([128, 8, 128])), ALU.mult)
                S.tt("pool", pre.v(pre.t[:, :].rearrange("p (h n) -> p h n", n=128)), O3,
                     sg_bc.v(sg_bc.t[:, :].unsqueeze(1).to_broadcast([128, 8, 128])), ALU.mult)
                for ec in range(8):
                    S.tr(psT[:, ec * 128:(ec + 1) * 128], pre[:, ec * 128:(ec + 1) * 128], G.ident_b[:])
                S.copy("act", preT[:], psT.v(psT.t[:, :].rearrange("p (a b) -> p a b", b=128)))
                for n in range(2):
                    for kc in range(8):
                        S.mm(psA[:, n * 512:(n + 1) * 512], preT[:, kc, :], Wo[:, kc, n * 512:(n + 1) * 512], start=(kc == 0), stop=(kc == 7))
                row = (qt * 4 + qs) * 128
                S.dma("sp", xin[:], x2_d.v(x2_d.t[b, CTX + row:CTX + row + 128, :]))
                S.tt("dve", scrq[:], psA[:], gate_bc[:], ALU.mult)
                S.tt("pool", xin[:], xin[:], scrq[:], ALU.add)
                S.dma("sp", x3_d.v(x3_d.t[b, row:row + 128, :]), xin[:])

        SK = 2
        for i in range(len(aitems) + SK):
            if i < len(aitems):
                emit_qk(i)
            if i >= SK:
                emit_pv(i - SK)
        S.pop_scope()
    S.pop_scope()

def build(cfg):
    nc = bass.Bass("TRN2", target_bir_lowering=False)
    S = Sched(nc)
    G = Ctx()
    setup_common(S, G, need=cfg.get("need", ("rwkv", "da", "moe")))
    outs = []
    dbg = {}
    stop = cfg.get("stop", "end")
    kind_x1 = "ExternalOutput" if stop == "rwkv" else "Internal"
    x1_d = S.dram("x1_d", [NB, TOK, D], F32, kind=kind_x1)
    if cfg.get("dbg_hT"):
        dbg["hT"] = S.dram("dbg_hT", [128, 8, LAT + 2], BF16, kind="ExternalOutput")
    wcast_phase(S, G, cfg.get("need", ("rwkv", "da", "moe")))
    if cfg.get("attn_in_ext"):
        G.x2_ext = S.dram("x2_ext", [NB, TOK, D], F32, kind="ExternalInput")
    if cfg.get("moe_in_ext"):
        G.x1_ext = S.dram("x1_ext", [NB, TOK, D], F32, kind="ExternalInput")
    if not cfg.get("skip_l0"):
        mod_phase(S, G, 0)
    if cfg.get("dbg_mod"):
        dm = S.dram("dbg_modT", [128, 48 * 4], F32, kind="ExternalOutput")
        S.dma("sp", dm[:], G.modT.v(G.modT.t[:, :, :].rearrange("p a b -> p (a b)")))
        dg = S.dram("dbg_gates", [2, 3, D], F32, kind="ExternalOutput")
        S.dma("sp", dg[:], G.gates_d[:])
    if stop == "mod":
        S.barrier()
        S.emit()
        return nc, S
    if not cfg.get("skip_rwkv") and not cfg.get("skip_l0"):
        rwkv_phase(S, G, x1_d, dbg=dbg, nb=cfg.get("nb", NB), **cfg.get("rw", {}))
    if stop == "rwkv":
        S.barrier()
        S.emit()
        return nc, S
    x2_d = S.dram("x2_d", [NB, TOK, D], F32, kind="ExternalOutput" if stop == "moe0" else "Internal")
    if not cfg.get("skip_l0"):
        moe0 = True
    else:
        moe0 = False
    tiles0 = [(b, ti * 128, 2 if ti < 2 else b) for b in range(NB) for ti in range(NT)]
    if cfg.get("dbg_gates"):
        dbg["gates"] = S.dram("dbg_gates32", [12, 128, 32], F32, kind="ExternalOutput")
    if moe0:
      moe_phase(S, G, 0, x1_d if not cfg.get("moe_in_ext") else G.x1_ext, tiles0[:cfg.get("moe_ntiles", len(tiles0))],
                lambda b, row0: x2_d.v(x2_d.t[b, row0:row0 + 128, :]), cfg.get("st0", 12), npairs=cfg.get("npairs", 16), dbg=dbg)
    if stop == "moe0":
        S.barrier()
        S.emit()
        return nc, S
    x3_d = S.dram("x3_d", [NB, LAT, D], F32, kind="ExternalOutput" if stop == "attn" else "Internal")
    mod_phase(S, G, 1)
    attn_phase(S, G, x2_d if not cfg.get("attn_in_ext") else G.x2_ext, x3_d, **cfg.get("at", {}))
    if stop == "attn":
        S.barrier()
        S.emit()
        return nc, S
    out_d = S.dram("out", [NB, LAT, D], F32, kind="ExternalOutput")
    tiles1 = [(b, ti * 128, b) for b in range(NB) for ti in range(LAT // 128)]
    moe_phase(S, G, 1, x3_d, tiles1, lambda b, row0: out_d.v(out_d.t[b, row0:row0 + 128, :]), cfg.get("st1", 8))
    S.barrier()
    S.emit()
    return nc, S


def prep_core_inputs(inputs, core, consts):
    b0 = core * NB
    f = lambda a: np.ascontiguousarray(a, dtype=np.float32)
    m = {}
    m["x"] = f(inputs["x"][b0:b0 + NB])
    m["ctx"] = f(inputs["ctx"][b0:b0 + NB])
    m["c3"] = f(np.concatenate([inputs["c"][b0:b0 + NB], inputs["c_ctx"][None, :]], axis=0))
    pv = np.concatenate([
        inputs["norm1_g"].reshape(16, 128), inputs["norm2_g"].reshape(16, 128),
        inputs["rw_mu"][0].reshape(48, 128), inputs["b_mod"].reshape(96, 128)], axis=0)
    m["pvec"] = f(pv)
    m["w_mod"] = f(inputs["w_mod"])
    m["b_mod"] = f(inputs["b_mod"])
    for k, v in consts.items():
        m[k] = v
    m["rw_w_rkv"] = f(inputs["rw_w_rkv"][0])
    for k in ("rw_w0", "rw_w1", "rw_w2", "rw_a0", "rw_a1", "rw_a2", "rw_g1", "rw_g2", "rw_w_o"):
        m[k] = f(inputs[k][0])
    for k in ("rw_k_k", "rw_k_a", "rw_lnx_g", "rw_lnx_b"):
        m[k] = f(inputs[k][0].reshape(1, D))
    m["rw_r_k"] = f(inputs["rw_r_k"][0].reshape(1, D))
    m["da_w_qkv"] = f(inputs["da_w_qkv"][0])
    m["da_q_norm_g"] = f(inputs["da_q_norm_g"][0].reshape(1, 64))
    m["da_k_norm_g"] = f(inputs["da_k_norm_g"][0].reshape(1, 64))
    m["da_lam"] = f(np.stack([inputs["da_lam_q1"][0], inputs["da_lam_k1"][0], inputs["da_lam_q2"][0], inputs["da_lam_k2"][0]]))
    m["da_subln_g"] = f(inputs["da_subln_g"][0].reshape(1, 128))
    m["da_w_o"] = f(inputs["da_w_o"][0])
    rt = np.concatenate([inputs["moe_router_g"], np.transpose(inputs["moe_router_e"], (0, 2, 1, 3)).reshape(2, D, 32)], axis=2)
    m["moe_router"] = f(rt)
    rb = np.concatenate([inputs["moe_router_g_b"], inputs["moe_router_e_b"].reshape(2, 32)], axis=1).reshape(2, 1, 36)
    m["moe_router_b"] = f(rb)
    m["moe_w_gate"] = f(inputs["moe_w_gate"]).reshape(2, NEXP, D, DFF)
    m["moe_w_up"] = f(inputs["moe_w_up"]).reshape(2, NEXP, D, DFF)
    m["moe_w_down"] = f(inputs["moe_w_down"]).reshape(2, NEXP, DFF, D)
    return m


_CACHE = {}


def kernel(**inputs):
    from concourse.bass_utils import run_bass_kernel_spmd
    n = 8
    if "nc" not in _CACHE:
        _CACHE["nc"] = build({})[0]
        _CACHE["consts"] = host_consts()
    nc = _CACHE["nc"]
    consts = _CACHE["consts"]
    inputs = {k: np.asarray(v) for k, v in inputs.items()}
    in_maps = [prep_core_inputs(inputs, c, consts) for c in range(n)]
    res = run_bass_kernel_spmd(nc, in_maps, core_ids=list(range(n)))
    out = np.concatenate([r["out"] for r in res.results], axis=0)
    return out.astype(np.float32)
```

```python
import numpy as np
import concourse.bass as bass
import concourse.mybir as mybir

F32 = mybir.dt.float32
BF16 = mybir.dt.bfloat16
I32 = mybir.dt.int32
U32 = mybir.dt.uint32
AF = mybir.ActivationFunctionType
ALU = mybir.AluOpType
AX = mybir.AxisListType

ENGS = ("pe", "dve", "act", "pool", "sp")
SEM_LIMIT = 30000
N_DMA_SEMS = 24


class V:
    __slots__ = ("ap", "toks", "excl")

    def __init__(self, ap, toks, excl=False):
        self.ap = ap
        self.toks = toks
        self.excl = excl


class Tl:
    def __init__(self, S, t, name, excl=False, toks=None):
        self.S = S
        self.t = t
        self.name = name
        self.excl = excl
        self.toks = toks

    def _tk(self, key):
        if self.toks is not None:
            return list(self.toks)
        return [(self.name, None if self.excl else key)]

    def __getitem__(self, idx):
        return V(self.t[idx], self._tk(None), self.excl)

    def k(self, key, idx=None):
        ap = self.t[idx] if idx is not None else None
        return V(ap, self._tk(key), self.excl)

    def v(self, ap, key=None):
        return V(ap, self._tk(key), self.excl)


class Sched:
    def __init__(self, nc, same_engine_sync=True):
        self.nc = nc
        self.q = {e: [] for e in ENGS}
        self.cnt = {e: 0 for e in ENGS}
        self.semi = {e: 0 for e in ENGS}
        self.nsem = {e: 1 for e in ENGS}
        self.state = {}
        self.waited = {e: {} for e in ENGS}
        self.same = same_engine_sync
        self.dma_i = 0
        self.dma_cnt = [0] * N_DMA_SEMS
        self.dma_last = [None] * N_DMA_SEMS
        self.ctx = []
        self.n_instr = 0
        self.out_deps = []

    def sbuf(self, name, shape, dtype=F32):
        cm = self.nc.sbuf_tensor(name, list(shape), dtype)
        t = cm.__enter__()
        self.ctx.append(cm)
        return Tl(self, t, name)

    def psum(self, name, shape, dtype=F32):
        cm = self.nc.psum_tensor(name, list(shape), dtype)
        t = cm.__enter__()
        self.ctx.append(cm)
        return Tl(self, t, name, excl=True)

    def dram(self, name, shape, dtype=F32, kind="Internal"):
        t = self.nc.dram_tensor(name, list(shape), dtype, kind=kind)
        return Tl(self, t.ap(), name)

    def push_scope(self):
        self.scopes = getattr(self, "scopes", [])
        self.scopes.append(len(self.ctx))

    def pop_scope(self):
        self.barrier()
        n = self.scopes.pop()
        while len(self.ctx) > n:
            self.ctx.pop().__exit__(None, None, None)

    def barrier(self):
        deps = []
        for e in ENGS:
            if self.cnt[e] > 0:
                deps.append(((e, self.semi[e]), self.cnt[e], e))
        for i in range(N_DMA_SEMS):
            if self.dma_last[i] is not None:
                deps.append(self.dma_last[i])
        for e in ENGS:
            d = [x for x in deps if x[2] != e]
            w = self._waits(e, d)
            if w:
                self.q[e].append(("wait", None, w, None))

    def _deps(self, reads, writes):
        deps = []
        for v in reads:
            for tok in v.toks:
                st = self.state.get(tok)
                if st and st[0] is not None:
                    deps.append(st[0])
        for v in writes:
            for tok in v.toks:
                st = self.state.get(tok)
                if st:
                    if st[0] is not None:
                        deps.append(st[0])
                    deps.extend(st[1])
        return deps

    def _update(self, reads, writes, me, real_writes=None):
        for v in reads:
            for tok in v.toks:
                st = self.state.setdefault(tok, [None, [], None])
                st[1].append(me)
                if len(st[1]) > 64:
                    best = {}
                    for d in st[1]:
                        if d[0] not in best or best[d[0]][1] < d[1]:
                            best[d[0]] = d
                    st[1] = list(best.values())
        rw = writes if real_writes is None else real_writes
        rwt = set()
        for v in rw:
            rwt.update(v.toks)
        for v in writes:
            for tok in v.toks:
                old = self.state.get(tok)
                lrw = me if tok in rwt else (old[2] if old else None)
                self.state[tok] = [me, [], lrw]

    def _waits(self, eng, deps, raw_toks_same=None):
        need = {}
        for d in deps:
            semkey, val, deng, is_raw = d[0], d[1], d[2], True
            if deng == eng and not self.same:
                continue
            w = self.waited[eng].get(semkey, 0)
            if val > w and val > need.get(semkey, 0):
                need[semkey] = val
        for sk, val in need.items():
            self.waited[eng][sk] = val
        return list(need.items())

    def op(self, eng, fn, reads=(), writes=(), same_ok=False):
        reads = list(reads)
        real_writes = list(writes)
        writes = real_writes + [v for v in reads if v.excl]
        deps = self._deps(reads, writes)
        if same_ok or eng == "pe":
            deps = [d for d in deps if d[2] != eng]
        elif self.same:
            rd = []
            for v in reads:
                for tok in v.toks:
                    st = self.state.get(tok)
                    if st and st[2] is not None and st[2][2] == eng:
                        rd.append(st[2])
            deps = [d for d in deps if d[2] != eng] + rd
        waits = self._waits(eng, deps)
        if self.cnt[eng] >= SEM_LIMIT:
            self.semi[eng] += 1
            self.nsem[eng] = max(self.nsem[eng], self.semi[eng] + 1)
            self.cnt[eng] = 0
        self.cnt[eng] += 1
        me = ((eng, self.semi[eng]), self.cnt[eng], eng)
        self.q[eng].append(("op", fn, waits, me[0]))
        self._update(reads, writes, me, real_writes)
        self.n_instr += 1
        return me

    def dma(self, queue, out, in_, split=0, **kw):
        reads, writes = [in_], [out]
        deps = self._deps(reads, writes)
        i = self.dma_i % N_DMA_SEMS
        self.dma_i += 1
        if self.dma_last[i] is not None:
            deps.append(self.dma_last[i])
        waits = self._waits(queue, deps)
        oa, ia = out.ap, in_.ap
        parts = [(oa, ia)]
        if split:
            step = 128 // split
            parts = [(oa[k * step:(k + 1) * step], ia[k * step:(k + 1) * step]) for k in range(split)]
        for k, (o_, a_) in enumerate(parts):
            self.dma_cnt[i] += 16
            self.q[queue].append(("dma", (o_, a_, kw), waits if k == 0 else [], ("dma", i)))
            self.n_instr += 1
        me = (("dma", i), self.dma_cnt[i], "dma")
        self.dma_last[i] = me
        self._update(reads, writes, me)
        return me

    def emit(self, final_deps=None):
        nc = self.nc
        sems = {}
        cms = []

        def mk(name):
            cm = nc.semaphore(name)
            s = cm.__enter__()
            cms.append(cm)
            return s
        for e in ENGS:
            for i in range(self.nsem[e]):
                sems[(e, i)] = mk(f"s_{e}_{i}")
        for i in range(N_DMA_SEMS):
            sems[("dma", i)] = mk(f"s_dma_{i}")
        engobj = {"pe": "tensor", "dve": "vector", "act": "scalar", "pool": "gpsimd", "sp": "sync"}
        if final_deps:
            w = self._waits("sp", final_deps)
            self.q["sp"].append(("wait", None, w, None))
        with nc.Block() as block:
            for e in ENGS:
                items = self.q[e]
                if not items:
                    continue

                def body(eng, items=items):
                    for kind, fn, waits, semkey in items:
                        for sk, val in waits:
                            eng.wait_ge(sems[sk], val)
                        if kind == "op":
                            ins = fn(eng)
                            ins.then_inc(sems[semkey], 1)
                        elif kind == "dma":
                            oa, ia, kw = fn
                            eng.dma_start(out=oa, in_=ia, **kw).then_inc(sems[semkey], 16)
                getattr(block, engobj[e])(body)
        for cm in reversed(cms):
            cm.__exit__(None, None, None)
        for cm in reversed(self.ctx):
            cm.__exit__(None, None, None)

    def mm(self, out, lhsT, rhs, start=True, stop=True, **kw):
        return self.op("pe", lambda e: e.matmul(out.ap, lhsT.ap, rhs.ap, start=start, stop=stop, **kw),
                       reads=[lhsT, rhs] + ([] if start else [out]), writes=[out])

    def tr(self, out, in_, ident):
        return self.op("pe", lambda e: e.transpose(out.ap, in_.ap, ident.ap),
                       reads=[in_, ident], writes=[out])

    def act(self, out, in_, func, bias=None, scale=None, accum_out=None, eng="act"):
        reads = [in_]
        kw = {}
        if isinstance(bias, V):
            reads.append(bias)
            kw["bias"] = bias.ap
        elif bias is not None:
            kw["bias"] = bias
        if isinstance(scale, V):
            reads.append(scale)
            kw["scale"] = scale.ap
        elif scale is not None:
            kw["scale"] = scale
        writes = [out]
        if accum_out is not None:
            writes.append(accum_out)
            kw["accum_out"] = accum_out.ap
        return self.op("act", lambda e: e.activation(out.ap, in_.ap, func, **kw), reads=reads, writes=writes)

    def tt(self, eng, out, in0, in1, op):
        return self.op(eng, lambda e: e.tensor_tensor(out.ap, in0.ap, in1.ap, op), reads=[in0, in1], writes=[out])

    def ts(self, eng, out, in0, s1, s2=None, op0=ALU.mult, op1=None, accum_out=None):
        reads = [in0]
        a1 = s1.ap if isinstance(s1, V) else s1
        a2 = s2.ap if isinstance(s2, V) else s2
        if isinstance(s1, V):
            reads.append(s1)
        if isinstance(s2, V):
            reads.append(s2)
        writes = [out]
        kw = {}
        if op1 is not None:
            kw["op1"] = op1
        if accum_out is not None:
            kw["accum_out"] = accum_out.ap
            writes.append(accum_out)
        return self.op(eng, lambda e: e.tensor_scalar(out.ap, in0.ap, a1, a2, op0, **kw), reads=reads, writes=writes)

    def stt(self, out, in0, scalar, in1, op0, op1, eng="dve"):
        reads = [in0, in1]
        a = scalar.ap if isinstance(scalar, V) else scalar
        if isinstance(scalar, V):
            reads.append(scalar)
        return self.op(eng, lambda e: e.scalar_tensor_tensor(out.ap, in0.ap, a, in1.ap, op0, op1), reads=reads, writes=[out])

    def copy(self, eng, out, in_):
        if eng == "act":
            return self.op("act", lambda e: e.copy(out.ap, in_.ap), reads=[in_], writes=[out])
        return self.op(eng, lambda e: e.tensor_copy(out.ap, in_.ap), reads=[in_], writes=[out])

    def memset(self, eng, out, val):
        return self.op(eng, lambda e: e.memset(out.ap, val), reads=[], writes=[out])

    def reduce(self, out, in_, op=ALU.add, axis=AX.X, eng="dve"):
        return self.op(eng, lambda e: e.tensor_reduce(out.ap, in_.ap, axis, op), reads=[in_], writes=[out])

    def recip(self, out, in_):
        return self.op("dve", lambda e: e.reciprocal(out.ap, in_.ap), reads=[in_], writes=[out])
D = 1024
NB = 2
LAT = 2048
CTX = 256
TOK = CTX + LAT
NT = TOK // 128
EPS = 1e-6
DECAY_C = -0.6065306597126334
LNX_EPS = 64e-5
NGRP = 4
NEXP = 32
DFF = 256

PV_N1 = (0, 16)
PV_N2 = (16, 32)
PV_MU = 32
PV_BMOD = 80
PV_ROWS = 176


def host_consts():
    c = {}
    c["ident_f"] = np.eye(128, dtype=np.float32)
    c["ones_f"] = np.ones((128, 128), dtype=np.float32)
    idx = np.arange(128)
    cs, ct = idx[:, None] // 64, idx[None, :] // 64
    same = (cs == ct)
    s_, t_ = idx[:, None], idx[None, :]
    maskA = np.zeros((2, 128, 256), np.float32)
    maskAT = np.zeros((2, 128, 128), np.float32)
    tri = np.zeros((2, 128, 384), np.float32)
    for z in range(2):
        prec = same & ((s_ < t_) if z == 0 else (s_ > t_))
        preceq = same & ((s_ <= t_) if z == 0 else (s_ >= t_))
        succ = same & ((s_ > t_) if z == 0 else (s_ < t_))
        maskA[z, :, 0:128] = prec
        maskA[z, :, 128:256] = preceq
        maskAT[z] = prec.T
        tri[z, :, 0:128] = preceq * DECAY_C
        tri[z, :, 128:256] = prec * DECAY_C
        tri[z, :, 256:384] = succ * DECAY_C
    c["maskA"] = maskA
    c["maskAT"] = maskAT
    c["tri"] = tri
    ind = np.zeros((128, 2), np.float32)
    ind[0:64, 0] = DECAY_C
    ind[64:128, 1] = DECAY_C
    c["ind"] = ind
    rows = LAT // 64
    row = np.repeat(np.arange(rows), 64)
    col = np.tile(np.arange(64), rows)
    inv = (10000.0 ** (-np.arange(16, dtype=np.float32) / 16)).astype(np.float32)
    ang = np.stack([row, col], axis=-1).astype(np.float32)[:, :, None] * inv
    c["rope_cos"] = np.cos(ang).astype(np.float32).reshape(LAT, 32)
    c["rope_sin"] = np.sin(ang).astype(np.float32).reshape(LAT, 32)
    sel = np.zeros((32, 32, 128), np.float32)
    for e in range(32):
        sel[e, e, :] = 1.0
    c["sel"] = sel.reshape(32, 32 * 128)
    return c


class Ctx:
    pass


def std_psum(S, G, tag):
    G.psA = S.psum(f"psA{tag}", [128, 1024], F32)
    nm = G.psA.name
    G.psA.toks = [(nm, 0), (nm, 1)]
    G.psA_h = [Tl(S, G.psA.t[:, i * 512:(i + 1) * 512].rearrange("p (a b) -> p a b", b=128), nm, excl=True,
                  toks=[(nm, i)]) for i in range(2)]
    G.psY = S.psum(f"psY{tag}", [128, 1024], F32)
    G.psT = S.psum(f"psT{tag}", [128, 1024], BF16)
    G.psH = [S.psum(f"psH{i}{tag}", [128, 4, 128], F32) for i in range(3)]


def setup_common(S, G, need=("rwkv", "da", "moe")):
    def I(name, shape, dt=F32):
        grp = name.split('_')[0]
        if grp in ('rw', 'da', 'moe') and {'rw': 'rwkv', 'da': 'da', 'moe': 'moe'}[grp] not in need:
            return None
        return S.dram(name, shape, dt, kind="ExternalInput")
    G.x = I("x", [NB, LAT, D])
    G.ctx = I("ctx", [NB, CTX, D])
    G.c3 = I("c3", [3, D])
    G.pvec = I("pvec", [PV_ROWS, 128])
    G.w_mod = I("w_mod", [2, D, 6 * D])
    G.b_mod = I("b_mod", [2, 6 * D])
    G.ident_f_d = I("ident_f", [128, 128])
    G.ones_f_d = I("ones_f", [128, 128])
    G.maskA_d = I("maskA", [2, 128, 256])
    G.maskAT_d = I("maskAT", [2, 128, 128])
    G.tri_d = I("tri", [2, 128, 384])
    G.ind_d = I("ind", [128, 2])
    G.rope_cos_d = I("rope_cos", [LAT, 32])
    G.rope_sin_d = I("rope_sin", [LAT, 32])
    G.sel_d = I("sel", [32, 32 * 128])
    G.rw_w_rkv = I("rw_w_rkv", [3, D, D])
    G.rw_w0 = I("rw_w0", [2, D])
    G.rw_w1 = I("rw_w1", [2, D, 64])
    G.rw_w2 = I("rw_w2", [2, 64, D])
    G.rw_a0 = I("rw_a0", [2, D])
    G.rw_a1 = I("rw_a1", [2, D, 64])
    G.rw_a2 = I("rw_a2", [2, 64, D])
    G.rw_g1 = I("rw_g1", [D, 128])
    G.rw_g2 = I("rw_g2", [128, D])
    G.rw_k_k = I("rw_k_k", [1, D])
    G.rw_k_a = I("rw_k_a", [1, D])
    G.rw_r_k = I("rw_r_k", [1, D])
    G.rw_lnx_g = I("rw_lnx_g", [1, D])
    G.rw_lnx_b = I("rw_lnx_b", [1, D])
    G.rw_w_o = I("rw_w_o", [D, D])
    G.da_w_qkv = I("da_w_qkv", [D, 3 * D])
    G.da_q_norm_g = I("da_q_norm_g", [1, 64])
    G.da_k_norm_g = I("da_k_norm_g", [1, 64])
    G.da_lam = I("da_lam", [4, 64])
    G.da_subln_g = I("da_subln_g", [1, 128])
    G.da_w_o = I("da_w_o", [D, D])
    G.moe_router = I("moe_router", [2, D, 36])
    G.moe_router_b = I("moe_router_b", [2, 1, 36])
    G.moe_w_gate = I("moe_w_gate", [2, NEXP, D, DFF])
    G.moe_w_up = I("moe_w_up", [2, NEXP, D, DFF])
    G.moe_w_down = I("moe_w_down", [2, NEXP, DFF, D])

    G.ident_f = S.sbuf("ident_f_s", [128, 128], F32)
    G.ident_b = S.sbuf("ident_b_s", [128, 128], BF16)
    G.ones_f = S.sbuf("ones_f_s", [128, 128], F32)
    S.dma("sp", G.ident_f[:], G.ident_f_d[:])
    S.dma("pool", G.ident_b[:], G.ident_f_d[:])
    S.dma("sp", G.ones_f[:], G.ones_f_d[:])
    G.pfm = S.sbuf("pfm", [128, PV_ROWS], F32)
    S.push_scope()
    std_psum(S, G, "c")
    pv = S.sbuf("pv_ld", [128, 2, 128], F32)
    S.memset("dve", pv[:], 0.0)
    S.dma("sp", pv[:, 0, :], G.pvec[0:128, :])
    S.dma("sp", pv[0:PV_ROWS - 128, 1, :], G.pvec[128:PV_ROWS, :])
    S.tr(G.psA[:, 0:128], pv[:, 0, :], G.ident_f[:])
    S.tr(G.psA[:, 128:256], pv[:, 1, :], G.ident_f[:])
    S.copy("dve", G.pfm[:], G.psA[:, 0:PV_ROWS])
    S.pop_scope()
    G.modT = S.sbuf("modT", [128, 48, 4], F32)
    G.A1 = S.sbuf("A1", [128, 3, 8], F32)
    G.A2 = S.sbuf("A2", [128, 3, 8], F32)
    G.gates_d = S.dram("gates_d", [2, 3, D], F32)


def mod_phase(S, G, li):
    S.push_scope()
    std_psum(S, G, f"m{li}")
    crow = S.sbuf(f"crow{li}", [4, D], F32)
    sc = S.sbuf(f"sc{li}", [4, D], F32)
    scT = S.sbuf(f"scT{li}", [128, 8, 4], F32)
    brow = S.sbuf(f"brow{li}", [1, 6 * D], F32)
    grow = S.sbuf(f"grow{li}", [4, 512], F32)
    wblk = [S.sbuf(f"wblk{i}_{li}", [128, 8, 512], F32) for i in range(2)]
    S.memset("dve", crow[:], 0.0)
    S.dma("sp", crow[0:3, :], G.c3[:])
    S.dma("sp", brow[:], G.b_mod[li:li + 1, :])
    S.act(sc[:], crow[:], AF.Silu)
    for kc in range(8):
        S.tr(G.psA[:, kc * 4:(kc + 1) * 4], sc[0:4, kc * 128:(kc + 1) * 128], G.ident_f[0:4, 0:4])
    S.copy("dve", scT[:], G.psA.v(G.psA.t[:, 0:32].rearrange("p (a b) -> p a b", b=4)))
    wsrc = G.w_mod.t[li].rearrange("(kc p) n -> p kc n", p=128)
    gate_blocks = {4: (0, 0), 5: (0, 1), 10: (1, 0), 11: (1, 1)}
    for blk in range(12):
        wb = wblk[blk % 2]
        S.dma("sp", wb[:], G.w_mod.v(wsrc[:, :, blk * 512:(blk + 1) * 512]), split=8)
        if blk in gate_blocks:
            which, half = gate_blocks[blk]
            ps = G.psY[0:4, 0:512]
            for kc in range(8):
                S.mm(ps, scT[:, kc, :], wb[:, kc, :], start=(kc == 0), stop=False)
            S.mm(ps, G.ones_f[0:1, 0:4], brow[0:1, blk * 512:(blk + 1) * 512], start=False, stop=True)
            S.copy("dve", grow[:], ps)
            S.dma("sp", G.gates_d.v(G.gates_d.t[which, :, half * 512:(half + 1) * 512]), grow[0:3, :])
        else:
            for ec in range(4):
                ch = blk * 4 + ec
                ps = G.psA[:, ch * 4:(ch + 1) * 4]
                for kc in range(8):
                    S.mm(ps, wb[:, kc, ec * 128:(ec + 1) * 128], scT[:, kc, :], start=(kc == 0), stop=(kc == 7))
                S.ts("dve", G.modT[:, ch, :], ps, G.pfm[:, PV_BMOD + li * 48 + ch:PV_BMOD + li * 48 + ch + 1], None, op0=ALU.add)
    for r in range(3):
        for (A, sc0, gofs) in ((G.A1, 8, PV_N1[0] + li * 8), (G.A2, 32, PV_N2[0] + li * 8)):
            S.ts("dve", A[:, r, :], G.modT[:, sc0:sc0 + 8, r], 1.0, None, op0=ALU.add)
            S.tt("dve", A[:, r, :], A[:, r, :], G.pfm[:, gofs:gofs + 8], ALU.mult)
    S.pop_scope()


def norm_tile_to_fm(S, G, xt, r, A, shift_ch0, out_fm, wk, fp32_out=None):
    st = wk["st"]
    S.act(wk["junk"][:], xt, AF.Square, accum_out=st[:, 0:1])
    S.ts("dve", st[:, 1:2], st[:, 0:1], 1.0 / D, EPS, op0=ALU.mult, op1=ALU.add)
    S.act(st[:, 2:3], st[:, 1:2], AF.Sqrt)
    S.recip(st[:, 3:4], st[:, 2:3])
    if fp32_out is None:
        xn = wk["xn"]
        S.ts("dve", xn[:], xt, st[:, 3:4], None, op0=ALU.mult)
        for kc in range(8):
            S.tr(G.psT[:, kc * 128:(kc + 1) * 128], xn[:, kc * 128:(kc + 1) * 128], G.ident_b[:])
        src = G.psT.v(G.psT.t[:, :].rearrange("p (a b) -> p a b", b=128))
    else:
        xn = wk["xn32"]
        S.ts("dve", xn[:], xt, st[:, 3:4], None, op0=ALU.mult)
        for kc in range(8):
            S.tr(G.psA[:, kc * 128:(kc + 1) * 128], xn[:, kc * 128:(kc + 1) * 128], G.ident_f[:])
        src = G.psA.v(G.psA.t[:, :].rearrange("p (a b) -> p a b", b=128))
    Abc = A.v(A.t[:, r, :].unsqueeze(2).to_broadcast([128, 8, 128]))
    shbc = G.modT.v(G.modT.t[:, shift_ch0:shift_ch0 + 8, r].unsqueeze(2).to_broadcast([128, 8, 128]))
    tmp = wk["fm32"]
    S.tt("dve", tmp[:], src, Abc, ALU.mult)
    if fp32_out is not None:
        S.tt("pool", fp32_out, tmp[:], shbc, ALU.add)
        S.copy("act", out_fm, fp32_out)
    else:
        S.tt("pool", out_fm, tmp[:], shbc, ALU.add)


def norm_tile_gen(S, G, xt, r, A, shift_ch0, out_fm, wk, fp32_out, psA):
    st = wk["st"]
    S.act(wk["junk"][:], xt, AF.Square, accum_out=st[:, 0:1])
    yield
    S.ts("dve", st[:, 1:2], st[:, 0:1], 1.0 / D, EPS, op0=ALU.mult, op1=ALU.add)
    yield
    S.act(st[:, 2:3], st[:, 1:2], AF.Ln)
    S.act(st[:, 3:4], st[:, 2:3], AF.Exp, scale=-0.5)
    yield
    xn = wk["xn32"]
    S.ts("dve", xn[:], xt, st[:, 3:4], None, op0=ALU.mult)
    yield
    for kc in range(8):
        S.tr(psA[:, kc * 128:(kc + 1) * 128], xn[:, kc * 128:(kc + 1) * 128], G.ident_f[:])
    yield
    src = psA.v(psA.t[:, :].rearrange("p (a b) -> p a b", b=128))
    Abc = A.v(A.t[:, r, :].unsqueeze(2).to_broadcast([128, 8, 128]))
    shbc = G.modT.v(G.modT.t[:, shift_ch0:shift_ch0 + 8, r].unsqueeze(2).to_broadcast([128, 8, 128]))
    tmp = wk["fm32"]
    S.tt("dve", tmp[:], src, Abc, ALU.mult)
    yield
    S.tt("pool", fp32_out, tmp[:], shbc, ALU.add)
    S.copy("act", out_fm, fp32_out)


def wcast_phase(S, G, need):
    items = []
    G.wbf = {}

    def add(name, src3, n):
        dst = S.dram("wbf_" + name, [128, n], BF16)
        G.wbf[name] = dst
        off = 0
        a, b = src3.shape[1], src3.shape[2]
        rows = max(1, 2048 // b)
        if b > 2048:
            for i in range(a):
                for c0 in range(0, b, 2048):
                    c1 = min(b, c0 + 2048)
                    items.append((src3[:, i:i + 1, c0:c1], dst, i * b + c0, c1 - c0, (1, c1 - c0)))
        else:
            for i in range(0, a, rows):
                i1 = min(a, i + rows)
                items.append((src3[:, i:i1, :], dst, i * b, (i1 - i) * b, (i1 - i, b)))

    if "rwkv" in need:
        for j, nm in enumerate(("Wr", "Wk", "Wv")):
            add(nm, G.rw_w_rkv.t[j].rearrange("(kc p) n -> p kc n", p=128), 8 * D)
        add("rwWo", G.rw_w_o.t.rearrange("(kc p) n -> p kc n", p=128), 8 * D)
    if "da" in need:
        add("Wqkv", G.da_w_qkv.t.rearrange("(kc p) n -> p kc n", p=128), 8 * 3 * D)
        add("daWo", G.da_w_o.t.rearrange("(kc p) n -> p kc n", p=128), 8 * D)
    if "moe" in need:
        for li in range(2):
            for e in range(NEXP):
                add(f"g{li}_{e}", G.moe_w_gate.t[li, e].rearrange("(kc p) n -> p kc n", p=128), 8 * DFF)
                add(f"u{li}_{e}", G.moe_w_up.t[li, e].rearrange("(kc p) n -> p kc n", p=128), 8 * DFF)
                add(f"d{li}_{e}", G.moe_w_down.t[li, e].rearrange("(fc p) n -> p fc n", p=128), 2 * D)
    S.push_scope()
    NBUF = 4
    stg = [S.sbuf(f"wc_stg{i}", [128, 2048], F32) for i in range(NBUF)]
    ob = [S.sbuf(f"wc_ob{i}", [128, 2048], BF16) for i in range(NBUF)]
    engs = ["dve", "pool", "dve"]

    def load(i):
        src3, dst, off, n, (a, b) = items[i]
        t = stg[i % NBUF]
        S.dma("sp", t.v(t.t[:, 0:n].rearrange("p (a b) -> p a b", b=b)), V(src3, [("wsrc", None)]))

    for i in range(min(NBUF - 1, len(items))):
        load(i)
    for i in range(len(items)):
        if i + NBUF - 1 < len(items):
            load(i + NBUF - 1)
        src3, dst, off, n, _ = items[i]
        S.copy(engs[i % 3], ob[i % NBUF][:, 0:n], stg[i % NBUF][:, 0:n])
        S.dma("act", dst.v(dst.t[:, off:off + n]), ob[i % NBUF][:, 0:n])
    S.pop_scope()

def rwkv_phase(S, G, x1_d, dbg=None, nb=NB, nt0=NT, do_dir=3, nt1=NT, nheads=16, fl=99):
    li = 0
    H = 16
    yf_d = S.dram("yf_d", [NB, NT, 128, 1040], F32)
    cache_d = S.dram("cache_d", [NB, NT, 128, 6 * D], BF16)
    sg1_d = S.dram("sg1_d", [NB, NT, 128, D], F32)

    def load_bc(name, src, dt=BF16, n=D, q="pool"):
        t = S.sbuf(name, [128, n], dt)
        S.dma(q, t[:], src.v(src.t[0:1, :].partition_broadcast(128)))
        return t

    S.push_scope()
    std_psum(S, G, "r")
    maskA = S.sbuf("maskA_s", [128, 2, 256], BF16)
    maskAT = S.sbuf("maskAT_s", [128, 2, 128], BF16)
    tri = S.sbuf("tri_s", [128, 2, 384], F32)
    ind = S.sbuf("ind_s", [128, 2], F32)
    for z in range(2):
        S.dma("pool", maskA[:, z, :], G.maskA_d.v(G.maskA_d.t[z]))
        S.dma("pool", maskAT[:, z, :], G.maskAT_d.v(G.maskAT_d.t[z]))
        S.dma("sp", tri[:, z, :], G.tri_d.v(G.tri_d.t[z]))
    S.dma("sp", ind[:], G.ind_d[:])
    k_a_bc = load_bc("k_a_bc", G.rw_k_a)
    r_k_bc = load_bc("r_k_bc", G.rw_r_k)
    scr1 = S.sbuf("scr1", [128, D], F32)
    scr2 = S.sbuf("scr2", [128, D], F32)
    sg_sb = S.sbuf("sg_sb", [128, D], F32)
    kdir_sb = S.sbuf("kdir_sb", [128, D], BF16)
    b_sb = S.sbuf("b_sb", [128, D], BF16)
    tm = [S.sbuf(f"tm{i}", [128, D], BF16) for i in range(2)]
    R19 = S.sbuf("R19", [128, H, 128], BF16)
    Bh = S.sbuf("Bh", [128, D], BF16)
    Kh = S.sbuf("Kh", [128, D], BF16)
    arT = S.sbuf("arT", [128, 8, 2, 128], BF16)
    btT = S.sbuf("btT", [128, 8, 128], BF16)
    ktT = S.sbuf("ktT", [128, 8, 128], BF16)
    gC = S.sbuf("gC", [128, 8, 2], F32)
    bon = S.sbuf("bon", [128, 2, 16], F32)
    NSET = 4
    M1 = [S.sbuf(f"M1_{i}", [128, 256], BF16) for i in range(NSET)]
    M2 = [S.sbuf(f"M2_{i}", [128, 256], BF16) for i in range(NSET)]
    MabT = [S.sbuf(f"MabT_{i}", [128, 128], BF16) for i in range(NSET)]
    Pb = [[S.sbuf(f"Pb_{i}_{j}", [128, 128], BF16) for j in range(2)] for i in range(NSET)]
    PTb = [[S.sbuf(f"PTb_{i}_{j}", [128, 128], BF16) for j in range(2)] for i in range(NSET)]
    Tb = [S.sbuf(f"Tb_{i}", [128, 128], BF16) for i in range(NSET)]
    WP = [S.sbuf(f"WP_{i}", [128, 128], BF16) for i in range(NSET)]
    G_all = S.sbuf("G_all", [128, 8, 128], BF16)
    Y0_all = S.sbuf("Y0_all", [128, H, 64], BF16)
    D_all = S.sbuf("D_all", [128, 8, 2, 128], BF16)
    E_all = S.sbuf("E_all", [128, 8, 2, 128], BF16)
    Sb = S.sbuf("Sb", [128, 8, 128], BF16)
    S.memset("pool", D_all[:], 0.0)
    S.memset("pool", E_all[:], 0.0)
    yfw = S.sbuf("yfw", [128, 1040], F32)
    banks = list(G.psH) + list(G.psA_h)
    NBK = len(banks)
    bank_ctr = [0]
    slot_ctr = [0] * NBK

    def slot(n=1):
        bk = bank_ctr[0] % NBK
        bank_ctr[0] += 1
        if n == 2:
            i = ((slot_ctr[bk] + 1) // 2 * 2) % 4
            slot_ctr[bk] = i + 2
        else:
            i = slot_ctr[bk] % 4
            slot_ctr[bk] = i + 1
        return (bk, i)

    def psl(s, p0=0, p1=128, c0=0, c1=128, n=1):
        bk, i = s
        t = banks[bk]
        if n == 2:
            return t.v(t.t[p0:p1, i:i + 2, :].rearrange("p a b -> p (a b)")[:, c0:c1])
        return t.v(t.t[p0:p1, i, c0:c1])

    def transposes_to(src_tm, dst_view):
        for ec in range(8):
            S.tr(G.psT[:, ec * 128:(ec + 1) * 128], src_tm[:, ec * 128:(ec + 1) * 128], G.ident_b[:])
        S.copy("act", dst_view, G.psT.v(G.psT.t[:, :].rearrange("p (a b) -> p a b", b=128)))

    def dir_part(z, r_v, k_v, v_v, kk_v, a_v, chunk_order):
        zsl = slice(z, z + 1)
        S.stt(scr1[:], a_v, -1.0, k_a_bc[:], ALU.add, ALU.mult)
        S.stt(kdir_sb[:], scr1[:], 1.0, k_v, ALU.add, ALU.mult)
        S.tt("pool", b_sb[:], kk_v, a_v, ALU.mult)
        S.tt("pool", scr1[:], r_v, kdir_sb[:], ALU.mult)
        S.tt("pool", scr1[:], scr1[:], r_k_bc[:], ALU.mult)
        S.reduce(bon[:, z, :], scr1.v(scr1.t[:, :].rearrange("p (h n) -> p h n", n=64)))
        def cum(which):
            for n in range(2):
                S.mm(G.psA[:, n * 512:(n + 1) * 512], tri[:, z, which * 128:(which + 1) * 128], sg_sb[:, n * 512:(n + 1) * 512])
        cum(0)
        S.act(scr2[:], G.psA[:], AF.Exp)
        S.tt("dve", tm[0][:], r_v, scr2[:], ALU.mult)
        transposes_to(tm[0], arT.v(arT.t[:, :, 1, :]))
        S.act(scr2[:], G.psA[:], AF.Exp, scale=-1.0)
        S.tt("dve", tm[1][:], b_sb[:], scr2[:], ALU.mult)
        transposes_to(tm[1], btT[:])
        S.tt("dve", tm[0][:], kdir_sb[:], scr2[:], ALU.mult)
        transposes_to(tm[0], ktT[:])
        cum(1)
        S.act(scr2[:], G.psA[:], AF.Exp)
        S.stt(tm[1][:], kk_v, -1.0, scr2[:], ALU.mult, ALU.mult)
        S.copy("pool", V(R19.t[:, :, 64:128], [("R19", h) for h in range(H)]),
               tm[1].v(tm[1].t[:, :].rearrange("p (h n) -> p h n", n=64)))
        transposes_to(tm[1], arT.v(arT.t[:, :, 0, :]))
        cum(2)
        S.act(scr2[:], G.psA[:], AF.Exp)
        S.tt("dve", Bh[:], b_sb[:], scr2[:], ALU.mult)
        S.tt("pool", Kh[:], kdir_sb[:], scr2[:], ALU.mult)
        sg_ = slot()
        for ec in range(8):
            S.mm(psl(sg_, c0=ec * 2, c1=ec * 2 + 2), sg_sb[:, ec * 128:(ec + 1) * 128], ind[:])
        S.act(gC[:], banks[sg_[0]].v(banks[sg_[0]].t[:, sg_[1], 0:16].rearrange("p (a b) -> p a b", b=2)), AF.Exp)

        if do_dir < 2:
            return
        def head_gen(h):
            ec, po = h // 2, (h % 2) * 64
            hc = slice(h * 64, (h + 1) * 64)
            pr = slice(po, po + 64)
            i2 = h % NSET
            bt_h = btT[pr, ec, :]
            kt_h = ktT[pr, ec, :]
            ar_h = arT.v(arT.t[pr, ec, :, :].rearrange("p a b -> p (a b)"))
            at_h = arT[pr, ec, 0, :]
            rt_h = arT[pr, ec, 1, :]
            s1 = slot(2)
            S.mm(psl(s1, n=2, c1=256), bt_h, ar_h)
            S.tt("dve", M1[i2][:], psl(s1, n=2, c1=256), maskA[:, z, :], ALU.mult)
            s3 = slot()
            S.mm(psl(s3), at_h, bt_h)
            S.tt("dve", MabT[i2][:], psl(s3), maskAT[:, z, :], ALU.mult)
            s2 = slot(2)
            S.mm(psl(s2, n=2, c1=256), kt_h, ar_h)
            S.tt("dve", M2[i2][:], psl(s2, n=2, c1=256), maskA[:, z, :], ALU.mult)
            T = Tb[i2]
            S.tt("pool", T[:], M1[i2][:, 0:128], G.ident_b[:], ALU.add)
            P, PT = M1[i2][:, 0:128], MabT[i2][:]
            yield
            for kstep in range(1, 6):
                if kstep < 5:
                    sa = slot()
                    S.mm(psl(sa), PT, P)
                    P2 = Pb[i2][kstep % 2]
                    S.copy("act", P2[:], psl(sa))
                sb_ = slot()
                S.mm(psl(sb_), P, PT)
                P2T = PTb[i2][kstep % 2]
                S.copy("act", P2T[:], psl(sb_))
                if kstep == 1:
                    sx = slot()
                    S.mm(psl(sx, c1=64), M2[i2][:, 0:128], v_v_slice(v_v, hc))
                    S.copy("act", R19.k(h, (slice(None), h, slice(0, 64))), psl(sx, c1=64))
                yield
                sc_ = slot()
                S.mm(psl(sc_), P2T[:], T[:])
                S.tt("dve", T[:], T[:], psl(sc_), ALU.add)
                if kstep < 5:
                    P, PT = P2[:], P2T[:]
            yield
            sw = slot()
            S.mm(psl(sw), T[:], R19.k(h, (slice(None), h, slice(None))))
            S.copy("act", WP[i2][:], psl(sw))
            yield
            sg2 = slot()
            S.mm(psl(sg2, p0=po, p1=po + 64), WP[i2][:, 64:128], M1[i2][:, 128:256])
            S.tt("dve", G_all.k(h, (pr, ec, slice(None))), psl(sg2, p0=po, p1=po + 64), rt_h, ALU.add)
            sy = slot()
            S.mm(psl(sy, c1=64), M1[i2][:, 128:256], WP[i2][:, 0:64], start=True, stop=False)
            S.mm(psl(sy, c1=64), M2[i2][:, 128:256], v_v_slice(v_v, hc), start=False, stop=True)
            S.copy("act", Y0_all.k(h, (slice(None), h, slice(None))), psl(sy, c1=64))
            sds = [slot(), slot()]
            for c in range(2):
                cr = slice(c * 64, (c + 1) * 64)
                S.mm(psl(sds[c], p0=po, p1=po + 64, c1=64), WP[i2][cr, 64:128], Bh[cr, hc])
            for c in range(2):
                S.stt(D_all.k(h, (pr, ec, c, slice(po, po + 64))), G.ident_f[pr, po:po + 64], gC[pr, ec, c:c + 1],
                      psl(sds[c], p0=po, p1=po + 64, c1=64), ALU.mult, ALU.add)
            ses = [slot(), slot()]
            for c in range(2):
                cr = slice(c * 64, (c + 1) * 64)
                S.mm(psl(ses[c], p0=po, p1=po + 64, c1=64), Bh[cr, hc], WP[i2][cr, 0:64], start=True, stop=False)
                S.mm(psl(ses[c], p0=po, p1=po + 64, c1=64), Kh[cr, hc], v_v_slice(v_v, hc, cr), start=False, stop=True)
            for c in range(2):
                S.copy("act", E_all.k(h, (pr, ec, c, slice(po, po + 64))), psl(ses[c], p0=po, p1=po + 64, c1=64))

        pending = list(range(nheads))
        active = []
        rnd, last_admit = 0, -99
        while pending or active:
            if pending and len(active) <= NSET - 2 and (rnd - last_admit >= 4 or not active):
                for _ in range(2):
                    if pending:
                        active.append(head_gen(pending.pop(0)))
                last_admit = rnd
            nxt = []
            for g in active:
                try:
                    next(g)
                    nxt.append(g)
                except StopIteration:
                    pass
            active = nxt
            rnd += 1
        if do_dir < 3:
            return
        for c in chunk_order:
            for ec in range(8):
                pair = [2 * ec, 2 * ec + 1]
                Gv = V(G_all.t[:, ec, c * 64:(c + 1) * 64], [("G_all", h) for h in pair])
                Dv = V(D_all.t[:, ec, c, :], [("D_all", h) for h in pair])
                S.mm(G.psY.v(G.psY.t[c * 64:(c + 1) * 64, ec * 128:(ec + 1) * 128]), Gv, Sb[:, ec, :])
                S.mm(G.psA.v(G.psA.t[:, ec * 128:(ec + 1) * 128]), Dv, Sb[:, ec, :])
            S.tt("dve", Sb[:], G.psA.v(G.psA.t[:, :].rearrange("p (a b) -> p a b", b=128)),
                 V(E_all.t[:, :, c, :], [("E_all", h) for h in range(H)]), ALU.add)

    def v_v_slice(v_v, hc, rows=slice(None)):
        return V(v_v.ap[rows, hc], v_v.toks)

    S.push_scope()
    Wr, Wk, Wv = [S.sbuf(n, [128, 8, D], BF16) for n in ("Wr", "Wk", "Wv")]
    for nm, W in (("Wr", Wr), ("Wk", Wk), ("Wv", Wv)):
        S.dma("sp", W.v(W.t[:, :, :].rearrange("p a b -> p (a b)")), G.wbf[nm][:])
    w1 = S.sbuf("w1", [128, 2, 8, 64], BF16)
    a1 = S.sbuf("a1", [128, 2, 8, 64], BF16)
    g1 = S.sbuf("g1", [128, 8, 128], BF16)
    w2x = S.sbuf("w2x", [65, 2, D], BF16)
    a2x = S.sbuf("a2x", [65, 2, D], BF16)
    g2 = S.sbuf("g2", [128, D], BF16)
    for z in range(2):
        S.dma("pool", w1[:, z, :, :], G.rw_w1.v(G.rw_w1.t[z].rearrange("(kc p) n -> p kc n", p=128)))
        S.dma("pool", a1[:, z, :, :], G.rw_a1.v(G.rw_a1.t[z].rearrange("(kc p) n -> p kc n", p=128)))
        S.dma("pool", w2x[0:64, z, :], G.rw_w2.v(G.rw_w2.t[z]))
        S.dma("pool", w2x[64:65, z, :], G.rw_w0.v(G.rw_w0.t[z:z + 1, :]))
        S.dma("pool", a2x[0:64, z, :], G.rw_a2.v(G.rw_a2.t[z]))
        S.dma("pool", a2x[64:65, z, :], G.rw_a0.v(G.rw_a0.t[z:z + 1, :]))
    S.dma("pool", g1[:], G.rw_g1.v(G.rw_g1.t.rearrange("(kc p) n -> p kc n", p=128)))
    S.dma("pool", g2[:], G.rw_g2[:])
    k_k_bc = load_bc("k_k_bc", G.rw_k_k)
    hTc = S.sbuf("hTc", [128, 8, CTX + 2], BF16)
    hTl = S.sbuf("hTl", [128, 8, LAT + 2], BF16)
    xin = scr2
    wk = {"junk": scr1, "st": S.sbuf("st", [128, 4], F32), "xn": tm[0],
          "fm32": S.sbuf("fm32", [128, 8, 128], F32)}
    dxt = S.sbuf("dxt", [128, 8, 128], F32)
    mix = [S.sbuf(f"mix{i}", [128, 8, 128], BF16) for i in range(2)]
    cach = S.sbuf("cach", [128, 6, D], BF16)
    a0_sb = S.sbuf("a0_sb", [128, D], BF16)
    sg1_v = yfw[:, 0:D]
    hwx = S.sbuf("hwx", [65, 2, 128], BF16)
    hax = S.sbuf("hax", [65, 2, 128], BF16)
    hgs = S.sbuf("hgs", [128, 128], BF16)
    st2 = S.sbuf("st2", [128, 3, 16], F32)
    S.memset("dve", hwx[:], 1.0)
    S.memset("dve", hax[:], 1.0)
    for hT in (hTc, hTl):
        S.memset("pool", hT[:], 0.0)

    mix_ctr = [0]

    def make_mix(hT, c0, j):
        m = mix[mix_ctr[0] % 2]
        mix_ctr[0] += 1
        mu = G.pfm.v(G.pfm.t[:, PV_MU + j * 8:PV_MU + j * 8 + 8].unsqueeze(2).to_broadcast([128, 8, 128]))
        S.tt("pool", wk["fm32"][:], dxt[:], mu, ALU.mult)
        S.tt("pool", m[:], wk["fm32"][:], hT[:, :, c0:c0 + 128], ALU.add)
        return m

    def proj_tm(ps, m, W):
        for n in range(2):
            for kc in range(8):
                S.mm(ps[:, n * 512:(n + 1) * 512], m[:, kc, :], W[:, kc, n * 512:(n + 1) * 512], start=(kc == 0), stop=(kc == 7))

    for b in range(nb):
        for ti in range(NT):
            if ti < 2:
                src, r, hT, t0 = G.ctx.v(G.ctx.t[b, ti * 128:(ti + 1) * 128, :]), 2, hTc, ti * 128
            else:
                src, r, hT, t0 = G.x.v(G.x.t[b, (ti - 2) * 128:(ti - 1) * 128, :]), b, hTl, (ti - 2) * 128
            S.dma("sp", xin[:], src, split=4)
            norm_tile_to_fm(S, G, xin[:], r, G.A1, 0, hT[:, :, t0 + 1:t0 + 129], wk)
        if dbg is not None and "hT" in dbg and b == 0:
            S.dma("sp", dbg["hT"][:], hTl[:])
        S.memset("dve", Sb[:], 0.0)
        for ti in range(nt0):
            hT, t0 = (hTc, ti * 128) if ti < 2 else (hTl, (ti - 2) * 128)
            c0 = t0 + 1
            S.tt("dve", dxt[:], hT[:, :, c0 - 1:c0 + 127], hT[:, :, c0 + 1:c0 + 129], ALU.add)
            S.stt(dxt[:], dxt[:], 0.5, hT[:, :, c0:c0 + 128], ALU.mult, ALU.subtract)
            if fl < 1:
                continue
            m = make_mix(hT, c0, 0)
            proj_tm(G.psA, m, Wr)
            S.copy("act", cach[:, 0, :], G.psA[:])
            if fl < 2:
                continue
            m = make_mix(hT, c0, 2)
            proj_tm(G.psY, m, Wv)
            S.copy("act", cach[:, 2, :], G.psY[:])
            if fl < 3:
                continue
            m = make_mix(hT, c0, 4)
            for z in range(2):
                sl_ = slot()
                for kc in range(8):
                    S.mm(psl(sl_, p1=64), a1[:, z, kc, :], m[:, kc, :], start=(kc == 0), stop=(kc == 7))
                S.copy("act", hax[0:64, z, :], psl(sl_, p1=64))
            for z in range(2):
                ps = G.psA if z == 0 else G.psY
                for n in range(2):
                    S.mm(ps[:, n * 512:(n + 1) * 512], hax[:, z, :], a2x[:, z, n * 512:(n + 1) * 512])
                S.act(a0_sb[:] if z == 0 else cach[:, 4, :], ps[:], AF.Sigmoid)
            if fl < 4:
                continue
            m = make_mix(hT, c0, 3)
            for z in range(2):
                sl_ = slot()
                for kc in range(8):
                    S.mm(psl(sl_, p1=64), w1[:, z, kc, :], m[:, kc, :], start=(kc == 0), stop=(kc == 7))
                S.act(hwx[0:64, z, :], psl(sl_, p1=64), AF.Tanh)
            for z in range(2):
                ps = G.psA if z == 0 else G.psY
                for n in range(2):
                    S.mm(ps[:, n * 512:(n + 1) * 512], hwx[:, z, :], w2x[:, z, n * 512:(n + 1) * 512])
                S.act(sg_sb[:] if z == 0 else sg1_v, ps[:], AF.Sigmoid)
            if fl < 5:
                continue
            m = make_mix(hT, c0, 5)
            sl_ = slot()
            for kc in range(8):
                S.mm(psl(sl_), g1[:, kc, :], m[:, kc, :], start=(kc == 0), stop=(kc == 7))
            S.act(hgs[:], psl(sl_), AF.Sigmoid)
            for n in range(2):
                S.mm(G.psY[:, n * 512:(n + 1) * 512], hgs[:], g2[:, n * 512:(n + 1) * 512])
            S.copy("act", cach[:, 5, :], G.psY[:])
            if fl < 6:
                continue
            m = make_mix(hT, c0, 1)
            proj_tm(G.psA, m, Wk)
            S.copy("act", cach[:, 1, :], G.psA[:])
            if fl < 6.1:
                continue
            S.tt("dve", scr1[:], G.psA[:], k_k_bc[:], ALU.mult)
            if fl < 6.2:
                continue
            S.act(scr2[:], scr1[:], AF.Square)
            S.reduce(st2[:, 0, :], scr2.v(scr2.t[:, :].rearrange("p (h n) -> p h n", n=64)))
            if fl < 6.3:
                continue
            S.ts("dve", st2[:, 1, :], st2[:, 0, :], 1e-12, None, op0=ALU.add)
            S.act(st2[:, 1, :], st2[:, 1, :], AF.Sqrt)
            S.recip(st2[:, 2, :], st2[:, 1, :])
            if fl < 6.4:
                continue
            S.tt("dve", cach.v(cach.t[:, 3, :].rearrange("p (h n) -> p h n", n=64)),
                 scr1.v(scr1.t[:, :].rearrange("p (h n) -> p h n", n=64)),
                 st2.v(st2.t[:, 2, :].unsqueeze(2).to_broadcast([128, 16, 64])), ALU.mult)
            if fl < 7:
                continue
            S.dma("sp", cache_d.v(cache_d.t[b, ti].rearrange("p (a n) -> p a n", n=D)), cach[:])
            S.dma("sp", sg1_d.v(sg1_d.t[b, ti]), sg1_v)
            if do_dir:
                dir_part(0, cach[:, 0, :], G.psA[:], cach[:, 2, :], cach[:, 3, :], a0_sb[:], (0, 1))
            S.tt("dve", yfw[:, 0:D], G.psY[:],
                 V(Y0_all.t[:, :, :].rearrange("p h n -> p (h n)"), [("Y0_all", h) for h in range(H)]), ALU.add)
            S.copy("pool", yfw[:, D:D + 16], bon[:, 0, :])
            S.dma("sp", yf_d.v(yf_d.t[b, ti]), yfw[:])
    S.pop_scope()

    S.push_scope()
    Wo = S.sbuf("Wo", [128, 8, D], BF16)
    S.dma("sp", Wo.v(Wo.t[:, :, :].rearrange("p a b -> p (a b)")), G.wbf["rwWo"][:])
    lnx_g_bc = load_bc("lnx_g_bc", G.rw_lnx_g)
    lnx_b_bc = load_bc("lnx_b_bc", G.rw_lnx_b)
    gate_bc = S.sbuf("gate_bc", [128, D], F32)
    cach = S.sbuf("cach1", [128, 6, D], BF16)
    xres = S.sbuf("xres", [128, D], F32)
    pre = S.sbuf("pre", [128, D], BF16)
    preT = S.sbuf("preT", [128, 8, 128], BF16)
    st3 = S.sbuf("st3", [128, 4, 16], F32)
    for b in range(nb):
        S.memset("dve", Sb[:], 0.0)
        order = ([1, 0] + list(range(NT - 1, 1, -1)))[:nt1]
        cur_r = None
        for ti in order:
            r = 2 if ti < 2 else b
            if r != cur_r:
                S.dma("sp", gate_bc[:], G.gates_d.v(G.gates_d.t[0, r:r + 1, :].partition_broadcast(128)))
                cur_r = r
            S.dma("sp", cach[:], cache_d.v(cache_d.t[b, ti].rearrange("p (a n) -> p a n", n=D)))
            S.dma("sp", sg_sb[:], sg1_d.v(sg1_d.t[b, ti]))
            S.dma("sp", yfw[:], yf_d.v(yf_d.t[b, ti]))
            dir_part(1, cach[:, 0, :], cach[:, 1, :], cach[:, 2, :], cach[:, 3, :], cach[:, 4, :], (1, 0))
            S.tt("dve", scr1[:], G.psY[:], V(Y0_all.t[:, :, :].rearrange("p h n -> p (h n)"), [("Y0_all", h) for h in range(H)]), ALU.add)
            S.tt("pool", scr1[:], scr1[:], yfw[:, 0:D], ALU.add)
            y3 = scr1.v(scr1.t[:, :].rearrange("p (h n) -> p h n", n=64))
            S.reduce(st3[:, 0, :], y3)
            S.ts("dve", st3[:, 0, :], st3[:, 0, :], 1.0 / 64, None, op0=ALU.mult)
            S.tt("dve", y3, y3, st3.v(st3.t[:, 0, :].unsqueeze(2).to_broadcast([128, 16, 64])), ALU.subtract)
            S.act(scr2[:], scr1[:], AF.Square)
            S.reduce(st3[:, 1, :], scr2.v(scr2.t[:, :].rearrange("p (h n) -> p h n", n=64)))
            S.ts("dve", st3[:, 1, :], st3[:, 1, :], 1.0 / 64, LNX_EPS, op0=ALU.mult, op1=ALU.add)
            S.act(st3[:, 1, :], st3[:, 1, :], AF.Sqrt)
            S.recip(st3[:, 2, :], st3[:, 1, :])
            S.tt("dve", y3, y3, st3.v(st3.t[:, 2, :].unsqueeze(2).to_broadcast([128, 16, 64])), ALU.mult)
            S.tt("pool", scr1[:], scr1[:], lnx_g_bc[:], ALU.mult)
            S.tt("pool", scr1[:], scr1[:], lnx_b_bc[:], ALU.add)
            S.tt("dve", st3[:, 3, :], bon[:, 1, :], yfw[:, D:D + 16], ALU.add)
            S.tt("dve", scr2.v(scr2.t[:, :].rearrange("p (h n) -> p h n", n=64)),
                 cach.v(cach.t[:, 2, :].rearrange("p (h n) -> p h n", n=64)),
                 st3.v(st3.t[:, 3, :].unsqueeze(2).to_broadcast([128, 16, 64])), ALU.mult)
            S.tt("pool", scr1[:], scr1[:], scr2[:], ALU.add)
            S.tt("pool", pre[:], scr1[:], cach[:, 5, :], ALU.mult)
            transposes_to(pre, preT[:])
            for n in range(2):
                for kc in range(8):
                    S.mm(G.psA[:, n * 512:(n + 1) * 512], preT[:, kc, :], Wo[:, kc, n * 512:(n + 1) * 512], start=(kc == 0), stop=(kc == 7))
            if ti < 2:
                xsrc = G.ctx.v(G.ctx.t[b, ti * 128:(ti + 1) * 128, :])
            else:
                xsrc = G.x.v(G.x.t[b, (ti - 2) * 128:(ti - 1) * 128, :])
            S.dma("sp", xres[:], xsrc)
            S.tt("dve", scr2[:], G.psA[:], gate_bc[:], ALU.mult)
            S.tt("pool", xres[:], xres[:], scr2[:], ALU.add)
            S.dma("sp", x1_d.v(x1_d.t[b, ti * 128:(ti + 1) * 128, :]), xres[:])
    S.pop_scope()
    S.pop_scope()

def moe_phase(S, G, li, xin_d, tiles, xout_fn, st_tiles, npairs=16, dbg=None):
    L = f"e{li}"
    S.push_scope()
    ysub = [S.psum(f"ysub{i}{L}", [128, 1024], F32) for i in range(2)]
    psG = [S.psum(f"psG{i}{L}", [128, 512], F32) for i in range(2)]
    psU = [S.psum(f"psU{i}{L}", [128, 512], F32) for i in range(2)]
    G.psA = ysub[0]
    STK = st_tiles * 128
    h2T = S.sbuf(f"h2T{L}", [128, 8, STK], BF16)
    y_acc = S.sbuf(f"yacc{L}", [128, st_tiles, D], F32)
    gatesT = S.sbuf(f"gatesT{L}", [32, STK], BF16)
    sel = S.sbuf(f"sel{L}", [32, 32, 128], BF16)
    S.dma("pool", sel[:], G.sel_d.v(G.sel_d.t[:, :].rearrange("p (a b) -> p a b", b=128)))
    Wrt = S.sbuf(f"Wrt{L}", [128, 8, 36], F32)
    S.dma("sp", Wrt[:], G.moe_router.v(G.moe_router.t[li].rearrange("(kc p) n -> p kc n", p=128)))
    rb = S.sbuf(f"rb{L}", [1, 36], F32)
    S.dma("sp", rb[:], G.moe_router_b.v(G.moe_router_b.t[li]))
    gate_bc = S.sbuf(f"gbc{L}", [128, 3, D], F32)
    rs_used = sorted(set(t[2] for t in tiles))
    for r in rs_used:
        S.dma("sp", gate_bc[:, r, :], G.gates_d.v(G.gates_d.t[1, r:r + 1, :].partition_broadcast(128)))
    Wg = [[S.sbuf(f"Wg{i}{e}{L}", [128, 8, DFF], BF16) for e in range(2)] for i in range(2)]
    Wu = [[S.sbuf(f"Wu{i}{e}{L}", [128, 8, DFF], BF16) for e in range(2)] for i in range(2)]
    Wd = [[S.sbuf(f"Wd{i}{e}{L}", [128, 2, D], BF16) for e in range(2)] for i in range(2)]
    NS1 = 2
    xin_s = [S.sbuf(f"xin{i}{L}", [128, D], F32) for i in range(NS1)]
    junk_s = [S.sbuf(f"junk{i}{L}", [128, D], F32) for i in range(NS1)]
    wk_s = [{"junk": junk_s[i], "st": S.sbuf(f"st{i}{L}", [128, 4], F32), "xn32": S.sbuf(f"xn32{i}{L}", [128, D], F32),
             "fm32": S.sbuf(f"fm32{i}{L}", [128, 8, 128], F32)} for i in range(NS1)]
    h32_s = [S.sbuf(f"h32{i}{L}", [128, 8, 128], F32) for i in range(NS1)]
    lg_s = [S.sbuf(f"lg{i}{L}", [128, 36], F32) for i in range(NS1)]
    sm_s = [S.sbuf(f"sm{i}{L}", [128, 64], F32) for i in range(NS1)]
    g32_s = [S.sbuf(f"g32{i}{L}", [128, 32], F32) for i in range(NS1)]
    xin3 = S.sbuf(f"xin3{L}", [128, D], F32)
    out3 = S.sbuf(f"out3{L}", [128, D], F32)
    s_sb = [S.sbuf(f"s_sb{i}{L}", [128, 256], F32) for i in range(2)]
    t_sb = [S.sbuf(f"t_sb{i}{L}", [128, 256], F32) for i in range(2)]
    hidT = [S.sbuf(f"hidT{i}{L}", [128, 256], BF16) for i in range(2)]

    def load_pair(p, buf):
        for e in range(2):
            eg = p * 2 + e
            for W, nm in ((Wg, "g"), (Wu, "u"), (Wd, "d")):
                t = W[buf][e]
                S.dma("sp", t.v(t.t[:, :, :].rearrange("p a b -> p (a b)")), G.wbf[f"{nm}{li}_{eg}"][:])

    n_super = len(tiles) // st_tiles
    assert n_super * st_tiles == len(tiles) and st_tiles % 2 == 0
    for su in range(n_super):
        stl = tiles[su * st_tiles:(su + 1) * st_tiles]
        load_pair(0, 0)
        def step1_gen(j, b, row0, r, s):
            xin, wk, h32, lg, sm, g32 = xin_s[s], wk_s[s], h32_s[s], lg_s[s], sm_s[s], g32_s[s]
            S.dma("sp", xin[:], xin_d.v(xin_d.t[b, row0:row0 + 128, :]), split=4)
            yield
            yield from norm_tile_gen(S, G, xin[:], r, G.A2, 24, h2T[:, :, j * 128:(j + 1) * 128], wk, h32[:], ysub[s])
            yield
            psr = (psG[0] if s == 0 else psU[0])[:, 0:36]
            for kc in range(8):
                S.mm(psr, h32[:, kc, :], Wrt[:, kc, :], start=(kc == 0), stop=False)
            S.mm(psr, G.ones_f[0:1, :], rb[:], start=False, stop=True)
            S.copy("dve", lg[:], psr)
            yield
            c = lambda i, n=1: sm[:, i:i + n]
            S.reduce(c(0), lg[:, 0:4], op=ALU.max)
            yield
            S.ts("dve", c(1), c(0), -1.0, None, op0=ALU.mult)
            yield
            S.ts("dve", c(4, 4), lg[:, 0:4], c(0), None, op0=ALU.is_ge)
            yield
            S.act(c(8, 4), lg[:, 0:4], AF.Exp, bias=c(1), accum_out=c(2))
            yield
            S.recip(c(3), c(2))
            yield
            S.ts("dve", c(16, 8), lg[:, 4:12], c(4), None, op0=ALU.mult)
            yield
            for g in range(1, 4):
                S.stt(c(16, 8), lg[:, 4 + 8 * g:12 + 8 * g], c(4 + g), c(16, 8), ALU.mult, ALU.add)
                yield
            S.reduce(c(12), c(16, 8), op=ALU.max)
            yield
            S.ts("dve", c(24, 8), c(16, 8), c(12), None, op0=ALU.is_ge)
            yield
            S.stt(c(32, 8), c(24, 8), -1e30, c(16, 8), ALU.mult, ALU.add)
            yield
            S.reduce(c(13), c(32, 8), op=ALU.max)
            yield
            S.ts("dve", c(40, 8), c(32, 8), c(13), None, op0=ALU.is_ge)
            yield
            S.tt("dve", c(14), c(13), c(12), ALU.subtract)
            yield
            S.act(c(15), c(14), AF.Exp)
            yield
            S.ts("dve", c(48), c(15), 1.0, None, op0=ALU.add)
            yield
            S.recip(c(49), c(48))
            yield
            S.tt("dve", c(50), c(49), c(3), ALU.mult)
            yield
            S.tt("dve", c(51), c(50), c(15), ALU.mult)
            yield
            S.ts("dve", c(52, 8), c(24, 8), c(50), None, op0=ALU.mult)
            yield
            S.stt(c(52, 8), c(40, 8), c(51), c(52, 8), ALU.mult, ALU.add)
            yield
            for g in range(4):
                S.ts("dve", g32[:, g * 8:(g + 1) * 8], c(52, 8), c(4 + g), None, op0=ALU.mult)
                yield
            pst = (psG[1] if s == 0 else psU[1])[0:32, 0:128]
            S.tr(pst, g32[:], G.ident_f[:])
            S.copy("dve", gatesT[:, j * 128:(j + 1) * 128], pst)
            if dbg is not None and "gates" in dbg and su == 0:
                S.dma("sp", dbg["gates"].v(dbg["gates"].t[j]), g32[:])

        pend1 = [step1_gen(j, b, row0, r, j % NS1) for j, (b, row0, r) in enumerate(stl)]
        act1 = []
        rnd, last1 = 0, -99
        while pend1 or act1:
            if pend1 and len(act1) < NS1 and (rnd - last1 >= 20 or not act1):
                act1.append(pend1.pop(0))
                last1 = rnd
            nxt = []
            for g_ in act1:
                try:
                    next(g_)
                    nxt.append(g_)
                except StopIteration:
                    pass
            act1 = nxt
            rnd += 1
        G.psA = ysub[0]
        items = [(p, t2, e, fc) for p in range(npairs) for t2 in range(st_tiles // 2) for e in range(2) for fc in range(2)]

        def emit_gu(idx):
            p, t2, e, fc = items[idx]
            buf, eg, ib = p % 2, p * 2 + e, idx % 2
            tok = slice(t2 * 256, (t2 + 1) * 256)
            pg, pu = psG[ib], psU[ib]
            for kc in range(8):
                S.mm(pg[:, 0:256], Wg[buf][e][:, kc, fc * 128:(fc + 1) * 128], h2T[:, kc, tok], start=(kc == 0), stop=(kc == 7))
            for kc in range(8):
                S.mm(pu[:, 0:256], Wu[buf][e][:, kc, fc * 128:(fc + 1) * 128], h2T[:, kc, tok], start=(kc == 0), stop=(kc == 7))
            S.mm(pu[:, 256:512], sel[:, eg, :], gatesT[:, tok])
            S.act(s_sb[ib][:], pg[:, 0:256], AF.Silu)
            S.tt("dve", t_sb[ib][:], s_sb[ib][:], pu[:, 0:256], ALU.mult)
            S.tt("dve", hidT[ib][:], t_sb[ib][:], pu[:, 256:512], ALU.mult)

        def emit_down(idx):
            p, t2, e, fc = items[idx]
            buf, ib, it = p % 2, idx % 2, e * 2 + fc
            for ts_ in range(2):
                for n in range(2):
                    S.mm(ysub[ts_][:, n * 512:(n + 1) * 512], hidT[ib][:, ts_ * 128:(ts_ + 1) * 128],
                         Wd[buf][e][:, fc, n * 512:(n + 1) * 512], start=(it == 0), stop=(it == 3))
            if it == 3:
                for ts_ in range(2):
                    j = t2 * 2 + ts_
                    if p == 0:
                        S.copy("dve", y_acc[:, j, :], ysub[ts_][:])
                    else:
                        S.tt("dve", y_acc[:, j, :], y_acc[:, j, :], ysub[ts_][:], ALU.add)
                    if p == npairs - 1:
                        b, row0, r = stl[j]
                        S.dma("sp", xin3[:], xin_d.v(xin_d.t[b, row0:row0 + 128, :]))
                        S.tt("pool", out3[:], y_acc[:, j, :], gate_bc[:, r, :], ALU.mult)
                        S.tt("pool", out3[:], out3[:], xin3[:], ALU.add)
                        S.dma("sp", xout_fn(b, row0), out3[:])

        for idx in range(len(items)):
            emit_gu(idx)
            if idx > 0:
                emit_down(idx - 1)
            p, t2, e, fc = items[idx]
            if t2 == 0 and e == 0 and fc == 0 and p + 1 < npairs:
                load_pair(p + 1, (p + 1) % 2)
        emit_down(len(items) - 1)
    S.pop_scope()

LAM_INIT1 = 0.8 - 0.6 * float(np.exp(-0.3 * 1))


def attn_phase(S, G, x2_d, x3_d, nb=NB, nqt=4, nh=8):
    li = 1
    NKT = NT
    S.push_scope()
    psA = S.psum("psA_a", [128, 1024], F32)
    psT = S.psum("psT_a", [128, 1024], BF16)
    psQ = [S.psum(f"psQ{i}_a", [128, 512], F32) for i in range(3)]
    psO = [S.psum(f"psO{i}_a", [128, 512], F32) for i in range(2)]
    G.psA, G.psT = psA, psT
    KT_all = S.sbuf("KT_all", [128, 8, TOK], BF16)
    QT_all = S.sbuf("QT_all", [128, 8, LAT], BF16)
    V_all = S.sbuf("V_all", [128, NKT, 8, 130], BF16)
    S.memset("pool", V_all[:], 1.0)
    gq_bc = S.sbuf("gq_bc", [128, 64], F32)
    gk_bc = S.sbuf("gk_bc", [128, 64], F32)
    sg_bc = S.sbuf("sg_bc", [128, 128], F32)
    S.dma("sp", gq_bc[:], G.da_q_norm_g.v(G.da_q_norm_g.t[0:1, :].partition_broadcast(128)))
    S.dma("sp", gk_bc[:], G.da_k_norm_g.v(G.da_k_norm_g.t[0:1, :].partition_broadcast(128)))
    S.dma("sp", sg_bc[:], G.da_subln_g.v(G.da_subln_g.t[0:1, :].partition_broadcast(128)))
    S.ts("dve", sg_bc[:], sg_bc[:], 1.0 - LAM_INIT1, None, op0=ALU.mult)
    lamv = S.sbuf("lamv", [128, 4, 64], F32)
    lsm = S.sbuf("lsm", [128, 8], F32)
    for i in range(4):
        S.dma("sp", lamv[:, i, :], G.da_lam.v(G.da_lam.t[i:i + 1, :].partition_broadcast(128)))
    S.tt("dve", lamv[:, 0, :], lamv[:, 0, :], lamv[:, 1, :], ALU.mult)
    S.tt("dve", lamv[:, 2, :], lamv[:, 2, :], lamv[:, 3, :], ALU.mult)
    S.reduce(lsm[:, 0:1], lamv[:, 0, :])
    S.reduce(lsm[:, 1:2], lamv[:, 2, :])
    S.act(lsm[:, 2:4], lsm[:, 0:2], AF.Exp)
    S.tt("dve", lsm[:, 4:5], lsm[:, 3:4], lsm[:, 2:3], ALU.subtract)
    S.ts("dve", lsm[:, 5:6], lsm[:, 4:5], -LAM_INIT1, None, op0=ALU.add)
    neglam = lsm[:, 5:6]
    junk = S.sbuf("junk_a", [128, D], F32)
    scrq = S.sbuf("scrq", [128, D], F32)
    xin = S.sbuf("xin_a", [128, D], F32)
    st = S.sbuf("st_a", [128, 4], F32)
    st16 = S.sbuf("st16_a", [128, 3, 16], F32)
    for b in range(nb):
        S.push_scope()
        Wqkv = S.sbuf(f"Wqkv{b}", [128, 8, 3 * D], BF16)
        S.dma("sp", Wqkv.v(Wqkv.t[:, :, :].rearrange("p a b -> p (a b)")), G.wbf["Wqkv"][:])
        hT_t = S.sbuf(f"hT_t{b}", [128, 8, 128], BF16)
        outq = S.sbuf(f"outq{b}", [128, D], BF16)
        wk = {"junk": junk, "st": st, "xn": S.sbuf(f"xn_a{b}", [128, D], BF16), "fm32": S.sbuf(f"fm32_a{b}", [128, 8, 128], F32)}
        cs_t = S.sbuf(f"cs_t{b}", [128, 2, 32], F32)
        tmpa = S.sbuf(f"tmpa{b}", [128, 512], F32)
        tmpb = S.sbuf(f"tmpb{b}", [128, 512], F32)

        def qk_norm(gain_bc, rope, dst_fm):
            S.act(junk[:], psA[:], AF.Square)
            S.reduce(st16[:, 0, :], junk.v(junk.t[:, :].rearrange("p (g n) -> p g n", n=64)))
            S.ts("dve", st16[:, 1, :], st16[:, 0, :], 1.0 / 64, EPS, op0=ALU.mult, op1=ALU.add)
            S.act(st16[:, 1, :], st16[:, 1, :], AF.Sqrt)
            S.recip(st16[:, 2, :], st16[:, 1, :])
            S.tt("dve", scrq.v(scrq.t[:, :].rearrange("p (g n) -> p g n", n=64)),
                 psA.v(psA.t[:, :].rearrange("p (g n) -> p g n", n=64)),
                 st16.v(st16.t[:, 2, :].unsqueeze(2).to_broadcast([128, 16, 64])), ALU.mult)
            gv = gain_bc.v(gain_bc.t[:, :].unsqueeze(1).to_broadcast([128, 16, 64]))
            if not rope:
                S.tt("pool", outq.v(outq.t[:, :].rearrange("p (g n) -> p g n", n=64)),
                     scrq.v(scrq.t[:, :].rearrange("p (g n) -> p g n", n=64)), gv, ALU.mult)
            else:
                S.tt("pool", scrq.v(scrq.t[:, :].rearrange("p (g n) -> p g n", n=64)),
                     scrq.v(scrq.t[:, :].rearrange("p (g n) -> p g n", n=64)), gv, ALU.mult)
                x5 = scrq.t[:, :].rearrange("p (g a h f) -> p g a h f", g=16, a=2, h=2)
                o5 = outq.t[:, :].rearrange("p (g a h f) -> p g a h f", g=16, a=2, h=2)
                x1, x2 = scrq.v(x5[:, :, :, 0, :]), scrq.v(x5[:, :, :, 1, :])
                o1, o2 = outq.v(o5[:, :, :, 0, :]), outq.v(o5[:, :, :, 1, :])
                cv = cs_t.v(cs_t.t[:, 0, :].rearrange("p (a f) -> p a f", a=2).unsqueeze(1).to_broadcast([128, 16, 2, 16]))
                sv = cs_t.v(cs_t.t[:, 1, :].rearrange("p (a f) -> p a f", a=2).unsqueeze(1).to_broadcast([128, 16, 2, 16]))
                ta = tmpa.v(tmpa.t[:, :].rearrange("p (g a f) -> p g a f", g=16, a=2))
                tb = tmpb.v(tmpb.t[:, :].rearrange("p (g a f) -> p g a f", g=16, a=2))
                S.tt("dve", ta, x1, cv, ALU.mult)
                S.tt("pool", tb, x2, sv, ALU.mult)
                S.tt("dve", o1, ta, tb, ALU.subtract)
                S.tt("dve", ta, x1, sv, ALU.mult)
                S.tt("pool", tb, x2, cv, ALU.mult)
                S.tt("pool", o2, ta, tb, ALU.add)
            for ec in range(8):
                S.tr(psT[:, ec * 128:(ec + 1) * 128], outq[:, ec * 128:(ec + 1) * 128], G.ident_b[:])
            S.copy("act", dst_fm, psT.v(psT.t[:, :].rearrange("p (a b) -> p a b", b=128)))

        def proj(c0):
            for n in range(2):
                for kc in range(8):
                    S.mm(psA[:, n * 512:(n + 1) * 512], hT_t[:, kc, :], Wqkv[:, kc, c0 + n * 512:c0 + (n + 1) * 512], start=(kc == 0), stop=(kc == 7))

        for ti in range(NT):
            r = 2 if ti < 2 else b
            S.dma("sp", xin[:], x2_d.v(x2_d.t[b, ti * 128:(ti + 1) * 128, :]), split=4)
            norm_tile_to_fm(S, G, xin[:], r, G.A1, 0, hT_t[:], wk)
            lat = ti >= 2
            if lat:
                t0 = (ti - 2) * 128
                S.dma("sp", cs_t[:, 0, :], G.rope_cos_d.v(G.rope_cos_d.t[t0:t0 + 128, :]))
                S.dma("sp", cs_t[:, 1, :], G.rope_sin_d.v(G.rope_sin_d.t[t0:t0 + 128, :]))
            proj(D)
            qk_norm(gk_bc, lat, KT_all[:, :, ti * 128:(ti + 1) * 128])
            proj(2 * D)
            S.copy("act", V_all[:, ti, :, 0:128], psA.v(psA.t[:, :].rearrange("p (h n) -> p h n", n=128)))
            if lat:
                proj(0)
                qk_norm(gq_bc, True, QT_all[:, :, t0:t0 + 128])
        S.pop_scope()
        S.push_scope()
        Wo = S.sbuf(f"Wo_a{b}", [128, 8, D], BF16)
        S.dma("sp", Wo.v(Wo.t[:, :, :].rearrange("p a b -> p (a b)")), G.wbf["daWo"][:])
        gate_bc = S.sbuf(f"gate_a{b}", [128, D], F32)
        S.dma("sp", gate_bc[:], G.gates_d.v(G.gates_d.t[0, b:b + 1, :].partition_broadcast(128)))
        O_all = S.sbuf(f"O_all{b}", [128, 4, D], F32)
        pT = [S.sbuf(f"pT{i}_{b}", [128, 512], BF16) for i in range(3)]
        rec = S.sbuf(f"rec{b}", [128, 8], F32)
        pre = S.sbuf(f"pre_a{b}", [128, D], BF16)
        preT = S.sbuf(f"preT_a{b}", [128, 8, 128], BF16)
        st8 = S.sbuf(f"st8_{b}", [128, 3, 8], F32)
        aitems = [(qt, h, m, kt) for qt in range(nqt) for h in range(nh) for m in range(2) for kt in range(NKT)]

        def emit_qk(i):
            qt, h, m, kt = aitems[i]
            pr = slice(m * 64, m * 64 + 64)
            ps = psQ[i % 3]
            S.mm(ps[:], KT_all[pr, h, kt * 128:(kt + 1) * 128], QT_all[pr, h, qt * 512:(qt + 1) * 512])
            S.act(pT[i % 3][:], ps[:], AF.Exp, scale=0.125)

        def emit_pv(i):
            qt, h, m, kt = aitems[i]
            for qs in range(4):
                acc = psO[qs // 2][:, (qs % 2) * 256:(qs % 2) * 256 + 129]
                S.mm(acc, pT[i % 3][:, qs * 128:(qs + 1) * 128], V_all[:, kt, h, 0:129],
                     start=(kt == 0 and qs % 2 == 0), stop=(kt == NKT - 1), skip_group_check=True)
            if kt != NKT - 1:
                return
            for qs in range(4):
                c0 = (qs % 2) * 256
                S.recip(rec[:, qs:qs + 1], psO[qs // 2][:, c0 + 128:c0 + 129])
                if m == 0:
                    S.ts("dve", O_all[:, qs, h * 128:(h + 1) * 128], psO[qs // 2][:, c0:c0 + 128], rec[:, qs:qs + 1], None, op0=ALU.mult)
                else:
                    S.tt("dve", rec[:, 4 + qs:5 + qs], rec[:, qs:qs + 1], neglam, ALU.mult)
                    S.stt(O_all[:, qs, h * 128:(h + 1) * 128], psO[qs // 2][:, c0:c0 + 128], rec[:, 4 + qs:5 + qs],
                          O_all[:, qs, h * 128:(h + 1) * 128], ALU.mult, ALU.add)
            if not (h == nh - 1 and m == 1):
                return
            for qs in range(4):
                O3 = O_all.v(O_all.t[:, qs, :].rearrange("p (h n) -> p h n", n=128))
                S.act(junk[:], O_all[:, qs, :], AF.Square)
                S.reduce(st8[:, 0, :], junk.v(junk.t[:, :].rearrange("p (h n) -> p h n", n=128)))
                S.ts("dve", st8[:, 1, :], st8[:, 0, :], 1.0 / 128, EPS, op0=ALU.mult, op1=ALU.add)
                S.act(st8[:, 1, :], st8[:, 1, :], AF.Sqrt)
                S.recip(st8[:, 2, :], st8[:, 1, :])
                S.tt("dve", O3, O3, st8.v(st8.t[:, 2, :].unsqueeze(2).to_broadcast([128, 8, 128])), ALU.mult)
                S.tt("pool", pre.v(pre.t[:, :].rearrange("p (h n) -> p h n", n=128)), O3,
                     sg_bc.v(sg_bc.t[:, :].unsqueeze(1).to_broadcast([128, 8, 128])), ALU.mult)
                for ec in range(8):
                    S.tr(psT[:, ec * 128:(ec + 1) * 128], pre[:, ec * 128:(ec + 1) * 128], G.ident_b[:])
                S.copy("act", preT[:], psT.v(psT.t[:, :].rearrange("p (a b) -> p a b", b=128)))
                for n in range(2):
                    for kc in range(8):
                        S.mm(psA[:, n * 512:(n + 1) * 512], preT[:, kc, :], Wo[:, kc, n * 512:(n + 1) * 512], start=(kc == 0), stop=(kc == 7))
                row = (qt * 4 + qs) * 128
                S.dma("sp", xin[:], x2_d.v(x2_d.t[b, CTX + row:CTX + row + 128, :]))
                S.tt("dve", scrq[:], psA[:], gate_bc[:], ALU.mult)
                S.tt("pool", xin[:], xin[:], scrq[:], ALU.add)
                S.dma("sp", x3_d.v(x3_d.t[b, row:row + 128, :]), xin[:])

        SK = 2
        for i in range(len(aitems) + SK):
            if i < len(aitems):
                emit_qk(i)
            if i >= SK:
                emit_pv(i - SK)
        S.pop_scope()
    S.pop_scope()

def build(cfg):
    nc = bass.Bass("TRN2", target_bir_lowering=False)
    S = Sched(nc)
    G = Ctx()
    setup_common(S, G, need=cfg.get("need", ("rwkv", "da", "moe")))
    outs = []
    dbg = {}
    stop = cfg.get("stop", "end")
    kind_x1 = "ExternalOutput" if stop == "rwkv" else "Internal"
    x1_d = S.dram("x1_d", [NB, TOK, D], F32, kind=kind_x1)
    if cfg.get("dbg_hT"):
        dbg["hT"] = S.dram("dbg_hT", [128, 8, LAT + 2], BF16, kind="ExternalOutput")
    wcast_phase(S, G, cfg.get("need", ("rwkv", "da", "moe")))
    if cfg.get("attn_in_ext"):
        G.x2_ext = S.dram("x2_ext", [NB, TOK, D], F32, kind="ExternalInput")
    if cfg.get("moe_in_ext"):
        G.x1_ext = S.dram("x1_ext", [NB, TOK, D], F32, kind="ExternalInput")
    if not cfg.get("skip_l0"):
        mod_phase(S, G, 0)
    if cfg.get("dbg_mod"):
        dm = S.dram("dbg_modT", [128, 48 * 4], F32, kind="ExternalOutput")
        S.dma("sp", dm[:], G.modT.v(G.modT.t[:, :, :].rearrange("p a b -> p (a b)")))
        dg = S.dram("dbg_gates", [2, 3, D], F32, kind="ExternalOutput")
        S.dma("sp", dg[:], G.gates_d[:])
    if stop == "mod":
        S.barrier()
        S.emit()
        return nc, S
    if not cfg.get("skip_rwkv") and not cfg.get("skip_l0"):
        rwkv_phase(S, G, x1_d, dbg=dbg, nb=cfg.get("nb", NB), **cfg.get("rw", {}))
    if stop == "rwkv":
        S.barrier()
        S.emit()
        return nc, S
    x2_d = S.dram("x2_d", [NB, TOK, D], F32, kind="ExternalOutput" if stop == "moe0" else "Internal")
    if not cfg.get("skip_l0"):
        moe0 = True
    else:
        moe0 = False
    tiles0 = [(b, ti * 128, 2 if ti < 2 else b) for b in range(NB) for ti in range(NT)]
    if cfg.get("dbg_gates"):
        dbg["gates"] = S.dram("dbg_gates32", [12, 128, 32], F32, kind="ExternalOutput")
    if moe0:
      moe_phase(S, G, 0, x1_d if not cfg.get("moe_in_ext") else G.x1_ext, tiles0[:cfg.get("moe_ntiles", len(tiles0))],
                lambda b, row0: x2_d.v(x2_d.t[b, row0:row0 + 128, :]), cfg.get("st0", 12), npairs=cfg.get("npairs", 16), dbg=dbg)
    if stop == "moe0":
        S.barrier()
        S.emit()
        return nc, S
    x3_d = S.dram("x3_d", [NB, LAT, D], F32, kind="ExternalOutput" if stop == "attn" else "Internal")
    mod_phase(S, G, 1)
    attn_phase(S, G, x2_d if not cfg.get("attn_in_ext") else G.x2_ext, x3_d, **cfg.get("at", {}))
    if stop == "attn":
        S.barrier()
        S.emit()
        return nc, S
    out_d = S.dram("out", [NB, LAT, D], F32, kind="ExternalOutput")
    tiles1 = [(b, ti * 128, b) for b in range(NB) for ti in range(LAT // 128)]
    moe_phase(S, G, 1, x3_d, tiles1, lambda b, row0: out_d.v(out_d.t[b, row0:row0 + 128, :]), cfg.get("st1", 8))
    S.barrier()
    S.emit()
    return nc, S


def prep_core_inputs(inputs, core, consts):
    b0 = core * NB
    f = lambda a: np.ascontiguousarray(a, dtype=np.float32)
    m = {}
    m["x"] = f(inputs["x"][b0:b0 + NB])
    m["ctx"] = f(inputs["ctx"][b0:b0 + NB])
    m["c3"] = f(np.concatenate([inputs["c"][b0:b0 + NB], inputs["c_ctx"][None, :]], axis=0))
    pv = np.concatenate([
        inputs["norm1_g"].reshape(16, 128), inputs["norm2_g"].reshape(16, 128),
        inputs["rw_mu"][0].reshape(48, 128), inputs["b_mod"].reshape(96, 128)], axis=0)
    m["pvec"] = f(pv)
    m["w_mod"] = f(inputs["w_mod"])
    m["b_mod"] = f(inputs["b_mod"])
    for k, v in consts.items():
        m[k] = v
    m["rw_w_rkv"] = f(inputs["rw_w_rkv"][0])
    for k in ("rw_w0", "rw_w1", "rw_w2", "rw_a0", "rw_a1", "rw_a2", "rw_g1", "rw_g2", "rw_w_o"):
        m[k] = f(inputs[k][0])
    for k in ("rw_k_k", "rw_k_a", "rw_lnx_g", "rw_lnx_b"):
        m[k] = f(inputs[k][0].reshape(1, D))
    m["rw_r_k"] = f(inputs["rw_r_k"][0].reshape(1, D))
    m["da_w_qkv"] = f(inputs["da_w_qkv"][0])
    m["da_q_norm_g"] = f(inputs["da_q_norm_g"][0].reshape(1, 64))
    m["da_k_norm_g"] = f(inputs["da_k_norm_g"][0].reshape(1, 64))
    m["da_lam"] = f(np.stack([inputs["da_lam_q1"][0], inputs["da_lam_k1"][0], inputs["da_lam_q2"][0], inputs["da_lam_k2"][0]]))
    m["da_subln_g"] = f(inputs["da_subln_g"][0].reshape(1, 128))
    m["da_w_o"] = f(inputs["da_w_o"][0])
    rt = np.concatenate([inputs["moe_router_g"], np.transpose(inputs["moe_router_e"], (0, 2, 1, 3)).reshape(2, D, 32)], axis=2)
    m["moe_router"] = f(rt)
    rb = np.concatenate([inputs["moe_router_g_b"], inputs["moe_router_e_b"].reshape(2, 32)], axis=1).reshape(2, 1, 36)
    m["moe_router_b"] = f(rb)
    m["moe_w_gate"] = f(inputs["moe_w_gate"]).reshape(2, NEXP, D, DFF)
    m["moe_w_up"] = f(inputs["moe_w_up"]).reshape(2, NEXP, D, DFF)
    m["moe_w_down"] = f(inputs["moe_w_down"]).reshape(2, NEXP, DFF, D)
    return m


_CACHE = {}


def kernel(**inputs):
    from concourse.bass_utils import run_bass_kernel_spmd
    n = 8
    if "nc" not in _CACHE:
        _CACHE["nc"] = build({})[0]
        _CACHE["consts"] = host_consts()
    nc = _CACHE["nc"]
    consts = _CACHE["consts"]
    inputs = {k: np.asarray(v) for k, v in inputs.items()}
    in_maps = [prep_core_inputs(inputs, c, consts) for c in range(n)]
    res = run_bass_kernel_spmd(nc, in_maps, core_ids=list(range(n)))
    out = np.concatenate([r["out"] for r in res.results], axis=0)
    return out.astype(np.float32)
```

```python
import numpy as np
import concourse.bass as bass
import concourse.mybir as mybir

F32 = mybir.dt.float32
BF16 = mybir.dt.bfloat16
I32 = mybir.dt.int32
U32 = mybir.dt.uint32
AF = mybir.ActivationFunctionType
ALU = mybir.AluOpType
AX = mybir.AxisListType

ENGS = ("pe", "dve", "act", "pool", "sp")
SEM_LIMIT = 30000
N_DMA_SEMS = 24


class V:
    __slots__ = ("ap", "toks", "excl")

    def __init__(self, ap, toks, excl=False):
        self.ap = ap
        self.toks = toks
        self.excl = excl


class Tl:
    def __init__(self, S, t, name, excl=False, toks=None):
        self.S = S
        self.t = t
        self.name = name
        self.excl = excl
        self.toks = toks

    def _tk(self, key):
        if self.toks is not None:
            return list(self.toks)
        return [(self.name, None if self.excl else key)]

    def __getitem__(self, idx):
        return V(self.t[idx], self._tk(None), self.excl)

    def k(self, key, idx=None):
        ap = self.t[idx] if idx is not None else None
        return V(ap, self._tk(key), self.excl)

    def v(self, ap, key=None):
        return V(ap, self._tk(key), self.excl)


class Sched:
    def __init__(self, nc, same_engine_sync=True):
        self.nc = nc
        self.q = {e: [] for e in ENGS}
        self.cnt = {e: 0 for e in ENGS}
        self.semi = {e: 0 for e in ENGS}
        self.nsem = {e: 1 for e in ENGS}
        self.state = {}
        self.waited = {e: {} for e in ENGS}
        self.same = same_engine_sync
        self.dma_i = 0
        self.dma_cnt = [0] * N_DMA_SEMS
        self.dma_last = [None] * N_DMA_SEMS
        self.ctx = []
        self.n_instr = 0
        self.out_deps = []

    def sbuf(self, name, shape, dtype=F32):
        cm = self.nc.sbuf_tensor(name, list(shape), dtype)
        t = cm.__enter__()
        self.ctx.append(cm)
        return Tl(self, t, name)

    def psum(self, name, shape, dtype=F32):
        cm = self.nc.psum_tensor(name, list(shape), dtype)
        t = cm.__enter__()
        self.ctx.append(cm)
        return Tl(self, t, name, excl=True)

    def dram(self, name, shape, dtype=F32, kind="Internal"):
        t = self.nc.dram_tensor(name, list(shape), dtype, kind=kind)
        return Tl(self, t.ap(), name)

    def push_scope(self):
        self.scopes = getattr(self, "scopes", [])
        self.scopes.append(len(self.ctx))

    def pop_scope(self):
        self.barrier()
        n = self.scopes.pop()
        while len(self.ctx) > n:
            self.ctx.pop().__exit__(None, None, None)

    def barrier(self):
        deps = []
        for e in ENGS:
            if self.cnt[e] > 0:
                deps.append(((e, self.semi[e]), self.cnt[e], e))
        for i in range(N_DMA_SEMS):
            if self.dma_last[i] is not None:
                deps.append(self.dma_last[i])
        for e in ENGS:
            d = [x for x in deps if x[2] != e]
            w = self._waits(e, d)
            if w:
                self.q[e].append(("wait", None, w, None))

    def _deps(self, reads, writes):
        deps = []
        for v in reads:
            for tok in v.toks:
                st = self.state.get(tok)
                if st and st[0] is not None:
                    deps.append(st[0])
        for v in writes:
            for tok in v.toks:
                st = self.state.get(tok)
                if st:
                    if st[0] is not None:
                        deps.append(st[0])
                    deps.extend(st[1])
        return deps

    def _update(self, reads, writes, me, real_writes=None):
        for v in reads:
            for tok in v.toks:
                st = self.state.setdefault(tok, [None, [], None])
                st[1].append(me)
                if len(st[1]) > 64:
                    best = {}
                    for d in st[1]:
                        if d[0] not in best or best[d[0]][1] < d[1]:
                            best[d[0]] = d
                    st[1] = list(best.values())
        rw = writes if real_writes is None else real_writes
        rwt = set()
        for v in rw:
            rwt.update(v.toks)
        for v in writes:
            for tok in v.toks:
                old = self.state.get(tok)
                lrw = me if tok in rwt else (old[2] if old else None)
                self.state[tok] = [me, [], lrw]

    def _waits(self, eng, deps, raw_toks_same=None):
        need = {}
        for d in deps:
            semkey, val, deng, is_raw = d[0], d[1], d[2], True
            if deng == eng and not self.same:
                continue
            w = self.waited[eng].get(semkey, 0)
            if val > w and val > need.get(semkey, 0):
                need[semkey] = val
        for sk, val in need.items():
            self.waited[eng][sk] = val
        return list(need.items())

    def op(self, eng, fn, reads=(), writes=(), same_ok=False):
        reads = list(reads)
        real_writes = list(writes)
        writes = real_writes + [v for v in reads if v.excl]
        deps = self._deps(reads, writes)
        if same_ok or eng == "pe":
            deps = [d for d in deps if d[2] != eng]
        elif self.same:
            rd = []
            for v in reads:
                for tok in v.toks:
                    st = self.state.get(tok)
                    if st and st[2] is not None and st[2][2] == eng:
                        rd.append(st[2])
            deps = [d for d in deps if d[2] != eng] + rd
        waits = self._waits(eng, deps)
        if self.cnt[eng] >= SEM_LIMIT:
            self.semi[eng] += 1
            self.nsem[eng] = max(self.nsem[eng], self.semi[eng] + 1)
            self.cnt[eng] = 0
        self.cnt[eng] += 1
        me = ((eng, self.semi[eng]), self.cnt[eng], eng)
        self.q[eng].append(("op", fn, waits, me[0]))
        self._update(reads, writes, me, real_writes)
        self.n_instr += 1
        return me

    def dma(self, queue, out, in_, split=0, **kw):
        reads, writes = [in_], [out]
        deps = self._deps(reads, writes)
        i = self.dma_i % N_DMA_SEMS
        self.dma_i += 1
        if self.dma_last[i] is not None:
            deps.append(self.dma_last[i])
        waits = self._waits(queue, deps)
        oa, ia = out.ap, in_.ap
        parts = [(oa, ia)]
        if split:
            step = 128 // split
            parts = [(oa[k * step:(k + 1) * step], ia[k * step:(k + 1) * step]) for k in range(split)]
        for k, (o_, a_) in enumerate(parts):
            self.dma_cnt[i] += 16
            self.q[queue].append(("dma", (o_, a_, kw), waits if k == 0 else [], ("dma", i)))
            self.n_instr += 1
        me = (("dma", i), self.dma_cnt[i], "dma")
        self.dma_last[i] = me
        self._update(reads, writes, me)
        return me

    def emit(self, final_deps=None):
        nc = self.nc
        sems = {}
        cms = []

        def mk(name):
            cm = nc.semaphore(name)
            s = cm.__enter__()
            cms.append(cm)
            return s
        for e in ENGS:
            for i in range(self.nsem[e]):
                sems[(e, i)] = mk(f"s_{e}_{i}")
        for i in range(N_DMA_SEMS):
            sems[("dma", i)] = mk(f"s_dma_{i}")
        engobj = {"pe": "tensor", "dve": "vector", "act": "scalar", "pool": "gpsimd", "sp": "sync"}
        if final_deps:
            w = self._waits("sp", final_deps)
            self.q["sp"].append(("wait", None, w, None))
        with nc.Block() as block:
            for e in ENGS:
                items = self.q[e]
                if not items:
                    continue

                def body(eng, items=items):
                    for kind, fn, waits, semkey in items:
                        for sk, val in waits:
                            eng.wait_ge(sems[sk], val)
                        if kind == "op":
                            ins = fn(eng)
                            ins.then_inc(sems[semkey], 1)
                        elif kind == "dma":
                            oa, ia, kw = fn
                            eng.dma_start(out=oa, in_=ia, **kw).then_inc(sems[semkey], 16)
                getattr(block, engobj[e])(body)
        for cm in reversed(cms):
            cm.__exit__(None, None, None)
        for cm in reversed(self.ctx):
            cm.__exit__(None, None, None)

    def mm(self, out, lhsT, rhs, start=True, stop=True, **kw):
        return self.op("pe", lambda e: e.matmul(out.ap, lhsT.ap, rhs.ap, start=start, stop=stop, **kw),
                       reads=[lhsT, rhs] + ([] if start else [out]), writes=[out])

    def tr(self, out, in_, ident):
        return self.op("pe", lambda e: e.transpose(out.ap, in_.ap, ident.ap),
                       reads=[in_, ident], writes=[out])

    def act(self, out, in_, func, bias=None, scale=None, accum_out=None, eng="act"):
        reads = [in_]
        kw = {}
        if isinstance(bias, V):
            reads.append(bias)
            kw["bias"] = bias.ap
        elif bias is not None:
            kw["bias"] = bias
        if isinstance(scale, V):
            reads.append(scale)
            kw["scale"] = scale.ap
        elif scale is not None:
            kw["scale"] = scale
        writes = [out]
        if accum_out is not None:
            writes.append(accum_out)
            kw["accum_out"] = accum_out.ap
        return self.op("act", lambda e: e.activation(out.ap, in_.ap, func, **kw), reads=reads, writes=writes)

    def tt(self, eng, out, in0, in1, op):
        return self.op(eng, lambda e: e.tensor_tensor(out.ap, in0.ap, in1.ap, op), reads=[in0, in1], writes=[out])

    def ts(self, eng, out, in0, s1, s2=None, op0=ALU.mult, op1=None, accum_out=None):
        reads = [in0]
        a1 = s1.ap if isinstance(s1, V) else s1
        a2 = s2.ap if isinstance(s2, V) else s2
        if isinstance(s1, V):
            reads.append(s1)
        if isinstance(s2, V):
            reads.append(s2)
        writes = [out]
        kw = {}
        if op1 is not None:
            kw["op1"] = op1
        if accum_out is not None:
            kw["accum_out"] = accum_out.ap
            writes.append(accum_out)
        return self.op(eng, lambda e: e.tensor_scalar(out.ap, in0.ap, a1, a2, op0, **kw), reads=reads, writes=writes)

    def stt(self, out, in0, scalar, in1, op0, op1, eng="dve"):
        reads = [in0, in1]
        a = scalar.ap if isinstance(scalar, V) else scalar
        if isinstance(scalar, V):
            reads.append(scalar)
        return self.op(eng, lambda e: e.scalar_tensor_tensor(out.ap, in0.ap, a, in1.ap, op0, op1), reads=reads, writes=[out])

    def copy(self, eng, out, in_):
        if eng == "act":
            return self.op("act", lambda e: e.copy(out.ap, in_.ap), reads=[in_], writes=[out])
        return self.op(eng, lambda e: e.tensor_copy(out.ap, in_.ap), reads=[in_], writes=[out])

    def memset(self, eng, out, val):
        return self.op(eng, lambda e: e.memset(out.ap, val), reads=[], writes=[out])

    def reduce(self, out, in_, op=ALU.add, axis=AX.X, eng="dve"):
        return self.op(eng, lambda e: e.tensor_reduce(out.ap, in_.ap, axis, op), reads=[in_], writes=[out])

    def recip(self, out, in_):
        return self.op("dve", lambda e: e.reciprocal(out.ap, in_.ap), reads=[in_], writes=[out])
D = 1024
NB = 2
LAT = 2048
CTX = 256
TOK = CTX + LAT
NT = TOK // 128
EPS = 1e-6
DECAY_C = -0.6065306597126334
LNX_EPS = 64e-5
NGRP = 4
NEXP = 32
DFF = 256

PV_N1 = (0, 16)
PV_N2 = (16, 32)
PV_MU = 32
PV_BMOD = 80
PV_ROWS = 176


def host_consts():
    c = {}
    c["ident_f"] = np.eye(128, dtype=np.float32)
    c["ones_f"] = np.ones((128, 128), dtype=np.float32)
    idx = np.arange(128)
    cs, ct = idx[:, None] // 64, idx[None, :] // 64
    same = (cs == ct)
    s_, t_ = idx[:, None], idx[None, :]
    maskA = np.zeros((2, 128, 256), np.float32)
    maskAT = np.zeros((2, 128, 128), np.float32)
    tri = np.zeros((2, 128, 384), np.float32)
    for z in range(2):
        prec = same & ((s_ < t_) if z == 0 else (s_ > t_))
        preceq = same & ((s_ <= t_) if z == 0 else (s_ >= t_))
        succ = same & ((s_ > t_) if z == 0 else (s_ < t_))
        maskA[z, :, 0:128] = prec
        maskA[z, :, 128:256] = preceq
        maskAT[z] = prec.T
        tri[z, :, 0:128] = preceq * DECAY_C
        tri[z, :, 128:256] = prec * DECAY_C
        tri[z, :, 256:384] = succ * DECAY_C
    c["maskA"] = maskA
    c["maskAT"] = maskAT
    c["tri"] = tri
    ind = np.zeros((128, 2), np.float32)
    ind[0:64, 0] = DECAY_C
    ind[64:128, 1] = DECAY_C
    c["ind"] = ind
    rows = LAT // 64
    row = np.repeat(np.arange(rows), 64)
    col = np.tile(np.arange(64), rows)
    inv = (10000.0 ** (-np.arange(16, dtype=np.float32) / 16)).astype(np.float32)
    ang = np.stack([row, col], axis=-1).astype(np.float32)[:, :, None] * inv
    c["rope_cos"] = np.cos(ang).astype(np.float32).reshape(LAT, 32)
    c["rope_sin"] = np.sin(ang).astype(np.float32).reshape(LAT, 32)
    sel = np.zeros((32, 32, 128), np.float32)
    for e in range(32):
        sel[e, e, :] = 1.0
    c["sel"] = sel.reshape(32, 32 * 128)
    return c


class Ctx:
    pass


def std_psum(S, G, tag):
    G.psA = S.psum(f"psA{tag}", [128, 1024], F32)
    nm = G.psA.name
    G.psA.toks = [(nm, 0), (nm, 1)]
    G.psA_h = [Tl(S, G.psA.t[:, i * 512:(i + 1) * 512].rearrange("p (a b) -> p a b", b=128), nm, excl=True,
                  toks=[(nm, i)]) for i in range(2)]
    G.psY = S.psum(f"psY{tag}", [128, 1024], F32)
    G.psT = S.psum(f"psT{tag}", [128, 1024], BF16)
    G.psH = [S.psum(f"psH{i}{tag}", [128, 4, 128], F32) for i in range(3)]


def setup_common(S, G, need=("rwkv", "da", "moe")):
    def I(name, shape, dt=F32):
        grp = name.split('_')[0]
        if grp in ('rw', 'da', 'moe') and {'rw': 'rwkv', 'da': 'da', 'moe': 'moe'}[grp] not in need:
            return None
        return S.dram(name, shape, dt, kind="ExternalInput")
    G.x = I("x", [NB, LAT, D])
    G.ctx = I("ctx", [NB, CTX, D])
    G.c3 = I("c3", [3, D])
    G.pvec = I("pvec", [PV_ROWS, 128])
    G.w_mod = I("w_mod", [2, D, 6 * D])
    G.b_mod = I("b_mod", [2, 6 * D])
    G.ident_f_d = I("ident_f", [128, 128])
    G.ones_f_d = I("ones_f", [128, 128])
    G.maskA_d = I("maskA", [2, 128, 256])
    G.maskAT_d = I("maskAT", [2, 128, 128])
    G.tri_d = I("tri", [2, 128, 384])
    G.ind_d = I("ind", [128, 2])
    G.rope_cos_d = I("rope_cos", [LAT, 32])
    G.rope_sin_d = I("rope_sin", [LAT, 32])
    G.sel_d = I("sel", [32, 32 * 128])
    G.rw_w_rkv = I("rw_w_rkv", [3, D, D])
    G.rw_w0 = I("rw_w0", [2, D])
    G.rw_w1 = I("rw_w1", [2, D, 64])
    G.rw_w2 = I("rw_w2", [2, 64, D])
    G.rw_a0 = I("rw_a0", [2, D])
    G.rw_a1 = I("rw_a1", [2, D, 64])
    G.rw_a2 = I("rw_a2", [2, 64, D])
    G.rw_g1 = I("rw_g1", [D, 128])
    G.rw_g2 = I("rw_g2", [128, D])
    G.rw_k_k = I("rw_k_k", [1, D])
    G.rw_k_a = I("rw_k_a", [1, D])
    G.rw_r_k = I("rw_r_k", [1, D])
    G.rw_lnx_g = I("rw_lnx_g", [1, D])
    G.rw_lnx_b = I("rw_lnx_b", [1, D])
    G.rw_w_o = I("rw_w_o", [D, D])
    G.da_w_qkv = I("da_w_qkv", [D, 3 * D])
    G.da_q_norm_g = I("da_q_norm_g", [1, 64])
    G.da_k_norm_g = I("da_k_norm_g", [1, 64])
    G.da_lam = I("da_lam", [4, 64])
    G.da_subln_g = I("da_subln_g", [1, 128])
    G.da_w_o = I("da_w_o", [D, D])
    G.moe_router = I("moe_router", [2, D, 36])
    G.moe_router_b = I("moe_router_b", [2, 1, 36])
    G.moe_w_gate = I("moe_w_gate", [2, NEXP, D, DFF])
    G.moe_w_up = I("moe_w_up", [2, NEXP, D, DFF])
    G.moe_w_down = I("moe_w_down", [2, NEXP, DFF, D])

    G.ident_f = S.sbuf("ident_f_s", [128, 128], F32)
    G.ident_b = S.sbuf("ident_b_s", [128, 128], BF16)
    G.ones_f = S.sbuf("ones_f_s", [128, 128], F32)
    S.dma("sp", G.ident_f[:], G.ident_f_d[:])
    S.dma("pool", G.ident_b[:], G.ident_f_d[:])
    S.dma("sp", G.ones_f[:], G.ones_f_d[:])
    G.pfm = S.sbuf("pfm", [128, PV_ROWS], F32)
    S.push_scope()
    std_psum(S, G, "c")
    pv = S.sbuf("pv_ld", [128, 2, 128], F32)
    S.memset("dve", pv[:], 0.0)
    S.dma("sp", pv[:, 0, :], G.pvec[0:128, :])
    S.dma("sp", pv[0:PV_ROWS - 128, 1, :], G.pvec[128:PV_ROWS, :])
    S.tr(G.psA[:, 0:128], pv[:, 0, :], G.ident_f[:])
    S.tr(G.psA[:, 128:256], pv[:, 1, :], G.ident_f[:])
    S.copy("dve", G.pfm[:], G.psA[:, 0:PV_ROWS])
    S.pop_scope()
    G.modT = S.sbuf("modT", [128, 48, 4], F32)
    G.A1 = S.sbuf("A1", [128, 3, 8], F32)
    G.A2 = S.sbuf("A2", [128, 3, 8], F32)
    G.gates_d = S.dram("gates_d", [2, 3, D], F32)


def mod_phase(S, G, li):
    S.push_scope()
    std_psum(S, G, f"m{li}")
    crow = S.sbuf(f"crow{li}", [4, D], F32)
    sc = S.sbuf(f"sc{li}", [4, D], F32)
    scT = S.sbuf(f"scT{li}", [128, 8, 4], F32)
    brow = S.sbuf(f"brow{li}", [1, 6 * D], F32)
    grow = S.sbuf(f"grow{li}", [4, 512], F32)
    wblk = [S.sbuf(f"wblk{i}_{li}", [128, 8, 512], F32) for i in range(2)]
    S.memset("dve", crow[:], 0.0)
    S.dma("sp", crow[0:3, :], G.c3[:])
    S.dma("sp", brow[:], G.b_mod[li:li + 1, :])
    S.act(sc[:], crow[:], AF.Silu)
    for kc in range(8):
        S.tr(G.psA[:, kc * 4:(kc + 1) * 4], sc[0:4, kc * 128:(kc + 1) * 128], G.ident_f[0:4, 0:4])
    S.copy("dve", scT[:], G.psA.v(G.psA.t[:, 0:32].rearrange("p (a b) -> p a b", b=4)))
    wsrc = G.w_mod.t[li].rearrange("(kc p) n -> p kc n", p=128)
    gate_blocks = {4: (0, 0), 5: (0, 1), 10: (1, 0), 11: (1, 1)}
    for blk in range(12):
        wb = wblk[blk % 2]
        S.dma("sp", wb[:], G.w_mod.v(wsrc[:, :, blk * 512:(blk + 1) * 512]), split=8)
        if blk in gate_blocks:
            which, half = gate_blocks[blk]
            ps = G.psY[0:4, 0:512]
            for kc in range(8):
                S.mm(ps, scT[:, kc, :], wb[:, kc, :], start=(kc == 0), stop=False)
            S.mm(ps, G.ones_f[0:1, 0:4], brow[0:1, blk * 512:(blk + 1) * 512], start=False, stop=True)
            S.copy("dve", grow[:], ps)
            S.dma("sp", G.gates_d.v(G.gates_d.t[which, :, half * 512:(half + 1) * 512]), grow[0:3, :])
        else:
            for ec in range(4):
                ch = blk * 4 + ec
                ps = G.psA[:, ch * 4:(ch + 1) * 4]
                for kc in range(8):
                    S.mm(ps, wb[:, kc, ec * 128:(ec + 1) * 128], scT[:, kc, :], start=(kc == 0), stop=(kc == 7))
                S.ts("dve", G.modT[:, ch, :], ps, G.pfm[:, PV_BMOD + li * 48 + ch:PV_BMOD + li * 48 + ch + 1], None, op0=ALU.add)
    for r in range(3):
        for (A, sc0, gofs) in ((G.A1, 8, PV_N1[0] + li * 8), (G.A2, 32, PV_N2[0] + li * 8)):
            S.ts("dve", A[:, r, :], G.modT[:, sc0:sc0 + 8, r], 1.0, None, op0=ALU.add)
            S.tt("dve", A[:, r, :], A[:, r, :], G.pfm[:, gofs:gofs + 8], ALU.mult)
    S.pop_scope()


def norm_tile_to_fm(S, G, xt, r, A, shift_ch0, out_fm, wk, fp32_out=None):
    st = wk["st"]
    S.act(wk["junk"][:], xt, AF.Square, accum_out=st[:, 0:1])
    S.ts("dve", st[:, 1:2], st[:, 0:1], 1.0 / D, EPS, op0=ALU.mult, op1=ALU.add)
    S.act(st[:, 2:3], st[:, 1:2], AF.Sqrt)
    S.recip(st[:, 3:4], st[:, 2:3])
    if fp32_out is None:
        xn = wk["xn"]
        S.ts("dve", xn[:], xt, st[:, 3:4], None, op0=ALU.mult)
        for kc in range(8):
            S.tr(G.psT[:, kc * 128:(kc + 1) * 128], xn[:, kc * 128:(kc + 1) * 128], G.ident_b[:])
        src = G.psT.v(G.psT.t[:, :].rearrange("p (a b) -> p a b", b=128))
    else:
        xn = wk["xn32"]
        S.ts("dve", xn[:], xt, st[:, 3:4], None, op0=ALU.mult)
        for kc in range(8):
            S.tr(G.psA[:, kc * 128:(kc + 1) * 128], xn[:, kc * 128:(kc + 1) * 128], G.ident_f[:])
        src = G.psA.v(G.psA.t[:, :].rearrange("p (a b) -> p a b", b=128))
    Abc = A.v(A.t[:, r, :].unsqueeze(2).to_broadcast([128, 8, 128]))
    shbc = G.modT.v(G.modT.t[:, shift_ch0:shift_ch0 + 8, r].unsqueeze(2).to_broadcast([128, 8, 128]))
    tmp = wk["fm32"]
    S.tt("dve", tmp[:], src, Abc, ALU.mult)
    if fp32_out is not None:
        S.tt("pool", fp32_out, tmp[:], shbc, ALU.add)
        S.copy("act", out_fm, fp32_out)
    else:
        S.tt("pool", out_fm, tmp[:], shbc, ALU.add)


def norm_tile_gen(S, G, xt, r, A, shift_ch0, out_fm, wk, fp32_out, psA):
    st = wk["st"]
    S.act(wk["junk"][:], xt, AF.Square, accum_out=st[:, 0:1])
    yield
    S.ts("dve", st[:, 1:2], st[:, 0:1], 1.0 / D, EPS, op0=ALU.mult, op1=ALU.add)
    yield
    S.act(st[:, 2:3], st[:, 1:2], AF.Ln)
    S.act(st[:, 3:4], st[:, 2:3], AF.Exp, scale=-0.5)
    yield
    xn = wk["xn32"]
    S.ts("dve", xn[:], xt, st[:, 3:4], None, op0=ALU.mult)
    yield
    for kc in range(8):
        S.tr(psA[:, kc * 128:(kc + 1) * 128], xn[:, kc * 128:(kc + 1) * 128], G.ident_f[:])
    yield
    src = psA.v(psA.t[:, :].rearrange("p (a b) -> p a b", b=128))
    Abc = A.v(A.t[:, r, :].unsqueeze(2).to_broadcast([128, 8, 128]))
    shbc = G.modT.v(G.modT.t[:, shift_ch0:shift_ch0 + 8, r].unsqueeze(2).to_broadcast([128, 8, 128]))
    tmp = wk["fm32"]
    S.tt("dve", tmp[:], src, Abc, ALU.mult)
    yield
    S.tt("pool", fp32_out, tmp[:], shbc, ALU.add)
    S.copy("act", out_fm, fp32_out)


def wcast_phase(S, G, need):
    items = []
    G.wbf = {}

    def add(name, src3, n):
        dst = S.dram("wbf_" + name, [128, n], BF16)
        G.wbf[name] = dst
        off = 0
        a, b = src3.shape[1], src3.shape[2]
        rows = max(1, 2048 // b)
        if b > 2048:
            for i in range(a):
                for c0 in range(0, b, 2048):
                    c1 = min(b, c0 + 2048)
                    items.append((src3[:, i:i + 1, c0:c1], dst, i * b + c0, c1 - c0, (1, c1 - c0)))
        else:
            for i in range(0, a, rows):
                i1 = min(a, i + rows)
                items.append((src3[:, i:i1, :], dst, i * b, (i1 - i) * b, (i1 - i, b)))

    if "rwkv" in need:
        for j, nm in enumerate(("Wr", "Wk", "Wv")):
            add(nm, G.rw_w_rkv.t[j].rearrange("(kc p) n -> p kc n", p=128), 8 * D)
        add("rwWo", G.rw_w_o.t.rearrange("(kc p) n -> p kc n", p=128), 8 * D)
    if "da" in need:
        add("Wqkv", G.da_w_qkv.t.rearrange("(kc p) n -> p kc n", p=128), 8 * 3 * D)
        add("daWo", G.da_w_o.t.rearrange("(kc p) n -> p kc n", p=128), 8 * D)
    if "moe" in need:
        for li in range(2):
            for e in range(NEXP):
                add(f"g{li}_{e}", G.moe_w_gate.t[li, e].rearrange("(kc p) n -> p kc n", p=128), 8 * DFF)
                add(f"u{li}_{e}", G.moe_w_up.t[li, e].rearrange("(kc p) n -> p kc n", p=128), 8 * DFF)
                add(f"d{li}_{e}", G.moe_w_down.t[li, e].rearrange("(fc p) n -> p fc n", p=128), 2 * D)
    S.push_scope()
    NBUF = 4
    stg = [S.sbuf(f"wc_stg{i}", [128, 2048], F32) for i in range(NBUF)]
    ob = [S.sbuf(f"wc_ob{i}", [128, 2048], BF16) for i in range(NBUF)]
    engs = ["dve", "pool", "dve"]

    def load(i):
        src3, dst, off, n, (a, b) = items[i]
        t = stg[i % NBUF]
        S.dma("sp", t.v(t.t[:, 0:n].rearrange("p (a b) -> p a b", b=b)), V(src3, [("wsrc", None)]))

    for i in range(min(NBUF - 1, len(items))):
        load(i)
    for i in range(len(items)):
        if i + NBUF - 1 < len(items):
            load(i + NBUF - 1)
        src3, dst, off, n, _ = items[i]
        S.copy(engs[i % 3], ob[i % NBUF][:, 0:n], stg[i % NBUF][:, 0:n])
        S.dma("act", dst.v(dst.t[:, off:off + n]), ob[i % NBUF][:, 0:n])
    S.pop_scope()

def rwkv_phase(S, G, x1_d, dbg=None, nb=NB, nt0=NT, do_dir=3, nt1=NT, nheads=16, fl=99):
    li = 0
    H = 16
    yf_d = S.dram("yf_d", [NB, NT, 128, 1040], F32)
    cache_d = S.dram("cache_d", [NB, NT, 128, 6 * D], BF16)
    sg1_d = S.dram("sg1_d", [NB, NT, 128, D], F32)

    def load_bc(name, src, dt=BF16, n=D, q="pool"):
        t = S.sbuf(name, [128, n], dt)
        S.dma(q, t[:], src.v(src.t[0:1, :].partition_broadcast(128)))
        return t

    S.push_scope()
    std_psum(S, G, "r")
    maskA = S.sbuf("maskA_s", [128, 2, 256], BF16)
    maskAT = S.sbuf("maskAT_s", [128, 2, 128], BF16)
    tri = S.sbuf("tri_s", [128, 2, 384], F32)
    ind = S.sbuf("ind_s", [128, 2], F32)
    for z in range(2):
        S.dma("pool", maskA[:, z, :], G.maskA_d.v(G.maskA_d.t[z]))
        S.dma("pool", maskAT[:, z, :], G.maskAT_d.v(G.maskAT_d.t[z]))
        S.dma("sp", tri[:, z, :], G.tri_d.v(G.tri_d.t[z]))
    S.dma("sp", ind[:], G.ind_d[:])
    k_a_bc = load_bc("k_a_bc", G.rw_k_a)
    r_k_bc = load_bc("r_k_bc", G.rw_r_k)
    scr1 = S.sbuf("scr1", [128, D], F32)
    scr2 = S.sbuf("scr2", [128, D], F32)
    sg_sb = S.sbuf("sg_sb", [128, D], F32)
    kdir_sb = S.sbuf("kdir_sb", [128, D], BF16)
    b_sb = S.sbuf("b_sb", [128, D], BF16)
    tm = [S.sbuf(f"tm{i}", [128, D], BF16) for i in range(2)]
    R19 = S.sbuf("R19", [128, H, 128], BF16)
    Bh = S.sbuf("Bh", [128, D], BF16)
    Kh = S.sbuf("Kh", [128, D], BF16)
    arT = S.sbuf("arT", [128, 8, 2, 128], BF16)
    btT = S.sbuf("btT", [128, 8, 128], BF16)
    ktT = S.sbuf("ktT", [128, 8, 128], BF16)
    gC = S.sbuf("gC", [128, 8, 2], F32)
    bon = S.sbuf("bon", [128, 2, 16], F32)
    NSET = 4
    M1 = [S.sbuf(f"M1_{i}", [128, 256], BF16) for i in range(NSET)]
    M2 = [S.sbuf(f"M2_{i}", [128, 256], BF16) for i in range(NSET)]
    MabT = [S.sbuf(f"MabT_{i}", [128, 128], BF16) for i in range(NSET)]
    Pb = [[S.sbuf(f"Pb_{i}_{j}", [128, 128], BF16) for j in range(2)] for i in range(NSET)]
    PTb = [[S.sbuf(f"PTb_{i}_{j}", [128, 128], BF16) for j in range(2)] for i in range(NSET)]
    Tb = [S.sbuf(f"Tb_{i}", [128, 128], BF16) for i in range(NSET)]
    WP = [S.sbuf(f"WP_{i}", [128, 128], BF16) for i in range(NSET)]
    G_all = S.sbuf("G_all", [128, 8, 128], BF16)
    Y0_all = S.sbuf("Y0_all", [128, H, 64], BF16)
    D_all = S.sbuf("D_all", [128, 8, 2, 128], BF16)
    E_all = S.sbuf("E_all", [128, 8, 2, 128], BF16)
    Sb = S.sbuf("Sb", [128, 8, 128], BF16)
    S.memset("pool", D_all[:], 0.0)
    S.memset("pool", E_all[:], 0.0)
    yfw = S.sbuf("yfw", [128, 1040], F32)
    banks = list(G.psH) + list(G.psA_h)
    NBK = len(banks)
    bank_ctr = [0]
    slot_ctr = [0] * NBK

    def slot(n=1):
        bk = bank_ctr[0] % NBK
        bank_ctr[0] += 1
        if n == 2:
            i = ((slot_ctr[bk] + 1) // 2 * 2) % 4
            slot_ctr[bk] = i + 2
        else:
            i = slot_ctr[bk] % 4
            slot_ctr[bk] = i + 1
        return (bk, i)

    def psl(s, p0=0, p1=128, c0=0, c1=128, n=1):
        bk, i = s
        t = banks[bk]
        if n == 2:
            return t.v(t.t[p0:p1, i:i + 2, :].rearrange("p a b -> p (a b)")[:, c0:c1])
        return t.v(t.t[p0:p1, i, c0:c1])

    def transposes_to(src_tm, dst_view):
        for ec in range(8):
            S.tr(G.psT[:, ec * 128:(ec + 1) * 128], src_tm[:, ec * 128:(ec + 1) * 128], G.ident_b[:])
        S.copy("act", dst_view, G.psT.v(G.psT.t[:, :].rearrange("p (a b) -> p a b", b=128)))

    def dir_part(z, r_v, k_v, v_v, kk_v, a_v, chunk_order):
        zsl = slice(z, z + 1)
        S.stt(scr1[:], a_v, -1.0, k_a_bc[:], ALU.add, ALU.mult)
        S.stt(kdir_sb[:], scr1[:], 1.0, k_v, ALU.add, ALU.mult)
        S.tt("pool", b_sb[:], kk_v, a_v, ALU.mult)
        S.tt("pool", scr1[:], r_v, kdir_sb[:], ALU.mult)
        S.tt("pool", scr1[:], scr1[:], r_k_bc[:], ALU.mult)
        S.reduce(bon[:, z, :], scr1.v(scr1.t[:, :].rearrange("p (h n) -> p h n", n=64)))
        def cum(which):
            for n in range(2):
                S.mm(G.psA[:, n * 512:(n + 1) * 512], tri[:, z, which * 128:(which + 1) * 128], sg_sb[:, n * 512:(n + 1) * 512])
        cum(0)
        S.act(scr2[:], G.psA[:], AF.Exp)
        S.tt("dve", tm[0][:], r_v, scr2[:], ALU.mult)
        transposes_to(tm[0], arT.v(arT.t[:, :, 1, :]))
        S.act(scr2[:], G.psA[:], AF.Exp, scale=-1.0)
        S.tt("dve", tm[1][:], b_sb[:], scr2[:], ALU.mult)
        transposes_to(tm[1], btT[:])
        S.tt("dve", tm[0][:], kdir_sb[:], scr2[:], ALU.mult)
        transposes_to(tm[0], ktT[:])
        cum(1)
        S.act(scr2[:], G.psA[:], AF.Exp)
        S.stt(tm[1][:], kk_v, -1.0, scr2[:], ALU.mult, ALU.mult)
        S.copy("pool", V(R19.t[:, :, 64:128], [("R19", h) for h in range(H)]),
               tm[1].v(tm[1].t[:, :].rearrange("p (h n) -> p h n", n=64)))
        transposes_to(tm[1], arT.v(arT.t[:, :, 0, :]))
        cum(2)
        S.act(scr2[:], G.psA[:], AF.Exp)
        S.tt("dve", Bh[:], b_sb[:], scr2[:], ALU.mult)
        S.tt("pool", Kh[:], kdir_sb[:], scr2[:], ALU.mult)
        sg_ = slot()
        for ec in range(8):
            S.mm(psl(sg_, c0=ec * 2, c1=ec * 2 + 2), sg_sb[:, ec * 128:(ec + 1) * 128], ind[:])
        S.act(gC[:], banks[sg_[0]].v(banks[sg_[0]].t[:, sg_[1], 0:16].rearrange("p (a b) -> p a b", b=2)), AF.Exp)

        if do_dir < 2:
            return
        def head_gen(h):
            ec, po = h // 2, (h % 2) * 64
            hc = slice(h * 64, (h + 1) * 64)
            pr = slice(po, po + 64)
            i2 = h % NSET
            bt_h = btT[pr, ec, :]
            kt_h = ktT[pr, ec, :]
            ar_h = arT.v(arT.t[pr, ec, :, :].rearrange("p a b -> p (a b)"))
            at_h = arT[pr, ec, 0, :]
            rt_h = arT[pr, ec, 1, :]
            s1 = slot(2)
            S.mm(psl(s1, n=2, c1=256), bt_h, ar_h)
            S.tt("dve", M1[i2][:], psl(s1, n=2, c1=256), maskA[:, z, :], ALU.mult)
            s3 = slot()
            S.mm(psl(s3), at_h, bt_h)
            S.tt("dve", MabT[i2][:], psl(s3), maskAT[:, z, :], ALU.mult)
            s2 = slot(2)
            S.mm(psl(s2, n=2, c1=256), kt_h, ar_h)
            S.tt("dve", M2[i2][:], psl(s2, n=2, c1=256), maskA[:, z, :], ALU.mult)
            T = Tb[i2]
            S.tt("pool", T[:], M1[i2][:, 0:128], G.ident_b[:], ALU.add)
            P, PT = M1[i2][:, 0:128], MabT[i2][:]
            yield
            for kstep in range(1, 6):
                if kstep < 5:
                    sa = slot()
                    S.mm(psl(sa), PT, P)
                    P2 = Pb[i2][kstep % 2]
                    S.copy("act", P2[:], psl(sa))
                sb_ = slot()
                S.mm(psl(sb_), P, PT)
                P2T = PTb[i2][kstep % 2]
                S.copy("act", P2T[:], psl(sb_))
                if kstep == 1:
                    sx = slot()
                    S.mm(psl(sx, c1=64), M2[i2][:, 0:128], v_v_slice(v_v, hc))
                    S.copy("act", R19.k(h, (slice(None), h, slice(0, 64))), psl(sx, c1=64))
                yield
                sc_ = slot()
                S.mm(psl(sc_), P2T[:], T[:])
                S.tt("dve", T[:], T[:], psl(sc_), ALU.add)
                if kstep < 5:
                    P, PT = P2[:], P2T[:]
            yield
            sw = slot()
            S.mm(psl(sw), T[:], R19.k(h, (slice(None), h, slice(None))))
            S.copy("act", WP[i2][:], psl(sw))
            yield
            sg2 = slot()
            S.mm(psl(sg2, p0=po, p1=po + 64), WP[i2][:, 64:128], M1[i2][:, 128:256])
            S.tt("dve", G_all.k(h, (pr, ec, slice(None))), psl(sg2, p0=po, p1=po + 64), rt_h, ALU.add)
            sy = slot()
            S.mm(psl(sy, c1=64), M1[i2][:, 128:256], WP[i2][:, 0:64], start=True, stop=False)
            S.mm(psl(sy, c1=64), M2[i2][:, 128:256], v_v_slice(v_v, hc), start=False, stop=True)
            S.copy("act", Y0_all.k(h, (slice(None), h, slice(None))), psl(sy, c1=64))
            sds = [slot(), slot()]
            for c in range(2):
                cr = slice(c * 64, (c + 1) * 64)
                S.mm(psl(sds[c], p0=po, p1=po + 64, c1=64), WP[i2][cr, 64:128], Bh[cr, hc])
            for c in range(2):
                S.stt(D_all.k(h, (pr, ec, c, slice(po, po + 64))), G.ident_f[pr, po:po + 64], gC[pr, ec, c:c + 1],
                      psl(sds[c], p0=po, p1=po + 64, c1=64), ALU.mult, ALU.add)
            ses = [slot(), slot()]
            for c in range(2):
                cr = slice(c * 64, (c + 1) * 64)
                S.mm(psl(ses[c], p0=po, p1=po + 64, c1=64), Bh[cr, hc], WP[i2][cr, 0:64], start=True, stop=False)
                S.mm(psl(ses[c], p0=po, p1=po + 64, c1=64), Kh[cr, hc], v_v_slice(v_v, hc, cr), start=False, stop=True)
            for c in range(2):
                S.copy("act", E_all.k(h, (pr, ec, c, slice(po, po + 64))), psl(ses[c], p0=po, p1=po + 64, c1=64))

        pending = list(range(nheads))
        active = []
        rnd, last_admit = 0, -99
        while pending or active:
            if pending and len(active) <= NSET - 2 and (rnd - last_admit >= 4 or not active):
                for _ in range(2):
                    if pending:
                        active.append(head_gen(pending.pop(0)))
                last_admit = rnd
            nxt = []
            for g in active:
                try:
                    next(g)
                    nxt.append(g)
                except StopIteration:
                    pass
            active = nxt
            rnd += 1
        if do_dir < 3:
            return
        for c in chunk_order:
            for ec in range(8):
                pair = [2 * ec, 2 * ec + 1]
                Gv = V(G_all.t[:, ec, c * 64:(c + 1) * 64], [("G_all", h) for h in pair])
                Dv = V(D_all.t[:, ec, c, :], [("D_all", h) for h in pair])
                S.mm(G.psY.v(G.psY.t[c * 64:(c + 1) * 64, ec * 128:(ec + 1) * 128]), Gv, Sb[:, ec, :])
                S.mm(G.psA.v(G.psA.t[:, ec * 128:(ec + 1) * 128]), Dv, Sb[:, ec, :])
            S.tt("dve", Sb[:], G.psA.v(G.psA.t[:, :].rearrange("p (a b) -> p a b", b=128)),
                 V(E_all.t[:, :, c, :], [("E_all", h) for h in range(H)]), ALU.add)

    def v_v_slice(v_v, hc, rows=slice(None)):
        return V(v_v.ap[rows, hc], v_v.toks)

    S.push_scope()
    Wr, Wk, Wv = [S.sbuf(n, [128, 8, D], BF16) for n in ("Wr", "Wk", "Wv")]
    for nm, W in (("Wr", Wr), ("Wk", Wk), ("Wv", Wv)):
        S.dma("sp", W.v(W.t[:, :, :].rearrange("p a b -> p (a b)")), G.wbf[nm][:])
    w1 = S.sbuf("w1", [128, 2, 8, 64], BF16)
    a1 = S.sbuf("a1", [128, 2, 8, 64], BF16)
    g1 = S.sbuf("g1", [128, 8, 128], BF16)
    w2x = S.sbuf("w2x", [65, 2, D], BF16)
    a2x = S.sbuf("a2x", [65, 2, D], BF16)
    g2 = S.sbuf("g2", [128, D], BF16)
    for z in range(2):
        S.dma("pool", w1[:, z, :, :], G.rw_w1.v(G.rw_w1.t[z].rearrange("(kc p) n -> p kc n", p=128)))
        S.dma("pool", a1[:, z, :, :], G.rw_a1.v(G.rw_a1.t[z].rearrange("(kc p) n -> p kc n", p=128)))
        S.dma("pool", w2x[0:64, z, :], G.rw_w2.v(G.rw_w2.t[z]))
        S.dma("pool", w2x[64:65, z, :], G.rw_w0.v(G.rw_w0.t[z:z + 1, :]))
        S.dma("pool", a2x[0:64, z, :], G.rw_a2.v(G.rw_a2.t[z]))
        S.dma("pool", a2x[64:65, z, :], G.rw_a0.v(G.rw_a0.t[z:z + 1, :]))
    S.dma("pool", g1[:], G.rw_g1.v(G.rw_g1.t.rearrange("(kc p) n -> p kc n", p=128)))
    S.dma("pool", g2[:], G.rw_g2[:])
    k_k_bc = load_bc("k_k_bc", G.rw_k_k)
    hTc = S.sbuf("hTc", [128, 8, CTX + 2], BF16)
    hTl = S.sbuf("hTl", [128, 8, LAT + 2], BF16)
    xin = scr2
    wk = {"junk": scr1, "st": S.sbuf("st", [128, 4], F32), "xn": tm[0],
          "fm32": S.sbuf("fm32", [128, 8, 128], F32)}
    dxt = S.sbuf("dxt", [128, 8, 128], F32)
    mix = [S.sbuf(f"mix{i}", [128, 8, 128], BF16) for i in range(2)]
    cach = S.sbuf("cach", [128, 6, D], BF16)
    a0_sb = S.sbuf("a0_sb", [128, D], BF16)
    sg1_v = yfw[:, 0:D]
    hwx = S.sbuf("hwx", [65, 2, 128], BF16)
    hax = S.sbuf("hax", [65, 2, 128], BF16)
    hgs = S.sbuf("hgs", [128, 128], BF16)
    st2 = S.sbuf("st2", [128, 3, 16], F32)
    S.memset("dve", hwx[:], 1.0)
    S.memset("dve", hax[:], 1.0)
    for hT in (hTc, hTl):
        S.memset("pool", hT[:], 0.0)

    mix_ctr = [0]

    def make_mix(hT, c0, j):
        m = mix[mix_ctr[0] % 2]
        mix_ctr[0] += 1
        mu = G.pfm.v(G.pfm.t[:, PV_MU + j * 8:PV_MU + j * 8 + 8].unsqueeze(2).to_broadcast([128, 8, 128]))
        S.tt("pool", wk["fm32"][:], dxt[:], mu, ALU.mult)
        S.tt("pool", m[:], wk["fm32"][:], hT[:, :, c0:c0 + 128], ALU.add)
        return m

    def proj_tm(ps, m, W):
        for n in range(2):
            for kc in range(8):
                S.mm(ps[:, n * 512:(n + 1) * 512], m[:, kc, :], W[:, kc, n * 512:(n + 1) * 512], start=(kc == 0), stop=(kc == 7))

    for b in range(nb):
        for ti in range(NT):
            if ti < 2:
                src, r, hT, t0 = G.ctx.v(G.ctx.t[b, ti * 128:(ti + 1) * 128, :]), 2, hTc, ti * 128
            else:
                src, r, hT, t0 = G.x.v(G.x.t[b, (ti - 2) * 128:(ti - 1) * 128, :]), b, hTl, (ti - 2) * 128
            S.dma("sp", xin[:], src, split=4)
            norm_tile_to_fm(S, G, xin[:], r, G.A1, 0, hT[:, :, t0 + 1:t0 + 129], wk)
        if dbg is not None and "hT" in dbg and b == 0:
            S.dma("sp", dbg["hT"][:], hTl[:])
        S.memset("dve", Sb[:], 0.0)
        for ti in range(nt0):
            hT, t0 = (hTc, ti * 128) if ti < 2 else (hTl, (ti - 2) * 128)
            c0 = t0 + 1
            S.tt("dve", dxt[:], hT[:, :, c0 - 1:c0 + 127], hT[:, :, c0 + 1:c0 + 129], ALU.add)
            S.stt(dxt[:], dxt[:], 0.5, hT[:, :, c0:c0 + 128], ALU.mult, ALU.subtract)
            if fl < 1:
                continue
            m = make_mix(hT, c0, 0)
            proj_tm(G.psA, m, Wr)
            S.copy("act", cach[:, 0, :], G.psA[:])
            if fl < 2:
                continue
            m = make_mix(hT, c0, 2)
            proj_tm(G.psY, m, Wv)
            S.copy("act", cach[:, 2, :], G.psY[:])
            if fl < 3:
                continue
            m = make_mix(hT, c0, 4)
            for z in range(2):
                sl_ = slot()
                for kc in range(8):
                    S.mm(psl(sl_, p1=64), a1[:, z, kc, :], m[:, kc, :], start=(kc == 0), stop=(kc == 7))
                S.copy("act", hax[0:64, z, :], psl(sl_, p1=64))
            for z in range(2):
                ps = G.psA if z == 0 else G.psY
                for n in range(2):
                    S.mm(ps[:, n * 512:(n + 1) * 512], hax[:, z, :], a2x[:, z, n * 512:(n + 1) * 512])
                S.act(a0_sb[:] if z == 0 else cach[:, 4, :], ps[:], AF.Sigmoid)
            if fl < 4:
                continue
            m = make_mix(hT, c0, 3)
            for z in range(2):
                sl_ = slot()
                for kc in range(8):
                    S.mm(psl(sl_, p1=64), w1[:, z, kc, :], m[:, kc, :], start=(kc == 0), stop=(kc == 7))
                S.act(hwx[0:64, z, :], psl(sl_, p1=64), AF.Tanh)
            for z in range(2):
                ps = G.psA if z == 0 else G.psY
                for n in range(2):
                    S.mm(ps[:, n * 512:(n + 1) * 512], hwx[:, z, :], w2x[:, z, n * 512:(n + 1) * 512])
                S.act(sg_sb[:] if z == 0 else sg1_v, ps[:], AF.Sigmoid)
            if fl < 5:
                continue
            m = make_mix(hT, c0, 5)
            sl_ = slot()
            for kc in range(8):
                S.mm(psl(sl_), g1[:, kc, :], m[:, kc, :], start=(kc == 0), stop=(kc == 7))
            S.act(hgs[:], psl(sl_), AF.Sigmoid)
            for n in range(2):
                S.mm(G.psY[:, n * 512:(n + 1) * 512], hgs[:], g2[:, n * 512:(n + 1) * 512])
            S.copy("act", cach[:, 5, :], G.psY[:])
            if fl < 6:
                continue
            m = make_mix(hT, c0, 1)
            proj_tm(G.psA, m, Wk)
            S.copy("act", cach[:, 1, :], G.psA[:])
            if fl < 6.1:
                continue
            S.tt("dve", scr1[:], G.psA[:], k_k_bc[:], ALU.mult)
            if fl < 6.2:
                continue
            S.act(scr2[:], scr1[:], AF.Square)
            S.reduce(st2[:, 0, :], scr2.v(scr2.t[:, :].rearrange("p (h n) -> p h n", n=64)))
            if fl < 6.3:
                continue
            S.ts("dve", st2[:, 1, :], st2[:, 0, :], 1e-12, None, op0=ALU.add)
            S.act(st2[:, 1, :], st2[:, 1, :], AF.Sqrt)
            S.recip(st2[:, 2, :], st2[:, 1, :])
            if fl < 6.4:
                continue
            S.tt("dve", cach.v(cach.t[:, 3, :].rearrange("p (h n) -> p h n", n=64)),
                 scr1.v(scr1.t[:, :].rearrange("p (h n) -> p h n", n=64)),
                 st2.v(st2.t[:, 2, :].unsqueeze(2).to_broadcast([128, 16, 64])), ALU.mult)
            if fl < 7:
                continue
            S.dma("sp", cache_d.v(cache_d.t[b, ti].rearrange("p (a n) -> p a n", n=D)), cach[:])
            S.dma("sp", sg1_d.v(sg1_d.t[b, ti]), sg1_v)
            if do_dir:
                dir_part(0, cach[:, 0, :], G.psA[:], cach[:, 2, :], cach[:, 3, :], a0_sb[:], (0, 1))
            S.tt("dve", yfw[:, 0:D], G.psY[:],
                 V(Y0_all.t[:, :, :].rearrange("p h n -> p (h n)"), [("Y0_all", h) for h in range(H)]), ALU.add)
            S.copy("pool", yfw[:, D:D + 16], bon[:, 0, :])
            S.dma("sp", yf_d.v(yf_d.t[b, ti]), yfw[:])
    S.pop_scope()

    S.push_scope()
    Wo = S.sbuf("Wo", [128, 8, D], BF16)
    S.dma("sp", Wo.v(Wo.t[:, :, :].rearrange("p a b -> p (a b)")), G.wbf["rwWo"][:])
    lnx_g_bc = load_bc("lnx_g_bc", G.rw_lnx_g)
    lnx_b_bc = load_bc("lnx_b_bc", G.rw_lnx_b)
    gate_bc = S.sbuf("gate_bc", [128, D], F32)
    cach = S.sbuf("cach1", [128, 6, D], BF16)
    xres = S.sbuf("xres", [128, D], F32)
    pre = S.sbuf("pre", [128, D], BF16)
    preT = S.sbuf("preT", [128, 8, 128], BF16)
    st3 = S.sbuf("st3", [128, 4, 16], F32)
    for b in range(nb):
        S.memset("dve", Sb[:], 0.0)
        order = ([1, 0] + list(range(NT - 1, 1, -1)))[:nt1]
        cur_r = None
        for ti in order:
            r = 2 if ti < 2 else b
            if r != cur_r:
                S.dma("sp", gate_bc[:], G.gates_d.v(G.gates_d.t[0, r:r + 1, :].partition_broadcast(128)))
                cur_r = r
            S.dma("sp", cach[:], cache_d.v(cache_d.t[b, ti].rearrange("p (a n) -> p a n", n=D)))
            S.dma("sp", sg_sb[:], sg1_d.v(sg1_d.t[b, ti]))
            S.dma("sp", yfw[:], yf_d.v(yf_d.t[b, ti]))
            dir_part(1, cach[:, 0, :], cach[:, 1, :], cach[:, 2, :], cach[:, 3, :], cach[:, 4, :], (1, 0))
            S.tt("dve", scr1[:], G.psY[:], V(Y0_all.t[:, :, :].rearrange("p h n -> p (h n)"), [("Y0_all", h) for h in range(H)]), ALU.add)
            S.tt("pool", scr1[:], scr1[:], yfw[:, 0:D], ALU.add)
            y3 = scr1.v(scr1.t[:, :].rearrange("p (h n) -> p h n", n=64))
            S.reduce(st3[:, 0, :], y3)
            S.ts("dve", st3[:, 0, :], st3[:, 0, :], 1.0 / 64, None, op0=ALU.mult)
            S.tt("dve", y3, y3, st3.v(st3.t[:, 0, :].unsqueeze(2).to_broadcast([128, 16, 64])), ALU.subtract)
            S.act(scr2[:], scr1[:], AF.Square)
            S.reduce(st3[:, 1, :], scr2.v(scr2.t[:, :].rearrange("p (h n) -> p h n", n=64)))
            S.ts("dve", st3[:, 1, :], st3[:, 1, :], 1.0 / 64, LNX_EPS, op0=ALU.mult, op1=ALU.add)
            S.act(st3[:, 1, :], st3[:, 1, :], AF.Sqrt)
            S.recip(st3[:, 2, :], st3[:, 1, :])
            S.tt("dve", y3, y3, st3.v(st3.t[:, 2, :].unsqueeze(2).to_broadcast([128, 16, 64])), ALU.mult)
            S.tt("pool", scr1[:], scr1[:], lnx_g_bc[:], ALU.mult)
            S.tt("pool", scr1[:], scr1[:], lnx_b_bc[:], ALU.add)
            S.tt("dve", st3[:, 3, :], bon[:, 1, :], yfw[:, D:D + 16], ALU.add)
            S.tt("dve", scr2.v(scr2.t[:, :].rearrange("p (h n) -> p h n", n=64)),
                 cach.v(cach.t[:, 2, :].rearrange("p (h n) -> p h n", n=64)),
                 st3.v(st3.t[:, 3, :].unsqueeze(2).to_broadcast([128, 16, 64])), ALU.mult)
            S.tt("pool", scr1[:], scr1[:], scr2[:], ALU.add)
            S.tt("pool", pre[:], scr1[:], cach[:, 5, :], ALU.mult)
            transposes_to(pre, preT[:])
            for n in range(2):
                for kc in range(8):
                    S.mm(G.psA[:, n * 512:(n + 1) * 512], preT[:, kc, :], Wo[:, kc, n * 512:(n + 1) * 512], start=(kc == 0), stop=(kc == 7))
            if ti < 2:
                xsrc = G.ctx.v(G.ctx.t[b, ti * 128:(ti + 1) * 128, :])
            else:
                xsrc = G.x.v(G.x.t[b, (ti - 2) * 128:(ti - 1) * 128, :])
            S.dma("sp", xres[:], xsrc)
            S.tt("dve", scr2[:], G.psA[:], gate_bc[:], ALU.mult)
            S.tt("pool", xres[:], xres[:], scr2[:], ALU.add)
            S.dma("sp", x1_d.v(x1_d.t[b, ti * 128:(ti + 1) * 128, :]), xres[:])
    S.pop_scope()
    S.pop_scope()

def moe_phase(S, G, li, xin_d, tiles, xout_fn, st_tiles, npairs=16, dbg=None):
    L = f"e{li}"
    S.push_scope()
    ysub = [S.psum(f"ysub{i}{L}", [128, 1024], F32) for i in range(2)]
    psG = [S.psum(f"psG{i}{L}", [128, 512], F32) for i in range(2)]
    psU = [S.psum(f"psU{i}{L}", [128, 512], F32) for i in range(2)]
    G.psA = ysub[0]
    STK = st_tiles * 128
    h2T = S.sbuf(f"h2T{L}", [128, 8, STK], BF16)
    y_acc = S.sbuf(f"yacc{L}", [128, st_tiles, D], F32)
    gatesT = S.sbuf(f"gatesT{L}", [32, STK], BF16)
    sel = S.sbuf(f"sel{L}", [32, 32, 128], BF16)
    S.dma("pool", sel[:], G.sel_d.v(G.sel_d.t[:, :].rearrange("p (a b) -> p a b", b=128)))
    Wrt = S.sbuf(f"Wrt{L}", [128, 8, 36], F32)
    S.dma("sp", Wrt[:], G.moe_router.v(G.moe_router.t[li].rearrange("(kc p) n -> p kc n", p=128)))
    rb = S.sbuf(f"rb{L}", [1, 36], F32)
    S.dma("sp", rb[:], G.moe_router_b.v(G.moe_router_b.t[li]))
    gate_bc = S.sbuf(f"gbc{L}", [128, 3, D], F32)
    rs_used = sorted(set(t[2] for t in tiles))
    for r in rs_used:
        S.dma("sp", gate_bc[:, r, :], G.gates_d.v(G.gates_d.t[1, r:r + 1, :].partition_broadcast(128)))
    Wg = [[S.sbuf(f"Wg{i}{e}{L}", [128, 8, DFF], BF16) for e in range(2)] for i in range(2)]
    Wu = [[S.sbuf(f"Wu{i}{e}{L}", [128, 8, DFF], BF16) for e in range(2)] for i in range(2)]
    Wd = [[S.sbuf(f"Wd{i}{e}{L}", [128, 2, D], BF16) for e in range(2)] for i in range(2)]
    NS1 = 2
    xin_s = [S.sbuf(f"xin{i}{L}", [128, D], F32) for i in range(NS1)]
    junk_s = [S.sbuf(f"junk{i}{L}", [128, D], F32) for i in range(NS1)]
    wk_s = [{"junk": junk_s[i], "st": S.sbuf(f"st{i}{L}", [128, 4], F32), "xn32": S.sbuf(f"xn32{i}{L}", [128, D], F32),
             "fm32": S.sbuf(f"fm32{i}{L}", [128, 8, 128], F32)} for i in range(NS1)]
    h32_s = [S.sbuf(f"h32{i}{L}", [128, 8, 128], F32) for i in range(NS1)]
    lg_s = [S.sbuf(f"lg{i}{L}", [128, 36], F32) for i in range(NS1)]
    sm_s = [S.sbuf(f"sm{i}{L}", [128, 64], F32) for i in range(NS1)]
    g32_s = [S.sbuf(f"g32{i}{L}", [128, 32], F32) for i in range(NS1)]
    xin3 = S.sbuf(f"xin3{L}", [128, D], F32)
    out3 = S.sbuf(f"out3{L}", [128, D], F32)
    s_sb = [S.sbuf(f"s_sb{i}{L}", [128, 256], F32) for i in range(2)]
    t_sb = [S.sbuf(f"t_sb{i}{L}", [128, 256], F32) for i in range(2)]
    hidT = [S.sbuf(f"hidT{i}{L}", [128, 256], BF16) for i in range(2)]

    def load_pair(p, buf):
        for e in range(2):
            eg = p * 2 + e
            for W, nm in ((Wg, "g"), (Wu, "u"), (Wd, "d")):
                t = W[buf][e]
                S.dma("sp", t.v(t.t[:, :, :].rearrange("p a b -> p (a b)")), G.wbf[f"{nm}{li}_{eg}"][:])

    n_super = len(tiles) // st_tiles
    assert n_super * st_tiles == len(tiles) and st_tiles % 2 == 0
    for su in range(n_super):
        stl = tiles[su * st_tiles:(su + 1) * st_tiles]
        load_pair(0, 0)
        def step1_gen(j, b, row0, r, s):
            xin, wk, h32, lg, sm, g32 = xin_s[s], wk_s[s], h32_s[s], lg_s[s], sm_s[s], g32_s[s]
            S.dma("sp", xin[:], xin_d.v(xin_d.t[b, row0:row0 + 128, :]), split=4)
            yield
            yield from norm_tile_gen(S, G, xin[:], r, G.A2, 24, h2T[:, :, j * 128:(j + 1) * 128], wk, h32[:], ysub[s])
            yield
            psr = (psG[0] if s == 0 else psU[0])[:, 0:36]
            for kc in range(8):
                S.mm(psr, h32[:, kc, :], Wrt[:, kc, :], start=(kc == 0), stop=False)
            S.mm(psr, G.ones_f[0:1, :], rb[:], start=False, stop=True)
            S.copy("dve", lg[:], psr)
            yield
            c = lambda i, n=1: sm[:, i:i + n]
            S.reduce(c(0), lg[:, 0:4], op=ALU.max)
            yield
            S.ts("dve", c(1), c(0), -1.0, None, op0=ALU.mult)
            yield
            S.ts("dve", c(4, 4), lg[:, 0:4], c(0), None, op0=ALU.is_ge)
            yield
            S.act(c(8, 4), lg[:, 0:4], AF.Exp, bias=c(1), accum_out=c(2))
            yield
            S.recip(c(3), c(2))
            yield
            S.ts("dve", c(16, 8), lg[:, 4:12], c(4), None, op0=ALU.mult)
            yield
            for g in range(1, 4):
                S.stt(c(16, 8), lg[:, 4 + 8 * g:12 + 8 * g], c(4 + g), c(16, 8), ALU.mult, ALU.add)
                yield
            S.reduce(c(12), c(16, 8), op=ALU.max)
            yield
            S.ts("dve", c(24, 8), c(16, 8), c(12), None, op0=ALU.is_ge)
            yield
            S.stt(c(32, 8), c(24, 8), -1e30, c(16, 8), ALU.mult, ALU.add)
            yield
            S.reduce(c(13), c(32, 8), op=ALU.max)
            yield
            S.ts("dve", c(40, 8), c(32, 8), c(13), None, op0=ALU.is_ge)
            yield
            S.tt("dve", c(14), c(13), c(12), ALU.subtract)
            yield
            S.act(c(15), c(14), AF.Exp)
            yield
            S.ts("dve", c(48), c(15), 1.0, None, op0=ALU.add)
            yield
            S.recip(c(49), c(48))
            yield
            S.tt("dve", c(50), c(49), c(3), ALU.mult)
            yield
            S.tt("dve", c(51), c(50), c(15), ALU.mult)
            yield
            S.ts("dve", c(52, 8), c(24, 8), c(50), None, op0=ALU.mult)
            yield
            S.stt(c(52, 8), c(40, 8), c(51), c(52, 8), ALU.mult, ALU.add)
            yield
            for g in range(4):
                S.ts("dve", g32[:, g * 8:(g + 1) * 8], c(52, 8), c(4 + g), None, op0=ALU.mult)
                yield
            pst = (psG[1] if s == 0 else psU[1])[0:32, 0:128]
            S.tr(pst, g32[:], G.ident_f[:])
            S.copy("dve", gatesT[:, j * 128:(j + 1) * 128], pst)
            if dbg is not None and "gates" in dbg and su == 0:
                S.dma("sp", dbg["gates"].v(dbg["gates"].t[j]), g32[:])

        pend1 = [step1_gen(j, b, row0, r, j % NS1) for j, (b, row0, r) in enumerate(stl)]
        act1 = []
        rnd, last1 = 0, -99
        while pend1 or act1:
            if pend1 and len(act1) < NS1 and (rnd - last1 >= 20 or not act1):
                act1.append(pend1.pop(0))
                last1 = rnd
            nxt = []
            for g_ in act1:
                try:
                    next(g_)
                    nxt.append(g_)
                except StopIteration:
                    pass
            act1 = nxt
            rnd += 1
        G.psA = ysub[0]
        items = [(p, t2, e, fc) for p in range(npairs) for t2 in range(st_tiles // 2) for e in range(2) for fc in range(2)]

        def emit_gu(idx):
            p, t2, e, fc = items[idx]
            buf, eg, ib = p % 2, p * 2 + e, idx % 2
            tok = slice(t2 * 256, (t2 + 1) * 256)
            pg, pu = psG[ib], psU[ib]
            for kc in range(8):
                S.mm(pg[:, 0:256], Wg[buf][e][:, kc, fc * 128:(fc + 1) * 128], h2T[:, kc, tok], start=(kc == 0), stop=(kc == 7))
            for kc in range(8):
                S.mm(pu[:, 0:256], Wu[buf][e][:, kc, fc * 128:(fc + 1) * 128], h2T[:, kc, tok], start=(kc == 0), stop=(kc == 7))
            S.mm(pu[:, 256:512], sel[:, eg, :], gatesT[:, tok])
            S.act(s_sb[ib][:], pg[:, 0:256], AF.Silu)
            S.tt("dve", t_sb[ib][:], s_sb[ib][:], pu[:, 0:256], ALU.mult)
            S.tt("dve", hidT[ib][:], t_sb[ib][:], pu[:, 256:512], ALU.mult)

        def emit_down(idx):
            p, t2, e, fc = items[idx]
            buf, ib, it = p % 2, idx % 2, e * 2 + fc
            for ts_ in range(2):
                for n in range(2):
                    S.mm(ysub[ts_][:, n * 512:(n + 1) * 512], hidT[ib][:, ts_ * 128:(ts_ + 1) * 128],
                         Wd[buf][e][:, fc, n * 512:(n + 1) * 512], start=(it == 0), stop=(it == 3))
            if it == 3:
                for ts_ in range(2):
                    j = t2 * 2 + ts_
                    if p == 0:
                        S.copy("dve", y_acc[:, j, :], ysub[ts_][:])
                    else:
                        S.tt("dve", y_acc[:, j, :], y_acc[:, j, :], ysub[ts_][:], ALU.add)
                    if p == npairs - 1:
                        b, row0, r = stl[j]
                        S.dma("sp", xin3[:], xin_d.v(xin_d.t[b, row0:row0 + 128, :]))
                        S.tt("pool", out3[:], y_acc[:, j, :], gate_bc[:, r, :], ALU.mult)
                        S.tt("pool", out3[:], out3[:], xin3[:], ALU.add)
                        S.dma("sp", xout_fn(b, row0), out3[:])

        for idx in range(len(items)):
            emit_gu(idx)
            if idx > 0:
                emit_down(idx - 1)
            p, t2, e, fc = items[idx]
            if t2 == 0 and e == 0 and fc == 0 and p + 1 < npairs:
                load_pair(p + 1, (p + 1) % 2)
        emit_down(len(items) - 1)
    S.pop_scope()

LAM_INIT1 = 0.8 - 0.6 * float(np.exp(-0.3 * 1))


def attn_phase(S, G, x2_d, x3_d, nb=NB, nqt=4, nh=8):
    li = 1
    NKT = NT
    S.push_scope()
    psA = S.psum("psA_a", [128, 1024], F32)
    psT = S.psum("psT_a", [128, 1024], BF16)
    psQ = [S.psum(f"psQ{i}_a", [128, 512], F32) for i in range(3)]
    psO = [S.psum(f"psO{i}_a", [128, 512], F32) for i in range(2)]
    G.psA, G.psT = psA, psT
    KT_all = S.sbuf("KT_all", [128, 8, TOK], BF16)
    QT_all = S.sbuf("QT_all", [128, 8, LAT], BF16)
    V_all = S.sbuf("V_all", [128, NKT, 8, 130], BF16)
    S.memset("pool", V_all[:], 1.0)
    gq_bc = S.sbuf("gq_bc", [128, 64], F32)
    gk_bc = S.sbuf("gk_bc", [128, 64], F32)
    sg_bc = S.sbuf("sg_bc", [128, 128], F32)
    S.dma("sp", gq_bc[:], G.da_q_norm_g.v(G.da_q_norm_g.t[0:1, :].partition_broadcast(128)))
    S.dma("sp", gk_bc[:], G.da_k_norm_g.v(G.da_k_norm_g.t[0:1, :].partition_broadcast(128)))
    S.dma("sp", sg_bc[:], G.da_subln_g.v(G.da_subln_g.t[0:1, :].partition_broadcast(128)))
    S.ts("dve", sg_bc[:], sg_bc[:], 1.0 - LAM_INIT1, None, op0=ALU.mult)
    lamv = S.sbuf("lamv", [128, 4, 64], F32)
    lsm = S.sbuf("lsm", [128, 8], F32)
    for i in range(4):
        S.dma("sp", lamv[:, i, :], G.da_lam.v(G.da_lam.t[i:i + 1, :].partition_broadcast(128)))
    S.tt("dve", lamv[:, 0, :], lamv[:, 0, :], lamv[:, 1, :], ALU.mult)
    S.tt("dve", lamv[:, 2, :], lamv[:, 2, :], lamv[:, 3, :], ALU.mult)
    S.reduce(lsm[:, 0:1], lamv[:, 0, :])
    S.reduce(lsm[:, 1:2], lamv[:, 2, :])
    S.act(lsm[:, 2:4], lsm[:, 0:2], AF.Exp)
    S.tt("dve", lsm[:, 4:5], lsm[:, 3:4], lsm[:, 2:3], ALU.subtract)
    S.ts("dve", lsm[:, 5:6], lsm[:, 4:5], -LAM_INIT1, None, op0=ALU.add)
    neglam = lsm[:, 5:6]
    junk = S.sbuf("junk_a", [128, D], F32)
    scrq = S.sbuf("scrq", [128, D], F32)
    xin = S.sbuf("xin_a", [128, D], F32)
    st = S.sbuf("st_a", [128, 4], F32)
    st16 = S.sbuf("st16_a", [128, 3, 16], F32)
    for b in range(nb):
        S.push_scope()
        Wqkv = S.sbuf(f"Wqkv{b}", [128, 8, 3 * D], BF16)
        S.dma("sp", Wqkv.v(Wqkv.t[:, :, :].rearrange("p a b -> p (a b)")), G.wbf["Wqkv"][:])
        hT_t = S.sbuf(f"hT_t{b}", [128, 8, 128], BF16)
        outq = S.sbuf(f"outq{b}", [128, D], BF16)
        wk = {"junk": junk, "st": st, "xn": S.sbuf(f"xn_a{b}", [128, D], BF16), "fm32": S.sbuf(f"fm32_a{b}", [128, 8, 128], F32)}
        cs_t = S.sbuf(f"cs_t{b}", [128, 2, 32], F32)
        tmpa = S.sbuf(f"tmpa{b}", [128, 512], F32)
        tmpb = S.sbuf(f"tmpb{b}", [128, 512], F32)

        qs_ = [dict(scrq=scrq, outq=outq, tmpa=tmpa, tmpb=tmpb, st16=st16, junk=junk)]
        qs_.append(dict(scrq=S.sbuf(f"scrq2_{b}", [128, D], F32), outq=S.sbuf(f"outq2_{b}", [128, D], BF16),
                        tmpa=S.sbuf(f"tmpa2_{b}", [128, 512], F32), tmpb=S.sbuf(f"tmpb2_{b}", [128, 512], F32),
                        st16=S.sbuf(f"st16_2_{b}", [128, 3, 16], F32), junk=S.sbuf(f"junk2_{b}", [128, D], F32)))
        wk["junk"] = S.sbuf(f"junkn_{b}", [128, D], F32)

        def qk_part1(X):
            s16, sq, jk = X["st16"], X["scrq"], X["junk"]
            S.act(jk[:], psA[:], AF.Square)
            S.reduce(s16[:, 0, :], jk.v(jk.t[:, :].rearrange("p (g n) -> p g n", n=64)))
            S.ts("dve", s16[:, 1, :], s16[:, 0, :], 1.0 / 64, EPS, op0=ALU.mult, op1=ALU.add)
            S.act(s16[:, 1, :], s16[:, 1, :], AF.Sqrt)
            S.recip(s16[:, 2, :], s16[:, 1, :])
            S.tt("dve", sq.v(sq.t[:, :].rearrange("p (g n) -> p g n", n=64)),
                 psA.v(psA.t[:, :].rearrange("p (g n) -> p g n", n=64)),
                 s16.v(s16.t[:, 2, :].unsqueeze(2).to_broadcast([128, 16, 64])), ALU.mult)

        def qk_part2a(X, gain_bc, rope):
            sq, oq, tmpa_, tmpb_ = X["scrq"], X["outq"], X["tmpa"], X["tmpb"]
            gv = gain_bc.v(gain_bc.t[:, :].unsqueeze(1).to_broadcast([128, 16, 64]))
            if not rope:
                S.tt("pool", oq.v(oq.t[:, :].rearrange("p (g n) -> p g n", n=64)),
                     sq.v(sq.t[:, :].rearrange("p (g n) -> p g n", n=64)), gv, ALU.mult)
                return
            S.tt("pool", sq.v(sq.t[:, :].rearrange("p (g n) -> p g n", n=64)),
                 sq.v(sq.t[:, :].rearrange("p (g n) -> p g n", n=64)), gv, ALU.mult)
            x5 = sq.t[:, :].rearrange("p (g a h f) -> p g a h f", g=16, a=2, h=2)
            o5 = oq.t[:, :].rearrange("p (g a h f) -> p g a h f", g=16, a=2, h=2)
            x1, x2 = sq.v(x5[:, :, :, 0, :]), sq.v(x5[:, :, :, 1, :])
            o1, o2 = oq.v(o5[:, :, :, 0, :]), oq.v(o5[:, :, :, 1, :])
            cv = cs_t.v(cs_t.t[:, 0, :].rearrange("p (a f) -> p a f", a=2).unsqueeze(1).to_broadcast([128, 16, 2, 16]))
            sv = cs_t.v(cs_t.t[:, 1, :].rearrange("p (a f) -> p a f", a=2).unsqueeze(1).to_broadcast([128, 16, 2, 16]))
            ta = tmpa_.v(tmpa_.t[:, :].rearrange("p (g a f) -> p g a f", g=16, a=2))
            tb = tmpb_.v(tmpb_.t[:, :].rearrange("p (g a f) -> p g a f", g=16, a=2))
            S.tt("dve", ta, x1, cv, ALU.mult)
            S.tt("pool", tb, x2, sv, ALU.mult)
            S.tt("dve", o1, ta, tb, ALU.subtract)
            S.tt("dve", ta, x1, sv, ALU.mult)
            S.tt("pool", tb, x2, cv, ALU.mult)
            S.tt("pool", o2, ta, tb, ALU.add)

        def qk_part2b(X, dst_fm):
            oq = X["outq"]
            for ec in range(8):
                S.tr(psT[:, ec * 128:(ec + 1) * 128], oq[:, ec * 128:(ec + 1) * 128], G.ident_b[:])
            S.copy("act", dst_fm, psT.v(psT.t[:, :].rearrange("p (a b) -> p a b", b=128)))

        def proj(c0):
            for n in range(2):
                for kc in range(8):
                    S.mm(psA[:, n * 512:(n + 1) * 512], hT_t[:, kc, :], Wqkv[:, kc, c0 + n * 512:c0 + (n + 1) * 512], start=(kc == 0), stop=(kc == 7))

        def load_norm(ti):
            r = 2 if ti < 2 else b
            S.dma("sp", xin[:], x2_d.v(x2_d.t[b, ti * 128:(ti + 1) * 128, :]), split=4)
            norm_tile_to_fm(S, G, xin[:], r, G.A1, 0, hT_t[:], wk)

        load_norm(0)
        pend_q = None
        for ti in range(NT):
            lat = ti >= 2
            if lat:
                t0 = (ti - 2) * 128
                S.dma("sp", cs_t[:, 0, :], G.rope_cos_d.v(G.rope_cos_d.t[t0:t0 + 128, :]))
                S.dma("sp", cs_t[:, 1, :], G.rope_sin_d.v(G.rope_sin_d.t[t0:t0 + 128, :]))
            proj(D)
            qk_part1(qs_[0])
            if pend_q is not None:
                qk_part2b(qs_[1], pend_q)
                pend_q = None
            proj(2 * D)
            S.copy("act", V_all[:, ti, :, 0:128], psA.v(psA.t[:, :].rearrange("p (h n) -> p h n", n=128)))
            if lat:
                proj(0)
            qk_part2a(qs_[0], gk_bc, lat)
            qk_part2b(qs_[0], KT_all[:, :, ti * 128:(ti + 1) * 128])
            if lat:
                qk_part1(qs_[1])
            if ti + 1 < NT:
                load_norm(ti + 1)
            if lat:
                qk_part2a(qs_[1], gq_bc, True)
                pend_q = QT_all[:, :, t0:t0 + 128]
        if pend_q is not None:
            qk_part2b(qs_[1], pend_q)
        S.pop_scope()
        S.push_scope()
        Wo = S.sbuf(f"Wo_a{b}", [128, 8, D], BF16)
        S.dma("sp", Wo.v(Wo.t[:, :, :].rearrange("p a b -> p (a b)")), G.wbf["daWo"][:])
        gate_bc = S.sbuf(f"gate_a{b}", [128, D], F32)
        S.dma("sp", gate_bc[:], G.gates_d.v(G.gates_d.t[0, b:b + 1, :].partition_broadcast(128)))
        O_all = S.sbuf(f"O_all{b}", [128, 4, D], F32)
        pT = [S.sbuf(f"pT{i}_{b}", [128, 512], BF16) for i in range(3)]
        rec = S.sbuf(f"rec{b}", [128, 8], F32)
        pre = S.sbuf(f"pre_a{b}", [128, D], BF16)
        preT = S.sbuf(f"preT_a{b}", [128, 8, 128], BF16)
        st8 = S.sbuf(f"st8_{b}", [128, 3, 8], F32)
        aitems = [(qt, h, m, kt) for qt in range(nqt) for h in range(nh) for m in range(2) for kt in range(NKT)]

        def emit_qk(i):
            qt, h, m, kt = aitems[i]
            pr = slice(m * 64, m * 64 + 64)
            ps = psQ[i % 3]
            S.mm(ps[:], KT_all[pr, h, kt * 128:(kt + 1) * 128], QT_all[pr, h, qt * 512:(qt + 1) * 512])
            S.act(pT[i % 3][:], ps[:], AF.Exp, scale=0.125)

        def emit_pv(i):
            qt, h, m, kt = aitems[i]
            for qs in range(4):
                acc = psO[qs // 2][:, (qs % 2) * 256:(qs % 2) * 256 + 129]
                S.mm(acc, pT[i % 3][:, qs * 128:(qs + 1) * 128], V_all[:, kt, h, 0:129],
                     start=(kt == 0 and qs % 2 == 0), stop=(kt == NKT - 1), skip_group_check=True)
            if kt != NKT - 1:
                return
            for qs in range(4):
                c0 = (qs % 2) * 256
                S.recip(rec[:, qs:qs + 1], psO[qs // 2][:, c0 + 128:c0 + 129])
                if m == 0:
                    S.ts("dve", O_all[:, qs, h * 128:(h + 1) * 128], psO[qs // 2][:, c0:c0 + 128], rec[:, qs:qs + 1], None, op0=ALU.mult)
                else:
                    S.tt("dve", rec[:, 4 + qs:5 + qs], rec[:, qs:qs + 1], neglam, ALU.mult)
                    S.stt(O_all[:, qs, h * 128:(h + 1) * 128], psO[qs // 2][:, c0:c0 + 128], rec[:, 4 + qs:5 + qs],
                          O_all[:, qs, h * 128:(h + 1) * 128], ALU.mult, ALU.add)
            if not (h == nh - 1 and m == 1):
                return
            for qs in range(4):
                O3 = O_all.v(O_all.t[:, qs, :].rearrange("p (h n) -> p h n", n=128))
                S.act(junk[:], O_all[:, qs, :], AF.Square)
                S.reduce(st8[:, 0, :], junk.v(junk.t[:, :].rearrange("p (h n) -> p h n", n=128)))
                S.ts("dve", st8[:, 1, :], st8[:, 0, :], 1.0 / 128, EPS, op0=ALU.mult, op1=ALU.add)
                S.act(st8[:, 1, :], st8[:, 1, :], AF.Sqrt)
                S.recip(st8[:, 2, :], st8[:, 1, :])
                S.tt("dve", O3, O3, st8.v(st8.t[:, 2, :].unsqueeze(2).to_broadcast([128, 8, 128])), ALU.mult)
                S.tt("pool", pre.v(pre.t[:, :].rearrange("p (h n) -> p h n", n=128)), O3,
                     sg_bc.v(sg_bc.t[:, :].unsqueeze(1).to_broadcast([128, 8, 128])), ALU.mult)
                for ec in range(8):
                    S.tr(psT[:, ec * 128:(ec + 1) * 128], pre[:, ec * 128:(ec + 1) * 128], G.ident_b[:])
                S.copy("act", preT[:], psT.v(psT.t[:, :].rearrange("p (a b) -> p a b", b=128)))
                for n in range(2):
                    for kc in range(8):
                        S.mm(psA[:, n * 512:(n + 1) * 512], preT[:, kc, :], Wo[:, kc, n * 512:(n + 1) * 512], start=(kc == 0), stop=(kc == 7))
                row = (qt * 4 + qs) * 128
                S.dma("sp", xin[:], x2_d.v(x2_d.t[b, CTX + row:CTX + row + 128, :]))
                S.tt("dve", scrq[:], psA[:], gate_bc[:], ALU.mult)
                S.tt("pool", xin[:], xin[:], scrq[:], ALU.add)
                S.dma("sp", x3_d.v(x3_d.t[b, row:row + 128, :]), xin[:])

        SK = 2
        for i in range(len(aitems) + SK):
            if i < len(aitems):
                emit_qk(i)
            if i >= SK:
                emit_pv(i - SK)
        S.pop_scope()
    S.pop_scope()

def build(cfg):
    nc = bass.Bass("TRN2", target_bir_lowering=False)
    S = Sched(nc)
    G = Ctx()
    setup_common(S, G, need=cfg.get("need", ("rwkv", "da", "moe")))
    outs = []
    dbg = {}
    stop = cfg.get("stop", "end")
    kind_x1 = "ExternalOutput" if stop == "rwkv" else "Internal"
    x1_d = S.dram("x1_d", [NB, TOK, D], F32, kind=kind_x1)
    if cfg.get("dbg_hT"):
        dbg["hT"] = S.dram("dbg_hT", [128, 8, LAT + 2], BF16, kind="ExternalOutput")
    wcast_phase(S, G, cfg.get("need", ("rwkv", "da", "moe")))
    if cfg.get("attn_in_ext"):
        G.x2_ext = S.dram("x2_ext", [NB, TOK, D], F32, kind="ExternalInput")
    if cfg.get("moe_in_ext"):
        G.x1_ext = S.dram("x1_ext", [NB, TOK, D], F32, kind="ExternalInput")
    if not cfg.get("skip_l0"):
        mod_phase(S, G, 0)
    if cfg.get("dbg_mod"):
        dm = S.dram("dbg_modT", [128, 48 * 4], F32, kind="ExternalOutput")
        S.dma("sp", dm[:], G.modT.v(G.modT.t[:, :, :].rearrange("p a b -> p (a b)")))
        dg = S.dram("dbg_gates", [2, 3, D], F32, kind="ExternalOutput")
        S.dma("sp", dg[:], G.gates_d[:])
    if stop == "mod":
        S.barrier()
        S.emit()
        return nc, S
    if not cfg.get("skip_rwkv") and not cfg.get("skip_l0"):
        rwkv_phase(S, G, x1_d, dbg=dbg, nb=cfg.get("nb", NB), **cfg.get("rw", {}))
    if stop == "rwkv":
        S.barrier()
        S.emit()
        return nc, S
    x2_d = S.dram("x2_d", [NB, TOK, D], F32, kind="ExternalOutput" if stop == "moe0" else "Internal")
    if not cfg.get("skip_l0"):
        moe0 = True
    else:
        moe0 = False
    tiles0 = [(b, ti * 128, 2 if ti < 2 else b) for b in range(NB) for ti in range(NT)]
    if cfg.get("dbg_gates"):
        dbg["gates"] = S.dram("dbg_gates32", [12, 128, 32], F32, kind="ExternalOutput")
    if moe0:
      moe_phase(S, G, 0, x1_d if not cfg.get("moe_in_ext") else G.x1_ext, tiles0[:cfg.get("moe_ntiles", len(tiles0))],
                lambda b, row0: x2_d.v(x2_d.t[b, row0:row0 + 128, :]), cfg.get("st0", 12), npairs=cfg.get("npairs", 16), dbg=dbg)
    if stop == "moe0":
        S.barrier()
        S.emit()
        return nc, S
    x3_d = S.dram("x3_d", [NB, LAT, D], F32, kind="ExternalOutput" if stop == "attn" else "Internal")
    mod_phase(S, G, 1)
    attn_phase(S, G, x2_d if not cfg.get("attn_in_ext") else G.x2_ext, x3_d, **cfg.get("at", {}))
    if stop == "attn":
        S.barrier()
        S.emit()
        return nc, S
    out_d = S.dram("out", [NB, LAT, D], F32, kind="ExternalOutput")
    tiles1 = [(b, ti * 128, b) for b in range(NB) for ti in range(LAT // 128)]
    moe_phase(S, G, 1, x3_d, tiles1, lambda b, row0: out_d.v(out_d.t[b, row0:row0 + 128, :]), cfg.get("st1", 8))
    S.barrier()
    S.emit()
    return nc, S


def prep_core_inputs(inputs, core, consts):
    b0 = core * NB
    f = lambda a: np.ascontiguousarray(a, dtype=np.float32)
    m = {}
    m["x"] = f(inputs["x"][b0:b0 + NB])
    m["ctx"] = f(inputs["ctx"][b0:b0 + NB])
    m["c3"] = f(np.concatenate([inputs["c"][b0:b0 + NB], inputs["c_ctx"][None, :]], axis=0))
    pv = np.concatenate([
        inputs["norm1_g"].reshape(16, 128), inputs["norm2_g"].reshape(16, 128),
        inputs["rw_mu"][0].reshape(48, 128), inputs["b_mod"].reshape(96, 128)], axis=0)
    m["pvec"] = f(pv)
    m["w_mod"] = f(inputs["w_mod"])
    m["b_mod"] = f(inputs["b_mod"])
    for k, v in consts.items():
        m[k] = v
    m["rw_w_rkv"] = f(inputs["rw_w_rkv"][0])
    for k in ("rw_w0", "rw_w1", "rw_w2", "rw_a0", "rw_a1", "rw_a2", "rw_g1", "rw_g2", "rw_w_o"):
        m[k] = f(inputs[k][0])
    for k in ("rw_k_k", "rw_k_a", "rw_lnx_g", "rw_lnx_b"):
        m[k] = f(inputs[k][0].reshape(1, D))
    m["rw_r_k"] = f(inputs["rw_r_k"][0].reshape(1, D))
    m["da_w_qkv"] = f(inputs["da_w_qkv"][0])
    m["da_q_norm_g"] = f(inputs["da_q_norm_g"][0].reshape(1, 64))
    m["da_k_norm_g"] = f(inputs["da_k_norm_g"][0].reshape(1, 64))
    m["da_lam"] = f(np.stack([inputs["da_lam_q1"][0], inputs["da_lam_k1"][0], inputs["da_lam_q2"][0], inputs["da_lam_k2"][0]]))
    m["da_subln_g"] = f(inputs["da_subln_g"][0].reshape(1, 128))
    m["da_w_o"] = f(inputs["da_w_o"][0])
    rt = np.concatenate([inputs["moe_router_g"], np.transpose(inputs["moe_router_e"], (0, 2, 1, 3)).reshape(2, D, 32)], axis=2)
    m["moe_router"] = f(rt)
    rb = np.concatenate([inputs["moe_router_g_b"], inputs["moe_router_e_b"].reshape(2, 32)], axis=1).reshape(2, 1, 36)
    m["moe_router_b"] = f(rb)
    m["moe_w_gate"] = f(inputs["moe_w_gate"]).reshape(2, NEXP, D, DFF)
    m["moe_w_up"] = f(inputs["moe_w_up"]).reshape(2, NEXP, D, DFF)
    m["moe_w_down"] = f(inputs["moe_w_down"]).reshape(2, NEXP, DFF, D)
    return m


_CACHE = {}


def kernel(**inputs):
    from concourse.bass_utils import run_bass_kernel_spmd
    n = 8
    if "nc" not in _CACHE:
        _CACHE["nc"] = build({})[0]
        _CACHE["consts"] = host_consts()
    nc = _CACHE["nc"]
    consts = _CACHE["consts"]
    inputs = {k: np.asarray(v) for k, v in inputs.items()}
    in_maps = [prep_core_inputs(inputs, c, consts) for c in range(n)]
    res = run_bass_kernel_spmd(nc, in_maps, core_ids=list(range(n)))
    out = np.concatenate([r["out"] for r in res.results], axis=0)
    return out.astype(np.float32)
```

```python
import numpy as np
import concourse.bass as bass
import concourse.mybir as mybir

F32 = mybir.dt.float32
BF16 = mybir.dt.bfloat16
I32 = mybir.dt.int32
U32 = mybir.dt.uint32
AF = mybir.ActivationFunctionType
ALU = mybir.AluOpType
AX = mybir.AxisListType

ENGS = ("pe", "dve", "act", "pool", "sp")
SEM_LIMIT = 30000
N_DMA_SEMS = 24


class V:
    __slots__ = ("ap", "toks", "excl")

    def __init__(self, ap, toks, excl=False):
        self.ap = ap
        self.toks = toks
        self.excl = excl


class Tl:
    def __init__(self, S, t, name, excl=False, toks=None):
        self.S = S
        self.t = t
        self.name = name
        self.excl = excl
        self.toks = toks

    def _tk(self, key):
        if self.toks is not None:
            return list(self.toks)
        return [(self.name, None if self.excl else key)]

    def __getitem__(self, idx):
        return V(self.t[idx], self._tk(None), self.excl)

    def k(self, key, idx=None):
        ap = self.t[idx] if idx is not None else None
        return V(ap, self._tk(key), self.excl)

    def v(self, ap, key=None):
        return V(ap, self._tk(key), self.excl)


class Sched:
    def __init__(self, nc, same_engine_sync=True):
        self.nc = nc
        self.q = {e: [] for e in ENGS}
        self.cnt = {e: 0 for e in ENGS}
        self.semi = {e: 0 for e in ENGS}
        self.nsem = {e: 1 for e in ENGS}
        self.state = {}
        self.waited = {e: {} for e in ENGS}
        self.same = same_engine_sync
        self.dma_i = 0
        self.dma_cnt = [0] * N_DMA_SEMS
        self.dma_last = [None] * N_DMA_SEMS
        self.ctx = []
        self.n_instr = 0
        self.out_deps = []

    def sbuf(self, name, shape, dtype=F32):
        cm = self.nc.sbuf_tensor(name, list(shape), dtype)
        t = cm.__enter__()
        self.ctx.append(cm)
        return Tl(self, t, name)

    def psum(self, name, shape, dtype=F32):
        cm = self.nc.psum_tensor(name, list(shape), dtype)
        t = cm.__enter__()
        self.ctx.append(cm)
        return Tl(self, t, name, excl=True)

    def dram(self, name, shape, dtype=F32, kind="Internal"):
        t = self.nc.dram_tensor(name, list(shape), dtype, kind=kind)
        return Tl(self, t.ap(), name)

    def push_scope(self):
        self.scopes = getattr(self, "scopes", [])
        self.scopes.append(len(self.ctx))

    def pop_scope(self):
        self.barrier()
        n = self.scopes.pop()
        while len(self.ctx) > n:
            self.ctx.pop().__exit__(None, None, None)

    def barrier(self):
        deps = []
        for e in ENGS:
            if self.cnt[e] > 0:
                deps.append(((e, self.semi[e]), self.cnt[e], e))
        for i in range(N_DMA_SEMS):
            if self.dma_last[i] is not None:
                deps.append(self.dma_last[i])
        for e in ENGS:
            d = [x for x in deps if x[2] != e]
            w = self._waits(e, d)
            if w:
                self.q[e].append(("wait", None, w, None))

    def _deps(self, reads, writes):
        deps = []
        for v in reads:
            for tok in v.toks:
                st = self.state.get(tok)
                if st and st[0] is not None:
                    deps.append(st[0])
        for v in writes:
            for tok in v.toks:
                st = self.state.get(tok)
                if st:
                    if st[0] is not None:
                        deps.append(st[0])
                    deps.extend(st[1])
        return deps

    def _update(self, reads, writes, me, real_writes=None):
        for v in reads:
            for tok in v.toks:
                st = self.state.setdefault(tok, [None, [], None])
                st[1].append(me)
                if len(st[1]) > 64:
                    best = {}
                    for d in st[1]:
                        if d[0] not in best or best[d[0]][1] < d[1]:
                            best[d[0]] = d
                    st[1] = list(best.values())
        rw = writes if real_writes is None else real_writes
        rwt = set()
        for v in rw:
            rwt.update(v.toks)
        for v in writes:
            for tok in v.toks:
                old = self.state.get(tok)
                lrw = me if tok in rwt else (old[2] if old else None)
                self.state[tok] = [me, [], lrw]

    def _waits(self, eng, deps, raw_toks_same=None):
        need = {}
        for d in deps:
            semkey, val, deng, is_raw = d[0], d[1], d[2], True
            if deng == eng and not self.same:
                continue
            w = self.waited[eng].get(semkey, 0)
            if val > w and val > need.get(semkey, 0):
                need[semkey] = val
        for sk, val in need.items():
            self.waited[eng][sk] = val
        return list(need.items())

    def op(self, eng, fn, reads=(), writes=(), same_ok=False):
        reads = list(reads)
        real_writes = list(writes)
        writes = real_writes + [v for v in reads if v.excl]
        deps = self._deps(reads, writes)
        if same_ok or eng == "pe":
            deps = [d for d in deps if d[2] != eng]
        elif self.same:
            rd = []
            for v in reads:
                for tok in v.toks:
                    st = self.state.get(tok)
                    if st and st[2] is not None and st[2][2] == eng:
                        rd.append(st[2])
            deps = [d for d in deps if d[2] != eng] + rd
        waits = self._waits(eng, deps)
        if self.cnt[eng] >= SEM_LIMIT:
            self.semi[eng] += 1
            self.nsem[eng] = max(self.nsem[eng], self.semi[eng] + 1)
            self.cnt[eng] = 0
        self.cnt[eng] += 1
        me = ((eng, self.semi[eng]), self.cnt[eng], eng)
        self.q[eng].append(("op", fn, waits, me[0]))
        self._update(reads, writes, me, real_writes)
        self.n_instr += 1
        return me

    def dma(self, queue, out, in_, split=0, **kw):
        reads, writes = [in_], [out]
        deps = self._deps(reads, writes)
        i = self.dma_i % N_DMA_SEMS
        self.dma_i += 1
        if self.dma_last[i] is not None:
            deps.append(self.dma_last[i])
        waits = self._waits(queue, deps)
        oa, ia = out.ap, in_.ap
        parts = [(oa, ia)]
        if split:
            step = 128 // split
            parts = [(oa[k * step:(k + 1) * step], ia[k * step:(k + 1) * step]) for k in range(split)]
        for k, (o_, a_) in enumerate(parts):
            self.dma_cnt[i] += 16
            self.q[queue].append(("dma", (o_, a_, kw), waits if k == 0 else [], ("dma", i)))
            self.n_instr += 1
        me = (("dma", i), self.dma_cnt[i], "dma")
        self.dma_last[i] = me
        self._update(reads, writes, me)
        return me

    def emit(self, final_deps=None):
        nc = self.nc
        sems = {}
        cms = []

        def mk(name):
            cm = nc.semaphore(name)
            s = cm.__enter__()
            cms.append(cm)
            return s
        for e in ENGS:
            for i in range(self.nsem[e]):
                sems[(e, i)] = mk(f"s_{e}_{i}")
        for i in range(N_DMA_SEMS):
            sems[("dma", i)] = mk(f"s_dma_{i}")
        engobj = {"pe": "tensor", "dve": "vector", "act": "scalar", "pool": "gpsimd", "sp": "sync"}
        if final_deps:
            w = self._waits("sp", final_deps)
            self.q["sp"].append(("wait", None, w, None))
        with nc.Block() as block:
            for e in ENGS:
                items = self.q[e]
                if not items:
                    continue

                def body(eng, items=items):
                    for kind, fn, waits, semkey in items:
                        for sk, val in waits:
                            eng.wait_ge(sems[sk], val)
                        if kind == "op":
                            ins = fn(eng)
                            ins.then_inc(sems[semkey], 1)
                        elif kind == "dma":
                            oa, ia, kw = fn
                            eng.dma_start(out=oa, in_=ia, **kw).then_inc(sems[semkey], 16)
                getattr(block, engobj[e])(body)
        for cm in reversed(cms):
            cm.__exit__(None, None, None)
        for cm in reversed(self.ctx):
            cm.__exit__(None, None, None)

    def mm(self, out, lhsT, rhs, start=True, stop=True, **kw):
        return self.op("pe", lambda e: e.matmul(out.ap, lhsT.ap, rhs.ap, start=start, stop=stop, **kw),
                       reads=[lhsT, rhs] + ([] if start else [out]), writes=[out])

    def tr(self, out, in_, ident):
        return self.op("pe", lambda e: e.transpose(out.ap, in_.ap, ident.ap),
                       reads=[in_, ident], writes=[out])

    def act(self, out, in_, func, bias=None, scale=None, accum_out=None, eng="act"):
        reads = [in_]
        kw = {}
        if isinstance(bias, V):
            reads.append(bias)
            kw["bias"] = bias.ap
        elif bias is not None:
            kw["bias"] = bias
        if isinstance(scale, V):
            reads.append(scale)
            kw["scale"] = scale.ap
        elif scale is not None:
            kw["scale"] = scale
        writes = [out]
        if accum_out is not None:
            writes.append(accum_out)
            kw["accum_out"] = accum_out.ap
        return self.op("act", lambda e: e.activation(out.ap, in_.ap, func, **kw), reads=reads, writes=writes)

    def tt(self, eng, out, in0, in1, op):
        return self.op(eng, lambda e: e.tensor_tensor(out.ap, in0.ap, in1.ap, op), reads=[in0, in1], writes=[out])

    def ts(self, eng, out, in0, s1, s2=None, op0=ALU.mult, op1=None, accum_out=None):
        reads = [in0]
        a1 = s1.ap if isinstance(s1, V) else s1
        a2 = s2.ap if isinstance(s2, V) else s2
        if isinstance(s1, V):
            reads.append(s1)
        if isinstance(s2, V):
            reads.append(s2)
        writes = [out]
        kw = {}
        if op1 is not None:
            kw["op1"] = op1
        if accum_out is not None:
            kw["accum_out"] = accum_out.ap
            writes.append(accum_out)
        return self.op(eng, lambda e: e.tensor_scalar(out.ap, in0.ap, a1, a2, op0, **kw), reads=reads, writes=writes)

    def stt(self, out, in0, scalar, in1, op0, op1, eng="dve"):
        reads = [in0, in1]
        a = scalar.ap if isinstance(scalar, V) else scalar
        if isinstance(scalar, V):
            reads.append(scalar)
        return self.op(eng, lambda e: e.scalar_tensor_tensor(out.ap, in0.ap, a, in1.ap, op0, op1), reads=reads, writes=[out])

    def copy(self, eng, out, in_):
        if eng == "act":
            return self.op("act", lambda e: e.copy(out.ap, in_.ap), reads=[in_], writes=[out])
        return self.op(eng, lambda e: e.tensor_copy(out.ap, in_.ap), reads=[in_], writes=[out])

    def memset(self, eng, out, val):
        return self.op(eng, lambda e: e.memset(out.ap, val), reads=[], writes=[out])

    def reduce(self, out, in_, op=ALU.add, axis=AX.X, eng="dve"):
        return self.op(eng, lambda e: e.tensor_reduce(out.ap, in_.ap, axis, op), reads=[in_], writes=[out])

    def recip(self, out, in_):
        return self.op("dve", lambda e: e.reciprocal(out.ap, in_.ap), reads=[in_], writes=[out])
D = 1024
NB = 2
LAT = 2048
CTX = 256
TOK = CTX + LAT
NT = TOK // 128
EPS = 1e-6
DECAY_C = -0.6065306597126334
LNX_EPS = 64e-5
NGRP = 4
NEXP = 32
DFF = 256

PV_N1 = (0, 16)
PV_N2 = (16, 32)
PV_MU = 32
PV_BMOD = 80
PV_ROWS = 176


def host_consts():
    c = {}
    c["ident_f"] = np.eye(128, dtype=np.float32)
    c["ones_f"] = np.ones((128, 128), dtype=np.float32)
    idx = np.arange(128)
    cs, ct = idx[:, None] // 64, idx[None, :] // 64
    same = (cs == ct)
    s_, t_ = idx[:, None], idx[None, :]
    maskA = np.zeros((2, 128, 256), np.float32)
    maskAT = np.zeros((2, 128, 128), np.float32)
    tri = np.zeros((2, 128, 384), np.float32)
    for z in range(2):
        prec = same & ((s_ < t_) if z == 0 else (s_ > t_))
        preceq = same & ((s_ <= t_) if z == 0 else (s_ >= t_))
        succ = same & ((s_ > t_) if z == 0 else (s_ < t_))
        maskA[z, :, 0:128] = prec
        maskA[z, :, 128:256] = preceq
        maskAT[z] = prec.T
        tri[z, :, 0:128] = preceq * DECAY_C
        tri[z, :, 128:256] = prec * DECAY_C
        tri[z, :, 256:384] = succ * DECAY_C
    c["maskA"] = maskA
    c["maskAT"] = maskAT
    c["tri"] = tri
    ind = np.zeros((128, 2), np.float32)
    ind[0:64, 0] = DECAY_C
    ind[64:128, 1] = DECAY_C
    c["ind"] = ind
    rows = LAT // 64
    row = np.repeat(np.arange(rows), 64)
    col = np.tile(np.arange(64), rows)
    inv = (10000.0 ** (-np.arange(16, dtype=np.float32) / 16)).astype(np.float32)
    ang = np.stack([row, col], axis=-1).astype(np.float32)[:, :, None] * inv
    c["rope_cos"] = np.cos(ang).astype(np.float32).reshape(LAT, 32)
    c["rope_sin"] = np.sin(ang).astype(np.float32).reshape(LAT, 32)
    sel = np.zeros((32, 32, 128), np.float32)
    for e in range(32):
        sel[e, e, :] = 1.0
    c["sel"] = sel.reshape(32, 32 * 128)
    return c


class Ctx:
    pass


def std_psum(S, G, tag):
    G.psA = S.psum(f"psA{tag}", [128, 1024], F32)
    nm = G.psA.name
    G.psA.toks = [(nm, 0), (nm, 1)]
    G.psA_h = [Tl(S, G.psA.t[:, i * 512:(i + 1) * 512].rearrange("p (a b) -> p a b", b=128), nm, excl=True,
                  toks=[(nm, i)]) for i in range(2)]
    G.psY = S.psum(f"psY{tag}", [128, 1024], F32)
    nmy = G.psY.name
    G.psY.toks = [(nmy, 0), (nmy, 1)]
    G.psY_h = [Tl(S, G.psY.t[:, i * 512:(i + 1) * 512].rearrange("p (a b) -> p a b", b=128), nmy, excl=True,
                  toks=[(nmy, i)]) for i in range(2)]
    G.psT = S.psum(f"psT{tag}", [128, 1024], BF16)
    G.psH = [S.psum(f"psH{i}{tag}", [128, 4, 128], F32) for i in range(3)]


def setup_common(S, G, need=("rwkv", "da", "moe")):
    def I(name, shape, dt=F32):
        grp = name.split('_')[0]
        if grp in ('rw', 'da', 'moe') and {'rw': 'rwkv', 'da': 'da', 'moe': 'moe'}[grp] not in need:
            return None
        return S.dram(name, shape, dt, kind="ExternalInput")
    G.x = I("x", [NB, LAT, D])
    G.ctx = I("ctx", [NB, CTX, D])
    G.c3 = I("c3", [3, D])
    G.pvec = I("pvec", [PV_ROWS, 128])
    G.w_mod = I("w_mod", [2, D, 6 * D])
    G.b_mod = I("b_mod", [2, 6 * D])
    G.ident_f_d = I("ident_f", [128, 128])
    G.ones_f_d = I("ones_f", [128, 128])
    G.maskA_d = I("maskA", [2, 128, 256])
    G.maskAT_d = I("maskAT", [2, 128, 128])
    G.tri_d = I("tri", [2, 128, 384])
    G.ind_d = I("ind", [128, 2])
    G.rope_cos_d = I("rope_cos", [LAT, 32])
    G.rope_sin_d = I("rope_sin", [LAT, 32])
    G.sel_d = I("sel", [32, 32 * 128])
    G.rw_w_rkv = I("rw_w_rkv", [3, D, D])
    G.rw_w0 = I("rw_w0", [2, D])
    G.rw_w1 = I("rw_w1", [2, D, 64])
    G.rw_w2 = I("rw_w2", [2, 64, D])
    G.rw_a0 = I("rw_a0", [2, D])
    G.rw_a1 = I("rw_a1", [2, D, 64])
    G.rw_a2 = I("rw_a2", [2, 64, D])
    G.rw_g1 = I("rw_g1", [D, 128])
    G.rw_g2 = I("rw_g2", [128, D])
    G.rw_k_k = I("rw_k_k", [1, D])
    G.rw_k_a = I("rw_k_a", [1, D])
    G.rw_r_k = I("rw_r_k", [1, D])
    G.rw_lnx_g = I("rw_lnx_g", [1, D])
    G.rw_lnx_b = I("rw_lnx_b", [1, D])
    G.rw_w_o = I("rw_w_o", [D, D])
    G.da_w_qkv = I("da_w_qkv", [D, 3 * D])
    G.da_q_norm_g = I("da_q_norm_g", [1, 64])
    G.da_k_norm_g = I("da_k_norm_g", [1, 64])
    G.da_lam = I("da_lam", [4, 64])
    G.da_subln_g = I("da_subln_g", [1, 128])
    G.da_w_o = I("da_w_o", [D, D])
    G.moe_router = I("moe_router", [2, D, 36])
    G.moe_router_b = I("moe_router_b", [2, 1, 36])
    G.moe_w_gate = I("moe_w_gate", [2, NEXP, D, DFF])
    G.moe_w_up = I("moe_w_up", [2, NEXP, D, DFF])
    G.moe_w_down = I("moe_w_down", [2, NEXP, DFF, D])

    G.ident_f = S.sbuf("ident_f_s", [128, 128], F32)
    G.ident_b = S.sbuf("ident_b_s", [128, 128], BF16)
    G.ones_f = S.sbuf("ones_f_s", [128, 128], F32)
    S.dma("sp", G.ident_f[:], G.ident_f_d[:])
    S.dma("pool", G.ident_b[:], G.ident_f_d[:])
    S.dma("sp", G.ones_f[:], G.ones_f_d[:])
    G.pfm = S.sbuf("pfm", [128, PV_ROWS], F32)
    S.push_scope()
    std_psum(S, G, "c")
    pv = S.sbuf("pv_ld", [128, 2, 128], F32)
    S.memset("dve", pv[:], 0.0)
    S.dma("sp", pv[:, 0, :], G.pvec[0:128, :])
    S.dma("sp", pv[0:PV_ROWS - 128, 1, :], G.pvec[128:PV_ROWS, :])
    S.tr(G.psA[:, 0:128], pv[:, 0, :], G.ident_f[:])
    S.tr(G.psA[:, 128:256], pv[:, 1, :], G.ident_f[:])
    S.copy("dve", G.pfm[:], G.psA[:, 0:PV_ROWS])
    S.pop_scope()
    G.modT = S.sbuf("modT", [128, 48, 4], F32)
    G.A1 = S.sbuf("A1", [128, 3, 8], F32)
    G.A2 = S.sbuf("A2", [128, 3, 8], F32)
    G.gates_d = S.dram("gates_d", [2, 3, D], F32)


def mod_phase(S, G, li):
    S.push_scope()
    std_psum(S, G, f"m{li}")
    crow = S.sbuf(f"crow{li}", [4, D], F32)
    sc = S.sbuf(f"sc{li}", [4, D], F32)
    scT = S.sbuf(f"scT{li}", [128, 8, 4], F32)
    brow = S.sbuf(f"brow{li}", [1, 6 * D], F32)
    grow = S.sbuf(f"grow{li}", [4, 512], F32)
    wblk = [S.sbuf(f"wblk{i}_{li}", [128, 8, 512], F32) for i in range(2)]
    S.memset("dve", crow[:], 0.0)
    S.dma("sp", crow[0:3, :], G.c3[:])
    S.dma("sp", brow[:], G.b_mod[li:li + 1, :])
    S.act(sc[:], crow[:], AF.Silu)
    for kc in range(8):
        S.tr(G.psA[:, kc * 4:(kc + 1) * 4], sc[0:4, kc * 128:(kc + 1) * 128], G.ident_f[0:4, 0:4])
    S.copy("dve", scT[:], G.psA.v(G.psA.t[:, 0:32].rearrange("p (a b) -> p a b", b=4)))
    wsrc = G.w_mod.t[li].rearrange("(kc p) n -> p kc n", p=128)
    gate_blocks = {4: (0, 0), 5: (0, 1), 10: (1, 0), 11: (1, 1)}
    for blk in range(12):
        wb = wblk[blk % 2]
        S.dma("sp", wb[:], G.w_mod.v(wsrc[:, :, blk * 512:(blk + 1) * 512]), split=8)
        if blk in gate_blocks:
            which, half = gate_blocks[blk]
            ps = G.psY[0:4, 0:512]
            for kc in range(8):
                S.mm(ps, scT[:, kc, :], wb[:, kc, :], start=(kc == 0), stop=False)
            S.mm(ps, G.ones_f[0:1, 0:4], brow[0:1, blk * 512:(blk + 1) * 512], start=False, stop=True)
            S.copy("dve", grow[:], ps)
            S.dma("sp", G.gates_d.v(G.gates_d.t[which, :, half * 512:(half + 1) * 512]), grow[0:3, :])
        else:
            for ec in range(4):
                ch = blk * 4 + ec
                ps = G.psA[:, ch * 4:(ch + 1) * 4]
                for kc in range(8):
                    S.mm(ps, wb[:, kc, ec * 128:(ec + 1) * 128], scT[:, kc, :], start=(kc == 0), stop=(kc == 7))
                S.ts("dve", G.modT[:, ch, :], ps, G.pfm[:, PV_BMOD + li * 48 + ch:PV_BMOD + li * 48 + ch + 1], None, op0=ALU.add)
    for r in range(3):
        for (A, sc0, gofs) in ((G.A1, 8, PV_N1[0] + li * 8), (G.A2, 32, PV_N2[0] + li * 8)):
            S.ts("dve", A[:, r, :], G.modT[:, sc0:sc0 + 8, r], 1.0, None, op0=ALU.add)
            S.tt("dve", A[:, r, :], A[:, r, :], G.pfm[:, gofs:gofs + 8], ALU.mult)
    S.pop_scope()


def norm_tile_to_fm(S, G, xt, r, A, shift_ch0, out_fm, wk, fp32_out=None):
    st = wk["st"]
    S.act(wk["junk"][:], xt, AF.Square, accum_out=st[:, 0:1])
    S.ts("dve", st[:, 1:2], st[:, 0:1], 1.0 / D, EPS, op0=ALU.mult, op1=ALU.add)
    S.act(st[:, 2:3], st[:, 1:2], AF.Sqrt)
    S.recip(st[:, 3:4], st[:, 2:3])
    if fp32_out is None:
        xn = wk["xn"]
        S.ts("dve", xn[:], xt, st[:, 3:4], None, op0=ALU.mult)
        for kc in range(8):
            S.tr(G.psT[:, kc * 128:(kc + 1) * 128], xn[:, kc * 128:(kc + 1) * 128], G.ident_b[:])
        src = G.psT.v(G.psT.t[:, :].rearrange("p (a b) -> p a b", b=128))
    else:
        xn = wk["xn32"]
        S.ts("dve", xn[:], xt, st[:, 3:4], None, op0=ALU.mult)
        for kc in range(8):
            S.tr(G.psA[:, kc * 128:(kc + 1) * 128], xn[:, kc * 128:(kc + 1) * 128], G.ident_f[:])
        src = G.psA.v(G.psA.t[:, :].rearrange("p (a b) -> p a b", b=128))
    Abc = A.v(A.t[:, r, :].unsqueeze(2).to_broadcast([128, 8, 128]))
    shbc = G.modT.v(G.modT.t[:, shift_ch0:shift_ch0 + 8, r].unsqueeze(2).to_broadcast([128, 8, 128]))
    tmp = wk["fm32"]
    S.tt("dve", tmp[:], src, Abc, ALU.mult)
    if fp32_out is not None:
        S.tt("pool", fp32_out, tmp[:], shbc, ALU.add)
        S.copy("act", out_fm, fp32_out)
    else:
        S.tt("pool", out_fm, tmp[:], shbc, ALU.add)


def norm_tile_gen(S, G, xt, r, A, shift_ch0, out_fm, wk, fp32_out, psA):
    st = wk["st"]
    S.act(wk["junk"][:], xt, AF.Square, accum_out=st[:, 0:1])
    yield
    S.ts("dve", st[:, 1:2], st[:, 0:1], 1.0 / D, EPS, op0=ALU.mult, op1=ALU.add)
    yield
    S.act(st[:, 2:3], st[:, 1:2], AF.Ln)
    S.act(st[:, 3:4], st[:, 2:3], AF.Exp, scale=-0.5)
    yield
    xn = wk["xn32"]
    S.ts("dve", xn[:], xt, st[:, 3:4], None, op0=ALU.mult)
    yield
    for kc in range(8):
        S.tr(psA[:, kc * 128:(kc + 1) * 128], xn[:, kc * 128:(kc + 1) * 128], G.ident_f[:])
    yield
    src = psA.v(psA.t[:, :].rearrange("p (a b) -> p a b", b=128))
    Abc = A.v(A.t[:, r, :].unsqueeze(2).to_broadcast([128, 8, 128]))
    shbc = G.modT.v(G.modT.t[:, shift_ch0:shift_ch0 + 8, r].unsqueeze(2).to_broadcast([128, 8, 128]))
    tmp = wk["fm32"]
    S.tt("dve", tmp[:], src, Abc, ALU.mult)
    yield
    S.tt("pool", fp32_out, tmp[:], shbc, ALU.add)
    S.copy("act", out_fm, fp32_out)


def wcast_phase(S, G, need):
    items = []
    G.wbf = {}

    def add(name, src3, n):
        dst = S.dram("wbf_" + name, [128, n], BF16)
        G.wbf[name] = dst
        off = 0
        a, b = src3.shape[1], src3.shape[2]
        rows = max(1, 2048 // b)
        if b > 2048:
            for i in range(a):
                for c0 in range(0, b, 2048):
                    c1 = min(b, c0 + 2048)
                    items.append((src3[:, i:i + 1, c0:c1], dst, i * b + c0, c1 - c0, (1, c1 - c0)))
        else:
            for i in range(0, a, rows):
                i1 = min(a, i + rows)
                items.append((src3[:, i:i1, :], dst, i * b, (i1 - i) * b, (i1 - i, b)))

    if "rwkv" in need:
        for j, nm in enumerate(("Wr", "Wk", "Wv")):
            add(nm, G.rw_w_rkv.t[j].rearrange("(kc p) n -> p kc n", p=128), 8 * D)
        add("rwWo", G.rw_w_o.t.rearrange("(kc p) n -> p kc n", p=128), 8 * D)
    if "da" in need:
        add("Wqkv", G.da_w_qkv.t.rearrange("(kc p) n -> p kc n", p=128), 8 * 3 * D)
        add("daWo", G.da_w_o.t.rearrange("(kc p) n -> p kc n", p=128), 8 * D)
    if "moe" in need:
        for li in range(2):
            for e in range(NEXP):
                add(f"g{li}_{e}", G.moe_w_gate.t[li, e].rearrange("(kc p) n -> p kc n", p=128), 8 * DFF)
                add(f"u{li}_{e}", G.moe_w_up.t[li, e].rearrange("(kc p) n -> p kc n", p=128), 8 * DFF)
                add(f"d{li}_{e}", G.moe_w_down.t[li, e].rearrange("(fc p) n -> p fc n", p=128), 2 * D)
    S.push_scope()
    NBUF = 4
    stg = [S.sbuf(f"wc_stg{i}", [128, 2048], F32) for i in range(NBUF)]
    ob = [S.sbuf(f"wc_ob{i}", [128, 2048], BF16) for i in range(NBUF)]
    engs = ["dve", "pool", "dve"]

    def load(i):
        src3, dst, off, n, (a, b) = items[i]
        t = stg[i % NBUF]
        S.dma("sp", t.v(t.t[:, 0:n].rearrange("p (a b) -> p a b", b=b)), V(src3, [("wsrc", None)]))

    for i in range(min(NBUF - 1, len(items))):
        load(i)
    for i in range(len(items)):
        if i + NBUF - 1 < len(items):
            load(i + NBUF - 1)
        src3, dst, off, n, _ = items[i]
        S.copy(engs[i % 3], ob[i % NBUF][:, 0:n], stg[i % NBUF][:, 0:n])
        S.dma("act", dst.v(dst.t[:, off:off + n]), ob[i % NBUF][:, 0:n])
    S.pop_scope()

def rwkv_phase(S, G, x1_d, dbg=None, nb=NB, nt0=NT, do_dir=3, nt1=NT, nheads=16, fl=99):
    li = 0
    H = 16
    yf_d = S.dram("yf_d", [NB, NT, 128, 1040], F32)
    cache_d = S.dram("cache_d", [NB, NT, 128, 6 * D], BF16)
    sg1_d = S.dram("sg1_d", [NB, NT, 128, D], F32)

    def load_bc(name, src, dt=BF16, n=D, q="pool"):
        t = S.sbuf(name, [128, n], dt)
        S.dma(q, t[:], src.v(src.t[0:1, :].partition_broadcast(128)))
        return t

    S.push_scope()
    std_psum(S, G, "r")
    maskA = S.sbuf("maskA_s", [128, 2, 256], BF16)
    maskAT = S.sbuf("maskAT_s", [128, 2, 128], BF16)
    tri = S.sbuf("tri_s", [128, 2, 384], F32)
    ind = S.sbuf("ind_s", [128, 2], F32)
    for z in range(2):
        S.dma("pool", maskA[:, z, :], G.maskA_d.v(G.maskA_d.t[z]))
        S.dma("pool", maskAT[:, z, :], G.maskAT_d.v(G.maskAT_d.t[z]))
        S.dma("sp", tri[:, z, :], G.tri_d.v(G.tri_d.t[z]))
    S.dma("sp", ind[:], G.ind_d[:])
    k_a_bc = load_bc("k_a_bc", G.rw_k_a)
    r_k_bc = load_bc("r_k_bc", G.rw_r_k)
    scr1 = S.sbuf("scr1", [128, D], F32)
    scr2 = S.sbuf("scr2", [128, D], F32)
    sg_sb = S.sbuf("sg_sb", [128, D], F32)
    kdir_sb = S.sbuf("kdir_sb", [128, D], BF16)
    b_sb = S.sbuf("b_sb", [128, D], BF16)
    tm = [S.sbuf(f"tm{i}", [128, D], BF16) for i in range(2)]
    R19 = S.sbuf("R19", [128, H, 128], BF16)
    Bh = S.sbuf("Bh", [128, D], BF16)
    Kh = S.sbuf("Kh", [128, D], BF16)
    arT = S.sbuf("arT", [128, 8, 2, 128], BF16)
    btT = S.sbuf("btT", [128, 8, 128], BF16)
    ktT = S.sbuf("ktT", [128, 8, 128], BF16)
    gC = S.sbuf("gC", [128, 8, 2], F32)
    bon = S.sbuf("bon", [128, 2, 16], F32)
    NSET = 4
    M1 = [S.sbuf(f"M1_{i}", [128, 256], BF16) for i in range(NSET)]
    M2 = [S.sbuf(f"M2_{i}", [128, 256], BF16) for i in range(NSET)]
    MabT = [S.sbuf(f"MabT_{i}", [128, 128], BF16) for i in range(NSET)]
    Pb = [[S.sbuf(f"Pb_{i}_{j}", [128, 128], BF16) for j in range(2)] for i in range(NSET)]
    PTb = [[S.sbuf(f"PTb_{i}_{j}", [128, 128], BF16) for j in range(2)] for i in range(NSET)]
    Tb = [S.sbuf(f"Tb_{i}", [128, 128], BF16) for i in range(NSET)]
    WP = [S.sbuf(f"WP_{i}", [128, 128], BF16) for i in range(NSET)]
    G_all = S.sbuf("G_all", [128, 8, 128], BF16)
    Y0_all = S.sbuf("Y0_all", [128, H, 64], BF16)
    D_all = S.sbuf("D_all", [128, 8, 2, 128], BF16)
    E_all = S.sbuf("E_all", [128, 8, 2, 128], BF16)
    Sb = S.sbuf("Sb", [128, 8, 128], BF16)
    S.memset("pool", D_all[:], 0.0)
    S.memset("pool", E_all[:], 0.0)
    yfw = S.sbuf("yfw", [128, 1040], F32)
    banks = list(G.psH) + list(G.psA_h) + list(G.psY_h)
    NBK = len(banks)
    bank_ctr = [0]
    slot_ctr = [0] * NBK

    def slot(n=1):
        bk = bank_ctr[0] % NBK
        bank_ctr[0] += 1
        if n == 2:
            i = ((slot_ctr[bk] + 1) // 2 * 2) % 4
            slot_ctr[bk] = i + 2
        else:
            i = slot_ctr[bk] % 4
            slot_ctr[bk] = i + 1
        return (bk, i)

    def psl(s, p0=0, p1=128, c0=0, c1=128, n=1):
        bk, i = s
        t = banks[bk]
        if n == 2:
            return t.v(t.t[p0:p1, i:i + 2, :].rearrange("p a b -> p (a b)")[:, c0:c1])
        return t.v(t.t[p0:p1, i, c0:c1])

    def transposes_to(src_tm, dst_view):
        for ec in range(8):
            S.tr(G.psT[:, ec * 128:(ec + 1) * 128], src_tm[:, ec * 128:(ec + 1) * 128], G.ident_b[:])
        S.copy("act", dst_view, G.psT.v(G.psT.t[:, :].rearrange("p (a b) -> p a b", b=128)))

    def dir_part(z, r_v, k_v, v_v, kk_v, a_v, chunk_order):
        zsl = slice(z, z + 1)
        S.stt(scr1[:], a_v, -1.0, k_a_bc[:], ALU.add, ALU.mult)
        S.stt(kdir_sb[:], scr1[:], 1.0, k_v, ALU.add, ALU.mult)
        S.tt("pool", b_sb[:], kk_v, a_v, ALU.mult)
        S.tt("pool", scr1[:], r_v, kdir_sb[:], ALU.mult)
        S.tt("pool", scr1[:], scr1[:], r_k_bc[:], ALU.mult)
        S.reduce(bon[:, z, :], scr1.v(scr1.t[:, :].rearrange("p (h n) -> p h n", n=64)))
        def cum(which):
            for n in range(2):
                S.mm(G.psA[:, n * 512:(n + 1) * 512], tri[:, z, which * 128:(which + 1) * 128], sg_sb[:, n * 512:(n + 1) * 512])
        cum(0)
        S.act(scr2[:], G.psA[:], AF.Exp)
        S.tt("dve", tm[0][:], r_v, scr2[:], ALU.mult)
        transposes_to(tm[0], arT.v(arT.t[:, :, 1, :]))
        S.act(scr2[:], G.psA[:], AF.Exp, scale=-1.0)
        S.tt("dve", tm[1][:], b_sb[:], scr2[:], ALU.mult)
        transposes_to(tm[1], btT[:])
        S.tt("dve", tm[0][:], kdir_sb[:], scr2[:], ALU.mult)
        transposes_to(tm[0], ktT[:])
        cum(1)
        S.act(scr2[:], G.psA[:], AF.Exp)
        S.stt(tm[1][:], kk_v, -1.0, scr2[:], ALU.mult, ALU.mult)
        S.copy("pool", V(R19.t[:, :, 64:128], [("R19", h) for h in range(H)]),
               tm[1].v(tm[1].t[:, :].rearrange("p (h n) -> p h n", n=64)))
        transposes_to(tm[1], arT.v(arT.t[:, :, 0, :]))
        cum(2)
        S.act(scr2[:], G.psA[:], AF.Exp)
        S.tt("dve", Bh[:], b_sb[:], scr2[:], ALU.mult)
        S.tt("pool", Kh[:], kdir_sb[:], scr2[:], ALU.mult)
        sg_ = slot()
        for ec in range(8):
            S.mm(psl(sg_, c0=ec * 2, c1=ec * 2 + 2), sg_sb[:, ec * 128:(ec + 1) * 128], ind[:])
        S.act(gC[:], banks[sg_[0]].v(banks[sg_[0]].t[:, sg_[1], 0:16].rearrange("p (a b) -> p a b", b=2)), AF.Exp)

        if do_dir < 2:
            return
        def head_gen(h):
            ec, po = h // 2, (h % 2) * 64
            hc = slice(h * 64, (h + 1) * 64)
            pr = slice(po, po + 64)
            i2 = h % NSET
            bt_h = btT[pr, ec, :]
            kt_h = ktT[pr, ec, :]
            ar_h = arT.v(arT.t[pr, ec, :, :].rearrange("p a b -> p (a b)"))
            at_h = arT[pr, ec, 0, :]
            rt_h = arT[pr, ec, 1, :]
            s1 = slot(2)
            S.mm(psl(s1, n=2, c1=256), bt_h, ar_h)
            S.tt("dve", M1[i2][:], psl(s1, n=2, c1=256), maskA[:, z, :], ALU.mult)
            s3 = slot()
            S.mm(psl(s3), at_h, bt_h)
            S.tt("dve", MabT[i2][:], psl(s3), maskAT[:, z, :], ALU.mult)
            s2 = slot(2)
            S.mm(psl(s2, n=2, c1=256), kt_h, ar_h)
            S.tt("dve", M2[i2][:], psl(s2, n=2, c1=256), maskA[:, z, :], ALU.mult)
            T = Tb[i2]
            S.tt("pool", T[:], M1[i2][:, 0:128], G.ident_b[:], ALU.add)
            P, PT = M1[i2][:, 0:128], MabT[i2][:]
            yield
            for kstep in range(1, 6):
                if kstep < 5:
                    sa = slot()
                    S.mm(psl(sa), PT, P)
                    P2 = Pb[i2][kstep % 2]
                    S.copy("act", P2[:], psl(sa))
                sb_ = slot()
                S.mm(psl(sb_), P, PT)
                P2T = PTb[i2][kstep % 2]
                S.copy("act", P2T[:], psl(sb_))
                if kstep == 1:
                    sx = slot()
                    S.mm(psl(sx, c1=64), M2[i2][:, 0:128], v_v_slice(v_v, hc))
                    S.copy("act", R19.k(h, (slice(None), h, slice(0, 64))), psl(sx, c1=64))
                yield
                sc_ = slot()
                S.mm(psl(sc_), P2T[:], T[:])
                S.tt("dve", T[:], T[:], psl(sc_), ALU.add)
                if kstep < 5:
                    P, PT = P2[:], P2T[:]
            yield
            sw = slot()
            S.mm(psl(sw), T[:], R19.k(h, (slice(None), h, slice(None))))
            S.copy("act", WP[i2][:], psl(sw))
            yield
            sg2 = slot()
            S.mm(psl(sg2, p0=po, p1=po + 64), WP[i2][:, 64:128], M1[i2][:, 128:256])
            S.tt("dve", G_all.k(h, (pr, ec, slice(None))), psl(sg2, p0=po, p1=po + 64), rt_h, ALU.add)
            sy = slot()
            S.mm(psl(sy, c1=64), M1[i2][:, 128:256], WP[i2][:, 0:64], start=True, stop=False)
            S.mm(psl(sy, c1=64), M2[i2][:, 128:256], v_v_slice(v_v, hc), start=False, stop=True)
            S.copy("act", Y0_all.k(h, (slice(None), h, slice(None))), psl(sy, c1=64))
            sds = [slot(), slot()]
            for c in range(2):
                cr = slice(c * 64, (c + 1) * 64)
                S.mm(psl(sds[c], p0=po, p1=po + 64, c1=64), WP[i2][cr, 64:128], Bh[cr, hc])
            for c in range(2):
                S.stt(D_all.k(h, (pr, ec, c, slice(po, po + 64))), G.ident_f[pr, po:po + 64], gC[pr, ec, c:c + 1],
                      psl(sds[c], p0=po, p1=po + 64, c1=64), ALU.mult, ALU.add)
            ses = [slot(), slot()]
            for c in range(2):
                cr = slice(c * 64, (c + 1) * 64)
                S.mm(psl(ses[c], p0=po, p1=po + 64, c1=64), Bh[cr, hc], WP[i2][cr, 0:64], start=True, stop=False)
                S.mm(psl(ses[c], p0=po, p1=po + 64, c1=64), Kh[cr, hc], v_v_slice(v_v, hc, cr), start=False, stop=True)
            for c in range(2):
                S.copy("act", E_all.k(h, (pr, ec, c, slice(po, po + 64))), psl(ses[c], p0=po, p1=po + 64, c1=64))

        pending = list(range(nheads))
        active = []
        rnd, last_admit = 0, -99
        while pending or active:
            if pending and len(active) <= NSET - 2 and (rnd - last_admit >= 4 or not active):
                for _ in range(2):
                    if pending:
                        active.append(head_gen(pending.pop(0)))
                last_admit = rnd
            nxt = []
            for g in active:
                try:
                    next(g)
                    nxt.append(g)
                except StopIteration:
                    pass
            active = nxt
            rnd += 1
        if do_dir < 3:
            return
        for c in chunk_order:
            for ec in range(8):
                pair = [2 * ec, 2 * ec + 1]
                Gv = V(G_all.t[:, ec, c * 64:(c + 1) * 64], [("G_all", h) for h in pair])
                Dv = V(D_all.t[:, ec, c, :], [("D_all", h) for h in pair])
                S.mm(G.psY.v(G.psY.t[c * 64:(c + 1) * 64, ec * 128:(ec + 1) * 128]), Gv, Sb[:, ec, :])
                S.mm(G.psA.v(G.psA.t[:, ec * 128:(ec + 1) * 128]), Dv, Sb[:, ec, :])
            S.tt("dve", Sb[:], G.psA.v(G.psA.t[:, :].rearrange("p (a b) -> p a b", b=128)),
                 V(E_all.t[:, :, c, :], [("E_all", h) for h in range(H)]), ALU.add)

    def v_v_slice(v_v, hc, rows=slice(None)):
        return V(v_v.ap[rows, hc], v_v.toks)

    S.push_scope()
    Wr, Wk, Wv = [S.sbuf(n, [128, 8, D], BF16) for n in ("Wr", "Wk", "Wv")]
    for nm, W in (("Wr", Wr), ("Wk", Wk), ("Wv", Wv)):
        S.dma("sp", W.v(W.t[:, :, :].rearrange("p a b -> p (a b)")), G.wbf[nm][:])
    w1 = S.sbuf("w1", [128, 2, 8, 64], BF16)
    a1 = S.sbuf("a1", [128, 2, 8, 64], BF16)
    g1 = S.sbuf("g1", [128, 8, 128], BF16)
    w2x = S.sbuf("w2x", [65, 2, D], BF16)
    a2x = S.sbuf("a2x", [65, 2, D], BF16)
    g2 = S.sbuf("g2", [128, D], BF16)
    for z in range(2):
        S.dma("pool", w1[:, z, :, :], G.rw_w1.v(G.rw_w1.t[z].rearrange("(kc p) n -> p kc n", p=128)))
        S.dma("pool", a1[:, z, :, :], G.rw_a1.v(G.rw_a1.t[z].rearrange("(kc p) n -> p kc n", p=128)))
        S.dma("pool", w2x[0:64, z, :], G.rw_w2.v(G.rw_w2.t[z]))
        S.dma("pool", w2x[64:65, z, :], G.rw_w0.v(G.rw_w0.t[z:z + 1, :]))
        S.dma("pool", a2x[0:64, z, :], G.rw_a2.v(G.rw_a2.t[z]))
        S.dma("pool", a2x[64:65, z, :], G.rw_a0.v(G.rw_a0.t[z:z + 1, :]))
    S.dma("pool", g1[:], G.rw_g1.v(G.rw_g1.t.rearrange("(kc p) n -> p kc n", p=128)))
    S.dma("pool", g2[:], G.rw_g2[:])
    k_k_bc = load_bc("k_k_bc", G.rw_k_k)
    hTc = S.sbuf("hTc", [128, 8, CTX + 2], BF16)
    hTl = S.sbuf("hTl", [128, 8, LAT + 2], BF16)
    xin = scr2
    wk = {"junk": scr1, "st": S.sbuf("st", [128, 4], F32), "xn": tm[0],
          "fm32": S.sbuf("fm32", [128, 8, 128], F32)}
    dxt = S.sbuf("dxt", [128, 8, 128], F32)
    mix = [S.sbuf(f"mix{i}", [128, 8, 128], BF16) for i in range(2)]
    cach = S.sbuf("cach", [128, 6, D], BF16)
    a0_sb = S.sbuf("a0_sb", [128, D], BF16)
    sg1_v = yfw[:, 0:D]
    hwx = S.sbuf("hwx", [65, 2, 128], BF16)
    hax = S.sbuf("hax", [65, 2, 128], BF16)
    hgs = S.sbuf("hgs", [128, 128], BF16)
    st2 = S.sbuf("st2", [128, 3, 16], F32)
    S.memset("dve", hwx[:], 1.0)
    S.memset("dve", hax[:], 1.0)
    for hT in (hTc, hTl):
        S.memset("pool", hT[:], 0.0)

    mix_ctr = [0]

    def make_mix(hT, c0, j):
        m = mix[mix_ctr[0] % 2]
        mix_ctr[0] += 1
        mu = G.pfm.v(G.pfm.t[:, PV_MU + j * 8:PV_MU + j * 8 + 8].unsqueeze(2).to_broadcast([128, 8, 128]))
        S.tt("pool", wk["fm32"][:], dxt[:], mu, ALU.mult)
        S.tt("pool", m[:], wk["fm32"][:], hT[:, :, c0:c0 + 128], ALU.add)
        return m

    def proj_tm(ps, m, W):
        for n in range(2):
            for kc in range(8):
                S.mm(ps[:, n * 512:(n + 1) * 512], m[:, kc, :], W[:, kc, n * 512:(n + 1) * 512], start=(kc == 0), stop=(kc == 7))

    for b in range(nb):
        for ti in range(NT):
            if ti < 2:
                src, r, hT, t0 = G.ctx.v(G.ctx.t[b, ti * 128:(ti + 1) * 128, :]), 2, hTc, ti * 128
            else:
                src, r, hT, t0 = G.x.v(G.x.t[b, (ti - 2) * 128:(ti - 1) * 128, :]), b, hTl, (ti - 2) * 128
            S.dma("sp", xin[:], src, split=4)
            norm_tile_to_fm(S, G, xin[:], r, G.A1, 0, hT[:, :, t0 + 1:t0 + 129], wk)
        if dbg is not None and "hT" in dbg and b == 0:
            S.dma("sp", dbg["hT"][:], hTl[:])
        S.memset("dve", Sb[:], 0.0)
        for ti in range(nt0):
            hT, t0 = (hTc, ti * 128) if ti < 2 else (hTl, (ti - 2) * 128)
            c0 = t0 + 1
            S.tt("dve", dxt[:], hT[:, :, c0 - 1:c0 + 127], hT[:, :, c0 + 1:c0 + 129], ALU.add)
            S.stt(dxt[:], dxt[:], 0.5, hT[:, :, c0:c0 + 128], ALU.mult, ALU.subtract)
            if fl < 1:
                continue
            m = make_mix(hT, c0, 0)
            proj_tm(G.psA, m, Wr)
            S.copy("act", cach[:, 0, :], G.psA[:])
            if fl < 2:
                continue
            m = make_mix(hT, c0, 2)
            proj_tm(G.psY, m, Wv)
            S.copy("act", cach[:, 2, :], G.psY[:])
            if fl < 3:
                continue
            m = make_mix(hT, c0, 4)
            for z in range(2):
                sl_ = slot()
                for kc in range(8):
                    S.mm(psl(sl_, p1=64), a1[:, z, kc, :], m[:, kc, :], start=(kc == 0), stop=(kc == 7))
                S.copy("act", hax[0:64, z, :], psl(sl_, p1=64))
            for z in range(2):
                ps = G.psA if z == 0 else G.psY
                for n in range(2):
                    S.mm(ps[:, n * 512:(n + 1) * 512], hax[:, z, :], a2x[:, z, n * 512:(n + 1) * 512])
                S.act(a0_sb[:] if z == 0 else cach[:, 4, :], ps[:], AF.Sigmoid)
            if fl < 4:
                continue
            m = make_mix(hT, c0, 3)
            for z in range(2):
                sl_ = slot()
                for kc in range(8):
                    S.mm(psl(sl_, p1=64), w1[:, z, kc, :], m[:, kc, :], start=(kc == 0), stop=(kc == 7))
                S.act(hwx[0:64, z, :], psl(sl_, p1=64), AF.Tanh)
            for z in range(2):
                ps = G.psA if z == 0 else G.psY
                for n in range(2):
                    S.mm(ps[:, n * 512:(n + 1) * 512], hwx[:, z, :], w2x[:, z, n * 512:(n + 1) * 512])
                S.act(sg_sb[:] if z == 0 else sg1_v, ps[:], AF.Sigmoid)
            if fl < 5:
                continue
            m = make_mix(hT, c0, 5)
            sl_ = slot()
            for kc in range(8):
                S.mm(psl(sl_), g1[:, kc, :], m[:, kc, :], start=(kc == 0), stop=(kc == 7))
            S.act(hgs[:], psl(sl_), AF.Sigmoid)
            for n in range(2):
                S.mm(G.psY[:, n * 512:(n + 1) * 512], hgs[:], g2[:, n * 512:(n + 1) * 512])
            S.copy("act", cach[:, 5, :], G.psY[:])
            if fl < 6:
                continue
            m = make_mix(hT, c0, 1)
            proj_tm(G.psA, m, Wk)
            S.copy("act", cach[:, 1, :], G.psA[:])
            if fl < 6.1:
                continue
            S.tt("dve", scr1[:], G.psA[:], k_k_bc[:], ALU.mult)
            if fl < 6.2:
                continue
            S.act(scr2[:], scr1[:], AF.Square)
            S.reduce(st2[:, 0, :], scr2.v(scr2.t[:, :].rearrange("p (h n) -> p h n", n=64)))
            if fl < 6.3:
                continue
            S.ts("dve", st2[:, 1, :], st2[:, 0, :], 1e-12, None, op0=ALU.add)
            S.act(st2[:, 1, :], st2[:, 1, :], AF.Sqrt)
            S.recip(st2[:, 2, :], st2[:, 1, :])
            if fl < 6.4:
                continue
            S.tt("dve", cach.v(cach.t[:, 3, :].rearrange("p (h n) -> p h n", n=64)),
                 scr1.v(scr1.t[:, :].rearrange("p (h n) -> p h n", n=64)),
                 st2.v(st2.t[:, 2, :].unsqueeze(2).to_broadcast([128, 16, 64])), ALU.mult)
            if fl < 7:
                continue
            S.dma("sp", cache_d.v(cache_d.t[b, ti].rearrange("p (a n) -> p a n", n=D)), cach[:])
            S.dma("sp", sg1_d.v(sg1_d.t[b, ti]), sg1_v)
            if do_dir:
                dir_part(0, cach[:, 0, :], G.psA[:], cach[:, 2, :], cach[:, 3, :], a0_sb[:], (0, 1))
            S.tt("dve", yfw[:, 0:D], G.psY[:],
                 V(Y0_all.t[:, :, :].rearrange("p h n -> p (h n)"), [("Y0_all", h) for h in range(H)]), ALU.add)
            S.copy("pool", yfw[:, D:D + 16], bon[:, 0, :])
            S.dma("sp", yf_d.v(yf_d.t[b, ti]), yfw[:])
    S.pop_scope()

    S.push_scope()
    Wo = S.sbuf("Wo", [128, 8, D], BF16)
    S.dma("sp", Wo.v(Wo.t[:, :, :].rearrange("p a b -> p (a b)")), G.wbf["rwWo"][:])
    lnx_g_bc = load_bc("lnx_g_bc", G.rw_lnx_g)
    lnx_b_bc = load_bc("lnx_b_bc", G.rw_lnx_b)
    gate_bc = S.sbuf("gate_bc", [128, D], F32)
    cach = S.sbuf("cach1", [128, 6, D], BF16)
    xres = S.sbuf("xres", [128, D], F32)
    pre = S.sbuf("pre", [128, D], BF16)
    preT = S.sbuf("preT", [128, 8, 128], BF16)
    st3 = S.sbuf("st3", [128, 4, 16], F32)
    for b in range(nb):
        S.memset("dve", Sb[:], 0.0)
        order = ([1, 0] + list(range(NT - 1, 1, -1)))[:nt1]
        cur_r = None
        for ti in order:
            r = 2 if ti < 2 else b
            if r != cur_r:
                S.dma("sp", gate_bc[:], G.gates_d.v(G.gates_d.t[0, r:r + 1, :].partition_broadcast(128)))
                cur_r = r
            S.dma("sp", cach[:], cache_d.v(cache_d.t[b, ti].rearrange("p (a n) -> p a n", n=D)))
            S.dma("sp", sg_sb[:], sg1_d.v(sg1_d.t[b, ti]))
            S.dma("sp", yfw[:], yf_d.v(yf_d.t[b, ti]))
            dir_part(1, cach[:, 0, :], cach[:, 1, :], cach[:, 2, :], cach[:, 3, :], cach[:, 4, :], (1, 0))
            S.tt("dve", scr1[:], G.psY[:], V(Y0_all.t[:, :, :].rearrange("p h n -> p (h n)"), [("Y0_all", h) for h in range(H)]), ALU.add)
            S.tt("pool", scr1[:], scr1[:], yfw[:, 0:D], ALU.add)
            y3 = scr1.v(scr1.t[:, :].rearrange("p (h n) -> p h n", n=64))
            S.reduce(st3[:, 0, :], y3)
            S.ts("dve", st3[:, 0, :], st3[:, 0, :], 1.0 / 64, None, op0=ALU.mult)
            S.tt("dve", y3, y3, st3.v(st3.t[:, 0, :].unsqueeze(2).to_broadcast([128, 16, 64])), ALU.subtract)
            S.act(scr2[:], scr1[:], AF.Square)
            S.reduce(st3[:, 1, :], scr2.v(scr2.t[:, :].rearrange("p (h n) -> p h n", n=64)))
            S.ts("dve", st3[:, 1, :], st3[:, 1, :], 1.0 / 64, LNX_EPS, op0=ALU.mult, op1=ALU.add)
            S.act(st3[:, 1, :], st3[:, 1, :], AF.Sqrt)
            S.recip(st3[:, 2, :], st3[:, 1, :])
            S.tt("dve", y3, y3, st3.v(st3.t[:, 2, :].unsqueeze(2).to_broadcast([128, 16, 64])), ALU.mult)
            S.tt("pool", scr1[:], scr1[:], lnx_g_bc[:], ALU.mult)
            S.tt("pool", scr1[:], scr1[:], lnx_b_bc[:], ALU.add)
            S.tt("dve", st3[:, 3, :], bon[:, 1, :], yfw[:, D:D + 16], ALU.add)
            S.tt("dve", scr2.v(scr2.t[:, :].rearrange("p (h n) -> p h n", n=64)),
                 cach.v(cach.t[:, 2, :].rearrange("p (h n) -> p h n", n=64)),
                 st3.v(st3.t[:, 3, :].unsqueeze(2).to_broadcast([128, 16, 64])), ALU.mult)
            S.tt("pool", scr1[:], scr1[:], scr2[:], ALU.add)
            S.tt("pool", pre[:], scr1[:], cach[:, 5, :], ALU.mult)
            transposes_to(pre, preT[:])
            for n in range(2):
                for kc in range(8):
                    S.mm(G.psA[:, n * 512:(n + 1) * 512], preT[:, kc, :], Wo[:, kc, n * 512:(n + 1) * 512], start=(kc == 0), stop=(kc == 7))
            if ti < 2:
                xsrc = G.ctx.v(G.ctx.t[b, ti * 128:(ti + 1) * 128, :])
            else:
                xsrc = G.x.v(G.x.t[b, (ti - 2) * 128:(ti - 1) * 128, :])
            S.dma("sp", xres[:], xsrc)
            S.tt("dve", scr2[:], G.psA[:], gate_bc[:], ALU.mult)
            S.tt("pool", xres[:], xres[:], scr2[:], ALU.add)
            S.dma("sp", x1_d.v(x1_d.t[b, ti * 128:(ti + 1) * 128, :]), xres[:])
    S.pop_scope()
    S.pop_scope()

def moe_phase(S, G, li, xin_d, tiles, xout_fn, st_tiles, npairs=16, dbg=None):
    L = f"e{li}"
    S.push_scope()
    ysub = [S.psum(f"ysub{i}{L}", [128, 1024], F32) for i in range(2)]
    psG = [S.psum(f"psG{i}{L}", [128, 512], F32) for i in range(2)]
    psU = [S.psum(f"psU{i}{L}", [128, 512], F32) for i in range(2)]
    G.psA = ysub[0]
    STK = st_tiles * 128
    h2T = S.sbuf(f"h2T{L}", [128, 8, STK], BF16)
    y_acc = S.sbuf(f"yacc{L}", [128, st_tiles, D], F32)
    gatesT = S.sbuf(f"gatesT{L}", [32, STK], BF16)
    sel = S.sbuf(f"sel{L}", [32, 32, 128], BF16)
    S.dma("pool", sel[:], G.sel_d.v(G.sel_d.t[:, :].rearrange("p (a b) -> p a b", b=128)))
    Wrt = S.sbuf(f"Wrt{L}", [128, 8, 36], F32)
    S.dma("sp", Wrt[:], G.moe_router.v(G.moe_router.t[li].rearrange("(kc p) n -> p kc n", p=128)))
    rb = S.sbuf(f"rb{L}", [1, 36], F32)
    S.dma("sp", rb[:], G.moe_router_b.v(G.moe_router_b.t[li]))
    gate_bc = S.sbuf(f"gbc{L}", [128, 3, D], F32)
    rs_used = sorted(set(t[2] for t in tiles))
    for r in rs_used:
        S.dma("sp", gate_bc[:, r, :], G.gates_d.v(G.gates_d.t[1, r:r + 1, :].partition_broadcast(128)))
    Wg = [[S.sbuf(f"Wg{i}{e}{L}", [128, 8, DFF], BF16) for e in range(2)] for i in range(2)]
    Wu = [[S.sbuf(f"Wu{i}{e}{L}", [128, 8, DFF], BF16) for e in range(2)] for i in range(2)]
    Wd = [[S.sbuf(f"Wd{i}{e}{L}", [128, 2, D], BF16) for e in range(2)] for i in range(2)]
    NS1 = 2
    xin_s = [S.sbuf(f"xin{i}{L}", [128, D], F32) for i in range(NS1)]
    junk_s = [S.sbuf(f"junk{i}{L}", [128, D], F32) for i in range(NS1)]
    wk_s = [{"junk": junk_s[i], "st": S.sbuf(f"st{i}{L}", [128, 4], F32), "xn32": S.sbuf(f"xn32{i}{L}", [128, D], F32),
             "fm32": S.sbuf(f"fm32{i}{L}", [128, 8, 128], F32)} for i in range(NS1)]
    h32_s = [S.sbuf(f"h32{i}{L}", [128, 8, 128], F32) for i in range(NS1)]
    lg_s = [S.sbuf(f"lg{i}{L}", [128, 36], F32) for i in range(NS1)]
    sm_s = [S.sbuf(f"sm{i}{L}", [128, 64], F32) for i in range(NS1)]
    g32_s = [S.sbuf(f"g32{i}{L}", [128, 32], F32) for i in range(NS1)]
    xin3 = S.sbuf(f"xin3{L}", [128, D], F32)
    out3 = S.sbuf(f"out3{L}", [128, D], F32)
    s_sb = [S.sbuf(f"s_sb{i}{L}", [128, 256], F32) for i in range(2)]
    t_sb = [S.sbuf(f"t_sb{i}{L}", [128, 256], F32) for i in range(2)]
    hidT = [S.sbuf(f"hidT{i}{L}", [128, 256], BF16) for i in range(2)]

    def load_pair(p, buf):
        for e in range(2):
            eg = p * 2 + e
            for W, nm in ((Wg, "g"), (Wu, "u"), (Wd, "d")):
                t = W[buf][e]
                S.dma("sp", t.v(t.t[:, :, :].rearrange("p a b -> p (a b)")), G.wbf[f"{nm}{li}_{eg}"][:])

    n_super = len(tiles) // st_tiles
    assert n_super * st_tiles == len(tiles) and st_tiles % 2 == 0
    for su in range(n_super):
        stl = tiles[su * st_tiles:(su + 1) * st_tiles]
        load_pair(0, 0)
        def step1_gen(j, b, row0, r, s):
            xin, wk, h32, lg, sm, g32 = xin_s[s], wk_s[s], h32_s[s], lg_s[s], sm_s[s], g32_s[s]
            S.dma("sp", xin[:], xin_d.v(xin_d.t[b, row0:row0 + 128, :]), split=4)
            yield
            yield from norm_tile_gen(S, G, xin[:], r, G.A2, 24, h2T[:, :, j * 128:(j + 1) * 128], wk, h32[:], ysub[s])
            yield
            psr = (psG[0] if s == 0 else psU[0])[:, 0:36]
            for kc in range(8):
                S.mm(psr, h32[:, kc, :], Wrt[:, kc, :], start=(kc == 0), stop=False)
            S.mm(psr, G.ones_f[0:1, :], rb[:], start=False, stop=True)
            S.copy("dve", lg[:], psr)
            yield
            c = lambda i, n=1: sm[:, i:i + n]
            S.reduce(c(0), lg[:, 0:4], op=ALU.max)
            yield
            S.ts("dve", c(1), c(0), -1.0, None, op0=ALU.mult)
            yield
            S.ts("dve", c(4, 4), lg[:, 0:4], c(0), None, op0=ALU.is_ge)
            yield
            S.act(c(8, 4), lg[:, 0:4], AF.Exp, bias=c(1), accum_out=c(2))
            yield
            S.recip(c(3), c(2))
            yield
            S.ts("dve", c(16, 8), lg[:, 4:12], c(4), None, op0=ALU.mult)
            yield
            for g in range(1, 4):
                S.stt(c(16, 8), lg[:, 4 + 8 * g:12 + 8 * g], c(4 + g), c(16, 8), ALU.mult, ALU.add)
                yield
            S.reduce(c(12), c(16, 8), op=ALU.max)
            yield
            S.ts("dve", c(24, 8), c(16, 8), c(12), None, op0=ALU.is_ge)
            yield
            S.stt(c(32, 8), c(24, 8), -1e30, c(16, 8), ALU.mult, ALU.add)
            yield
            S.reduce(c(13), c(32, 8), op=ALU.max)
            yield
            S.ts("dve", c(40, 8), c(32, 8), c(13), None, op0=ALU.is_ge)
            yield
            S.tt("dve", c(14), c(13), c(12), ALU.subtract)
            yield
            S.act(c(15), c(14), AF.Exp)
            yield
            S.ts("dve", c(48), c(15), 1.0, None, op0=ALU.add)
            yield
            S.recip(c(49), c(48))
            yield
            S.tt("dve", c(50), c(49), c(3), ALU.mult)
            yield
            S.tt("dve", c(51), c(50), c(15), ALU.mult)
            yield
            S.ts("dve", c(52, 8), c(24, 8), c(50), None, op0=ALU.mult)
            yield
            S.stt(c(52, 8), c(40, 8), c(51), c(52, 8), ALU.mult, ALU.add)
            yield
            for g in range(4):
                S.ts("dve", g32[:, g * 8:(g + 1) * 8], c(52, 8), c(4 + g), None, op0=ALU.mult)
                yield
            pst = (psG[1] if s == 0 else psU[1])[0:32, 0:128]
            S.tr(pst, g32[:], G.ident_f[:])
            S.copy("dve", gatesT[:, j * 128:(j + 1) * 128], pst)
            if dbg is not None and "gates" in dbg and su == 0:
                S.dma("sp", dbg["gates"].v(dbg["gates"].t[j]), g32[:])

        pend1 = [step1_gen(j, b, row0, r, j % NS1) for j, (b, row0, r) in enumerate(stl)]
        act1 = []
        rnd, last1 = 0, -99
        while pend1 or act1:
            if pend1 and len(act1) < NS1 and (rnd - last1 >= 20 or not act1):
                act1.append(pend1.pop(0))
                last1 = rnd
            nxt = []
            for g_ in act1:
                try:
                    next(g_)
                    nxt.append(g_)
                except StopIteration:
                    pass
            act1 = nxt
            rnd += 1
        G.psA = ysub[0]
        items = [(p, t2, e, fc) for p in range(npairs) for t2 in range(st_tiles // 2) for e in range(2) for fc in range(2)]

        def emit_gu(idx):
            p, t2, e, fc = items[idx]
            buf, eg, ib = p % 2, p * 2 + e, idx % 2
            tok = slice(t2 * 256, (t2 + 1) * 256)
            pg, pu = psG[ib], psU[ib]
            for kc in range(8):
                S.mm(pg[:, 0:256], Wg[buf][e][:, kc, fc * 128:(fc + 1) * 128], h2T[:, kc, tok], start=(kc == 0), stop=(kc == 7))
            for kc in range(8):
                S.mm(pu[:, 0:256], Wu[buf][e][:, kc, fc * 128:(fc + 1) * 128], h2T[:, kc, tok], start=(kc == 0), stop=(kc == 7))
            S.mm(pu[:, 256:512], sel[:, eg, :], gatesT[:, tok])
            S.act(s_sb[ib][:], pg[:, 0:256], AF.Silu)
            S.tt("dve", t_sb[ib][:], s_sb[ib][:], pu[:, 0:256], ALU.mult)
            S.tt("dve", hidT[ib][:], t_sb[ib][:], pu[:, 256:512], ALU.mult)

        def emit_down(idx):
            p, t2, e, fc = items[idx]
            buf, ib, it = p % 2, idx % 2, e * 2 + fc
            for ts_ in range(2):
                for n in range(2):
                    S.mm(ysub[ts_][:, n * 512:(n + 1) * 512], hidT[ib][:, ts_ * 128:(ts_ + 1) * 128],
                         Wd[buf][e][:, fc, n * 512:(n + 1) * 512], start=(it == 0), stop=(it == 3))
            if it == 3:
                for ts_ in range(2):
                    j = t2 * 2 + ts_
                    if p == 0:
                        S.copy("dve", y_acc[:, j, :], ysub[ts_][:])
                    else:
                        S.tt("dve", y_acc[:, j, :], y_acc[:, j, :], ysub[ts_][:], ALU.add)
                    if p == npairs - 1:
                        b, row0, r = stl[j]
                        S.dma("sp", xin3[:], xin_d.v(xin_d.t[b, row0:row0 + 128, :]))
                        S.tt("pool", out3[:], y_acc[:, j, :], gate_bc[:, r, :], ALU.mult)
                        S.tt("pool", out3[:], out3[:], xin3[:], ALU.add)
                        S.dma("sp", xout_fn(b, row0), out3[:])

        for idx in range(len(items)):
            emit_gu(idx)
            if idx > 0:
                emit_down(idx - 1)
            p, t2, e, fc = items[idx]
            if t2 == 0 and e == 0 and fc == 0 and p + 1 < npairs:
                load_pair(p + 1, (p + 1) % 2)
        emit_down(len(items) - 1)
    S.pop_scope()

LAM_INIT1 = 0.8 - 0.6 * float(np.exp(-0.3 * 1))


def attn_phase(S, G, x2_d, x3_d, nb=NB, nqt=4, nh=8):
    li = 1
    NKT = NT
    S.push_scope()
    psA = S.psum("psA_a", [128, 1024], F32)
    psT = S.psum("psT_a", [128, 1024], BF16)
    psQ = [S.psum(f"psQ{i}_a", [128, 512], F32) for i in range(3)]
    psO = [S.psum(f"psO{i}_a", [128, 512], F32) for i in range(2)]
    G.psA, G.psT = psA, psT
    KT_all = S.sbuf("KT_all", [128, 8, TOK], BF16)
    QT_all = S.sbuf("QT_all", [128, 8, LAT], BF16)
    V_all = S.sbuf("V_all", [128, NKT, 8, 130], BF16)
    S.memset("pool", V_all[:], 1.0)
    gq_bc = S.sbuf("gq_bc", [128, 64], F32)
    gk_bc = S.sbuf("gk_bc", [128, 64], F32)
    sg_bc = S.sbuf("sg_bc", [128, 128], F32)
    S.dma("sp", gq_bc[:], G.da_q_norm_g.v(G.da_q_norm_g.t[0:1, :].partition_broadcast(128)))
    S.dma("sp", gk_bc[:], G.da_k_norm_g.v(G.da_k_norm_g.t[0:1, :].partition_broadcast(128)))
    S.dma("sp", sg_bc[:], G.da_subln_g.v(G.da_subln_g.t[0:1, :].partition_broadcast(128)))
    S.ts("dve", sg_bc[:], sg_bc[:], 1.0 - LAM_INIT1, None, op0=ALU.mult)
    lamv = S.sbuf("lamv", [128, 4, 64], F32)
    lsm = S.sbuf("lsm", [128, 8], F32)
    for i in range(4):
        S.dma("sp", lamv[:, i, :], G.da_lam.v(G.da_lam.t[i:i + 1, :].partition_broadcast(128)))
    S.tt("dve", lamv[:, 0, :], lamv[:, 0, :], lamv[:, 1, :], ALU.mult)
    S.tt("dve", lamv[:, 2, :], lamv[:, 2, :], lamv[:, 3, :], ALU.mult)
    S.reduce(lsm[:, 0:1], lamv[:, 0, :])
    S.reduce(lsm[:, 1:2], lamv[:, 2, :])
    S.act(lsm[:, 2:4], lsm[:, 0:2], AF.Exp)
    S.tt("dve", lsm[:, 4:5], lsm[:, 3:4], lsm[:, 2:3], ALU.subtract)
    S.ts("dve", lsm[:, 5:6], lsm[:, 4:5], -LAM_INIT1, None, op0=ALU.add)
    neglam = lsm[:, 5:6]
    junk = S.sbuf("junk_a", [128, D], F32)
    scrq = S.sbuf("scrq", [128, D], F32)
    xin = S.sbuf("xin_a", [128, D], F32)
    st = S.sbuf("st_a", [128, 4], F32)
    st16 = S.sbuf("st16_a", [128, 3, 16], F32)
    for b in range(nb):
        S.push_scope()
        Wqkv = S.sbuf(f"Wqkv{b}", [128, 8, 3 * D], BF16)
        S.dma("sp", Wqkv.v(Wqkv.t[:, :, :].rearrange("p a b -> p (a b)")), G.wbf["Wqkv"][:])
        hT_t = S.sbuf(f"hT_t{b}", [128, 8, 128], BF16)
        outq = S.sbuf(f"outq{b}", [128, D], BF16)
        wk = {"junk": junk, "st": st, "xn": S.sbuf(f"xn_a{b}", [128, D], BF16), "fm32": S.sbuf(f"fm32_a{b}", [128, 8, 128], F32)}
        cs_t = S.sbuf(f"cs_t{b}", [128, 2, 32], F32)
        tmpa = S.sbuf(f"tmpa{b}", [128, 512], F32)
        tmpb = S.sbuf(f"tmpb{b}", [128, 512], F32)

        qs_ = [dict(scrq=scrq, outq=outq, tmpa=tmpa, tmpb=tmpb, st16=st16, junk=junk)]
        qs_.append(dict(scrq=S.sbuf(f"scrq2_{b}", [128, D], F32), outq=S.sbuf(f"outq2_{b}", [128, D], BF16),
                        tmpa=S.sbuf(f"tmpa2_{b}", [128, 512], F32), tmpb=S.sbuf(f"tmpb2_{b}", [128, 512], F32),
                        st16=S.sbuf(f"st16_2_{b}", [128, 3, 16], F32), junk=S.sbuf(f"junk2_{b}", [128, D], F32)))
        wk["junk"] = S.sbuf(f"junkn_{b}", [128, D], F32)

        def qk_part1(X):
            s16, sq, jk = X["st16"], X["scrq"], X["junk"]
            S.act(jk[:], psA[:], AF.Square)
            S.reduce(s16[:, 0, :], jk.v(jk.t[:, :].rearrange("p (g n) -> p g n", n=64)))
            S.ts("dve", s16[:, 1, :], s16[:, 0, :], 1.0 / 64, EPS, op0=ALU.mult, op1=ALU.add)
            S.act(s16[:, 1, :], s16[:, 1, :], AF.Sqrt)
            S.recip(s16[:, 2, :], s16[:, 1, :])
            S.tt("dve", sq.v(sq.t[:, :].rearrange("p (g n) -> p g n", n=64)),
                 psA.v(psA.t[:, :].rearrange("p (g n) -> p g n", n=64)),
                 s16.v(s16.t[:, 2, :].unsqueeze(2).to_broadcast([128, 16, 64])), ALU.mult)

        def qk_part2a(X, gain_bc, rope):
            sq, oq, tmpa_, tmpb_ = X["scrq"], X["outq"], X["tmpa"], X["tmpb"]
            gv = gain_bc.v(gain_bc.t[:, :].unsqueeze(1).to_broadcast([128, 16, 64]))
            if not rope:
                S.tt("pool", oq.v(oq.t[:, :].rearrange("p (g n) -> p g n", n=64)),
                     sq.v(sq.t[:, :].rearrange("p (g n) -> p g n", n=64)), gv, ALU.mult)
                return
            S.tt("pool", sq.v(sq.t[:, :].rearrange("p (g n) -> p g n", n=64)),
                 sq.v(sq.t[:, :].rearrange("p (g n) -> p g n", n=64)), gv, ALU.mult)
            x5 = sq.t[:, :].rearrange("p (g a h f) -> p g a h f", g=16, a=2, h=2)
            o5 = oq.t[:, :].rearrange("p (g a h f) -> p g a h f", g=16, a=2, h=2)
            x1, x2 = sq.v(x5[:, :, :, 0, :]), sq.v(x5[:, :, :, 1, :])
            o1, o2 = oq.v(o5[:, :, :, 0, :]), oq.v(o5[:, :, :, 1, :])
            cv = cs_t.v(cs_t.t[:, 0, :].rearrange("p (a f) -> p a f", a=2).unsqueeze(1).to_broadcast([128, 16, 2, 16]))
            sv = cs_t.v(cs_t.t[:, 1, :].rearrange("p (a f) -> p a f", a=2).unsqueeze(1).to_broadcast([128, 16, 2, 16]))
            ta = tmpa_.v(tmpa_.t[:, :].rearrange("p (g a f) -> p g a f", g=16, a=2))
            tb = tmpb_.v(tmpb_.t[:, :].rearrange("p (g a f) -> p g a f", g=16, a=2))
            S.tt("dve", ta, x1, cv, ALU.mult)
            S.tt("pool", tb, x2, sv, ALU.mult)
            S.tt("dve", o1, ta, tb, ALU.subtract)
            S.tt("dve", ta, x1, sv, ALU.mult)
            S.tt("pool", tb, x2, cv, ALU.mult)
            S.tt("pool", o2, ta, tb, ALU.add)

        def qk_part2b(X, dst_fm):
            oq = X["outq"]
            for ec in range(8):
                S.tr(psT[:, ec * 128:(ec + 1) * 128], oq[:, ec * 128:(ec + 1) * 128], G.ident_b[:])
            S.copy("act", dst_fm, psT.v(psT.t[:, :].rearrange("p (a b) -> p a b", b=128)))

        def proj(c0):
            for n in range(2):
                for kc in range(8):
                    S.mm(psA[:, n * 512:(n + 1) * 512], hT_t[:, kc, :], Wqkv[:, kc, c0 + n * 512:c0 + (n + 1) * 512], start=(kc == 0), stop=(kc == 7))

        def load_norm(ti):
            r = 2 if ti < 2 else b
            S.dma("sp", xin[:], x2_d.v(x2_d.t[b, ti * 128:(ti + 1) * 128, :]), split=4)
            norm_tile_to_fm(S, G, xin[:], r, G.A1, 0, hT_t[:], wk)

        load_norm(0)
        pend_q = None
        for ti in range(NT):
            lat = ti >= 2
            if lat:
                t0 = (ti - 2) * 128
                S.dma("sp", cs_t[:, 0, :], G.rope_cos_d.v(G.rope_cos_d.t[t0:t0 + 128, :]))
                S.dma("sp", cs_t[:, 1, :], G.rope_sin_d.v(G.rope_sin_d.t[t0:t0 + 128, :]))
            proj(D)
            qk_part1(qs_[0])
            if pend_q is not None:
                qk_part2b(qs_[1], pend_q)
                pend_q = None
            proj(2 * D)
            S.copy("act", V_all[:, ti, :, 0:128], psA.v(psA.t[:, :].rearrange("p (h n) -> p h n", n=128)))
            if lat:
                proj(0)
            qk_part2a(qs_[0], gk_bc, lat)
            qk_part2b(qs_[0], KT_all[:, :, ti * 128:(ti + 1) * 128])
            if lat:
                qk_part1(qs_[1])
            if ti + 1 < NT:
                load_norm(ti + 1)
            if lat:
                qk_part2a(qs_[1], gq_bc, True)
                pend_q = QT_all[:, :, t0:t0 + 128]
        if pend_q is not None:
            qk_part2b(qs_[1], pend_q)
        S.pop_scope()
        S.push_scope()
        Wo = S.sbuf(f"Wo_a{b}", [128, 8, D], BF16)
        S.dma("sp", Wo.v(Wo.t[:, :, :].rearrange("p a b -> p (a b)")), G.wbf["daWo"][:])
        gate_bc = S.sbuf(f"gate_a{b}", [128, D], F32)
        S.dma("sp", gate_bc[:], G.gates_d.v(G.gates_d.t[0, b:b + 1, :].partition_broadcast(128)))
        O_all = S.sbuf(f"O_all{b}", [128, 4, D], F32)
        pT = [S.sbuf(f"pT{i}_{b}", [128, 512], BF16) for i in range(3)]
        rec = S.sbuf(f"rec{b}", [128, 8], F32)
        pre = S.sbuf(f"pre_a{b}", [128, D], BF16)
        preT = S.sbuf(f"preT_a{b}", [128, 8, 128], BF16)
        st8 = S.sbuf(f"st8_{b}", [128, 3, 8], F32)
        aitems = [(qt, h, m, kt) for qt in range(nqt) for h in range(nh) for m in range(2) for kt in range(NKT)]

        def emit_qk(i):
            qt, h, m, kt = aitems[i]
            pr = slice(m * 64, m * 64 + 64)
            ps = psQ[i % 3]
            S.mm(ps[:], KT_all[pr, h, kt * 128:(kt + 1) * 128], QT_all[pr, h, qt * 512:(qt + 1) * 512])
            S.act(pT[i % 3][:], ps[:], AF.Exp, scale=0.125)

        def emit_pv(i):
            qt, h, m, kt = aitems[i]
            for qs in range(4):
                acc = psO[qs // 2][:, (qs % 2) * 256:(qs % 2) * 256 + 129]
                S.mm(acc, pT[i % 3][:, qs * 128:(qs + 1) * 128], V_all[:, kt, h, 0:129],
                     start=(kt == 0 and qs % 2 == 0), stop=(kt == NKT - 1), skip_group_check=True)
            if kt != NKT - 1:
                return
            for qs in range(4):
                c0 = (qs % 2) * 256
                S.recip(rec[:, qs:qs + 1], psO[qs // 2][:, c0 + 128:c0 + 129])
                if m == 0:
                    S.ts("dve", O_all[:, qs, h * 128:(h + 1) * 128], psO[qs // 2][:, c0:c0 + 128], rec[:, qs:qs + 1], None, op0=ALU.mult)
                else:
                    S.tt("dve", rec[:, 4 + qs:5 + qs], rec[:, qs:qs + 1], neglam, ALU.mult)
                    S.stt(O_all[:, qs, h * 128:(h + 1) * 128], psO[qs // 2][:, c0:c0 + 128], rec[:, 4 + qs:5 + qs],
                          O_all[:, qs, h * 128:(h + 1) * 128], ALU.mult, ALU.add)
            if not (h == nh - 1 and m == 1):
                return
            for qs in range(4):
                O3 = O_all.v(O_all.t[:, qs, :].rearrange("p (h n) -> p h n", n=128))
                S.act(junk[:], O_all[:, qs, :], AF.Square)
                S.reduce(st8[:, 0, :], junk.v(junk.t[:, :].rearrange("p (h n) -> p h n", n=128)))
                S.ts("dve", st8[:, 1, :], st8[:, 0, :], 1.0 / 128, EPS, op0=ALU.mult, op1=ALU.add)
                S.act(st8[:, 1, :], st8[:, 1, :], AF.Sqrt)
                S.recip(st8[:, 2, :], st8[:, 1, :])
                S.tt("dve", O3, O3, st8.v(st8.t[:, 2, :].unsqueeze(2).to_broadcast([128, 8, 128])), ALU.mult)
                S.tt("pool", pre.v(pre.t[:, :].rearrange("p (h n) -> p h n", n=128)), O3,
                     sg_bc.v(sg_bc.t[:, :].unsqueeze(1).to_broadcast([128, 8, 128])), ALU.mult)
                for ec in range(8):
                    S.tr(psT[:, ec * 128:(ec + 1) * 128], pre[:, ec * 128:(ec + 1) * 128], G.ident_b[:])
                S.copy("act", preT[:], psT.v(psT.t[:, :].rearrange("p (a b) -> p a b", b=128)))
                for n in range(2):
                    for kc in range(8):
                        S.mm(psA[:, n * 512:(n + 1) * 512], preT[:, kc, :], Wo[:, kc, n * 512:(n + 1) * 512], start=(kc == 0), stop=(kc == 7))
                row = (qt * 4 + qs) * 128
                S.dma("sp", xin[:], x2_d.v(x2_d.t[b, CTX + row:CTX + row + 128, :]))
                S.tt("dve", scrq[:], psA[:], gate_bc[:], ALU.mult)
                S.tt("pool", xin[:], xin[:], scrq[:], ALU.add)
                S.dma("sp", x3_d.v(x3_d.t[b, row:row + 128, :]), xin[:])

        SK = 2
        for i in range(len(aitems) + SK):
            if i < len(aitems):
                emit_qk(i)
            if i >= SK:
                emit_pv(i - SK)
        S.pop_scope()
    S.pop_scope()

def build(cfg):
    nc = bass.Bass("TRN2", target_bir_lowering=False)
    S = Sched(nc)
    G = Ctx()
    setup_common(S, G, need=cfg.get("need", ("rwkv", "da", "moe")))
    outs = []
    dbg = {}
    stop = cfg.get("stop", "end")
    kind_x1 = "ExternalOutput" if stop == "rwkv" else "Internal"
    x1_d = S.dram("x1_d", [NB, TOK, D], F32, kind=kind_x1)
    if cfg.get("dbg_hT"):
        dbg["hT"] = S.dram("dbg_hT", [128, 8, LAT + 2], BF16, kind="ExternalOutput")
    wcast_phase(S, G, cfg.get("need", ("rwkv", "da", "moe")))
    if cfg.get("attn_in_ext"):
        G.x2_ext = S.dram("x2_ext", [NB, TOK, D], F32, kind="ExternalInput")
    if cfg.get("moe_in_ext"):
        G.x1_ext = S.dram("x1_ext", [NB, TOK, D], F32, kind="ExternalInput")
    if not cfg.get("skip_l0"):
        mod_phase(S, G, 0)
    if cfg.get("dbg_mod"):
        dm = S.dram("dbg_modT", [128, 48 * 4], F32, kind="ExternalOutput")
        S.dma("sp", dm[:], G.modT.v(G.modT.t[:, :, :].rearrange("p a b -> p (a b)")))
        dg = S.dram("dbg_gates", [2, 3, D], F32, kind="ExternalOutput")
        S.dma("sp", dg[:], G.gates_d[:])
    if stop == "mod":
        S.barrier()
        S.emit()
        return nc, S
    if not cfg.get("skip_rwkv") and not cfg.get("skip_l0"):
        rwkv_phase(S, G, x1_d, dbg=dbg, nb=cfg.get("nb", NB), **cfg.get("rw", {}))
    if stop == "rwkv":
        S.barrier()
        S.emit()
        return nc, S
    x2_d = S.dram("x2_d", [NB, TOK, D], F32, kind="ExternalOutput" if stop == "moe0" else "Internal")
    if not cfg.get("skip_l0"):
        moe0 = True
    else:
        moe0 = False
    tiles0 = [(b, ti * 128, 2 if ti < 2 else b) for b in range(NB) for ti in range(NT)]
    if cfg.get("dbg_gates"):
        dbg["gates"] = S.dram("dbg_gates32", [12, 128, 32], F32, kind="ExternalOutput")
    if moe0:
      moe_phase(S, G, 0, x1_d if not cfg.get("moe_in_ext") else G.x1_ext, tiles0[:cfg.get("moe_ntiles", len(tiles0))],
                lambda b, row0: x2_d.v(x2_d.t[b, row0:row0 + 128, :]), cfg.get("st0", 12), npairs=cfg.get("npairs", 16), dbg=dbg)
    if stop == "moe0":
        S.barrier()
        S.emit()
        return nc, S
    x3_d = S.dram("x3_d", [NB, LAT, D], F32, kind="ExternalOutput" if stop == "attn" else "Internal")
    mod_phase(S, G, 1)
    attn_phase(S, G, x2_d if not cfg.get("attn_in_ext") else G.x2_ext, x3_d, **cfg.get("at", {}))
    if stop == "attn":
        S.barrier()
        S.emit()
        return nc, S
    out_d = S.dram("out", [NB, LAT, D], F32, kind="ExternalOutput")
    tiles1 = [(b, ti * 128, b) for b in range(NB) for ti in range(LAT // 128)]
    moe_phase(S, G, 1, x3_d, tiles1, lambda b, row0: out_d.v(out_d.t[b, row0:row0 + 128, :]), cfg.get("st1", 8))
    S.barrier()
    S.emit()
    return nc, S


def prep_core_inputs(inputs, core, consts):
    b0 = core * NB
    f = lambda a: np.ascontiguousarray(a, dtype=np.float32)
    m = {}
    m["x"] = f(inputs["x"][b0:b0 + NB])
    m["ctx"] = f(inputs["ctx"][b0:b0 + NB])
    m["c3"] = f(np.concatenate([inputs["c"][b0:b0 + NB], inputs["c_ctx"][None, :]], axis=0))
    pv = np.concatenate([
        inputs["norm1_g"].reshape(16, 128), inputs["norm2_g"].reshape(16, 128),
        inputs["rw_mu"][0].reshape(48, 128), inputs["b_mod"].reshape(96, 128)], axis=0)
    m["pvec"] = f(pv)
    m["w_mod"] = f(inputs["w_mod"])
    m["b_mod"] = f(inputs["b_mod"])
    for k, v in consts.items():
        m[k] = v
    m["rw_w_rkv"] = f(inputs["rw_w_rkv"][0])
    for k in ("rw_w0", "rw_w1", "rw_w2", "rw_a0", "rw_a1", "rw_a2", "rw_g1", "rw_g2", "rw_w_o"):
        m[k] = f(inputs[k][0])
    for k in ("rw_k_k", "rw_k_a", "rw_lnx_g", "rw_lnx_b"):
        m[k] = f(inputs[k][0].reshape(1, D))
    m["rw_r_k"] = f(inputs["rw_r_k"][0].reshape(1, D))
    m["da_w_qkv"] = f(inputs["da_w_qkv"][0])
    m["da_q_norm_g"] = f(inputs["da_q_norm_g"][0].reshape(1, 64))
    m["da_k_norm_g"] = f(inputs["da_k_norm_g"][0].reshape(1, 64))
    m["da_lam"] = f(np.stack([inputs["da_lam_q1"][0], inputs["da_lam_k1"][0], inputs["da_lam_q2"][0], inputs["da_lam_k2"][0]]))
    m["da_subln_g"] = f(inputs["da_subln_g"][0].reshape(1, 128))
    m["da_w_o"] = f(inputs["da_w_o"][0])
    rt = np.concatenate([inputs["moe_router_g"], np.transpose(inputs["moe_router_e"], (0, 2, 1, 3)).reshape(2, D, 32)], axis=2)
    m["moe_router"] = f(rt)
    rb = np.concatenate([inputs["moe_router_g_b"], inputs["moe_router_e_b"].reshape(2, 32)], axis=1).reshape(2, 1, 36)
    m["moe_router_b"] = f(rb)
    m["moe_w_gate"] = f(inputs["moe_w_gate"]).reshape(2, NEXP, D, DFF)
    m["moe_w_up"] = f(inputs["moe_w_up"]).reshape(2, NEXP, D, DFF)
    m["moe_w_down"] = f(inputs["moe_w_down"]).reshape(2, NEXP, DFF, D)
    return m


_CACHE = {}


def kernel(**inputs):
    from concourse.bass_utils import run_bass_kernel_spmd
    n = 8
    if "nc" not in _CACHE:
        _CACHE["nc"] = build({})[0]
        _CACHE["consts"] = host_consts()
    nc = _CACHE["nc"]
    consts = _CACHE["consts"]
    inputs = {k: np.asarray(v) for k, v in inputs.items()}
    in_maps = [prep_core_inputs(inputs, c, consts) for c in range(n)]
    res = run_bass_kernel_spmd(nc, in_maps, core_ids=list(range(n)))
    out = np.concatenate([r["out"] for r in res.results], axis=0)
    return out.astype(np.float32)
```

```python
import numpy as np
import concourse.bass as bass
import concourse.mybir as mybir

F32 = mybir.dt.float32
BF16 = mybir.dt.bfloat16
I32 = mybir.dt.int32
U32 = mybir.dt.uint32
AF = mybir.ActivationFunctionType
ALU = mybir.AluOpType
AX = mybir.AxisListType

ENGS = ("pe", "dve", "act", "pool", "sp")
SEM_LIMIT = 30000
N_DMA_SEMS = 24


class V:
    __slots__ = ("ap", "toks", "excl")

    def __init__(self, ap, toks, excl=False):
        self.ap = ap
        self.toks = toks
        self.excl = excl


class Tl:
    def __init__(self, S, t, name, excl=False, toks=None):
        self.S = S
        self.t = t
        self.name = name
        self.excl = excl
        self.toks = toks

    def _tk(self, key):
        if self.toks is not None:
            return list(self.toks)
        return [(self.name, None if self.excl else key)]

    def __getitem__(self, idx):
        return V(self.t[idx], self._tk(None), self.excl)

    def k(self, key, idx=None):
        ap = self.t[idx] if idx is not None else None
        return V(ap, self._tk(key), self.excl)

    def v(self, ap, key=None):
        return V(ap, self._tk(key), self.excl)


class Sched:
    def __init__(self, nc, same_engine_sync=True):
        self.nc = nc
        self.q = {e: [] for e in ENGS}
        self.cnt = {e: 0 for e in ENGS}
        self.semi = {e: 0 for e in ENGS}
        self.nsem = {e: 1 for e in ENGS}
        self.state = {}
        self.waited = {e: {} for e in ENGS}
        self.same = same_engine_sync
        self.dma_i = 0
        self.dma_cnt = [0] * N_DMA_SEMS
        self.dma_last = [None] * N_DMA_SEMS
        self.ctx = []
        self.n_instr = 0
        self.out_deps = []

    def sbuf(self, name, shape, dtype=F32):
        cm = self.nc.sbuf_tensor(name, list(shape), dtype)
        t = cm.__enter__()
        self.ctx.append(cm)
        return Tl(self, t, name)

    def psum(self, name, shape, dtype=F32):
        cm = self.nc.psum_tensor(name, list(shape), dtype)
        t = cm.__enter__()
        self.ctx.append(cm)
        return Tl(self, t, name, excl=True)

    def dram(self, name, shape, dtype=F32, kind="Internal"):
        t = self.nc.dram_tensor(name, list(shape), dtype, kind=kind)
        return Tl(self, t.ap(), name)

    def push_scope(self):
        self.scopes = getattr(self, "scopes", [])
        self.scopes.append(len(self.ctx))

    def pop_scope(self):
        self.barrier()
        n = self.scopes.pop()
        while len(self.ctx) > n:
            self.ctx.pop().__exit__(None, None, None)

    def barrier(self):
        deps = []
        for e in ENGS:
            if self.cnt[e] > 0:
                deps.append(((e, self.semi[e]), self.cnt[e], e))
        for i in range(N_DMA_SEMS):
            if self.dma_last[i] is not None:
                deps.append(self.dma_last[i])
        for e in ENGS:
            d = [x for x in deps if x[2] != e]
            w = self._waits(e, d)
            if w:
                self.q[e].append(("wait", None, w, None))

    def _deps(self, reads, writes):
        deps = []
        for v in reads:
            for tok in v.toks:
                st = self.state.get(tok)
                if st and st[0] is not None:
                    deps.append(st[0])
        for v in writes:
            for tok in v.toks:
                st = self.state.get(tok)
                if st:
                    if st[0] is not None:
                        deps.append(st[0])
                    deps.extend(st[1])
        return deps

    def _update(self, reads, writes, me, real_writes=None):
        for v in reads:
            for tok in v.toks:
                st = self.state.setdefault(tok, [None, [], None])
                st[1].append(me)
                if len(st[1]) > 64:
                    best = {}
                    for d in st[1]:
                        if d[0] not in best or best[d[0]][1] < d[1]:
                            best[d[0]] = d
                    st[1] = list(best.values())
        rw = writes if real_writes is None else real_writes
        rwt = set()
        for v in rw:
            rwt.update(v.toks)
        for v in writes:
            for tok in v.toks:
                old = self.state.get(tok)
                lrw = me if tok in rwt else (old[2] if old else None)
                self.state[tok] = [me, [], lrw]

    def _waits(self, eng, deps, raw_toks_same=None):
        need = {}
        for d in deps:
            semkey, val, deng, is_raw = d[0], d[1], d[2], True
            if deng == eng and not self.same:
                continue
            w = self.waited[eng].get(semkey, 0)
            if val > w and val > need.get(semkey, 0):
                need[semkey] = val
        for sk, val in need.items():
            self.waited[eng][sk] = val
        return list(need.items())

    def op(self, eng, fn, reads=(), writes=(), same_ok=False):
        reads = list(reads)
        real_writes = list(writes)
        writes = real_writes + [v for v in reads if v.excl]
        deps = self._deps(reads, writes)
        if same_ok or eng == "pe":
            deps = [d for d in deps if d[2] != eng]
        elif self.same:
            rd = []
            for v in reads:
                for tok in v.toks:
                    st = self.state.get(tok)
                    if st and st[2] is not None and st[2][2] == eng:
                        rd.append(st[2])
            deps = [d for d in deps if d[2] != eng] + rd
        waits = self._waits(eng, deps)
        if self.cnt[eng] >= SEM_LIMIT:
            self.semi[eng] += 1
            self.nsem[eng] = max(self.nsem[eng], self.semi[eng] + 1)
            self.cnt[eng] = 0
        self.cnt[eng] += 1
        me = ((eng, self.semi[eng]), self.cnt[eng], eng)
        self.q[eng].append(("op", fn, waits, me[0]))
        self._update(reads, writes, me, real_writes)
        self.n_instr += 1
        return me

    def dma(self, queue, out, in_, split=0, **kw):
        reads, writes = [in_], [out]
        deps = self._deps(reads, writes)
        i = self.dma_i % N_DMA_SEMS
        self.dma_i += 1
        if self.dma_last[i] is not None:
            deps.append(self.dma_last[i])
        waits = self._waits(queue, deps)
        oa, ia = out.ap, in_.ap
        parts = [(oa, ia)]
        if split:
            step = 128 // split
            parts = [(oa[k * step:(k + 1) * step], ia[k * step:(k + 1) * step]) for k in range(split)]
        for k, (o_, a_) in enumerate(parts):
            self.dma_cnt[i] += 16
            self.q[queue].append(("dma", (o_, a_, kw), waits if k == 0 else [], ("dma", i)))
            self.n_instr += 1
        me = (("dma", i), self.dma_cnt[i], "dma")
        self.dma_last[i] = me
        self._update(reads, writes, me)
        return me

    def emit(self, final_deps=None):
        nc = self.nc
        sems = {}
        cms = []

        def mk(name):
            cm = nc.semaphore(name)
            s = cm.__enter__()
            cms.append(cm)
            return s
        for e in ENGS:
            for i in range(self.nsem[e]):
                sems[(e, i)] = mk(f"s_{e}_{i}")
        for i in range(N_DMA_SEMS):
            sems[("dma", i)] = mk(f"s_dma_{i}")
        engobj = {"pe": "tensor", "dve": "vector", "act": "scalar", "pool": "gpsimd", "sp": "sync"}
        if final_deps:
            w = self._waits("sp", final_deps)
            self.q["sp"].append(("wait", None, w, None))
        with nc.Block() as block:
            for e in ENGS:
                items = self.q[e]
                if not items:
                    continue

                def body(eng, items=items):
                    for kind, fn, waits, semkey in items:
                        for sk, val in waits:
                            eng.wait_ge(sems[sk], val)
                        if kind == "op":
                            ins = fn(eng)
                            ins.then_inc(sems[semkey], 1)
                        elif kind == "dma":
                            oa, ia, kw = fn
                            eng.dma_start(out=oa, in_=ia, **kw).then_inc(sems[semkey], 16)
                getattr(block, engobj[e])(body)
        for cm in reversed(cms):
            cm.__exit__(None, None, None)
        for cm in reversed(self.ctx):
            cm.__exit__(None, None, None)

    def mm(self, out, lhsT, rhs, start=True, stop=True, **kw):
        return self.op("pe", lambda e: e.matmul(out.ap, lhsT.ap, rhs.ap, start=start, stop=stop, **kw),
                       reads=[lhsT, rhs] + ([] if start else [out]), writes=[out])

    def tr(self, out, in_, ident):
        return self.op("pe", lambda e: e.transpose(out.ap, in_.ap, ident.ap),
                       reads=[in_, ident], writes=[out])

    def act(self, out, in_, func, bias=None, scale=None, accum_out=None, eng="act"):
        reads = [in_]
        kw = {}
        if isinstance(bias, V):
            reads.append(bias)
            kw["bias"] = bias.ap
        elif bias is not None:
            kw["bias"] = bias
        if isinstance(scale, V):
            reads.append(scale)
            kw["scale"] = scale.ap
        elif scale is not None:
            kw["scale"] = scale
        writes = [out]
        if accum_out is not None:
            writes.append(accum_out)
            kw["accum_out"] = accum_out.ap
        return self.op("act", lambda e: e.activation(out.ap, in_.ap, func, **kw), reads=reads, writes=writes)

    def tt(self, eng, out, in0, in1, op):
        return self.op(eng, lambda e: e.tensor_tensor(out.ap, in0.ap, in1.ap, op), reads=[in0, in1], writes=[out])

    def ts(self, eng, out, in0, s1, s2=None, op0=ALU.mult, op1=None, accum_out=None):
        reads = [in0]
        a1 = s1.ap if isinstance(s1, V) else s1
        a2 = s2.ap if isinstance(s2, V) else s2
        if isinstance(s1, V):
            reads.append(s1)
        if isinstance(s2, V):
            reads.append(s2)
        writes = [out]
        kw = {}
        if op1 is not None:
            kw["op1"] = op1
        if accum_out is not None:
            kw["accum_out"] = accum_out.ap
            writes.append(accum_out)
        return self.op(eng, lambda e: e.tensor_scalar(out.ap, in0.ap, a1, a2, op0, **kw), reads=reads, writes=writes)

    def stt(self, out, in0, scalar, in1, op0, op1, eng="dve"):
        reads = [in0, in1]
        a = scalar.ap if isinstance(scalar, V) else scalar
        if isinstance(scalar, V):
            reads.append(scalar)
        return self.op(eng, lambda e: e.scalar_tensor_tensor(out.ap, in0.ap, a, in1.ap, op0, op1), reads=reads, writes=[out])

    def copy(self, eng, out, in_):
        if eng == "act":
            return self.op("act", lambda e: e.copy(out.ap, in_.ap), reads=[in_], writes=[out])
        return self.op(eng, lambda e: e.tensor_copy(out.ap, in_.ap), reads=[in_], writes=[out])

    def memset(self, eng, out, val):
        return self.op(eng, lambda e: e.memset(out.ap, val), reads=[], writes=[out])

    def reduce(self, out, in_, op=ALU.add, axis=AX.X, eng="dve"):
        return self.op(eng, lambda e: e.tensor_reduce(out.ap, in_.ap, axis, op), reads=[in_], writes=[out])

    def recip(self, out, in_):
        return self.op("dve", lambda e: e.reciprocal(out.ap, in_.ap), reads=[in_], writes=[out])
D = 1024
NB = 2
LAT = 2048
CTX = 256
TOK = CTX + LAT
NT = TOK // 128
EPS = 1e-6
DECAY_C = -0.6065306597126334
LNX_EPS = 64e-5
NGRP = 4
NEXP = 32
DFF = 256

PV_N1 = (0, 16)
PV_N2 = (16, 32)
PV_MU = 32
PV_BMOD = 80
PV_ROWS = 176


def host_consts():
    c = {}
    c["ident_f"] = np.eye(128, dtype=np.float32)
    c["ones_f"] = np.ones((128, 128), dtype=np.float32)
    idx = np.arange(128)
    cs, ct = idx[:, None] // 64, idx[None, :] // 64
    same = (cs == ct)
    s_, t_ = idx[:, None], idx[None, :]
    maskA = np.zeros((2, 128, 256), np.float32)
    maskAT = np.zeros((2, 128, 128), np.float32)
    tri = np.zeros((2, 128, 384), np.float32)
    for z in range(2):
        prec = same & ((s_ < t_) if z == 0 else (s_ > t_))
        preceq = same & ((s_ <= t_) if z == 0 else (s_ >= t_))
        succ = same & ((s_ > t_) if z == 0 else (s_ < t_))
        maskA[z, :, 0:128] = prec
        maskA[z, :, 128:256] = preceq
        maskAT[z] = prec.T
        tri[z, :, 0:128] = preceq * DECAY_C
        tri[z, :, 128:256] = prec * DECAY_C
        tri[z, :, 256:384] = succ * DECAY_C
    c["maskA"] = maskA
    c["maskAT"] = maskAT
    c["tri"] = tri
    ind = np.zeros((128, 2), np.float32)
    ind[0:64, 0] = DECAY_C
    ind[64:128, 1] = DECAY_C
    c["ind"] = ind
    rows = LAT // 64
    row = np.repeat(np.arange(rows), 64)
    col = np.tile(np.arange(64), rows)
    inv = (10000.0 ** (-np.arange(16, dtype=np.float32) / 16)).astype(np.float32)
    ang = np.stack([row, col], axis=-1).astype(np.float32)[:, :, None] * inv
    c["rope_cos"] = np.cos(ang).astype(np.float32).reshape(LAT, 32)
    c["rope_sin"] = np.sin(ang).astype(np.float32).reshape(LAT, 32)
    sel = np.zeros((32, 32, 128), np.float32)
    for e in range(32):
        sel[e, e, :] = 1.0
    c["sel"] = sel.reshape(32, 32 * 128)
    return c


class Ctx:
    pass


def std_psum(S, G, tag):
    G.psA = S.psum(f"psA{tag}", [128, 1024], F32)
    nm = G.psA.name
    G.psA.toks = [(nm, 0), (nm, 1)]
    G.psA_h = [Tl(S, G.psA.t[:, i * 512:(i + 1) * 512].rearrange("p (a b) -> p a b", b=128), nm, excl=True,
                  toks=[(nm, i)]) for i in range(2)]
    G.psY = S.psum(f"psY{tag}", [128, 1024], F32)
    nmy = G.psY.name
    G.psY.toks = [(nmy, 0), (nmy, 1)]
    G.psY_h = [Tl(S, G.psY.t[:, i * 512:(i + 1) * 512].rearrange("p (a b) -> p a b", b=128), nmy, excl=True,
                  toks=[(nmy, i)]) for i in range(2)]
    G.psT = S.psum(f"psT{tag}", [128, 1024], BF16)
    G.psH = [S.psum(f"psH{i}{tag}", [128, 4, 128], F32) for i in range(3)]


def setup_common(S, G, need=("rwkv", "da", "moe")):
    def I(name, shape, dt=F32):
        grp = name.split('_')[0]
        if grp in ('rw', 'da', 'moe') and {'rw': 'rwkv', 'da': 'da', 'moe': 'moe'}[grp] not in need:
            return None
        return S.dram(name, shape, dt, kind="ExternalInput")
    G.x = I("x", [NB, LAT, D])
    G.ctx = I("ctx", [NB, CTX, D])
    G.c3 = I("c3", [3, D])
    G.pvec = I("pvec", [PV_ROWS, 128])
    G.w_mod = I("w_mod", [2, D, 6 * D])
    G.b_mod = I("b_mod", [2, 6 * D])
    G.ident_f_d = I("ident_f", [128, 128])
    G.ones_f_d = I("ones_f", [128, 128])
    G.maskA_d = I("maskA", [2, 128, 256])
    G.maskAT_d = I("maskAT", [2, 128, 128])
    G.tri_d = I("tri", [2, 128, 384])
    G.ind_d = I("ind", [128, 2])
    G.rope_cos_d = I("rope_cos", [LAT, 32])
    G.rope_sin_d = I("rope_sin", [LAT, 32])
    G.sel_d = I("sel", [32, 32 * 128])
    G.rw_w_rkv = I("rw_w_rkv", [3, D, D])
    G.rw_w0 = I("rw_w0", [2, D])
    G.rw_w1 = I("rw_w1", [2, D, 64])
    G.rw_w2 = I("rw_w2", [2, 64, D])
    G.rw_a0 = I("rw_a0", [2, D])
    G.rw_a1 = I("rw_a1", [2, D, 64])
    G.rw_a2 = I("rw_a2", [2, 64, D])
    G.rw_g1 = I("rw_g1", [D, 128])
    G.rw_g2 = I("rw_g2", [128, D])
    G.rw_k_k = I("rw_k_k", [1, D])
    G.rw_k_a = I("rw_k_a", [1, D])
    G.rw_r_k = I("rw_r_k", [1, D])
    G.rw_lnx_g = I("rw_lnx_g", [1, D])
    G.rw_lnx_b = I("rw_lnx_b", [1, D])
    G.rw_w_o = I("rw_w_o", [D, D])
    G.da_w_qkv = I("da_w_qkv", [D, 3 * D])
    G.da_q_norm_g = I("da_q_norm_g", [1, 64])
    G.da_k_norm_g = I("da_k_norm_g", [1, 64])
    G.da_lam = I("da_lam", [4, 64])
    G.da_subln_g = I("da_subln_g", [1, 128])
    G.da_w_o = I("da_w_o", [D, D])
    G.moe_router = I("moe_router", [2, D, 36])
    G.moe_router_b = I("moe_router_b", [2, 1, 36])
    G.moe_w_gate = I("moe_w_gate", [2, NEXP, D, DFF])
    G.moe_w_up = I("moe_w_up", [2, NEXP, D, DFF])
    G.moe_w_down = I("moe_w_down", [2, NEXP, DFF, D])

    G.ident_f = S.sbuf("ident_f_s", [128, 128], F32)
    G.ident_b = S.sbuf("ident_b_s", [128, 128], BF16)
    G.ones_f = S.sbuf("ones_f_s", [128, 128], F32)
    S.dma("sp", G.ident_f[:], G.ident_f_d[:])
    S.dma("pool", G.ident_b[:], G.ident_f_d[:])
    S.dma("sp", G.ones_f[:], G.ones_f_d[:])
    G.pfm = S.sbuf("pfm", [128, PV_ROWS], F32)
    S.push_scope()
    std_psum(S, G, "c")
    pv = S.sbuf("pv_ld", [128, 2, 128], F32)
    S.memset("dve", pv[:], 0.0)
    S.dma("sp", pv[:, 0, :], G.pvec[0:128, :])
    S.dma("sp", pv[0:PV_ROWS - 128, 1, :], G.pvec[128:PV_ROWS, :])
    S.tr(G.psA[:, 0:128], pv[:, 0, :], G.ident_f[:])
    S.tr(G.psA[:, 128:256], pv[:, 1, :], G.ident_f[:])
    S.copy("dve", G.pfm[:], G.psA[:, 0:PV_ROWS])
    S.pop_scope()
    G.modT = S.sbuf("modT", [128, 48, 4], F32)
    G.A1 = S.sbuf("A1", [128, 3, 8], F32)
    G.A2 = S.sbuf("A2", [128, 3, 8], F32)
    G.gates_d = S.dram("gates_d", [2, 3, D], F32)


def mod_phase(S, G, li):
    S.push_scope()
    std_psum(S, G, f"m{li}")
    crow = S.sbuf(f"crow{li}", [4, D], F32)
    sc = S.sbuf(f"sc{li}", [4, D], F32)
    scT = S.sbuf(f"scT{li}", [128, 8, 4], F32)
    brow = S.sbuf(f"brow{li}", [1, 6 * D], F32)
    grow = S.sbuf(f"grow{li}", [4, 512], F32)
    wblk = [S.sbuf(f"wblk{i}_{li}", [128, 8, 512], F32) for i in range(2)]
    S.memset("dve", crow[:], 0.0)
    S.dma("sp", crow[0:3, :], G.c3[:])
    S.dma("sp", brow[:], G.b_mod[li:li + 1, :])
    S.act(sc[:], crow[:], AF.Silu)
    for kc in range(8):
        S.tr(G.psA[:, kc * 4:(kc + 1) * 4], sc[0:4, kc * 128:(kc + 1) * 128], G.ident_f[0:4, 0:4])
    S.copy("dve", scT[:], G.psA.v(G.psA.t[:, 0:32].rearrange("p (a b) -> p a b", b=4)))
    wsrc = G.w_mod.t[li].rearrange("(kc p) n -> p kc n", p=128)
    gate_blocks = {4: (0, 0), 5: (0, 1), 10: (1, 0), 11: (1, 1)}
    for blk in range(12):
        wb = wblk[blk % 2]
        S.dma("sp", wb[:], G.w_mod.v(wsrc[:, :, blk * 512:(blk + 1) * 512]), split=8)
        if blk in gate_blocks:
            which, half = gate_blocks[blk]
            ps = G.psY[0:4, 0:512]
            for kc in range(8):
                S.mm(ps, scT[:, kc, :], wb[:, kc, :], start=(kc == 0), stop=False)
            S.mm(ps, G.ones_f[0:1, 0:4], brow[0:1, blk * 512:(blk + 1) * 512], start=False, stop=True)
            S.copy("dve", grow[:], ps)
            S.dma("sp", G.gates_d.v(G.gates_d.t[which, :, half * 512:(half + 1) * 512]), grow[0:3, :])
        else:
            for ec in range(4):
                ch = blk * 4 + ec
                ps = G.psA[:, ch * 4:(ch + 1) * 4]
                for kc in range(8):
                    S.mm(ps, wb[:, kc, ec * 128:(ec + 1) * 128], scT[:, kc, :], start=(kc == 0), stop=(kc == 7))
                S.ts("dve", G.modT[:, ch, :], ps, G.pfm[:, PV_BMOD + li * 48 + ch:PV_BMOD + li * 48 + ch + 1], None, op0=ALU.add)
    for r in range(3):
        for (A, sc0, gofs) in ((G.A1, 8, PV_N1[0] + li * 8), (G.A2, 32, PV_N2[0] + li * 8)):
            S.ts("dve", A[:, r, :], G.modT[:, sc0:sc0 + 8, r], 1.0, None, op0=ALU.add)
            S.tt("dve", A[:, r, :], A[:, r, :], G.pfm[:, gofs:gofs + 8], ALU.mult)
    S.pop_scope()


def norm_tile_to_fm(S, G, xt, r, A, shift_ch0, out_fm, wk, fp32_out=None):
    st = wk["st"]
    S.act(wk["junk"][:], xt, AF.Square, accum_out=st[:, 0:1])
    S.ts("dve", st[:, 1:2], st[:, 0:1], 1.0 / D, EPS, op0=ALU.mult, op1=ALU.add)
    S.act(st[:, 2:3], st[:, 1:2], AF.Sqrt)
    S.recip(st[:, 3:4], st[:, 2:3])
    if fp32_out is None:
        xn = wk["xn"]
        S.ts("dve", xn[:], xt, st[:, 3:4], None, op0=ALU.mult)
        for kc in range(8):
            S.tr(G.psT[:, kc * 128:(kc + 1) * 128], xn[:, kc * 128:(kc + 1) * 128], G.ident_b[:])
        src = G.psT.v(G.psT.t[:, :].rearrange("p (a b) -> p a b", b=128))
    else:
        xn = wk["xn32"]
        S.ts("dve", xn[:], xt, st[:, 3:4], None, op0=ALU.mult)
        for kc in range(8):
            S.tr(G.psA[:, kc * 128:(kc + 1) * 128], xn[:, kc * 128:(kc + 1) * 128], G.ident_f[:])
        src = G.psA.v(G.psA.t[:, :].rearrange("p (a b) -> p a b", b=128))
    Abc = A.v(A.t[:, r, :].unsqueeze(2).to_broadcast([128, 8, 128]))
    shbc = G.modT.v(G.modT.t[:, shift_ch0:shift_ch0 + 8, r].unsqueeze(2).to_broadcast([128, 8, 128]))
    tmp = wk["fm32"]
    S.tt("dve", tmp[:], src, Abc, ALU.mult)
    if fp32_out is not None:
        S.tt("pool", fp32_out, tmp[:], shbc, ALU.add)
        S.copy("act", out_fm, fp32_out)
    else:
        S.tt("pool", out_fm, tmp[:], shbc, ALU.add)


def norm_tile_gen(S, G, xt, r, A, shift_ch0, out_fm, wk, fp32_out, psA):
    st = wk["st"]
    S.act(wk["junk"][:], xt, AF.Square, accum_out=st[:, 0:1])
    yield
    S.ts("dve", st[:, 1:2], st[:, 0:1], 1.0 / D, EPS, op0=ALU.mult, op1=ALU.add)
    yield
    S.act(st[:, 2:3], st[:, 1:2], AF.Ln)
    S.act(st[:, 3:4], st[:, 2:3], AF.Exp, scale=-0.5)
    yield
    xn = wk["xn32"]
    S.ts("dve", xn[:], xt, st[:, 3:4], None, op0=ALU.mult)
    yield
    for kc in range(8):
        S.tr(psA[:, kc * 128:(kc + 1) * 128], xn[:, kc * 128:(kc + 1) * 128], G.ident_f[:])
    yield
    src = psA.v(psA.t[:, :].rearrange("p (a b) -> p a b", b=128))
    Abc = A.v(A.t[:, r, :].unsqueeze(2).to_broadcast([128, 8, 128]))
    shbc = G.modT.v(G.modT.t[:, shift_ch0:shift_ch0 + 8, r].unsqueeze(2).to_broadcast([128, 8, 128]))
    tmp = wk["fm32"]
    S.tt("dve", tmp[:], src, Abc, ALU.mult)
    yield
    S.tt("pool", fp32_out, tmp[:], shbc, ALU.add)
    S.copy("act", out_fm, fp32_out)


def wcast_phase(S, G, need):
    items = []
    G.wbf = {}

    def add(name, src3, n):
        dst = S.dram("wbf_" + name, [128, n], BF16)
        G.wbf[name] = dst
        off = 0
        a, b = src3.shape[1], src3.shape[2]
        rows = max(1, 2048 // b)
        if b > 2048:
            for i in range(a):
                for c0 in range(0, b, 2048):
                    c1 = min(b, c0 + 2048)
                    items.append((src3[:, i:i + 1, c0:c1], dst, i * b + c0, c1 - c0, (1, c1 - c0)))
        else:
            for i in range(0, a, rows):
                i1 = min(a, i + rows)
                items.append((src3[:, i:i1, :], dst, i * b, (i1 - i) * b, (i1 - i, b)))

    if "rwkv" in need:
        for j, nm in enumerate(("Wr", "Wk", "Wv")):
            add(nm, G.rw_w_rkv.t[j].rearrange("(kc p) n -> p kc n", p=128), 8 * D)
        add("rwWo", G.rw_w_o.t.rearrange("(kc p) n -> p kc n", p=128), 8 * D)
    if "da" in need:
        add("Wqkv", G.da_w_qkv.t.rearrange("(kc p) n -> p kc n", p=128), 8 * 3 * D)
        add("daWo", G.da_w_o.t.rearrange("(kc p) n -> p kc n", p=128), 8 * D)
    if "moe" in need:
        for li in range(2):
            for e in range(NEXP):
                add(f"g{li}_{e}", G.moe_w_gate.t[li, e].rearrange("(kc p) n -> p kc n", p=128), 8 * DFF)
                add(f"u{li}_{e}", G.moe_w_up.t[li, e].rearrange("(kc p) n -> p kc n", p=128), 8 * DFF)
                add(f"d{li}_{e}", G.moe_w_down.t[li, e].rearrange("(fc p) n -> p fc n", p=128), 2 * D)
    S.push_scope()
    NBUF = 4
    stg = [S.sbuf(f"wc_stg{i}", [128, 2048], F32) for i in range(NBUF)]
    ob = [S.sbuf(f"wc_ob{i}", [128, 2048], BF16) for i in range(NBUF)]
    engs = ["dve", "pool", "dve"]

    def load(i):
        src3, dst, off, n, (a, b) = items[i]
        t = stg[i % NBUF]
        S.dma("sp", t.v(t.t[:, 0:n].rearrange("p (a b) -> p a b", b=b)), V(src3, [("wsrc", None)]))

    for i in range(min(NBUF - 1, len(items))):
        load(i)
    for i in range(len(items)):
        if i + NBUF - 1 < len(items):
            load(i + NBUF - 1)
        src3, dst, off, n, _ = items[i]
        S.copy(engs[i % 3], ob[i % NBUF][:, 0:n], stg[i % NBUF][:, 0:n])
        S.dma("act", dst.v(dst.t[:, off:off + n]), ob[i % NBUF][:, 0:n])
    S.pop_scope()

def rwkv_phase(S, G, x1_d, dbg=None, nb=NB, nt0=NT, do_dir=3, nt1=NT, nheads=16, fl=99):
    li = 0
    H = 16
    yf_d = S.dram("yf_d", [NB, NT, 128, 1040], F32)
    cache_d = S.dram("cache_d", [NB, NT, 128, 6 * D], BF16)
    sg1_d = S.dram("sg1_d", [NB, NT, 128, D], F32)

    def load_bc(name, src, dt=BF16, n=D, q="pool"):
        t = S.sbuf(name, [128, n], dt)
        S.dma(q, t[:], src.v(src.t[0:1, :].partition_broadcast(128)))
        return t

    S.push_scope()
    std_psum(S, G, "r")
    maskA = S.sbuf("maskA_s", [128, 2, 256], BF16)
    maskAT = S.sbuf("maskAT_s", [128, 2, 128], BF16)
    tri = S.sbuf("tri_s", [128, 2, 384], F32)
    ind = S.sbuf("ind_s", [128, 2], F32)
    for z in range(2):
        S.dma("pool", maskA[:, z, :], G.maskA_d.v(G.maskA_d.t[z]))
        S.dma("pool", maskAT[:, z, :], G.maskAT_d.v(G.maskAT_d.t[z]))
        S.dma("sp", tri[:, z, :], G.tri_d.v(G.tri_d.t[z]))
    S.dma("sp", ind[:], G.ind_d[:])
    k_a_bc = load_bc("k_a_bc", G.rw_k_a)
    r_k_bc = load_bc("r_k_bc", G.rw_r_k)
    scr1 = S.sbuf("scr1", [128, D], F32)
    scr2 = S.sbuf("scr2", [128, D], F32)
    sg_sb = S.sbuf("sg_sb", [128, D], F32)
    kdir_sb = S.sbuf("kdir_sb", [128, D], BF16)
    b_sb = S.sbuf("b_sb", [128, D], BF16)
    tm = [S.sbuf(f"tm{i}", [128, D], BF16) for i in range(2)]
    R19 = S.sbuf("R19", [128, H, 128], BF16)
    Bh = S.sbuf("Bh", [128, D], BF16)
    Kh = S.sbuf("Kh", [128, D], BF16)
    arT = S.sbuf("arT", [128, 8, 2, 128], BF16)
    btT = S.sbuf("btT", [128, 8, 128], BF16)
    ktT = S.sbuf("ktT", [128, 8, 128], BF16)
    gC = S.sbuf("gC", [128, 8, 2], F32)
    bon = S.sbuf("bon", [128, 2, 16], F32)
    NSET = 4
    M1 = [S.sbuf(f"M1_{i}", [128, 256], BF16) for i in range(NSET)]
    M2 = [S.sbuf(f"M2_{i}", [128, 256], BF16) for i in range(NSET)]
    MabT = [S.sbuf(f"MabT_{i}", [128, 128], BF16) for i in range(NSET)]
    Pb = [[S.sbuf(f"Pb_{i}_{j}", [128, 128], BF16) for j in range(2)] for i in range(NSET)]
    PTb = [[S.sbuf(f"PTb_{i}_{j}", [128, 128], BF16) for j in range(2)] for i in range(NSET)]
    Tb = [S.sbuf(f"Tb_{i}", [128, 128], BF16) for i in range(NSET)]
    WP = [S.sbuf(f"WP_{i}", [128, 128], BF16) for i in range(NSET)]
    G_all = S.sbuf("G_all", [128, 8, 128], BF16)
    Y0_all = S.sbuf("Y0_all", [128, H, 64], BF16)
    D_all = S.sbuf("D_all", [128, 8, 2, 128], BF16)
    E_all = S.sbuf("E_all", [128, 8, 2, 128], BF16)
    Sb = S.sbuf("Sb", [128, 8, 128], BF16)
    S.memset("pool", D_all[:], 0.0)
    S.memset("pool", E_all[:], 0.0)
    yfw = S.sbuf("yfw", [128, 1040], F32)
    banks = list(G.psH) + list(G.psA_h) + list(G.psY_h)
    NBK = len(banks)
    bank_ctr = [0]
    slot_ctr = [0] * NBK

    def slot(n=1):
        bk = bank_ctr[0] % NBK
        bank_ctr[0] += 1
        if n == 2:
            i = ((slot_ctr[bk] + 1) // 2 * 2) % 4
            slot_ctr[bk] = i + 2
        else:
            i = slot_ctr[bk] % 4
            slot_ctr[bk] = i + 1
        return (bk, i)

    def psl(s, p0=0, p1=128, c0=0, c1=128, n=1):
        bk, i = s
        t = banks[bk]
        if n == 2:
            return t.v(t.t[p0:p1, i:i + 2, :].rearrange("p a b -> p (a b)")[:, c0:c1])
        return t.v(t.t[p0:p1, i, c0:c1])

    def transposes_to(src_tm, dst_view):
        for ec in range(8):
            S.tr(G.psT[:, ec * 128:(ec + 1) * 128], src_tm[:, ec * 128:(ec + 1) * 128], G.ident_b[:])
        S.copy("act", dst_view, G.psT.v(G.psT.t[:, :].rearrange("p (a b) -> p a b", b=128)))

    def dir_part(z, r_v, k_v, v_v, kk_v, a_v, chunk_order, sg_sb=sg_sb):
        zsl = slice(z, z + 1)
        S.stt(scr1[:], a_v, -1.0, k_a_bc[:], ALU.add, ALU.mult)
        S.stt(kdir_sb[:], scr1[:], 1.0, k_v, ALU.add, ALU.mult)
        S.tt("pool", b_sb[:], kk_v, a_v, ALU.mult)
        S.tt("pool", scr1[:], r_v, kdir_sb[:], ALU.mult)
        S.tt("pool", scr1[:], scr1[:], r_k_bc[:], ALU.mult)
        S.reduce(bon[:, z, :], scr1.v(scr1.t[:, :].rearrange("p (h n) -> p h n", n=64)))
        def cum(which):
            for n in range(2):
                S.mm(G.psA[:, n * 512:(n + 1) * 512], tri[:, z, which * 128:(which + 1) * 128], sg_sb[:, n * 512:(n + 1) * 512])
        cum(0)
        S.act(scr2[:], G.psA[:], AF.Exp)
        S.tt("dve", tm[0][:], r_v, scr2[:], ALU.mult)
        transposes_to(tm[0], arT.v(arT.t[:, :, 1, :]))
        S.act(scr2[:], G.psA[:], AF.Exp, scale=-1.0)
        S.tt("dve", tm[1][:], b_sb[:], scr2[:], ALU.mult)
        transposes_to(tm[1], btT[:])
        S.tt("dve", tm[0][:], kdir_sb[:], scr2[:], ALU.mult)
        transposes_to(tm[0], ktT[:])
        cum(1)
        S.act(scr2[:], G.psA[:], AF.Exp)
        S.stt(tm[1][:], kk_v, -1.0, scr2[:], ALU.mult, ALU.mult)
        S.copy("pool", V(R19.t[:, :, 64:128], [("R19", h) for h in range(H)]),
               tm[1].v(tm[1].t[:, :].rearrange("p (h n) -> p h n", n=64)))
        transposes_to(tm[1], arT.v(arT.t[:, :, 0, :]))
        cum(2)
        S.act(scr2[:], G.psA[:], AF.Exp)
        S.tt("dve", Bh[:], b_sb[:], scr2[:], ALU.mult)
        S.tt("pool", Kh[:], kdir_sb[:], scr2[:], ALU.mult)
        sg_ = slot()
        for ec in range(8):
            S.mm(psl(sg_, c0=ec * 2, c1=ec * 2 + 2), sg_sb[:, ec * 128:(ec + 1) * 128], ind[:])
        S.act(gC[:], banks[sg_[0]].v(banks[sg_[0]].t[:, sg_[1], 0:16].rearrange("p (a b) -> p a b", b=2)), AF.Exp)

        if do_dir < 2:
            return
        def head_gen(h):
            ec, po = h // 2, (h % 2) * 64
            hc = slice(h * 64, (h + 1) * 64)
            pr = slice(po, po + 64)
            i2 = h % NSET
            bt_h = btT[pr, ec, :]
            kt_h = ktT[pr, ec, :]
            ar_h = arT.v(arT.t[pr, ec, :, :].rearrange("p a b -> p (a b)"))
            at_h = arT[pr, ec, 0, :]
            rt_h = arT[pr, ec, 1, :]
            s1 = slot(2)
            S.mm(psl(s1, n=2, c1=256), bt_h, ar_h)
            S.tt("dve", M1[i2][:], psl(s1, n=2, c1=256), maskA[:, z, :], ALU.mult)
            s3 = slot()
            S.mm(psl(s3), at_h, bt_h)
            S.tt("dve", MabT[i2][:], psl(s3), maskAT[:, z, :], ALU.mult)
            s2 = slot(2)
            S.mm(psl(s2, n=2, c1=256), kt_h, ar_h)
            S.tt("dve", M2[i2][:], psl(s2, n=2, c1=256), maskA[:, z, :], ALU.mult)
            T = Tb[i2]
            S.tt("pool", T[:], M1[i2][:, 0:128], G.ident_b[:], ALU.add)
            P, PT = M1[i2][:, 0:128], MabT[i2][:]
            yield
            for kstep in range(1, 6):
                if kstep < 5:
                    sa = slot()
                    S.mm(psl(sa), PT, P)
                    P2 = Pb[i2][kstep % 2]
                    S.copy("act", P2[:], psl(sa))
                sb_ = slot()
                S.mm(psl(sb_), P, PT)
                P2T = PTb[i2][kstep % 2]
                S.copy("act", P2T[:], psl(sb_))
                if kstep == 1:
                    sx = slot()
                    S.mm(psl(sx, c1=64), M2[i2][:, 0:128], v_v_slice(v_v, hc))
                    S.copy("act", R19.k(h, (slice(None), h, slice(0, 64))), psl(sx, c1=64))
                yield
                sc_ = slot()
                S.mm(psl(sc_), P2T[:], T[:])
                S.tt("dve", T[:], T[:], psl(sc_), ALU.add)
                if kstep < 5:
                    P, PT = P2[:], P2T[:]
            yield
            sw = slot()
            S.mm(psl(sw), T[:], R19.k(h, (slice(None), h, slice(None))))
            S.copy("act", WP[i2][:], psl(sw))
            yield
            sg2 = slot()
            S.mm(psl(sg2, p0=po, p1=po + 64), WP[i2][:, 64:128], M1[i2][:, 128:256])
            S.tt("dve", G_all.k(h, (pr, ec, slice(None))), psl(sg2, p0=po, p1=po + 64), rt_h, ALU.add)
            sy = slot()
            S.mm(psl(sy, c1=64), M1[i2][:, 128:256], WP[i2][:, 0:64], start=True, stop=False)
            S.mm(psl(sy, c1=64), M2[i2][:, 128:256], v_v_slice(v_v, hc), start=False, stop=True)
            S.copy("act", Y0_all.k(h, (slice(None), h, slice(None))), psl(sy, c1=64))
            sds = [slot(), slot()]
            for c in range(2):
                cr = slice(c * 64, (c + 1) * 64)
                S.mm(psl(sds[c], p0=po, p1=po + 64, c1=64), WP[i2][cr, 64:128], Bh[cr, hc])
            for c in range(2):
                S.stt(D_all.k(h, (pr, ec, c, slice(po, po + 64))), G.ident_f[pr, po:po + 64], gC[pr, ec, c:c + 1],
                      psl(sds[c], p0=po, p1=po + 64, c1=64), ALU.mult, ALU.add)
            ses = [slot(), slot()]
            for c in range(2):
                cr = slice(c * 64, (c + 1) * 64)
                S.mm(psl(ses[c], p0=po, p1=po + 64, c1=64), Bh[cr, hc], WP[i2][cr, 0:64], start=True, stop=False)
                S.mm(psl(ses[c], p0=po, p1=po + 64, c1=64), Kh[cr, hc], v_v_slice(v_v, hc, cr), start=False, stop=True)
            for c in range(2):
                S.copy("act", E_all.k(h, (pr, ec, c, slice(po, po + 64))), psl(ses[c], p0=po, p1=po + 64, c1=64))

        pending = list(range(nheads))
        active = []
        rnd, last_admit = 0, -99
        while pending or active:
            if pending and len(active) <= NSET - 2 and (rnd - last_admit >= 4 or not active):
                for _ in range(2):
                    if pending:
                        active.append(head_gen(pending.pop(0)))
                last_admit = rnd
            nxt = []
            for g in active:
                try:
                    next(g)
                    nxt.append(g)
                except StopIteration:
                    pass
            active = nxt
            rnd += 1
        if do_dir < 3:
            return
        for c in chunk_order:
            for ec in range(8):
                pair = [2 * ec, 2 * ec + 1]
                Gv = V(G_all.t[:, ec, c * 64:(c + 1) * 64], [("G_all", h) for h in pair])
                Dv = V(D_all.t[:, ec, c, :], [("D_all", h) for h in pair])
                S.mm(G.psY.v(G.psY.t[c * 64:(c + 1) * 64, ec * 128:(ec + 1) * 128]), Gv, Sb[:, ec, :])
                S.mm(G.psA.v(G.psA.t[:, ec * 128:(ec + 1) * 128]), Dv, Sb[:, ec, :])
            S.tt("dve", Sb[:], G.psA.v(G.psA.t[:, :].rearrange("p (a b) -> p a b", b=128)),
                 V(E_all.t[:, :, c, :], [("E_all", h) for h in range(H)]), ALU.add)

    def v_v_slice(v_v, hc, rows=slice(None)):
        return V(v_v.ap[rows, hc], v_v.toks)

    S.push_scope()
    Wr, Wk, Wv = [S.sbuf(n, [128, 8, D], BF16) for n in ("Wr", "Wk", "Wv")]
    for nm, W in (("Wr", Wr), ("Wk", Wk), ("Wv", Wv)):
        S.dma("sp", W.v(W.t[:, :, :].rearrange("p a b -> p (a b)")), G.wbf[nm][:])
    w1 = S.sbuf("w1", [128, 2, 8, 64], BF16)
    a1 = S.sbuf("a1", [128, 2, 8, 64], BF16)
    g1 = S.sbuf("g1", [128, 8, 128], BF16)
    w2x = S.sbuf("w2x", [65, 2, D], BF16)
    a2x = S.sbuf("a2x", [65, 2, D], BF16)
    g2 = S.sbuf("g2", [128, D], BF16)
    for z in range(2):
        S.dma("pool", w1[:, z, :, :], G.rw_w1.v(G.rw_w1.t[z].rearrange("(kc p) n -> p kc n", p=128)))
        S.dma("pool", a1[:, z, :, :], G.rw_a1.v(G.rw_a1.t[z].rearrange("(kc p) n -> p kc n", p=128)))
        S.dma("pool", w2x[0:64, z, :], G.rw_w2.v(G.rw_w2.t[z]))
        S.dma("pool", w2x[64:65, z, :], G.rw_w0.v(G.rw_w0.t[z:z + 1, :]))
        S.dma("pool", a2x[0:64, z, :], G.rw_a2.v(G.rw_a2.t[z]))
        S.dma("pool", a2x[64:65, z, :], G.rw_a0.v(G.rw_a0.t[z:z + 1, :]))
    S.dma("pool", g1[:], G.rw_g1.v(G.rw_g1.t.rearrange("(kc p) n -> p kc n", p=128)))
    S.dma("pool", g2[:], G.rw_g2[:])
    k_k_bc = load_bc("k_k_bc", G.rw_k_k)
    hTc = S.sbuf("hTc", [128, 8, CTX + 2], BF16)
    hTl = S.sbuf("hTl", [128, 8, LAT + 2], BF16)
    xin = scr2
    wk = {"junk": scr1, "st": S.sbuf("st", [128, 4], F32), "xn": tm[0],
          "fm32": S.sbuf("fm32", [128, 8, 128], F32)}
    dxt = S.sbuf("dxt", [128, 8, 128], F32)
    mix = [S.sbuf(f"mix{i}", [128, 8, 128], BF16) for i in range(2)]
    cach = S.sbuf("cach", [128, 6, D], BF16)
    a0_sb = S.sbuf("a0_sb", [128, D], BF16)
    sg1_v = yfw[:, 0:D]
    hwx = S.sbuf("hwx", [65, 2, 128], BF16)
    hax = S.sbuf("hax", [65, 2, 128], BF16)
    hgs = S.sbuf("hgs", [128, 128], BF16)
    st2 = S.sbuf("st2", [128, 3, 16], F32)
    S.memset("dve", hwx[:], 1.0)
    S.memset("dve", hax[:], 1.0)
    for hT in (hTc, hTl):
        S.memset("pool", hT[:], 0.0)

    mix_ctr = [0]

    def make_mix(hT, c0, j):
        m = mix[mix_ctr[0] % 2]
        mix_ctr[0] += 1
        mu = G.pfm.v(G.pfm.t[:, PV_MU + j * 8:PV_MU + j * 8 + 8].unsqueeze(2).to_broadcast([128, 8, 128]))
        S.tt("pool", wk["fm32"][:], dxt[:], mu, ALU.mult)
        S.tt("pool", m[:], wk["fm32"][:], hT[:, :, c0:c0 + 128], ALU.add)
        return m

    def proj_tm(ps, m, W):
        for n in range(2):
            for kc in range(8):
                S.mm(ps[:, n * 512:(n + 1) * 512], m[:, kc, :], W[:, kc, n * 512:(n + 1) * 512], start=(kc == 0), stop=(kc == 7))

    for b in range(nb):
        for ti in range(NT):
            if ti < 2:
                src, r, hT, t0 = G.ctx.v(G.ctx.t[b, ti * 128:(ti + 1) * 128, :]), 2, hTc, ti * 128
            else:
                src, r, hT, t0 = G.x.v(G.x.t[b, (ti - 2) * 128:(ti - 1) * 128, :]), b, hTl, (ti - 2) * 128
            S.dma("sp", xin[:], src, split=4)
            norm_tile_to_fm(S, G, xin[:], r, G.A1, 0, hT[:, :, t0 + 1:t0 + 129], wk)
        if dbg is not None and "hT" in dbg and b == 0:
            S.dma("sp", dbg["hT"][:], hTl[:])
        S.memset("dve", Sb[:], 0.0)
        for ti in range(nt0):
            hT, t0 = (hTc, ti * 128) if ti < 2 else (hTl, (ti - 2) * 128)
            c0 = t0 + 1
            S.tt("dve", dxt[:], hT[:, :, c0 - 1:c0 + 127], hT[:, :, c0 + 1:c0 + 129], ALU.add)
            S.stt(dxt[:], dxt[:], 0.5, hT[:, :, c0:c0 + 128], ALU.mult, ALU.subtract)
            if fl < 1:
                continue
            m = make_mix(hT, c0, 0)
            proj_tm(G.psA, m, Wr)
            S.copy("act", cach[:, 0, :], G.psA[:])
            if fl < 2:
                continue
            m = make_mix(hT, c0, 2)
            proj_tm(G.psY, m, Wv)
            S.copy("act", cach[:, 2, :], G.psY[:])
            if fl < 3:
                continue
            m = make_mix(hT, c0, 4)
            for z in range(2):
                sl_ = slot()
                for kc in range(8):
                    S.mm(psl(sl_, p1=64), a1[:, z, kc, :], m[:, kc, :], start=(kc == 0), stop=(kc == 7))
                S.copy("act", hax[0:64, z, :], psl(sl_, p1=64))
            for z in range(2):
                ps = G.psA if z == 0 else G.psY
                for n in range(2):
                    S.mm(ps[:, n * 512:(n + 1) * 512], hax[:, z, :], a2x[:, z, n * 512:(n + 1) * 512])
                S.act(a0_sb[:] if z == 0 else cach[:, 4, :], ps[:], AF.Sigmoid)
            if fl < 4:
                continue
            m = make_mix(hT, c0, 3)
            for z in range(2):
                sl_ = slot()
                for kc in range(8):
                    S.mm(psl(sl_, p1=64), w1[:, z, kc, :], m[:, kc, :], start=(kc == 0), stop=(kc == 7))
                S.act(hwx[0:64, z, :], psl(sl_, p1=64), AF.Tanh)
            for z in range(2):
                ps = G.psA if z == 0 else G.psY
                for n in range(2):
                    S.mm(ps[:, n * 512:(n + 1) * 512], hwx[:, z, :], w2x[:, z, n * 512:(n + 1) * 512])
                S.act(sg_sb[:] if z == 0 else sg1_v, ps[:], AF.Sigmoid)
            if fl < 5:
                continue
            m = make_mix(hT, c0, 5)
            sl_ = slot()
            for kc in range(8):
                S.mm(psl(sl_), g1[:, kc, :], m[:, kc, :], start=(kc == 0), stop=(kc == 7))
            S.act(hgs[:], psl(sl_), AF.Sigmoid)
            for n in range(2):
                S.mm(G.psY[:, n * 512:(n + 1) * 512], hgs[:], g2[:, n * 512:(n + 1) * 512])
            S.copy("act", cach[:, 5, :], G.psY[:])
            if fl < 6:
                continue
            m = make_mix(hT, c0, 1)
            proj_tm(G.psA, m, Wk)
            S.copy("act", cach[:, 1, :], G.psA[:])
            if fl < 6.1:
                continue
            S.tt("dve", scr1[:], G.psA[:], k_k_bc[:], ALU.mult)
            if fl < 6.2:
                continue
            S.act(scr2[:], scr1[:], AF.Square)
            S.reduce(st2[:, 0, :], scr2.v(scr2.t[:, :].rearrange("p (h n) -> p h n", n=64)))
            if fl < 6.3:
                continue
            S.ts("dve", st2[:, 1, :], st2[:, 0, :], 1e-12, None, op0=ALU.add)
            S.act(st2[:, 1, :], st2[:, 1, :], AF.Sqrt)
            S.recip(st2[:, 2, :], st2[:, 1, :])
            if fl < 6.4:
                continue
            S.tt("dve", cach.v(cach.t[:, 3, :].rearrange("p (h n) -> p h n", n=64)),
                 scr1.v(scr1.t[:, :].rearrange("p (h n) -> p h n", n=64)),
                 st2.v(st2.t[:, 2, :].unsqueeze(2).to_broadcast([128, 16, 64])), ALU.mult)
            if fl < 7:
                continue
            S.dma("sp", cache_d.v(cache_d.t[b, ti].rearrange("p (a n) -> p a n", n=D)), cach[:])
            S.dma("sp", sg1_d.v(sg1_d.t[b, ti]), sg1_v)
            if do_dir:
                dir_part(0, cach[:, 0, :], G.psA[:], cach[:, 2, :], cach[:, 3, :], a0_sb[:], (0, 1))
            S.tt("dve", yfw[:, 0:D], G.psY[:],
                 V(Y0_all.t[:, :, :].rearrange("p h n -> p (h n)"), [("Y0_all", h) for h in range(H)]), ALU.add)
            S.copy("pool", yfw[:, D:D + 16], bon[:, 0, :])
            S.dma("sp", yf_d.v(yf_d.t[b, ti]), yfw[:])
    S.pop_scope()

    S.push_scope()
    Wo = S.sbuf("Wo", [128, 8, D], BF16)
    S.dma("sp", Wo.v(Wo.t[:, :, :].rearrange("p a b -> p (a b)")), G.wbf["rwWo"][:])
    lnx_g_bc = load_bc("lnx_g_bc", G.rw_lnx_g)
    lnx_b_bc = load_bc("lnx_b_bc", G.rw_lnx_b)
    gate_bc = S.sbuf("gate_bc", [128, D], F32)
    cachs = [S.sbuf(f"cach1_{i}", [128, 6, D], BF16) for i in range(2)]
    sgs = [S.sbuf(f"sg1b_{i}", [128, D], F32) for i in range(2)]
    yfs = [S.sbuf(f"yf1_{i}", [128, 1040], F32) for i in range(2)]
    xres = S.sbuf("xres", [128, D], F32)
    pre = S.sbuf("pre", [128, D], BF16)
    preT = S.sbuf("preT", [128, 8, 128], BF16)
    st3 = S.sbuf("st3", [128, 4, 16], F32)
    order1 = ([1, 0] + list(range(NT - 1, 1, -1)))[:nt1]
    seq1 = [(b, ti) for b in range(nb) for ti in order1]

    def load1(n):
        b_, ti_ = seq1[n]
        i = n % 2
        S.dma("sp", cachs[i][:], cache_d.v(cache_d.t[b_, ti_].rearrange("p (a n) -> p a n", n=D)))
        S.dma("sp", sgs[i][:], sg1_d.v(sg1_d.t[b_, ti_]))
        S.dma("sp", yfs[i][:], yf_d.v(yf_d.t[b_, ti_]))

    load1(0)
    cur_r = None
    for n1, (b, ti) in enumerate(seq1):
        if True:
            if ti == order1[0]:
                S.memset("dve", Sb[:], 0.0)
            if n1 + 1 < len(seq1):
                load1(n1 + 1)
            cach, sg1t, yfw = cachs[n1 % 2], sgs[n1 % 2], yfs[n1 % 2]
            r = 2 if ti < 2 else b
            if r != cur_r:
                S.dma("sp", gate_bc[:], G.gates_d.v(G.gates_d.t[0, r:r + 1, :].partition_broadcast(128)))
                cur_r = r
            dir_part(1, cach[:, 0, :], cach[:, 1, :], cach[:, 2, :], cach[:, 3, :], cach[:, 4, :], (1, 0), sg_sb=sg1t)
            S.tt("dve", scr1[:], G.psY[:], V(Y0_all.t[:, :, :].rearrange("p h n -> p (h n)"), [("Y0_all", h) for h in range(H)]), ALU.add)
            S.tt("pool", scr1[:], scr1[:], yfw[:, 0:D], ALU.add)
            y3 = scr1.v(scr1.t[:, :].rearrange("p (h n) -> p h n", n=64))
            S.reduce(st3[:, 0, :], y3)
            S.ts("dve", st3[:, 0, :], st3[:, 0, :], 1.0 / 64, None, op0=ALU.mult)
            S.tt("dve", y3, y3, st3.v(st3.t[:, 0, :].unsqueeze(2).to_broadcast([128, 16, 64])), ALU.subtract)
            S.act(scr2[:], scr1[:], AF.Square)
            S.reduce(st3[:, 1, :], scr2.v(scr2.t[:, :].rearrange("p (h n) -> p h n", n=64)))
            S.ts("dve", st3[:, 1, :], st3[:, 1, :], 1.0 / 64, LNX_EPS, op0=ALU.mult, op1=ALU.add)
            S.act(st3[:, 1, :], st3[:, 1, :], AF.Sqrt)
            S.recip(st3[:, 2, :], st3[:, 1, :])
            S.tt("dve", y3, y3, st3.v(st3.t[:, 2, :].unsqueeze(2).to_broadcast([128, 16, 64])), ALU.mult)
            S.tt("pool", scr1[:], scr1[:], lnx_g_bc[:], ALU.mult)
            S.tt("pool", scr1[:], scr1[:], lnx_b_bc[:], ALU.add)
            S.tt("dve", st3[:, 3, :], bon[:, 1, :], yfw[:, D:D + 16], ALU.add)
            S.tt("dve", scr2.v(scr2.t[:, :].rearrange("p (h n) -> p h n", n=64)),
                 cach.v(cach.t[:, 2, :].rearrange("p (h n) -> p h n", n=64)),
                 st3.v(st3.t[:, 3, :].unsqueeze(2).to_broadcast([128, 16, 64])), ALU.mult)
            S.tt("pool", scr1[:], scr1[:], scr2[:], ALU.add)
            S.tt("pool", pre[:], scr1[:], cach[:, 5, :], ALU.mult)
            transposes_to(pre, preT[:])
            for n in range(2):
                for kc in range(8):
                    S.mm(G.psA[:, n * 512:(n + 1) * 512], preT[:, kc, :], Wo[:, kc, n * 512:(n + 1) * 512], start=(kc == 0), stop=(kc == 7))
            if ti < 2:
                xsrc = G.ctx.v(G.ctx.t[b, ti * 128:(ti + 1) * 128, :])
            else:
                xsrc = G.x.v(G.x.t[b, (ti - 2) * 128:(ti - 1) * 128, :])
            S.dma("sp", xres[:], xsrc)
            S.tt("dve", scr2[:], G.psA[:], gate_bc[:], ALU.mult)
            S.tt("pool", xres[:], xres[:], scr2[:], ALU.add)
            S.dma("sp", x1_d.v(x1_d.t[b, ti * 128:(ti + 1) * 128, :]), xres[:])
    S.pop_scope()
    S.pop_scope()

def moe_phase(S, G, li, xin_d, tiles, xout_fn, st_tiles, npairs=16, dbg=None):
    L = f"e{li}"
    S.push_scope()
    ysub = [S.psum(f"ysub{i}{L}", [128, 1024], F32) for i in range(2)]
    psG = [S.psum(f"psG{i}{L}", [128, 512], F32) for i in range(2)]
    psU = [S.psum(f"psU{i}{L}", [128, 512], F32) for i in range(2)]
    G.psA = ysub[0]
    STK = st_tiles * 128
    h2T = S.sbuf(f"h2T{L}", [128, 8, STK], BF16)
    y_acc = S.sbuf(f"yacc{L}", [128, st_tiles, D], F32)
    gatesT = S.sbuf(f"gatesT{L}", [32, STK], BF16)
    sel = S.sbuf(f"sel{L}", [32, 32, 128], BF16)
    S.dma("pool", sel[:], G.sel_d.v(G.sel_d.t[:, :].rearrange("p (a b) -> p a b", b=128)))
    Wrt = S.sbuf(f"Wrt{L}", [128, 8, 36], F32)
    S.dma("sp", Wrt[:], G.moe_router.v(G.moe_router.t[li].rearrange("(kc p) n -> p kc n", p=128)))
    rb = S.sbuf(f"rb{L}", [1, 36], F32)
    S.dma("sp", rb[:], G.moe_router_b.v(G.moe_router_b.t[li]))
    gate_bc = S.sbuf(f"gbc{L}", [128, 3, D], F32)
    rs_used = sorted(set(t[2] for t in tiles))
    for r in rs_used:
        S.dma("sp", gate_bc[:, r, :], G.gates_d.v(G.gates_d.t[1, r:r + 1, :].partition_broadcast(128)))
    Wg = [[S.sbuf(f"Wg{i}{e}{L}", [128, 8, DFF], BF16) for e in range(2)] for i in range(2)]
    Wu = [[S.sbuf(f"Wu{i}{e}{L}", [128, 8, DFF], BF16) for e in range(2)] for i in range(2)]
    Wd = [[S.sbuf(f"Wd{i}{e}{L}", [128, 2, D], BF16) for e in range(2)] for i in range(2)]
    NS1 = 2
    xin_s = [S.sbuf(f"xin{i}{L}", [128, D], F32) for i in range(NS1)]
    junk_s = [S.sbuf(f"junk{i}{L}", [128, D], F32) for i in range(NS1)]
    wk_s = [{"junk": junk_s[i], "st": S.sbuf(f"st{i}{L}", [128, 4], F32), "xn32": S.sbuf(f"xn32{i}{L}", [128, D], F32),
             "fm32": S.sbuf(f"fm32{i}{L}", [128, 8, 128], F32)} for i in range(NS1)]
    h32_s = [S.sbuf(f"h32{i}{L}", [128, 8, 128], F32) for i in range(NS1)]
    lg_s = [S.sbuf(f"lg{i}{L}", [128, 36], F32) for i in range(NS1)]
    sm_s = [S.sbuf(f"sm{i}{L}", [128, 64], F32) for i in range(NS1)]
    g32_s = [S.sbuf(f"g32{i}{L}", [128, 32], F32) for i in range(NS1)]
    xin3 = S.sbuf(f"xin3{L}", [128, D], F32)
    out3 = S.sbuf(f"out3{L}", [128, D], F32)
    s_sb = [S.sbuf(f"s_sb{i}{L}", [128, 256], F32) for i in range(2)]
    t_sb = [S.sbuf(f"t_sb{i}{L}", [128, 256], F32) for i in range(2)]
    hidT = [S.sbuf(f"hidT{i}{L}", [128, 256], BF16) for i in range(2)]

    def load_pair(p, buf):
        for e in range(2):
            eg = p * 2 + e
            for W, nm in ((Wg, "g"), (Wu, "u"), (Wd, "d")):
                t = W[buf][e]
                S.dma("sp", t.v(t.t[:, :, :].rearrange("p a b -> p (a b)")), G.wbf[f"{nm}{li}_{eg}"][:])

    n_super = len(tiles) // st_tiles
    assert n_super * st_tiles == len(tiles) and st_tiles % 2 == 0
    for su in range(n_super):
        stl = tiles[su * st_tiles:(su + 1) * st_tiles]
        load_pair(0, 0)
        def step1_gen(j, b, row0, r, s):
            xin, wk, h32, lg, sm, g32 = xin_s[s], wk_s[s], h32_s[s], lg_s[s], sm_s[s], g32_s[s]
            S.dma("sp", xin[:], xin_d.v(xin_d.t[b, row0:row0 + 128, :]), split=4)
            yield
            yield from norm_tile_gen(S, G, xin[:], r, G.A2, 24, h2T[:, :, j * 128:(j + 1) * 128], wk, h32[:], ysub[s])
            yield
            psr = (psG[0] if s == 0 else psU[0])[:, 0:36]
            for kc in range(8):
                S.mm(psr, h32[:, kc, :], Wrt[:, kc, :], start=(kc == 0), stop=False)
            S.mm(psr, G.ones_f[0:1, :], rb[:], start=False, stop=True)
            S.copy("dve", lg[:], psr)
            yield
            c = lambda i, n=1: sm[:, i:i + n]
            S.reduce(c(0), lg[:, 0:4], op=ALU.max)
            yield
            S.ts("dve", c(1), c(0), -1.0, None, op0=ALU.mult)
            yield
            S.ts("dve", c(4, 4), lg[:, 0:4], c(0), None, op0=ALU.is_ge)
            yield
            S.act(c(8, 4), lg[:, 0:4], AF.Exp, bias=c(1), accum_out=c(2))
            yield
            S.recip(c(3), c(2))
            yield
            S.ts("dve", c(16, 8), lg[:, 4:12], c(4), None, op0=ALU.mult)
            yield
            for g in range(1, 4):
                S.stt(c(16, 8), lg[:, 4 + 8 * g:12 + 8 * g], c(4 + g), c(16, 8), ALU.mult, ALU.add)
                yield
            S.reduce(c(12), c(16, 8), op=ALU.max)
            yield
            S.ts("dve", c(24, 8), c(16, 8), c(12), None, op0=ALU.is_ge)
            yield
            S.stt(c(32, 8), c(24, 8), -1e30, c(16, 8), ALU.mult, ALU.add)
            yield
            S.reduce(c(13), c(32, 8), op=ALU.max)
            yield
            S.ts("dve", c(40, 8), c(32, 8), c(13), None, op0=ALU.is_ge)
            yield
            S.tt("dve", c(14), c(13), c(12), ALU.subtract)
            yield
            S.act(c(15), c(14), AF.Exp)
            yield
            S.ts("dve", c(48), c(15), 1.0, None, op0=ALU.add)
            yield
            S.recip(c(49), c(48))
            yield
            S.tt("dve", c(50), c(49), c(3), ALU.mult)
            yield
            S.tt("dve", c(51), c(50), c(15), ALU.mult)
            yield
            S.ts("dve", c(52, 8), c(24, 8), c(50), None, op0=ALU.mult)
            yield
            S.stt(c(52, 8), c(40, 8), c(51), c(52, 8), ALU.mult, ALU.add)
            yield
            for g in range(4):
                S.ts("dve", g32[:, g * 8:(g + 1) * 8], c(52, 8), c(4 + g), None, op0=ALU.mult)
                yield
            pst = (psG[1] if s == 0 else psU[1])[0:32, 0:128]
            S.tr(pst, g32[:], G.ident_f[:])
            S.copy("dve", gatesT[:, j * 128:(j + 1) * 128], pst)
            if dbg is not None and "gates" in dbg and su == 0:
                S.dma("sp", dbg["gates"].v(dbg["gates"].t[j]), g32[:])

        pend1 = [step1_gen(j, b, row0, r, j % NS1) for j, (b, row0, r) in enumerate(stl)]
        act1 = []
        rnd, last1 = 0, -99
        while pend1 or act1:
            if pend1 and len(act1) < NS1 and (rnd - last1 >= 20 or not act1):
                act1.append(pend1.pop(0))
                last1 = rnd
            nxt = []
            for g_ in act1:
                try:
                    next(g_)
                    nxt.append(g_)
                except StopIteration:
                    pass
            act1 = nxt
            rnd += 1
        G.psA = ysub[0]
        items = [(p, t2, e, fc) for p in range(npairs) for t2 in range(st_tiles // 2) for e in range(2) for fc in range(2)]

        def emit_gu(idx):
            p, t2, e, fc = items[idx]
            buf, eg, ib = p % 2, p * 2 + e, idx % 2
            tok = slice(t2 * 256, (t2 + 1) * 256)
            pg, pu = psG[ib], psU[ib]
            for kc in range(8):
                S.mm(pg[:, 0:256], Wg[buf][e][:, kc, fc * 128:(fc + 1) * 128], h2T[:, kc, tok], start=(kc == 0), stop=(kc == 7))
            for kc in range(8):
                S.mm(pu[:, 0:256], Wu[buf][e][:, kc, fc * 128:(fc + 1) * 128], h2T[:, kc, tok], start=(kc == 0), stop=(kc == 7))
            S.mm(pu[:, 256:512], sel[:, eg, :], gatesT[:, tok])
            S.act(s_sb[ib][:], pg[:, 0:256], AF.Silu)
            S.tt("dve", t_sb[ib][:], s_sb[ib][:], pu[:, 0:256], ALU.mult)
            S.tt("dve", hidT[ib][:], t_sb[ib][:], pu[:, 256:512], ALU.mult)

        def emit_down(idx):
            p, t2, e, fc = items[idx]
            buf, ib, it = p % 2, idx % 2, e * 2 + fc
            for ts_ in range(2):
                for n in range(2):
                    S.mm(ysub[ts_][:, n * 512:(n + 1) * 512], hidT[ib][:, ts_ * 128:(ts_ + 1) * 128],
                         Wd[buf][e][:, fc, n * 512:(n + 1) * 512], start=(it == 0), stop=(it == 3))
            if it == 3:
                for ts_ in range(2):
                    j = t2 * 2 + ts_
                    if p == 0:
                        S.copy("dve", y_acc[:, j, :], ysub[ts_][:])
                    else:
                        S.tt("dve", y_acc[:, j, :], y_acc[:, j, :], ysub[ts_][:], ALU.add)
                    if p == npairs - 1:
                        b, row0, r = stl[j]
                        S.dma("sp", xin3[:], xin_d.v(xin_d.t[b, row0:row0 + 128, :]))
                        S.tt("pool", out3[:], y_acc[:, j, :], gate_bc[:, r, :], ALU.mult)
                        S.tt("pool", out3[:], out3[:], xin3[:], ALU.add)
                        S.dma("sp", xout_fn(b, row0), out3[:])

        for idx in range(len(items)):
            emit_gu(idx)
            if idx > 0:
                emit_down(idx - 1)
            p, t2, e, fc = items[idx]
            if t2 == 0 and e == 0 and fc == 0 and p + 1 < npairs:
                load_pair(p + 1, (p + 1) % 2)
        emit_down(len(items) - 1)
    S.pop_scope()

LAM_INIT1 = 0.8 - 0.6 * float(np.exp(-0.3 * 1))


def attn_phase(S, G, x2_d, x3_d, nb=NB, nqt=4, nh=8):
    li = 1
    NKT = NT
    S.push_scope()
    psA = S.psum("psA_a", [128, 1024], F32)
    psT = S.psum("psT_a", [128, 1024], BF16)
    psQ = [S.psum(f"psQ{i}_a", [128, 512], F32) for i in range(3)]
    psO = [S.psum(f"psO{i}_a", [128, 512], F32) for i in range(2)]
    G.psA, G.psT = psA, psT
    KT_all = S.sbuf("KT_all", [128, 8, TOK], BF16)
    QT_all = S.sbuf("QT_all", [128, 8, LAT], BF16)
    V_all = S.sbuf("V_all", [128, NKT, 8, 130], BF16)
    S.memset("pool", V_all[:], 1.0)
    gq_bc = S.sbuf("gq_bc", [128, 64], F32)
    gk_bc = S.sbuf("gk_bc", [128, 64], F32)
    sg_bc = S.sbuf("sg_bc", [128, 128], F32)
    S.dma("sp", gq_bc[:], G.da_q_norm_g.v(G.da_q_norm_g.t[0:1, :].partition_broadcast(128)))
    S.dma("sp", gk_bc[:], G.da_k_norm_g.v(G.da_k_norm_g.t[0:1, :].partition_broadcast(128)))
    S.dma("sp", sg_bc[:], G.da_subln_g.v(G.da_subln_g.t[0:1, :].partition_broadcast(128)))
    S.ts("dve", sg_bc[:], sg_bc[:], 1.0 - LAM_INIT1, None, op0=ALU.mult)
    lamv = S.sbuf("lamv", [128, 4, 64], F32)
    lsm = S.sbuf("lsm", [128, 8], F32)
    for i in range(4):
        S.dma("sp", lamv[:, i, :], G.da_lam.v(G.da_lam.t[i:i + 1, :].partition_broadcast(128)))
    S.tt("dve", lamv[:, 0, :], lamv[:, 0, :], lamv[:, 1, :], ALU.mult)
    S.tt("dve", lamv[:, 2, :], lamv[:, 2, :], lamv[:, 3, :], ALU.mult)
    S.reduce(lsm[:, 0:1], lamv[:, 0, :])
    S.reduce(lsm[:, 1:2], lamv[:, 2, :])
    S.act(lsm[:, 2:4], lsm[:, 0:2], AF.Exp)
    S.tt("dve", lsm[:, 4:5], lsm[:, 3:4], lsm[:, 2:3], ALU.subtract)
    S.ts("dve", lsm[:, 5:6], lsm[:, 4:5], -LAM_INIT1, None, op0=ALU.add)
    neglam = lsm[:, 5:6]
    junk = S.sbuf("junk_a", [128, D], F32)
    scrq = S.sbuf("scrq", [128, D], F32)
    xin = S.sbuf("xin_a", [128, D], F32)
    st = S.sbuf("st_a", [128, 4], F32)
    st16 = S.sbuf("st16_a", [128, 3, 16], F32)
    for b in range(nb):
        S.push_scope()
        Wqkv = S.sbuf(f"Wqkv{b}", [128, 8, 3 * D], BF16)
        S.dma("sp", Wqkv.v(Wqkv.t[:, :, :].rearrange("p a b -> p (a b)")), G.wbf["Wqkv"][:])
        hT_t = S.sbuf(f"hT_t{b}", [128, 8, 128], BF16)
        outq = S.sbuf(f"outq{b}", [128, D], BF16)
        wk = {"junk": junk, "st": st, "xn": S.sbuf(f"xn_a{b}", [128, D], BF16), "fm32": S.sbuf(f"fm32_a{b}", [128, 8, 128], F32)}
        cs_t = S.sbuf(f"cs_t{b}", [128, 2, 32], F32)
        tmpa = S.sbuf(f"tmpa{b}", [128, 512], F32)
        tmpb = S.sbuf(f"tmpb{b}", [128, 512], F32)

        qs_ = [dict(scrq=scrq, outq=outq, tmpa=tmpa, tmpb=tmpb, st16=st16, junk=junk)]
        qs_.append(dict(scrq=S.sbuf(f"scrq2_{b}", [128, D], F32), outq=S.sbuf(f"outq2_{b}", [128, D], BF16),
                        tmpa=S.sbuf(f"tmpa2_{b}", [128, 512], F32), tmpb=S.sbuf(f"tmpb2_{b}", [128, 512], F32),
                        st16=S.sbuf(f"st16_2_{b}", [128, 3, 16], F32), junk=S.sbuf(f"junk2_{b}", [128, D], F32)))
        wk["junk"] = S.sbuf(f"junkn_{b}", [128, D], F32)

        def qk_part1(X):
            s16, sq, jk = X["st16"], X["scrq"], X["junk"]
            S.act(jk[:], psA[:], AF.Square)
            S.reduce(s16[:, 0, :], jk.v(jk.t[:, :].rearrange("p (g n) -> p g n", n=64)))
            S.ts("dve", s16[:, 1, :], s16[:, 0, :], 1.0 / 64, EPS, op0=ALU.mult, op1=ALU.add)
            S.act(s16[:, 1, :], s16[:, 1, :], AF.Sqrt)
            S.recip(s16[:, 2, :], s16[:, 1, :])
            S.tt("dve", sq.v(sq.t[:, :].rearrange("p (g n) -> p g n", n=64)),
                 psA.v(psA.t[:, :].rearrange("p (g n) -> p g n", n=64)),
                 s16.v(s16.t[:, 2, :].unsqueeze(2).to_broadcast([128, 16, 64])), ALU.mult)

        def qk_part2a(X, gain_bc, rope):
            sq, oq, tmpa_, tmpb_ = X["scrq"], X["outq"], X["tmpa"], X["tmpb"]
            gv = gain_bc.v(gain_bc.t[:, :].unsqueeze(1).to_broadcast([128, 16, 64]))
            if not rope:
                S.tt("pool", oq.v(oq.t[:, :].rearrange("p (g n) -> p g n", n=64)),
                     sq.v(sq.t[:, :].rearrange("p (g n) -> p g n", n=64)), gv, ALU.mult)
                return
            S.tt("pool", sq.v(sq.t[:, :].rearrange("p (g n) -> p g n", n=64)),
                 sq.v(sq.t[:, :].rearrange("p (g n) -> p g n", n=64)), gv, ALU.mult)
            x5 = sq.t[:, :].rearrange("p (g a h f) -> p g a h f", g=16, a=2, h=2)
            o5 = oq.t[:, :].rearrange("p (g a h f) -> p g a h f", g=16, a=2, h=2)
            x1, x2 = sq.v(x5[:, :, :, 0, :]), sq.v(x5[:, :, :, 1, :])
            o1, o2 = oq.v(o5[:, :, :, 0, :]), oq.v(o5[:, :, :, 1, :])
            cv = cs_t.v(cs_t.t[:, 0, :].rearrange("p (a f) -> p a f", a=2).unsqueeze(1).to_broadcast([128, 16, 2, 16]))
            sv = cs_t.v(cs_t.t[:, 1, :].rearrange("p (a f) -> p a f", a=2).unsqueeze(1).to_broadcast([128, 16, 2, 16]))
            ta = tmpa_.v(tmpa_.t[:, :].rearrange("p (g a f) -> p g a f", g=16, a=2))
            tb = tmpb_.v(tmpb_.t[:, :].rearrange("p (g a f) -> p g a f", g=16, a=2))
            S.tt("dve", ta, x1, cv, ALU.mult)
            S.tt("pool", tb, x2, sv, ALU.mult)
            S.tt("dve", o1, ta, tb, ALU.subtract)
            S.tt("dve", ta, x1, sv, ALU.mult)
            S.tt("pool", tb, x2, cv, ALU.mult)
            S.tt("pool", o2, ta, tb, ALU.add)

        def qk_part2b(X, dst_fm):
            oq = X["outq"]
            for ec in range(8):
                S.tr(psT[:, ec * 128:(ec + 1) * 128], oq[:, ec * 128:(ec + 1) * 128], G.ident_b[:])
            S.copy("act", dst_fm, psT.v(psT.t[:, :].rearrange("p (a b) -> p a b", b=128)))

        def proj(c0):
            for n in range(2):
                for kc in range(8):
                    S.mm(psA[:, n * 512:(n + 1) * 512], hT_t[:, kc, :], Wqkv[:, kc, c0 + n * 512:c0 + (n + 1) * 512], start=(kc == 0), stop=(kc == 7))

        def load_norm(ti):
            r = 2 if ti < 2 else b
            S.dma("sp", xin[:], x2_d.v(x2_d.t[b, ti * 128:(ti + 1) * 128, :]), split=4)
            norm_tile_to_fm(S, G, xin[:], r, G.A1, 0, hT_t[:], wk)

        load_norm(0)
        pend_q = None
        for ti in range(NT):
            lat = ti >= 2
            if lat:
                t0 = (ti - 2) * 128
                S.dma("sp", cs_t[:, 0, :], G.rope_cos_d.v(G.rope_cos_d.t[t0:t0 + 128, :]))
                S.dma("sp", cs_t[:, 1, :], G.rope_sin_d.v(G.rope_sin_d.t[t0:t0 + 128, :]))
            proj(D)
            qk_part1(qs_[0])
            if pend_q is not None:
                qk_part2b(qs_[1], pend_q)
                pend_q = None
            proj(2 * D)
            S.copy("act", V_all[:, ti, :, 0:128], psA.v(psA.t[:, :].rearrange("p (h n) -> p h n", n=128)))
            if lat:
                proj(0)
            qk_part2a(qs_[0], gk_bc, lat)
            qk_part2b(qs_[0], KT_all[:, :, ti * 128:(ti + 1) * 128])
            if lat:
                qk_part1(qs_[1])
            if ti + 1 < NT:
                load_norm(ti + 1)
            if lat:
                qk_part2a(qs_[1], gq_bc, True)
                pend_q = QT_all[:, :, t0:t0 + 128]
        if pend_q is not None:
            qk_part2b(qs_[1], pend_q)
        S.pop_scope()
        S.push_scope()
        Wo = S.sbuf(f"Wo_a{b}", [128, 8, D], BF16)
        S.dma("sp", Wo.v(Wo.t[:, :, :].rearrange("p a b -> p (a b)")), G.wbf["daWo"][:])
        gate_bc = S.sbuf(f"gate_a{b}", [128, D], F32)
        S.dma("sp", gate_bc[:], G.gates_d.v(G.gates_d.t[0, b:b + 1, :].partition_broadcast(128)))
        O_all = S.sbuf(f"O_all{b}", [128, 4, D], F32)
        pT = [S.sbuf(f"pT{i}_{b}", [128, 512], BF16) for i in range(3)]
        rec = S.sbuf(f"rec{b}", [128, 8], F32)
        pre = S.sbuf(f"pre_a{b}", [128, D], BF16)
        preT = S.sbuf(f"preT_a{b}", [128, 8, 128], BF16)
        st8 = S.sbuf(f"st8_{b}", [128, 3, 8], F32)
        aitems = [(qt, h, m, kt) for qt in range(nqt) for h in range(nh) for m in range(2) for kt in range(NKT)]

        def emit_qk(i):
            qt, h, m, kt = aitems[i]
            pr = slice(m * 64, m * 64 + 64)
            ps = psQ[i % 3]
            S.mm(ps[:], KT_all[pr, h, kt * 128:(kt + 1) * 128], QT_all[pr, h, qt * 512:(qt + 1) * 512])
            S.act(pT[i % 3][:], ps[:], AF.Exp, scale=0.125)

        def emit_pv(i):
            qt, h, m, kt = aitems[i]
            for qs in range(4):
                acc = psO[qs // 2][:, (qs % 2) * 256:(qs % 2) * 256 + 129]
                S.mm(acc, pT[i % 3][:, qs * 128:(qs + 1) * 128], V_all[:, kt, h, 0:129],
                     start=(kt == 0 and qs % 2 == 0), stop=(kt == NKT - 1), skip_group_check=True)
            if kt != NKT - 1:
                return
            for qs in range(4):
                c0 = (qs % 2) * 256
                S.recip(rec[:, qs:qs + 1], psO[qs // 2][:, c0 + 128:c0 + 129])
                if m == 0:
                    S.ts("dve", O_all[:, qs, h * 128:(h + 1) * 128], psO[qs // 2][:, c0:c0 + 128], rec[:, qs:qs + 1], None, op0=ALU.mult)
                else:
                    S.tt("dve", rec[:, 4 + qs:5 + qs], rec[:, qs:qs + 1], neglam, ALU.mult)
                    S.stt(O_all[:, qs, h * 128:(h + 1) * 128], psO[qs // 2][:, c0:c0 + 128], rec[:, 4 + qs:5 + qs],
                          O_all[:, qs, h * 128:(h + 1) * 128], ALU.mult, ALU.add)
            if not (h == nh - 1 and m == 1):
                return
            for qs in range(4):
                O3 = O_all.v(O_all.t[:, qs, :].rearrange("p (h n) -> p h n", n=128))
                S.act(junk[:], O_all[:, qs, :], AF.Square)
                S.reduce(st8[:, 0, :], junk.v(junk.t[:, :].rearrange("p (h n) -> p h n", n=128)))
                S.ts("dve", st8[:, 1, :], st8[:, 0, :], 1.0 / 128, EPS, op0=ALU.mult, op1=ALU.add)
                S.act(st8[:, 1, :], st8[:, 1, :], AF.Sqrt)
                S.recip(st8[:, 2, :], st8[:, 1, :])
                S.tt("dve", O3, O3, st8.v(st8.t[:, 2, :].unsqueeze(2).to_broadcast([128, 8, 128])), ALU.mult)
                S.tt("pool", pre.v(pre.t[:, :].rearrange("p (h n) -> p h n", n=128)), O3,
                     sg_bc.v(sg_bc.t[:, :].unsqueeze(1).to_broadcast([128, 8, 128])), ALU.mult)
                for ec in range(8):
                    S.tr(psT[:, ec * 128:(ec + 1) * 128], pre[:, ec * 128:(ec + 1) * 128], G.ident_b[:])
                S.copy("act", preT[:], psT.v(psT.t[:, :].rearrange("p (a b) -> p a b", b=128)))
                for n in range(2):
                    for kc in range(8):
                        S.mm(psA[:, n * 512:(n + 1) * 512], preT[:, kc, :], Wo[:, kc, n * 512:(n + 1) * 512], start=(kc == 0), stop=(kc == 7))
                row = (qt * 4 + qs) * 128
                S.dma("sp", xin[:], x2_d.v(x2_d.t[b, CTX + row:CTX + row + 128, :]))
                S.tt("dve", scrq[:], psA[:], gate_bc[:], ALU.mult)
                S.tt("pool", xin[:], xin[:], scrq[:], ALU.add)
                S.dma("sp", x3_d.v(x3_d.t[b, row:row + 128, :]), xin[:])

        SK = 2
        for i in range(len(aitems) + SK):
            if i < len(aitems):
                emit_qk(i)
            if i >= SK:
                emit_pv(i - SK)
        S.pop_scope()
    S.pop_scope()

def build(cfg):
    nc = bass.Bass("TRN2", target_bir_lowering=False)
    S = Sched(nc)
    G = Ctx()
    setup_common(S, G, need=cfg.get("need", ("rwkv", "da", "moe")))
    outs = []
    dbg = {}
    stop = cfg.get("stop", "end")
    kind_x1 = "ExternalOutput" if stop == "rwkv" else "Internal"
    x1_d = S.dram("x1_d", [NB, TOK, D], F32, kind=kind_x1)
    if cfg.get("dbg_hT"):
        dbg["hT"] = S.dram("dbg_hT", [128, 8, LAT + 2], BF16, kind="ExternalOutput")
    wcast_phase(S, G, cfg.get("need", ("rwkv", "da", "moe")))
    if cfg.get("attn_in_ext"):
        G.x2_ext = S.dram("x2_ext", [NB, TOK, D], F32, kind="ExternalInput")
    if cfg.get("moe_in_ext"):
        G.x1_ext = S.dram("x1_ext", [NB, TOK, D], F32, kind="ExternalInput")
    if not cfg.get("skip_l0"):
        mod_phase(S, G, 0)
    if cfg.get("dbg_mod"):
        dm = S.dram("dbg_modT", [128, 48 * 4], F32, kind="ExternalOutput")
        S.dma("sp", dm[:], G.modT.v(G.modT.t[:, :, :].rearrange("p a b -> p (a b)")))
        dg = S.dram("dbg_gates", [2, 3, D], F32, kind="ExternalOutput")
        S.dma("sp", dg[:], G.gates_d[:])
    if stop == "mod":
        S.barrier()
        S.emit()
        return nc, S
    if not cfg.get("skip_rwkv") and not cfg.get("skip_l0"):
        rwkv_phase(S, G, x1_d, dbg=dbg, nb=cfg.get("nb", NB), **cfg.get("rw", {}))
    if stop == "rwkv":
        S.barrier()
        S.emit()
        return nc, S
    x2_d = S.dram("x2_d", [NB, TOK, D], F32, kind="ExternalOutput" if stop == "moe0" else "Internal")
    if not cfg.get("skip_l0"):
        moe0 = True
    else:
        moe0 = False
    tiles0 = [(b, ti * 128, 2 if ti < 2 else b) for b in range(NB) for ti in range(NT)]
    if cfg.get("dbg_gates"):
        dbg["gates"] = S.dram("dbg_gates32", [12, 128, 32], F32, kind="ExternalOutput")
    if moe0:
      moe_phase(S, G, 0, x1_d if not cfg.get("moe_in_ext") else G.x1_ext, tiles0[:cfg.get("moe_ntiles", len(tiles0))],
                lambda b, row0: x2_d.v(x2_d.t[b, row0:row0 + 128, :]), cfg.get("st0", 12), npairs=cfg.get("npairs", 16), dbg=dbg)
    if stop == "moe0":
        S.barrier()
        S.emit()
        return nc, S
    x3_d = S.dram("x3_d", [NB, LAT, D], F32, kind="ExternalOutput" if stop == "attn" else "Internal")
    mod_phase(S, G, 1)
    attn_phase(S, G, x2_d if not cfg.get("attn_in_ext") else G.x2_ext, x3_d, **cfg.get("at", {}))
    if stop == "attn":
        S.barrier()
        S.emit()
        return nc, S
    out_d = S.dram("out", [NB, LAT, D], F32, kind="ExternalOutput")
    tiles1 = [(b, ti * 128, b) for b in range(NB) for ti in range(LAT // 128)]
    moe_phase(S, G, 1, x3_d, tiles1, lambda b, row0: out_d.v(out_d.t[b, row0:row0 + 128, :]), cfg.get("st1", 8))
    S.barrier()
    S.emit()
    return nc, S


def prep_core_inputs(inputs, core, consts):
    b0 = core * NB
    f = lambda a: np.ascontiguousarray(a, dtype=np.float32)
    m = {}
    m["x"] = f(inputs["x"][b0:b0 + NB])
    m["ctx"] = f(inputs["ctx"][b0:b0 + NB])
    m["c3"] = f(np.concatenate([inputs["c"][b0:b0 + NB], inputs["c_ctx"][None, :]], axis=0))
    pv = np.concatenate([
        inputs["norm1_g"].reshape(16, 128), inputs["norm2_g"].reshape(16, 128),
        inputs["rw_mu"][0].reshape(48, 128), inputs["b_mod"].reshape(96, 128)], axis=0)
    m["pvec"] = f(pv)
    m["w_mod"] = f(inputs["w_mod"])
    m["b_mod"] = f(inputs["b_mod"])
    for k, v in consts.items():
        m[k] = v
    m["rw_w_rkv"] = f(inputs["rw_w_rkv"][0])
    for k in ("rw_w0", "rw_w1", "rw_w2", "rw_a0", "rw_a1", "rw_a2", "rw_g1", "rw_g2", "rw_w_o"):
        m[k] = f(inputs[k][0])
    for k in ("rw_k_k", "rw_k_a", "rw_lnx_g", "rw_lnx_b"):
        m[k] = f(inputs[k][0].reshape(1, D))
    m["rw_r_k"] = f(inputs["rw_r_k"][0].reshape(1, D))
    m["da_w_qkv"] = f(inputs["da_w_qkv"][0])
    m["da_q_norm_g"] = f(inputs["da_q_norm_g"][0].reshape(1, 64))
    m["da_k_norm_g"] = f(inputs["da_k_norm_g"][0].reshape(1, 64))
    m["da_lam"] = f(np.stack([inputs["da_lam_q1"][0], inputs["da_lam_k1"][0], inputs["da_lam_q2"][0], inputs["da_lam_k2"][0]]))
    m["da_subln_g"] = f(inputs["da_subln_g"][0].reshape(1, 128))
    m["da_w_o"] = f(inputs["da_w_o"][0])
    rt = np.concatenate([inputs["moe_router_g"], np.transpose(inputs["moe_router_e"], (0, 2, 1, 3)).reshape(2, D, 32)], axis=2)
    m["moe_router"] = f(rt)
    rb = np.concatenate([inputs["moe_router_g_b"], inputs["moe_router_e_b"].reshape(2, 32)], axis=1).reshape(2, 1, 36)
    m["moe_router_b"] = f(rb)
    m["moe_w_gate"] = f(inputs["moe_w_gate"]).reshape(2, NEXP, D, DFF)
    m["moe_w_up"] = f(inputs["moe_w_up"]).reshape(2, NEXP, D, DFF)
    m["moe_w_down"] = f(inputs["moe_w_down"]).reshape(2, NEXP, DFF, D)
    return m


_CACHE = {}


def kernel(**inputs):
    from concourse.bass_utils import run_bass_kernel_spmd
    n = 8
    if "nc" not in _CACHE:
        _CACHE["nc"] = build({})[0]
        _CACHE["consts"] = host_consts()
    nc = _CACHE["nc"]
    consts = _CACHE["consts"]
    inputs = {k: np.asarray(v) for k, v in inputs.items()}
    in_maps = [prep_core_inputs(inputs, c, consts) for c in range(n)]
    res = run_bass_kernel_spmd(nc, in_maps, core_ids=list(range(n)))
    out = np.concatenate([r["out"] for r in res.results], axis=0)
    return out.astype(np.float32)
```

```python
import numpy as np
import concourse.bass as bass
import concourse.mybir as mybir

F32 = mybir.dt.float32
BF16 = mybir.dt.bfloat16
I32 = mybir.dt.int32
U32 = mybir.dt.uint32
AF = mybir.ActivationFunctionType
ALU = mybir.AluOpType
AX = mybir.AxisListType

ENGS = ("pe", "dve", "act", "pool", "sp")
SEM_LIMIT = 30000
N_DMA_SEMS = 28
N_POOL_SEMS = 4


class V:
    __slots__ = ("ap", "toks", "excl")

    def __init__(self, ap, toks, excl=False):
        self.ap = ap
        self.toks = toks
        self.excl = excl


class Tl:
    def __init__(self, S, t, name, excl=False, toks=None):
        self.S = S
        self.t = t
        self.name = name
        self.excl = excl
        self.toks = toks

    def _tk(self, key):
        if self.toks is not None:
            return list(self.toks)
        return [(self.name, None if self.excl else key)]

    def __getitem__(self, idx):
        return V(self.t[idx], self._tk(None), self.excl)

    def k(self, key, idx=None):
        ap = self.t[idx] if idx is not None else None
        return V(ap, self._tk(key), self.excl)

    def v(self, ap, key=None):
        return V(ap, self._tk(key), self.excl)


class Sched:
    def __init__(self, nc, same_engine_sync=True):
        self.nc = nc
        self.q = {e: [] for e in ENGS}
        self.cnt = {e: 0 for e in ENGS}
        self.semi = {e: 0 for e in ENGS}
        self.nsem = {e: 1 for e in ENGS}
        self.state = {}
        self.waited = {e: {} for e in ENGS}
        self.same = same_engine_sync
        self.dma_i = 0
        self.dma_cnt = [0] * N_DMA_SEMS
        self.dma_last = [None] * N_DMA_SEMS
        self.ctx = []
        self.n_instr = 0
        self.out_deps = []

    def sbuf(self, name, shape, dtype=F32):
        cm = self.nc.sbuf_tensor(name, list(shape), dtype)
        t = cm.__enter__()
        self.ctx.append(cm)
        return Tl(self, t, name)

    def psum(self, name, shape, dtype=F32):
        cm = self.nc.psum_tensor(name, list(shape), dtype)
        t = cm.__enter__()
        self.ctx.append(cm)
        return Tl(self, t, name, excl=True)

    def dram(self, name, shape, dtype=F32, kind="Internal"):
        t = self.nc.dram_tensor(name, list(shape), dtype, kind=kind)
        return Tl(self, t.ap(), name)

    def push_scope(self):
        self.scopes = getattr(self, "scopes", [])
        self.scopes.append(len(self.ctx))

    def pop_scope(self):
        self.barrier()
        n = self.scopes.pop()
        while len(self.ctx) > n:
            self.ctx.pop().__exit__(None, None, None)

    def barrier(self):
        deps = []
        for e in ENGS:
            if self.cnt[e] > 0:
                deps.append(((e, self.semi[e]), self.cnt[e], e))
        for i in range(N_DMA_SEMS):
            if self.dma_last[i] is not None:
                deps.append(self.dma_last[i])
        for e in ENGS:
            d = [x for x in deps if x[2] != e]
            w = self._waits(e, d)
            if w:
                self.q[e].append(("wait", None, w, None))

    def _deps(self, reads, writes):
        deps = []
        for v in reads:
            for tok in v.toks:
                st = self.state.get(tok)
                if st and st[0] is not None:
                    deps.append(st[0])
        for v in writes:
            for tok in v.toks:
                st = self.state.get(tok)
                if st:
                    if st[0] is not None:
                        deps.append(st[0])
                    deps.extend(st[1])
        return deps

    def _update(self, reads, writes, me, real_writes=None):
        for v in reads:
            for tok in v.toks:
                st = self.state.setdefault(tok, [None, [], None])
                st[1].append(me)
                if len(st[1]) > 64:
                    best = {}
                    for d in st[1]:
                        if d[0] not in best or best[d[0]][1] < d[1]:
                            best[d[0]] = d
                    st[1] = list(best.values())
        rw = writes if real_writes is None else real_writes
        rwt = set()
        for v in rw:
            rwt.update(v.toks)
        for v in writes:
            for tok in v.toks:
                old = self.state.get(tok)
                lrw = me if tok in rwt else (old[2] if old else None)
                self.state[tok] = [me, [], lrw]

    def _waits(self, eng, deps, raw_toks_same=None):
        need = {}
        for d in deps:
            semkey, val, deng, is_raw = d[0], d[1], d[2], True
            if deng == eng and not self.same:
                continue
            w = self.waited[eng].get(semkey, 0)
            if val > w and val > need.get(semkey, 0):
                need[semkey] = val
        for sk, val in need.items():
            self.waited[eng][sk] = val
        return list(need.items())

    def op(self, eng, fn, reads=(), writes=(), same_ok=False):
        reads = list(reads)
        real_writes = list(writes)
        writes = real_writes + [v for v in reads if v.excl]
        deps = self._deps(reads, writes)
        if same_ok or eng == "pe":
            deps = [d for d in deps if d[2] != eng]
        elif self.same:
            rd = []
            for v in reads:
                for tok in v.toks:
                    st = self.state.get(tok)
                    if st and st[2] is not None and st[2][2] == eng:
                        rd.append(st[2])
            deps = [d for d in deps if d[2] != eng] + rd
        waits = self._waits(eng, deps)
        if self.cnt[eng] >= SEM_LIMIT:
            self.semi[eng] += 1
            self.nsem[eng] = max(self.nsem[eng], self.semi[eng] + 1)
            self.cnt[eng] = 0
        self.cnt[eng] += 1
        me = ((eng, self.semi[eng]), self.cnt[eng], eng)
        self.q[eng].append(("op", fn, waits, me[0]))
        self._update(reads, writes, me, real_writes)
        self.n_instr += 1
        return me

    def dma(self, queue, out, in_, split=0, **kw):
        reads, writes = [in_], [out]
        deps = self._deps(reads, writes)
        if queue == "pool":
            self.dma_ip = getattr(self, "dma_ip", 0)
            i = N_DMA_SEMS - N_POOL_SEMS + (self.dma_ip % N_POOL_SEMS)
            self.dma_ip += 1
        else:
            i = self.dma_i % (N_DMA_SEMS - N_POOL_SEMS)
            self.dma_i += 1
        if self.dma_last[i] is not None:
            deps.append(self.dma_last[i])
        waits = self._waits(queue, deps)
        oa, ia = out.ap, in_.ap
        parts = [(oa, ia)]
        if split:
            step = 128 // split
            parts = [(oa[k * step:(k + 1) * step], ia[k * step:(k + 1) * step]) for k in range(split)]
        for k, (o_, a_) in enumerate(parts):
            self.dma_cnt[i] += 16
            self.q[queue].append(("dma", (o_, a_, kw), waits if k == 0 else [], ("dma", i)))
            self.n_instr += 1
        me = (("dma", i), self.dma_cnt[i], "dma")
        self.dma_last[i] = me
        self._update(reads, writes, me)
        return me

    def emit(self, final_deps=None):
        nc = self.nc
        sems = {}
        cms = []

        def mk(name):
            cm = nc.semaphore(name)
            s = cm.__enter__()
            cms.append(cm)
            return s
        for e in ENGS:
            for i in range(self.nsem[e]):
                sems[(e, i)] = mk(f"s_{e}_{i}")
        for i in range(N_DMA_SEMS):
            sems[("dma", i)] = mk(f"s_dma_{i}")
        engobj = {"pe": "tensor", "dve": "vector", "act": "scalar", "pool": "gpsimd", "sp": "sync"}
        if final_deps:
            w = self._waits("sp", final_deps)
            self.q["sp"].append(("wait", None, w, None))
        with nc.Block() as block:
            for e in ENGS:
                items = self.q[e]
                if not items:
                    continue

                def body(eng, items=items):
                    for kind, fn, waits, semkey in items:
                        for sk, val in waits:
                            eng.wait_ge(sems[sk], val)
                        if kind == "op":
                            ins = fn(eng)
                            ins.then_inc(sems[semkey], 1)
                        elif kind == "dma":
                            oa, ia, kw = fn
                            eng.dma_start(out=oa, in_=ia, **kw).then_inc(sems[semkey], 16)
                getattr(block, engobj[e])(body)
        for cm in reversed(cms):
            cm.__exit__(None, None, None)
        for cm in reversed(self.ctx):
            cm.__exit__(None, None, None)

    def mm(self, out, lhsT, rhs, start=True, stop=True, **kw):
        return self.op("pe", lambda e: e.matmul(out.ap, lhsT.ap, rhs.ap, start=start, stop=stop, **kw),
                       reads=[lhsT, rhs] + ([] if start else [out]), writes=[out])

    def tr(self, out, in_, ident):
        return self.op("pe", lambda e: e.transpose(out.ap, in_.ap, ident.ap),
                       reads=[in_, ident], writes=[out])

    def act(self, out, in_, func, bias=None, scale=None, accum_out=None, eng="act"):
        reads = [in_]
        kw = {}
        if isinstance(bias, V):
            reads.append(bias)
            kw["bias"] = bias.ap
        elif bias is not None:
            kw["bias"] = bias
        if isinstance(scale, V):
            reads.append(scale)
            kw["scale"] = scale.ap
        elif scale is not None:
            kw["scale"] = scale
        writes = [out]
        if accum_out is not None:
            writes.append(accum_out)
            kw["accum_out"] = accum_out.ap
        return self.op("act", lambda e: e.activation(out.ap, in_.ap, func, **kw), reads=reads, writes=writes)

    def tt(self, eng, out, in0, in1, op):
        return self.op(eng, lambda e: e.tensor_tensor(out.ap, in0.ap, in1.ap, op), reads=[in0, in1], writes=[out])

    def ts(self, eng, out, in0, s1, s2=None, op0=ALU.mult, op1=None, accum_out=None):
        reads = [in0]
        a1 = s1.ap if isinstance(s1, V) else s1
        a2 = s2.ap if isinstance(s2, V) else s2
        if isinstance(s1, V):
            reads.append(s1)
        if isinstance(s2, V):
            reads.append(s2)
        writes = [out]
        kw = {}
        if op1 is not None:
            kw["op1"] = op1
        if accum_out is not None:
            kw["accum_out"] = accum_out.ap
            writes.append(accum_out)
        return self.op(eng, lambda e: e.tensor_scalar(out.ap, in0.ap, a1, a2, op0, **kw), reads=reads, writes=writes)

    def stt(self, out, in0, scalar, in1, op0, op1, eng="dve"):
        reads = [in0, in1]
        a = scalar.ap if isinstance(scalar, V) else scalar
        if isinstance(scalar, V):
            reads.append(scalar)
        return self.op(eng, lambda e: e.scalar_tensor_tensor(out.ap, in0.ap, a, in1.ap, op0, op1), reads=reads, writes=[out])

    def copy(self, eng, out, in_):
        if eng == "act":
            return self.op("act", lambda e: e.copy(out.ap, in_.ap), reads=[in_], writes=[out])
        return self.op(eng, lambda e: e.tensor_copy(out.ap, in_.ap), reads=[in_], writes=[out])

    def memset(self, eng, out, val):
        return self.op(eng, lambda e: e.memset(out.ap, val), reads=[], writes=[out])

    def reduce(self, out, in_, op=ALU.add, axis=AX.X, eng="dve"):
        return self.op(eng, lambda e: e.tensor_reduce(out.ap, in_.ap, axis, op), reads=[in_], writes=[out])

    def recip(self, out, in_):
        return self.op("dve", lambda e: e.reciprocal(out.ap, in_.ap), reads=[in_], writes=[out])
D = 1024
NB = 2
LAT = 2048
CTX = 256
TOK = CTX + LAT
NT = TOK // 128
EPS = 1e-6
DECAY_C = -0.6065306597126334
LNX_EPS = 64e-5
NGRP = 4
NEXP = 32
DFF = 256

PV_N1 = (0, 16)
PV_N2 = (16, 32)
PV_MU = 32
PV_BMOD = 80
PV_ROWS = 176


def host_consts():
    c = {}
    c["ident_f"] = np.eye(128, dtype=np.float32)
    c["ones_f"] = np.ones((128, 128), dtype=np.float32)
    idx = np.arange(128)
    cs, ct = idx[:, None] // 64, idx[None, :] // 64
    same = (cs == ct)
    s_, t_ = idx[:, None], idx[None, :]
    maskA = np.zeros((2, 128, 256), np.float32)
    maskAT = np.zeros((2, 128, 128), np.float32)
    tri = np.zeros((2, 128, 384), np.float32)
    for z in range(2):
        prec = same & ((s_ < t_) if z == 0 else (s_ > t_))
        preceq = same & ((s_ <= t_) if z == 0 else (s_ >= t_))
        succ = same & ((s_ > t_) if z == 0 else (s_ < t_))
        maskA[z, :, 0:128] = prec
        maskA[z, :, 128:256] = preceq
        maskAT[z] = prec.T
        tri[z, :, 0:128] = preceq * DECAY_C
        tri[z, :, 128:256] = prec * DECAY_C
        tri[z, :, 256:384] = succ * DECAY_C
    c["maskA"] = maskA
    c["maskAT"] = maskAT
    c["tri"] = tri
    ind = np.zeros((128, 2), np.float32)
    ind[0:64, 0] = DECAY_C
    ind[64:128, 1] = DECAY_C
    c["ind"] = ind
    rows = LAT // 64
    row = np.repeat(np.arange(rows), 64)
    col = np.tile(np.arange(64), rows)
    inv = (10000.0 ** (-np.arange(16, dtype=np.float32) / 16)).astype(np.float32)
    ang = np.stack([row, col], axis=-1).astype(np.float32)[:, :, None] * inv
    c["rope_cos"] = np.cos(ang).astype(np.float32).reshape(LAT, 32)
    c["rope_sin"] = np.sin(ang).astype(np.float32).reshape(LAT, 32)
    sel = np.zeros((32, 32, 128), np.float32)
    for e in range(32):
        sel[e, e, :] = 1.0
    c["sel"] = sel.reshape(32, 32 * 128)
    return c


class Ctx:
    pass


def std_psum(S, G, tag):
    G.psA = S.psum(f"psA{tag}", [128, 1024], F32)
    nm = G.psA.name
    G.psA.toks = [(nm, 0), (nm, 1)]
    G.psA_h = [Tl(S, G.psA.t[:, i * 512:(i + 1) * 512].rearrange("p (a b) -> p a b", b=128), nm, excl=True,
                  toks=[(nm, i)]) for i in range(2)]
    G.psY = S.psum(f"psY{tag}", [128, 1024], F32)
    nmy = G.psY.name
    G.psY.toks = [(nmy, 0), (nmy, 1)]
    G.psY_h = [Tl(S, G.psY.t[:, i * 512:(i + 1) * 512].rearrange("p (a b) -> p a b", b=128), nmy, excl=True,
                  toks=[(nmy, i)]) for i in range(2)]
    G.psT = S.psum(f"psT{tag}", [128, 1024], BF16)
    G.psH = [S.psum(f"psH{i}{tag}", [128, 4, 128], F32) for i in range(3)]


def setup_common(S, G, need=("rwkv", "da", "moe")):
    def I(name, shape, dt=F32):
        grp = name.split('_')[0]
        if grp in ('rw', 'da', 'moe') and {'rw': 'rwkv', 'da': 'da', 'moe': 'moe'}[grp] not in need:
            return None
        return S.dram(name, shape, dt, kind="ExternalInput")
    G.x = I("x", [NB, LAT, D])
    G.ctx = I("ctx", [NB, CTX, D])
    G.c3 = I("c3", [3, D])
    G.pvec = I("pvec", [PV_ROWS, 128])
    G.w_mod = I("w_mod", [2, D, 6 * D])
    G.b_mod = I("b_mod", [2, 6 * D])
    G.ident_f_d = I("ident_f", [128, 128])
    G.ones_f_d = I("ones_f", [128, 128])
    G.maskA_d = I("maskA", [2, 128, 256])
    G.maskAT_d = I("maskAT", [2, 128, 128])
    G.tri_d = I("tri", [2, 128, 384])
    G.ind_d = I("ind", [128, 2])
    G.rope_cos_d = I("rope_cos", [LAT, 32])
    G.rope_sin_d = I("rope_sin", [LAT, 32])
    G.sel_d = I("sel", [32, 32 * 128])
    G.rw_w_rkv = I("rw_w_rkv", [3, D, D])
    G.rw_w0 = I("rw_w0", [2, D])
    G.rw_w1 = I("rw_w1", [2, D, 64])
    G.rw_w2 = I("rw_w2", [2, 64, D])
    G.rw_a0 = I("rw_a0", [2, D])
    G.rw_a1 = I("rw_a1", [2, D, 64])
    G.rw_a2 = I("rw_a2", [2, 64, D])
    G.rw_g1 = I("rw_g1", [D, 128])
    G.rw_g2 = I("rw_g2", [128, D])
    G.rw_k_k = I("rw_k_k", [1, D])
    G.rw_k_a = I("rw_k_a", [1, D])
    G.rw_r_k = I("rw_r_k", [1, D])
    G.rw_lnx_g = I("rw_lnx_g", [1, D])
    G.rw_lnx_b = I("rw_lnx_b", [1, D])
    G.rw_w_o = I("rw_w_o", [D, D])
    G.da_w_qkv = I("da_w_qkv", [D, 3 * D])
    G.da_q_norm_g = I("da_q_norm_g", [1, 64])
    G.da_k_norm_g = I("da_k_norm_g", [1, 64])
    G.da_lam = I("da_lam", [4, 64])
    G.da_subln_g = I("da_subln_g", [1, 128])
    G.da_w_o = I("da_w_o", [D, D])
    G.moe_router = I("moe_router", [2, D, 36])
    G.moe_router_b = I("moe_router_b", [2, 1, 36])
    G.moe_w_gate = I("moe_w_gate", [2, NEXP, D, DFF])
    G.moe_w_up = I("moe_w_up", [2, NEXP, D, DFF])
    G.moe_w_down = I("moe_w_down", [2, NEXP, DFF, D])

    G.ident_f = S.sbuf("ident_f_s", [128, 128], F32)
    G.ident_b = S.sbuf("ident_b_s", [128, 128], BF16)
    G.ones_f = S.sbuf("ones_f_s", [128, 128], F32)
    S.dma("sp", G.ident_f[:], G.ident_f_d[:])
    S.dma("pool", G.ident_b[:], G.ident_f_d[:])
    S.dma("sp", G.ones_f[:], G.ones_f_d[:])
    G.pfm = S.sbuf("pfm", [128, PV_ROWS], F32)
    S.push_scope()
    std_psum(S, G, "c")
    pv = S.sbuf("pv_ld", [128, 2, 128], F32)
    S.memset("dve", pv[:], 0.0)
    S.dma("sp", pv[:, 0, :], G.pvec[0:128, :])
    S.dma("sp", pv[0:PV_ROWS - 128, 1, :], G.pvec[128:PV_ROWS, :])
    S.tr(G.psA[:, 0:128], pv[:, 0, :], G.ident_f[:])
    S.tr(G.psA[:, 128:256], pv[:, 1, :], G.ident_f[:])
    S.copy("dve", G.pfm[:], G.psA[:, 0:PV_ROWS])
    S.pop_scope()
    G.modT = S.sbuf("modT", [128, 48, 4], F32)
    G.A1 = S.sbuf("A1", [128, 3, 8], F32)
    G.A2 = S.sbuf("A2", [128, 3, 8], F32)
    G.gates_d = S.dram("gates_d", [2, 3, D], F32)


def mod_phase(S, G, li):
    S.push_scope()
    std_psum(S, G, f"m{li}")
    crow = S.sbuf(f"crow{li}", [4, D], F32)
    sc = S.sbuf(f"sc{li}", [4, D], F32)
    scT = S.sbuf(f"scT{li}", [128, 8, 4], F32)
    brow = S.sbuf(f"brow{li}", [1, 6 * D], F32)
    grow = S.sbuf(f"grow{li}", [4, 512], F32)
    wblk = [S.sbuf(f"wblk{i}_{li}", [128, 8, 512], F32) for i in range(2)]
    S.memset("dve", crow[:], 0.0)
    S.dma("sp", crow[0:3, :], G.c3[:])
    S.dma("sp", brow[:], G.b_mod[li:li + 1, :])
    S.act(sc[:], crow[:], AF.Silu)
    for kc in range(8):
        S.tr(G.psA[:, kc * 4:(kc + 1) * 4], sc[0:4, kc * 128:(kc + 1) * 128], G.ident_f[0:4, 0:4])
    S.copy("dve", scT[:], G.psA.v(G.psA.t[:, 0:32].rearrange("p (a b) -> p a b", b=4)))
    wsrc = G.w_mod.t[li].rearrange("(kc p) n -> p kc n", p=128)
    gate_blocks = {4: (0, 0), 5: (0, 1), 10: (1, 0), 11: (1, 1)}
    for blk in range(12):
        wb = wblk[blk % 2]
        S.dma("sp", wb[:], G.w_mod.v(wsrc[:, :, blk * 512:(blk + 1) * 512]), split=8)
        if blk in gate_blocks:
            which, half = gate_blocks[blk]
            ps = G.psY[0:4, 0:512]
            for kc in range(8):
                S.mm(ps, scT[:, kc, :], wb[:, kc, :], start=(kc == 0), stop=False)
            S.mm(ps, G.ones_f[0:1, 0:4], brow[0:1, blk * 512:(blk + 1) * 512], start=False, stop=True)
            S.copy("dve", grow[:], ps)
            S.dma("sp", G.gates_d.v(G.gates_d.t[which, :, half * 512:(half + 1) * 512]), grow[0:3, :])
        else:
            for ec in range(4):
                ch = blk * 4 + ec
                ps = G.psA[:, ch * 4:(ch + 1) * 4]
                for kc in range(8):
                    S.mm(ps, wb[:, kc, ec * 128:(ec + 1) * 128], scT[:, kc, :], start=(kc == 0), stop=(kc == 7))
                S.ts("dve", G.modT[:, ch, :], ps, G.pfm[:, PV_BMOD + li * 48 + ch:PV_BMOD + li * 48 + ch + 1], None, op0=ALU.add)
    for r in range(3):
        for (A, sc0, gofs) in ((G.A1, 8, PV_N1[0] + li * 8), (G.A2, 32, PV_N2[0] + li * 8)):
            S.ts("dve", A[:, r, :], G.modT[:, sc0:sc0 + 8, r], 1.0, None, op0=ALU.add)
            S.tt("dve", A[:, r, :], A[:, r, :], G.pfm[:, gofs:gofs + 8], ALU.mult)
    S.pop_scope()


def norm_tile_to_fm(S, G, xt, r, A, shift_ch0, out_fm, wk, fp32_out=None):
    st = wk["st"]
    S.act(wk["junk"][:], xt, AF.Square, accum_out=st[:, 0:1])
    S.ts("dve", st[:, 1:2], st[:, 0:1], 1.0 / D, EPS, op0=ALU.mult, op1=ALU.add)
    S.act(st[:, 2:3], st[:, 1:2], AF.Sqrt)
    S.recip(st[:, 3:4], st[:, 2:3])
    if fp32_out is None:
        xn = wk["xn"]
        S.ts("dve", xn[:], xt, st[:, 3:4], None, op0=ALU.mult)
        for kc in range(8):
            S.tr(G.psT[:, kc * 128:(kc + 1) * 128], xn[:, kc * 128:(kc + 1) * 128], G.ident_b[:])
        src = G.psT.v(G.psT.t[:, :].rearrange("p (a b) -> p a b", b=128))
    else:
        xn = wk["xn32"]
        S.ts("dve", xn[:], xt, st[:, 3:4], None, op0=ALU.mult)
        for kc in range(8):
            S.tr(G.psA[:, kc * 128:(kc + 1) * 128], xn[:, kc * 128:(kc + 1) * 128], G.ident_f[:])
        src = G.psA.v(G.psA.t[:, :].rearrange("p (a b) -> p a b", b=128))
    Abc = A.v(A.t[:, r, :].unsqueeze(2).to_broadcast([128, 8, 128]))
    shbc = G.modT.v(G.modT.t[:, shift_ch0:shift_ch0 + 8, r].unsqueeze(2).to_broadcast([128, 8, 128]))
    tmp = wk["fm32"]
    S.tt("dve", tmp[:], src, Abc, ALU.mult)
    if fp32_out is not None:
        S.tt("pool", fp32_out, tmp[:], shbc, ALU.add)
        S.copy("act", out_fm, fp32_out)
    else:
        S.tt("pool", out_fm, tmp[:], shbc, ALU.add)


def norm_tile_gen(S, G, xt, r, A, shift_ch0, out_fm, wk, fp32_out, psA):
    st = wk["st"]
    S.act(wk["junk"][:], xt, AF.Square, accum_out=st[:, 0:1])
    yield
    S.ts("dve", st[:, 1:2], st[:, 0:1], 1.0 / D, EPS, op0=ALU.mult, op1=ALU.add)
    yield
    S.act(st[:, 2:3], st[:, 1:2], AF.Ln)
    S.act(st[:, 3:4], st[:, 2:3], AF.Exp, scale=-0.5)
    yield
    xn = wk["xn32"]
    S.ts("dve", xn[:], xt, st[:, 3:4], None, op0=ALU.mult)
    yield
    for kc in range(8):
        S.tr(psA[:, kc * 128:(kc + 1) * 128], xn[:, kc * 128:(kc + 1) * 128], G.ident_f[:])
    yield
    src = psA.v(psA.t[:, :].rearrange("p (a b) -> p a b", b=128))
    Abc = A.v(A.t[:, r, :].unsqueeze(2).to_broadcast([128, 8, 128]))
    shbc = G.modT.v(G.modT.t[:, shift_ch0:shift_ch0 + 8, r].unsqueeze(2).to_broadcast([128, 8, 128]))
    tmp = wk["fm32"]
    S.tt("dve", tmp[:], src, Abc, ALU.mult)
    yield
    S.tt("pool", fp32_out, tmp[:], shbc, ALU.add)
    S.copy("act", out_fm, fp32_out)


def wcast_phase(S, G, need):
    items = []
    G.wbf = {}

    def add(name, src3, n):
        dst = S.dram("wbf_" + name, [128, n], BF16)
        G.wbf[name] = dst
        off = 0
        a, b = src3.shape[1], src3.shape[2]
        rows = max(1, 2048 // b)
        if b > 2048:
            for i in range(a):
                for c0 in range(0, b, 2048):
                    c1 = min(b, c0 + 2048)
                    items.append((src3[:, i:i + 1, c0:c1], dst, i * b + c0, c1 - c0, (1, c1 - c0)))
        else:
            for i in range(0, a, rows):
                i1 = min(a, i + rows)
                items.append((src3[:, i:i1, :], dst, i * b, (i1 - i) * b, (i1 - i, b)))

    if "rwkv" in need:
        for j, nm in enumerate(("Wr", "Wk", "Wv")):
            add(nm, G.rw_w_rkv.t[j].rearrange("(kc p) n -> p kc n", p=128), 8 * D)
        add("rwWo", G.rw_w_o.t.rearrange("(kc p) n -> p kc n", p=128), 8 * D)
    if "da" in need:
        add("Wqkv", G.da_w_qkv.t.rearrange("(kc p) n -> p kc n", p=128), 8 * 3 * D)
        add("daWo", G.da_w_o.t.rearrange("(kc p) n -> p kc n", p=128), 8 * D)
    if "moe" in need:
        for li in range(2):
            for e in range(NEXP):
                add(f"g{li}_{e}", G.moe_w_gate.t[li, e].rearrange("(kc p) n -> p kc n", p=128), 8 * DFF)
                add(f"u{li}_{e}", G.moe_w_up.t[li, e].rearrange("(kc p) n -> p kc n", p=128), 8 * DFF)
                add(f"d{li}_{e}", G.moe_w_down.t[li, e].rearrange("(fc p) n -> p fc n", p=128), 2 * D)
    S.push_scope()
    NBUF = 4
    stg = [S.sbuf(f"wc_stg{i}", [128, 2048], F32) for i in range(NBUF)]
    ob = [S.sbuf(f"wc_ob{i}", [128, 2048], BF16) for i in range(NBUF)]
    engs = ["dve", "pool", "dve"]

    def load(i):
        src3, dst, off, n, (a, b) = items[i]
        t = stg[i % NBUF]
        S.dma("sp", t.v(t.t[:, 0:n].rearrange("p (a b) -> p a b", b=b)), V(src3, [("wsrc", None)]))

    for i in range(min(NBUF - 1, len(items))):
        load(i)
    for i in range(len(items)):
        if i + NBUF - 1 < len(items):
            load(i + NBUF - 1)
        src3, dst, off, n, _ = items[i]
        S.copy(engs[i % 3], ob[i % NBUF][:, 0:n], stg[i % NBUF][:, 0:n])
        S.dma("act", dst.v(dst.t[:, off:off + n]), ob[i % NBUF][:, 0:n])
    S.pop_scope()

def rwkv_phase(S, G, x1_d, dbg=None, nb=NB, nt0=NT, do_dir=3, nt1=NT, nheads=16, fl=99):
    li = 0
    H = 16
    yf_d = S.dram("yf_d", [NB, NT, 128, 1040], F32)
    cache_d = S.dram("cache_d", [NB, NT, 128, 6 * D], BF16)
    sg1_d = S.dram("sg1_d", [NB, NT, 128, D], F32)

    def load_bc(name, src, dt=BF16, n=D, q="pool"):
        t = S.sbuf(name, [128, n], dt)
        S.dma(q, t[:], src.v(src.t[0:1, :].partition_broadcast(128)))
        return t

    S.push_scope()
    std_psum(S, G, "r")
    maskA = S.sbuf("maskA_s", [128, 2, 256], BF16)
    maskAT = S.sbuf("maskAT_s", [128, 2, 128], BF16)
    tri = S.sbuf("tri_s", [128, 2, 384], F32)
    ind = S.sbuf("ind_s", [128, 2], F32)
    for z in range(2):
        S.dma("pool", maskA[:, z, :], G.maskA_d.v(G.maskA_d.t[z]))
        S.dma("pool", maskAT[:, z, :], G.maskAT_d.v(G.maskAT_d.t[z]))
        S.dma("sp", tri[:, z, :], G.tri_d.v(G.tri_d.t[z]))
    S.dma("sp", ind[:], G.ind_d[:])
    k_a_bc = load_bc("k_a_bc", G.rw_k_a)
    r_k_bc = load_bc("r_k_bc", G.rw_r_k)
    scr1 = S.sbuf("scr1", [128, D], F32)
    scr2 = S.sbuf("scr2", [128, D], F32)
    sg_sb = S.sbuf("sg_sb", [128, D], F32)
    kdir_sb = S.sbuf("kdir_sb", [128, D], BF16)
    b_sb = S.sbuf("b_sb", [128, D], BF16)
    tm = [S.sbuf(f"tm{i}", [128, D], BF16) for i in range(2)]
    R19 = S.sbuf("R19", [128, H, 128], BF16)
    Bh = S.sbuf("Bh", [128, D], BF16)
    Kh = S.sbuf("Kh", [128, D], BF16)
    arT = S.sbuf("arT", [128, 8, 2, 128], BF16)
    btT = S.sbuf("btT", [128, 8, 128], BF16)
    ktT = S.sbuf("ktT", [128, 8, 128], BF16)
    gC = S.sbuf("gC", [128, 8, 2], F32)
    bon = S.sbuf("bon", [128, 2, 16], F32)
    NSET = 4
    M1 = [S.sbuf(f"M1_{i}", [128, 256], BF16) for i in range(NSET)]
    M2 = [S.sbuf(f"M2_{i}", [128, 256], BF16) for i in range(NSET)]
    MabT = [S.sbuf(f"MabT_{i}", [128, 128], BF16) for i in range(NSET)]
    Pb = [[S.sbuf(f"Pb_{i}_{j}", [128, 128], BF16) for j in range(2)] for i in range(NSET)]
    PTb = [[S.sbuf(f"PTb_{i}_{j}", [128, 128], BF16) for j in range(2)] for i in range(NSET)]
    Tb = [S.sbuf(f"Tb_{i}", [128, 128], BF16) for i in range(NSET)]
    WP = [S.sbuf(f"WP_{i}", [128, 128], BF16) for i in range(NSET)]
    G_all = S.sbuf("G_all", [128, 8, 128], BF16)
    Y0_all = S.sbuf("Y0_all", [128, H, 64], BF16)
    D_all = S.sbuf("D_all", [128, 8, 2, 128], BF16)
    E_all = S.sbuf("E_all", [128, 8, 2, 128], BF16)
    Sb = S.sbuf("Sb", [128, 8, 128], BF16)
    S.memset("pool", D_all[:], 0.0)
    S.memset("pool", E_all[:], 0.0)
    yfw = S.sbuf("yfw", [128, 1040], F32)
    banks = list(G.psH) + list(G.psA_h) + list(G.psY_h)
    NBK = len(banks)
    bank_ctr = [0]
    slot_ctr = [0] * NBK

    def slot(n=1):
        bk = bank_ctr[0] % NBK
        bank_ctr[0] += 1
        if n == 2:
            i = ((slot_ctr[bk] + 1) // 2 * 2) % 4
            slot_ctr[bk] = i + 2
        else:
            i = slot_ctr[bk] % 4
            slot_ctr[bk] = i + 1
        return (bk, i)

    def psl(s, p0=0, p1=128, c0=0, c1=128, n=1):
        bk, i = s
        t = banks[bk]
        if n == 2:
            return t.v(t.t[p0:p1, i:i + 2, :].rearrange("p a b -> p (a b)")[:, c0:c1])
        return t.v(t.t[p0:p1, i, c0:c1])

    def transposes_to(src_tm, dst_view):
        for ec in range(8):
            S.tr(G.psT[:, ec * 128:(ec + 1) * 128], src_tm[:, ec * 128:(ec + 1) * 128], G.ident_b[:])
        S.copy("act", dst_view, G.psT.v(G.psT.t[:, :].rearrange("p (a b) -> p a b", b=128)))

    def dir_part(z, r_v, k_v, v_v, kk_v, a_v, chunk_order, sg_sb=sg_sb):
        zsl = slice(z, z + 1)
        S.stt(scr1[:], a_v, -1.0, k_a_bc[:], ALU.add, ALU.mult)
        S.stt(kdir_sb[:], scr1[:], 1.0, k_v, ALU.add, ALU.mult)
        S.tt("pool", b_sb[:], kk_v, a_v, ALU.mult)
        S.tt("pool", scr1[:], r_v, kdir_sb[:], ALU.mult)
        S.tt("pool", scr1[:], scr1[:], r_k_bc[:], ALU.mult)
        S.reduce(bon[:, z, :], scr1.v(scr1.t[:, :].rearrange("p (h n) -> p h n", n=64)))
        def cum(which):
            for n in range(2):
                S.mm(G.psA[:, n * 512:(n + 1) * 512], tri[:, z, which * 128:(which + 1) * 128], sg_sb[:, n * 512:(n + 1) * 512])
        cum(0)
        S.act(scr2[:], G.psA[:], AF.Exp)
        S.tt("dve", tm[0][:], r_v, scr2[:], ALU.mult)
        transposes_to(tm[0], arT.v(arT.t[:, :, 1, :]))
        S.act(scr2[:], G.psA[:], AF.Exp, scale=-1.0)
        S.tt("dve", tm[1][:], b_sb[:], scr2[:], ALU.mult)
        transposes_to(tm[1], btT[:])
        S.tt("dve", tm[0][:], kdir_sb[:], scr2[:], ALU.mult)
        transposes_to(tm[0], ktT[:])
        cum(1)
        S.act(scr2[:], G.psA[:], AF.Exp)
        S.stt(tm[1][:], kk_v, -1.0, scr2[:], ALU.mult, ALU.mult)
        S.copy("pool", V(R19.t[:, :, 64:128], [("R19", h) for h in range(H)]),
               tm[1].v(tm[1].t[:, :].rearrange("p (h n) -> p h n", n=64)))
        transposes_to(tm[1], arT.v(arT.t[:, :, 0, :]))
        cum(2)
        S.act(scr2[:], G.psA[:], AF.Exp)
        S.tt("dve", Bh[:], b_sb[:], scr2[:], ALU.mult)
        S.tt("pool", Kh[:], kdir_sb[:], scr2[:], ALU.mult)
        sg_ = slot()
        for ec in range(8):
            S.mm(psl(sg_, c0=ec * 2, c1=ec * 2 + 2), sg_sb[:, ec * 128:(ec + 1) * 128], ind[:])
        S.act(gC[:], banks[sg_[0]].v(banks[sg_[0]].t[:, sg_[1], 0:16].rearrange("p (a b) -> p a b", b=2)), AF.Exp)

        if do_dir < 2:
            return
        def head_gen(h):
            ec, po = h // 2, (h % 2) * 64
            hc = slice(h * 64, (h + 1) * 64)
            pr = slice(po, po + 64)
            i2 = h % NSET
            bt_h = btT[pr, ec, :]
            kt_h = ktT[pr, ec, :]
            ar_h = arT.v(arT.t[pr, ec, :, :].rearrange("p a b -> p (a b)"))
            at_h = arT[pr, ec, 0, :]
            rt_h = arT[pr, ec, 1, :]
            s1 = slot(2)
            S.mm(psl(s1, n=2, c1=256), bt_h, ar_h)
            S.tt("dve", M1[i2][:], psl(s1, n=2, c1=256), maskA[:, z, :], ALU.mult)
            s3 = slot()
            S.mm(psl(s3), at_h, bt_h)
            S.tt("dve", MabT[i2][:], psl(s3), maskAT[:, z, :], ALU.mult)
            s2 = slot(2)
            S.mm(psl(s2, n=2, c1=256), kt_h, ar_h)
            S.tt("dve", M2[i2][:], psl(s2, n=2, c1=256), maskA[:, z, :], ALU.mult)
            T = Tb[i2]
            S.tt("pool", T[:], M1[i2][:, 0:128], G.ident_b[:], ALU.add)
            P, PT = M1[i2][:, 0:128], MabT[i2][:]
            yield
            for kstep in range(1, 6):
                if kstep < 5:
                    sa = slot()
                    S.mm(psl(sa), PT, P)
                    P2 = Pb[i2][kstep % 2]
                    S.copy("act", P2[:], psl(sa))
                sb_ = slot()
                S.mm(psl(sb_), P, PT)
                P2T = PTb[i2][kstep % 2]
                S.copy("act", P2T[:], psl(sb_))
                if kstep == 1:
                    sx = slot()
                    S.mm(psl(sx, c1=64), M2[i2][:, 0:128], v_v_slice(v_v, hc))
                    S.copy("act", R19.k(h, (slice(None), h, slice(0, 64))), psl(sx, c1=64))
                yield
                sc_ = slot()
                S.mm(psl(sc_), P2T[:], T[:])
                S.tt("dve", T[:], T[:], psl(sc_), ALU.add)
                if kstep < 5:
                    P, PT = P2[:], P2T[:]
            yield
            sw = slot()
            S.mm(psl(sw), T[:], R19.k(h, (slice(None), h, slice(None))))
            S.copy("act", WP[i2][:], psl(sw))
            yield
            sg2 = slot()
            S.mm(psl(sg2, p0=po, p1=po + 64), WP[i2][:, 64:128], M1[i2][:, 128:256])
            S.tt("dve", G_all.k(h, (pr, ec, slice(None))), psl(sg2, p0=po, p1=po + 64), rt_h, ALU.add)
            sy = slot()
            S.mm(psl(sy, c1=64), M1[i2][:, 128:256], WP[i2][:, 0:64], start=True, stop=False)
            S.mm(psl(sy, c1=64), M2[i2][:, 128:256], v_v_slice(v_v, hc), start=False, stop=True)
            S.copy("act", Y0_all.k(h, (slice(None), h, slice(None))), psl(sy, c1=64))
            sds = [slot(), slot()]
            for c in range(2):
                cr = slice(c * 64, (c + 1) * 64)
                S.mm(psl(sds[c], p0=po, p1=po + 64, c1=64), WP[i2][cr, 64:128], Bh[cr, hc])
            for c in range(2):
                S.stt(D_all.k(h, (pr, ec, c, slice(po, po + 64))), G.ident_f[pr, po:po + 64], gC[pr, ec, c:c + 1],
                      psl(sds[c], p0=po, p1=po + 64, c1=64), ALU.mult, ALU.add)
            ses = [slot(), slot()]
            for c in range(2):
                cr = slice(c * 64, (c + 1) * 64)
                S.mm(psl(ses[c], p0=po, p1=po + 64, c1=64), Bh[cr, hc], WP[i2][cr, 0:64], start=True, stop=False)
                S.mm(psl(ses[c], p0=po, p1=po + 64, c1=64), Kh[cr, hc], v_v_slice(v_v, hc, cr), start=False, stop=True)
            for c in range(2):
                S.copy("act", E_all.k(h, (pr, ec, c, slice(po, po + 64))), psl(ses[c], p0=po, p1=po + 64, c1=64))

        pending = list(range(nheads))
        active = []
        rnd, last_admit = 0, -99
        while pending or active:
            if pending and len(active) <= NSET - 2 and (rnd - last_admit >= 4 or not active):
                for _ in range(2):
                    if pending:
                        active.append(head_gen(pending.pop(0)))
                last_admit = rnd
            nxt = []
            for g in active:
                try:
                    next(g)
                    nxt.append(g)
                except StopIteration:
                    pass
            active = nxt
            rnd += 1
        if do_dir < 3:
            return
        for c in chunk_order:
            for ec in range(8):
                pair = [2 * ec, 2 * ec + 1]
                Gv = V(G_all.t[:, ec, c * 64:(c + 1) * 64], [("G_all", h) for h in pair])
                Dv = V(D_all.t[:, ec, c, :], [("D_all", h) for h in pair])
                S.mm(G.psY.v(G.psY.t[c * 64:(c + 1) * 64, ec * 128:(ec + 1) * 128]), Gv, Sb[:, ec, :])
                S.mm(G.psA.v(G.psA.t[:, ec * 128:(ec + 1) * 128]), Dv, Sb[:, ec, :])
            S.tt("dve", Sb[:], G.psA.v(G.psA.t[:, :].rearrange("p (a b) -> p a b", b=128)),
                 V(E_all.t[:, :, c, :], [("E_all", h) for h in range(H)]), ALU.add)

    def v_v_slice(v_v, hc, rows=slice(None)):
        return V(v_v.ap[rows, hc], v_v.toks)

    S.push_scope()
    Wr, Wk, Wv = [S.sbuf(n, [128, 8, D], BF16) for n in ("Wr", "Wk", "Wv")]
    for nm, W in (("Wr", Wr), ("Wk", Wk), ("Wv", Wv)):
        S.dma("sp", W.v(W.t[:, :, :].rearrange("p a b -> p (a b)")), G.wbf[nm][:])
    w1 = S.sbuf("w1", [128, 2, 8, 64], BF16)
    a1 = S.sbuf("a1", [128, 2, 8, 64], BF16)
    g1 = S.sbuf("g1", [128, 8, 128], BF16)
    w2x = S.sbuf("w2x", [65, 2, D], BF16)
    a2x = S.sbuf("a2x", [65, 2, D], BF16)
    g2 = S.sbuf("g2", [128, D], BF16)
    for z in range(2):
        S.dma("pool", w1[:, z, :, :], G.rw_w1.v(G.rw_w1.t[z].rearrange("(kc p) n -> p kc n", p=128)))
        S.dma("pool", a1[:, z, :, :], G.rw_a1.v(G.rw_a1.t[z].rearrange("(kc p) n -> p kc n", p=128)))
        S.dma("pool", w2x[0:64, z, :], G.rw_w2.v(G.rw_w2.t[z]))
        S.dma("pool", w2x[64:65, z, :], G.rw_w0.v(G.rw_w0.t[z:z + 1, :]))
        S.dma("pool", a2x[0:64, z, :], G.rw_a2.v(G.rw_a2.t[z]))
        S.dma("pool", a2x[64:65, z, :], G.rw_a0.v(G.rw_a0.t[z:z + 1, :]))
    S.dma("pool", g1[:], G.rw_g1.v(G.rw_g1.t.rearrange("(kc p) n -> p kc n", p=128)))
    S.dma("pool", g2[:], G.rw_g2[:])
    k_k_bc = load_bc("k_k_bc", G.rw_k_k)
    hTc = S.sbuf("hTc", [128, 8, CTX + 2], BF16)
    hTl = S.sbuf("hTl", [128, 8, LAT + 2], BF16)
    xin = scr2
    wk = {"junk": scr1, "st": S.sbuf("st", [128, 4], F32), "xn": tm[0],
          "fm32": S.sbuf("fm32", [128, 8, 128], F32)}
    dxt = S.sbuf("dxt", [128, 8, 128], F32)
    mix = [S.sbuf(f"mix{i}", [128, 8, 128], BF16) for i in range(2)]
    cach = S.sbuf("cach", [128, 6, D], BF16)
    a0_sb = S.sbuf("a0_sb", [128, D], BF16)
    sg1_v = yfw[:, 0:D]
    hwx = S.sbuf("hwx", [65, 2, 128], BF16)
    hax = S.sbuf("hax", [65, 2, 128], BF16)
    hgs = S.sbuf("hgs", [128, 128], BF16)
    st2 = S.sbuf("st2", [128, 3, 16], F32)
    S.memset("dve", hwx[:], 1.0)
    S.memset("dve", hax[:], 1.0)
    for hT in (hTc, hTl):
        S.memset("pool", hT[:], 0.0)

    mix_ctr = [0]

    def make_mix(hT, c0, j):
        m = mix[mix_ctr[0] % 2]
        mix_ctr[0] += 1
        mu = G.pfm.v(G.pfm.t[:, PV_MU + j * 8:PV_MU + j * 8 + 8].unsqueeze(2).to_broadcast([128, 8, 128]))
        S.tt("pool", wk["fm32"][:], dxt[:], mu, ALU.mult)
        S.tt("pool", m[:], wk["fm32"][:], hT[:, :, c0:c0 + 128], ALU.add)
        return m

    def proj_tm(ps, m, W):
        for n in range(2):
            for kc in range(8):
                S.mm(ps[:, n * 512:(n + 1) * 512], m[:, kc, :], W[:, kc, n * 512:(n + 1) * 512], start=(kc == 0), stop=(kc == 7))

    for b in range(nb):
        for ti in range(NT):
            if ti < 2:
                src, r, hT, t0 = G.ctx.v(G.ctx.t[b, ti * 128:(ti + 1) * 128, :]), 2, hTc, ti * 128
            else:
                src, r, hT, t0 = G.x.v(G.x.t[b, (ti - 2) * 128:(ti - 1) * 128, :]), b, hTl, (ti - 2) * 128
            S.dma("sp", xin[:], src, split=4)
            norm_tile_to_fm(S, G, xin[:], r, G.A1, 0, hT[:, :, t0 + 1:t0 + 129], wk)
        if dbg is not None and "hT" in dbg and b == 0:
            S.dma("sp", dbg["hT"][:], hTl[:])
        S.memset("dve", Sb[:], 0.0)
        for ti in range(nt0):
            hT, t0 = (hTc, ti * 128) if ti < 2 else (hTl, (ti - 2) * 128)
            c0 = t0 + 1
            S.tt("dve", dxt[:], hT[:, :, c0 - 1:c0 + 127], hT[:, :, c0 + 1:c0 + 129], ALU.add)
            S.stt(dxt[:], dxt[:], 0.5, hT[:, :, c0:c0 + 128], ALU.mult, ALU.subtract)
            if fl < 1:
                continue
            m = make_mix(hT, c0, 0)
            proj_tm(G.psA, m, Wr)
            S.copy("act", cach[:, 0, :], G.psA[:])
            if fl < 2:
                continue
            m = make_mix(hT, c0, 2)
            proj_tm(G.psY, m, Wv)
            S.copy("act", cach[:, 2, :], G.psY[:])
            if fl < 3:
                continue
            m = make_mix(hT, c0, 4)
            for z in range(2):
                sl_ = slot()
                for kc in range(8):
                    S.mm(psl(sl_, p1=64), a1[:, z, kc, :], m[:, kc, :], start=(kc == 0), stop=(kc == 7))
                S.copy("act", hax[0:64, z, :], psl(sl_, p1=64))
            for z in range(2):
                ps = G.psA if z == 0 else G.psY
                for n in range(2):
                    S.mm(ps[:, n * 512:(n + 1) * 512], hax[:, z, :], a2x[:, z, n * 512:(n + 1) * 512])
                S.act(a0_sb[:] if z == 0 else cach[:, 4, :], ps[:], AF.Sigmoid)
            if fl < 4:
                continue
            m = make_mix(hT, c0, 3)
            for z in range(2):
                sl_ = slot()
                for kc in range(8):
                    S.mm(psl(sl_, p1=64), w1[:, z, kc, :], m[:, kc, :], start=(kc == 0), stop=(kc == 7))
                S.act(hwx[0:64, z, :], psl(sl_, p1=64), AF.Tanh)
            for z in range(2):
                ps = G.psA if z == 0 else G.psY
                for n in range(2):
                    S.mm(ps[:, n * 512:(n + 1) * 512], hwx[:, z, :], w2x[:, z, n * 512:(n + 1) * 512])
                S.act(sg_sb[:] if z == 0 else sg1_v, ps[:], AF.Sigmoid)
            if fl < 5:
                continue
            m = make_mix(hT, c0, 5)
            sl_ = slot()
            for kc in range(8):
                S.mm(psl(sl_), g1[:, kc, :], m[:, kc, :], start=(kc == 0), stop=(kc == 7))
            S.act(hgs[:], psl(sl_), AF.Sigmoid)
            for n in range(2):
                S.mm(G.psY[:, n * 512:(n + 1) * 512], hgs[:], g2[:, n * 512:(n + 1) * 512])
            S.copy("act", cach[:, 5, :], G.psY[:])
            if fl < 6:
                continue
            m = make_mix(hT, c0, 1)
            proj_tm(G.psA, m, Wk)
            S.copy("act", cach[:, 1, :], G.psA[:])
            if fl < 6.1:
                continue
            S.tt("dve", scr1[:], G.psA[:], k_k_bc[:], ALU.mult)
            if fl < 6.2:
                continue
            S.act(scr2[:], scr1[:], AF.Square)
            S.reduce(st2[:, 0, :], scr2.v(scr2.t[:, :].rearrange("p (h n) -> p h n", n=64)))
            if fl < 6.3:
                continue
            S.ts("dve", st2[:, 1, :], st2[:, 0, :], 1e-12, None, op0=ALU.add)
            S.act(st2[:, 1, :], st2[:, 1, :], AF.Sqrt)
            S.recip(st2[:, 2, :], st2[:, 1, :])
            if fl < 6.4:
                continue
            S.tt("dve", cach.v(cach.t[:, 3, :].rearrange("p (h n) -> p h n", n=64)),
                 scr1.v(scr1.t[:, :].rearrange("p (h n) -> p h n", n=64)),
                 st2.v(st2.t[:, 2, :].unsqueeze(2).to_broadcast([128, 16, 64])), ALU.mult)
            if fl < 7:
                continue
            S.dma("sp", cache_d.v(cache_d.t[b, ti].rearrange("p (a n) -> p a n", n=D)), cach[:])
            S.dma("sp", sg1_d.v(sg1_d.t[b, ti]), sg1_v)
            if do_dir:
                dir_part(0, cach[:, 0, :], G.psA[:], cach[:, 2, :], cach[:, 3, :], a0_sb[:], (0, 1))
            S.tt("dve", yfw[:, 0:D], G.psY[:],
                 V(Y0_all.t[:, :, :].rearrange("p h n -> p (h n)"), [("Y0_all", h) for h in range(H)]), ALU.add)
            S.copy("pool", yfw[:, D:D + 16], bon[:, 0, :])
            S.dma("sp", yf_d.v(yf_d.t[b, ti]), yfw[:])
    S.pop_scope()

    S.push_scope()
    Wo = S.sbuf("Wo", [128, 8, D], BF16)
    S.dma("sp", Wo.v(Wo.t[:, :, :].rearrange("p a b -> p (a b)")), G.wbf["rwWo"][:])
    lnx_g_bc = load_bc("lnx_g_bc", G.rw_lnx_g)
    lnx_b_bc = load_bc("lnx_b_bc", G.rw_lnx_b)
    gate_bc = S.sbuf("gate_bc", [128, D], F32)
    cachs = [S.sbuf(f"cach1_{i}", [128, 6, D], BF16) for i in range(2)]
    sgs = [S.sbuf(f"sg1b_{i}", [128, D], F32) for i in range(2)]
    yfs = [S.sbuf(f"yf1_{i}", [128, 1040], F32) for i in range(2)]
    xres = S.sbuf("xres", [128, D], F32)
    pre = S.sbuf("pre", [128, D], BF16)
    preT = S.sbuf("preT", [128, 8, 128], BF16)
    st3 = S.sbuf("st3", [128, 4, 16], F32)
    order1 = ([1, 0] + list(range(NT - 1, 1, -1)))[:nt1]
    seq1 = [(b, ti) for b in range(nb) for ti in order1]

    def load1(n):
        b_, ti_ = seq1[n]
        i = n % 2
        S.dma("sp", cachs[i][:], cache_d.v(cache_d.t[b_, ti_].rearrange("p (a n) -> p a n", n=D)))
        S.dma("sp", sgs[i][:], sg1_d.v(sg1_d.t[b_, ti_]))
        S.dma("sp", yfs[i][:], yf_d.v(yf_d.t[b_, ti_]))

    load1(0)
    cur_r = None
    for n1, (b, ti) in enumerate(seq1):
        if True:
            if ti == order1[0]:
                S.memset("dve", Sb[:], 0.0)
            if n1 + 1 < len(seq1):
                load1(n1 + 1)
            cach, sg1t, yfw = cachs[n1 % 2], sgs[n1 % 2], yfs[n1 % 2]
            r = 2 if ti < 2 else b
            if r != cur_r:
                S.dma("sp", gate_bc[:], G.gates_d.v(G.gates_d.t[0, r:r + 1, :].partition_broadcast(128)))
                cur_r = r
            dir_part(1, cach[:, 0, :], cach[:, 1, :], cach[:, 2, :], cach[:, 3, :], cach[:, 4, :], (1, 0), sg_sb=sg1t)
            S.tt("dve", scr1[:], G.psY[:], V(Y0_all.t[:, :, :].rearrange("p h n -> p (h n)"), [("Y0_all", h) for h in range(H)]), ALU.add)
            S.tt("pool", scr1[:], scr1[:], yfw[:, 0:D], ALU.add)
            y3 = scr1.v(scr1.t[:, :].rearrange("p (h n) -> p h n", n=64))
            S.reduce(st3[:, 0, :], y3)
            S.ts("dve", st3[:, 0, :], st3[:, 0, :], 1.0 / 64, None, op0=ALU.mult)
            S.tt("dve", y3, y3, st3.v(st3.t[:, 0, :].unsqueeze(2).to_broadcast([128, 16, 64])), ALU.subtract)
            S.act(scr2[:], scr1[:], AF.Square)
            S.reduce(st3[:, 1, :], scr2.v(scr2.t[:, :].rearrange("p (h n) -> p h n", n=64)))
            S.ts("dve", st3[:, 1, :], st3[:, 1, :], 1.0 / 64, LNX_EPS, op0=ALU.mult, op1=ALU.add)
            S.act(st3[:, 1, :], st3[:, 1, :], AF.Sqrt)
            S.recip(st3[:, 2, :], st3[:, 1, :])
            S.tt("dve", y3, y3, st3.v(st3.t[:, 2, :].unsqueeze(2).to_broadcast([128, 16, 64])), ALU.mult)
            S.tt("pool", scr1[:], scr1[:], lnx_g_bc[:], ALU.mult)
            S.tt("pool", scr1[:], scr1[:], lnx_b_bc[:], ALU.add)
            S.tt("dve", st3[:, 3, :], bon[:, 1, :], yfw[:, D:D + 16], ALU.add)
            S.tt("dve", scr2.v(scr2.t[:, :].rearrange("p (h n) -> p h n", n=64)),
                 cach.v(cach.t[:, 2, :].rearrange("p (h n) -> p h n", n=64)),
                 st3.v(st3.t[:, 3, :].unsqueeze(2).to_broadcast([128, 16, 64])), ALU.mult)
            S.tt("pool", scr1[:], scr1[:], scr2[:], ALU.add)
            S.tt("pool", pre[:], scr1[:], cach[:, 5, :], ALU.mult)
            transposes_to(pre, preT[:])
            for n in range(2):
                for kc in range(8):
                    S.mm(G.psA[:, n * 512:(n + 1) * 512], preT[:, kc, :], Wo[:, kc, n * 512:(n + 1) * 512], start=(kc == 0), stop=(kc == 7))
            if ti < 2:
                xsrc = G.ctx.v(G.ctx.t[b, ti * 128:(ti + 1) * 128, :])
            else:
                xsrc = G.x.v(G.x.t[b, (ti - 2) * 128:(ti - 1) * 128, :])
            S.dma("sp", xres[:], xsrc)
            S.tt("dve", scr2[:], G.psA[:], gate_bc[:], ALU.mult)
            S.tt("pool", xres[:], xres[:], scr2[:], ALU.add)
            S.dma("sp", x1_d.v(x1_d.t[b, ti * 128:(ti + 1) * 128, :]), xres[:])
    S.pop_scope()
    S.pop_scope()

def moe_phase(S, G, li, xin_d, tiles, xout_fn, st_tiles, npairs=16, dbg=None):
    L = f"e{li}"
    S.push_scope()
    ysub = [S.psum(f"ysub{i}{L}", [128, 1024], F32) for i in range(2)]
    psG = [S.psum(f"psG{i}{L}", [128, 512], F32) for i in range(2)]
    psU = [S.psum(f"psU{i}{L}", [128, 512], F32) for i in range(2)]
    G.psA = ysub[0]
    STK = st_tiles * 128
    h2T = S.sbuf(f"h2T{L}", [128, 8, STK], BF16)
    y_acc = S.sbuf(f"yacc{L}", [128, st_tiles, D], F32)
    gatesT = S.sbuf(f"gatesT{L}", [32, STK], BF16)
    sel = S.sbuf(f"sel{L}", [32, 32, 128], BF16)
    S.dma("pool", sel[:], G.sel_d.v(G.sel_d.t[:, :].rearrange("p (a b) -> p a b", b=128)))
    Wrt = S.sbuf(f"Wrt{L}", [128, 8, 36], F32)
    S.dma("sp", Wrt[:], G.moe_router.v(G.moe_router.t[li].rearrange("(kc p) n -> p kc n", p=128)))
    rb = S.sbuf(f"rb{L}", [1, 36], F32)
    S.dma("sp", rb[:], G.moe_router_b.v(G.moe_router_b.t[li]))
    gate_bc = S.sbuf(f"gbc{L}", [128, 3, D], F32)
    rs_used = sorted(set(t[2] for t in tiles))
    for r in rs_used:
        S.dma("sp", gate_bc[:, r, :], G.gates_d.v(G.gates_d.t[1, r:r + 1, :].partition_broadcast(128)))
    Wg = [[S.sbuf(f"Wg{i}{e}{L}", [128, 8, DFF], BF16) for e in range(2)] for i in range(2)]
    Wu = [[S.sbuf(f"Wu{i}{e}{L}", [128, 8, DFF], BF16) for e in range(2)] for i in range(2)]
    Wd = [[S.sbuf(f"Wd{i}{e}{L}", [128, 2, D], BF16) for e in range(2)] for i in range(2)]
    NS1 = 2
    xin_s = [S.sbuf(f"xin{i}{L}", [128, D], F32) for i in range(NS1)]
    junk_s = [S.sbuf(f"junk{i}{L}", [128, D], F32) for i in range(NS1)]
    wk_s = [{"junk": junk_s[i], "st": S.sbuf(f"st{i}{L}", [128, 4], F32), "xn32": S.sbuf(f"xn32{i}{L}", [128, D], F32),
             "fm32": S.sbuf(f"fm32{i}{L}", [128, 8, 128], F32)} for i in range(NS1)]
    h32_s = [S.sbuf(f"h32{i}{L}", [128, 8, 128], F32) for i in range(NS1)]
    lg_s = [S.sbuf(f"lg{i}{L}", [128, 36], F32) for i in range(NS1)]
    sm_s = [S.sbuf(f"sm{i}{L}", [128, 64], F32) for i in range(NS1)]
    g32_s = [S.sbuf(f"g32{i}{L}", [128, 32], F32) for i in range(NS1)]
    xin3 = S.sbuf(f"xin3{L}", [128, D], F32)
    out3 = S.sbuf(f"out3{L}", [128, D], F32)
    s_sb = [S.sbuf(f"s_sb{i}{L}", [128, 256], F32) for i in range(2)]
    t_sb = [S.sbuf(f"t_sb{i}{L}", [128, 256], F32) for i in range(2)]
    hidT = [S.sbuf(f"hidT{i}{L}", [128, 256], BF16) for i in range(2)]

    def load_pair(p, buf):
        for e in range(2):
            eg = p * 2 + e
            for W, nm in ((Wg, "g"), (Wu, "u"), (Wd, "d")):
                t = W[buf][e]
                S.dma("sp", t.v(t.t[:, :, :].rearrange("p a b -> p (a b)")), G.wbf[f"{nm}{li}_{eg}"][:])

    n_super = len(tiles) // st_tiles
    assert n_super * st_tiles == len(tiles) and st_tiles % 2 == 0
    for su in range(n_super):
        stl = tiles[su * st_tiles:(su + 1) * st_tiles]
        load_pair(0, 0)
        def step1_gen(j, b, row0, r, s):
            xin, wk, h32, lg, sm, g32 = xin_s[s], wk_s[s], h32_s[s], lg_s[s], sm_s[s], g32_s[s]
            S.dma("sp", xin[:], xin_d.v(xin_d.t[b, row0:row0 + 128, :]), split=4)
            yield
            yield from norm_tile_gen(S, G, xin[:], r, G.A2, 24, h2T[:, :, j * 128:(j + 1) * 128], wk, h32[:], ysub[s])
            yield
            psr = (psG[0] if s == 0 else psU[0])[:, 0:36]
            for kc in range(8):
                S.mm(psr, h32[:, kc, :], Wrt[:, kc, :], start=(kc == 0), stop=False)
            S.mm(psr, G.ones_f[0:1, :], rb[:], start=False, stop=True)
            S.copy("dve", lg[:], psr)
            yield
            c = lambda i, n=1: sm[:, i:i + n]
            S.reduce(c(0), lg[:, 0:4], op=ALU.max)
            yield
            S.ts("dve", c(1), c(0), -1.0, None, op0=ALU.mult)
            yield
            S.ts("dve", c(4, 4), lg[:, 0:4], c(0), None, op0=ALU.is_ge)
            yield
            S.act(c(8, 4), lg[:, 0:4], AF.Exp, bias=c(1), accum_out=c(2))
            yield
            S.recip(c(3), c(2))
            yield
            S.ts("dve", c(16, 8), lg[:, 4:12], c(4), None, op0=ALU.mult)
            yield
            for g in range(1, 4):
                S.stt(c(16, 8), lg[:, 4 + 8 * g:12 + 8 * g], c(4 + g), c(16, 8), ALU.mult, ALU.add)
                yield
            S.reduce(c(12), c(16, 8), op=ALU.max)
            yield
            S.ts("dve", c(24, 8), c(16, 8), c(12), None, op0=ALU.is_ge)
            yield
            S.stt(c(32, 8), c(24, 8), -1e30, c(16, 8), ALU.mult, ALU.add)
            yield
            S.reduce(c(13), c(32, 8), op=ALU.max)
            yield
            S.ts("dve", c(40, 8), c(32, 8), c(13), None, op0=ALU.is_ge)
            yield
            S.tt("dve", c(14), c(13), c(12), ALU.subtract)
            yield
            S.act(c(15), c(14), AF.Exp)
            yield
            S.ts("dve", c(48), c(15), 1.0, None, op0=ALU.add)
            yield
            S.recip(c(49), c(48))
            yield
            S.tt("dve", c(50), c(49), c(3), ALU.mult)
            yield
            S.tt("dve", c(51), c(50), c(15), ALU.mult)
            yield
            S.ts("dve", c(52, 8), c(24, 8), c(50), None, op0=ALU.mult)
            yield
            S.stt(c(52, 8), c(40, 8), c(51), c(52, 8), ALU.mult, ALU.add)
            yield
            for g in range(4):
                S.ts("dve", g32[:, g * 8:(g + 1) * 8], c(52, 8), c(4 + g), None, op0=ALU.mult)
                yield
            pst = (psG[1] if s == 0 else psU[1])[0:32, 0:128]
            S.tr(pst, g32[:], G.ident_f[:])
            S.copy("dve", gatesT[:, j * 128:(j + 1) * 128], pst)
            if dbg is not None and "gates" in dbg and su == 0:
                S.dma("sp", dbg["gates"].v(dbg["gates"].t[j]), g32[:])

        pend1 = [step1_gen(j, b, row0, r, j % NS1) for j, (b, row0, r) in enumerate(stl)]
        act1 = []
        rnd, last1 = 0, -99
        while pend1 or act1:
            if pend1 and len(act1) < NS1 and (rnd - last1 >= 20 or not act1):
                act1.append(pend1.pop(0))
                last1 = rnd
            nxt = []
            for g_ in act1:
                try:
                    next(g_)
                    nxt.append(g_)
                except StopIteration:
                    pass
            act1 = nxt
            rnd += 1
        G.psA = ysub[0]
        items = [(p, t2, e, fc) for p in range(npairs) for t2 in range(st_tiles // 2) for e in range(2) for fc in range(2)]

        def emit_gu(idx):
            p, t2, e, fc = items[idx]
            buf, eg, ib = p % 2, p * 2 + e, idx % 2
            tok = slice(t2 * 256, (t2 + 1) * 256)
            pg, pu = psG[ib], psU[ib]
            for kc in range(8):
                S.mm(pg[:, 0:256], Wg[buf][e][:, kc, fc * 128:(fc + 1) * 128], h2T[:, kc, tok], start=(kc == 0), stop=(kc == 7))
            for kc in range(8):
                S.mm(pu[:, 0:256], Wu[buf][e][:, kc, fc * 128:(fc + 1) * 128], h2T[:, kc, tok], start=(kc == 0), stop=(kc == 7))
            S.mm(pu[:, 256:512], sel[:, eg, :], gatesT[:, tok])
            S.act(s_sb[ib][:], pg[:, 0:256], AF.Silu)
            S.tt("dve", t_sb[ib][:], s_sb[ib][:], pu[:, 0:256], ALU.mult)
            S.tt("dve", hidT[ib][:], t_sb[ib][:], pu[:, 256:512], ALU.mult)

        def emit_down(idx):
            p, t2, e, fc = items[idx]
            buf, ib, it = p % 2, idx % 2, e * 2 + fc
            for ts_ in range(2):
                for n in range(2):
                    S.mm(ysub[ts_][:, n * 512:(n + 1) * 512], hidT[ib][:, ts_ * 128:(ts_ + 1) * 128],
                         Wd[buf][e][:, fc, n * 512:(n + 1) * 512], start=(it == 0), stop=(it == 3))
            if it == 3:
                for ts_ in range(2):
                    j = t2 * 2 + ts_
                    if p == 0:
                        S.copy("dve", y_acc[:, j, :], ysub[ts_][:])
                    else:
                        S.tt("dve", y_acc[:, j, :], y_acc[:, j, :], ysub[ts_][:], ALU.add)
                    if p == npairs - 1:
                        b, row0, r = stl[j]
                        S.dma("sp", xin3[:], xin_d.v(xin_d.t[b, row0:row0 + 128, :]))
                        S.tt("pool", out3[:], y_acc[:, j, :], gate_bc[:, r, :], ALU.mult)
                        S.tt("pool", out3[:], out3[:], xin3[:], ALU.add)
                        S.dma("sp", xout_fn(b, row0), out3[:])

        for idx in range(len(items)):
            emit_gu(idx)
            if idx > 0:
                emit_down(idx - 1)
            p, t2, e, fc = items[idx]
            if t2 == 0 and e == 0 and fc == 0 and p + 1 < npairs:
                load_pair(p + 1, (p + 1) % 2)
        emit_down(len(items) - 1)
    S.pop_scope()

LAM_INIT1 = 0.8 - 0.6 * float(np.exp(-0.3 * 1))


def attn_phase(S, G, x2_d, x3_d, nb=NB, nqt=4, nh=8):
    li = 1
    NKT = NT
    S.push_scope()
    psA = S.psum("psA_a", [128, 1024], F32)
    psT = S.psum("psT_a", [128, 1024], BF16)
    psQ = [S.psum(f"psQ{i}_a", [128, 512], F32) for i in range(3)]
    psO = [S.psum(f"psO{i}_a", [128, 512], F32) for i in range(2)]
    G.psA, G.psT = psA, psT
    KT_all = S.sbuf("KT_all", [128, 8, TOK], BF16)
    QT_all = S.sbuf("QT_all", [128, 8, LAT], BF16)
    V_all = S.sbuf("V_all", [128, NKT, 8, 130], BF16)
    S.memset("pool", V_all[:], 1.0)
    gq_bc = S.sbuf("gq_bc", [128, 64], F32)
    gk_bc = S.sbuf("gk_bc", [128, 64], F32)
    sg_bc = S.sbuf("sg_bc", [128, 128], F32)
    S.dma("sp", gq_bc[:], G.da_q_norm_g.v(G.da_q_norm_g.t[0:1, :].partition_broadcast(128)))
    S.dma("sp", gk_bc[:], G.da_k_norm_g.v(G.da_k_norm_g.t[0:1, :].partition_broadcast(128)))
    S.dma("sp", sg_bc[:], G.da_subln_g.v(G.da_subln_g.t[0:1, :].partition_broadcast(128)))
    S.ts("dve", sg_bc[:], sg_bc[:], 1.0 - LAM_INIT1, None, op0=ALU.mult)
    lamv = S.sbuf("lamv", [128, 4, 64], F32)
    lsm = S.sbuf("lsm", [128, 8], F32)
    for i in range(4):
        S.dma("sp", lamv[:, i, :], G.da_lam.v(G.da_lam.t[i:i + 1, :].partition_broadcast(128)))
    S.tt("dve", lamv[:, 0, :], lamv[:, 0, :], lamv[:, 1, :], ALU.mult)
    S.tt("dve", lamv[:, 2, :], lamv[:, 2, :], lamv[:, 3, :], ALU.mult)
    S.reduce(lsm[:, 0:1], lamv[:, 0, :])
    S.reduce(lsm[:, 1:2], lamv[:, 2, :])
    S.act(lsm[:, 2:4], lsm[:, 0:2], AF.Exp)
    S.tt("dve", lsm[:, 4:5], lsm[:, 3:4], lsm[:, 2:3], ALU.subtract)
    S.ts("dve", lsm[:, 5:6], lsm[:, 4:5], -LAM_INIT1, None, op0=ALU.add)
    neglam = lsm[:, 5:6]
    junk = S.sbuf("junk_a", [128, D], F32)
    scrq = S.sbuf("scrq", [128, D], F32)
    xin = S.sbuf("xin_a", [128, D], F32)
    st = S.sbuf("st_a", [128, 4], F32)
    st16 = S.sbuf("st16_a", [128, 3, 16], F32)
    for b in range(nb):
        S.push_scope()
        Wqkv = S.sbuf(f"Wqkv{b}", [128, 8, 3 * D], BF16)
        S.dma("sp", Wqkv.v(Wqkv.t[:, :, :].rearrange("p a b -> p (a b)")), G.wbf["Wqkv"][:])
        hT_t = S.sbuf(f"hT_t{b}", [128, 8, 128], BF16)
        outq = S.sbuf(f"outq{b}", [128, D], BF16)
        wk = {"junk": junk, "st": st, "xn": S.sbuf(f"xn_a{b}", [128, D], BF16), "fm32": S.sbuf(f"fm32_a{b}", [128, 8, 128], F32)}
        cs_t = S.sbuf(f"cs_t{b}", [128, 2, 32], F32)
        tmpa = S.sbuf(f"tmpa{b}", [128, 512], F32)
        tmpb = S.sbuf(f"tmpb{b}", [128, 512], F32)

        qs_ = [dict(scrq=scrq, outq=outq, tmpa=tmpa, tmpb=tmpb, st16=st16, junk=junk)]
        qs_.append(dict(scrq=S.sbuf(f"scrq2_{b}", [128, D], F32), outq=S.sbuf(f"outq2_{b}", [128, D], BF16),
                        tmpa=S.sbuf(f"tmpa2_{b}", [128, 512], F32), tmpb=S.sbuf(f"tmpb2_{b}", [128, 512], F32),
                        st16=S.sbuf(f"st16_2_{b}", [128, 3, 16], F32), junk=S.sbuf(f"junk2_{b}", [128, D], F32)))
        wk["junk"] = S.sbuf(f"junkn_{b}", [128, D], F32)

        def qk_part1(X):
            s16, sq, jk = X["st16"], X["scrq"], X["junk"]
            S.act(jk[:], psA[:], AF.Square)
            S.reduce(s16[:, 0, :], jk.v(jk.t[:, :].rearrange("p (g n) -> p g n", n=64)))
            S.ts("dve", s16[:, 1, :], s16[:, 0, :], 1.0 / 64, EPS, op0=ALU.mult, op1=ALU.add)
            S.act(s16[:, 1, :], s16[:, 1, :], AF.Sqrt)
            S.recip(s16[:, 2, :], s16[:, 1, :])
            S.tt("dve", sq.v(sq.t[:, :].rearrange("p (g n) -> p g n", n=64)),
                 psA.v(psA.t[:, :].rearrange("p (g n) -> p g n", n=64)),
                 s16.v(s16.t[:, 2, :].unsqueeze(2).to_broadcast([128, 16, 64])), ALU.mult)

        def qk_part2a(X, gain_bc, rope):
            sq, oq, tmpa_, tmpb_ = X["scrq"], X["outq"], X["tmpa"], X["tmpb"]
            gv = gain_bc.v(gain_bc.t[:, :].unsqueeze(1).to_broadcast([128, 16, 64]))
            if not rope:
                S.tt("pool", oq.v(oq.t[:, :].rearrange("p (g n) -> p g n", n=64)),
                     sq.v(sq.t[:, :].rearrange("p (g n) -> p g n", n=64)), gv, ALU.mult)
                return
            S.tt("pool", sq.v(sq.t[:, :].rearrange("p (g n) -> p g n", n=64)),
                 sq.v(sq.t[:, :].rearrange("p (g n) -> p g n", n=64)), gv, ALU.mult)
            x5 = sq.t[:, :].rearrange("p (g a h f) -> p g a h f", g=16, a=2, h=2)
            o5 = oq.t[:, :].rearrange("p (g a h f) -> p g a h f", g=16, a=2, h=2)
            x1, x2 = sq.v(x5[:, :, :, 0, :]), sq.v(x5[:, :, :, 1, :])
            o1, o2 = oq.v(o5[:, :, :, 0, :]), oq.v(o5[:, :, :, 1, :])
            cv = cs_t.v(cs_t.t[:, 0, :].rearrange("p (a f) -> p a f", a=2).unsqueeze(1).to_broadcast([128, 16, 2, 16]))
            sv = cs_t.v(cs_t.t[:, 1, :].rearrange("p (a f) -> p a f", a=2).unsqueeze(1).to_broadcast([128, 16, 2, 16]))
            ta = tmpa_.v(tmpa_.t[:, :].rearrange("p (g a f) -> p g a f", g=16, a=2))
            tb = tmpb_.v(tmpb_.t[:, :].rearrange("p (g a f) -> p g a f", g=16, a=2))
            S.tt("dve", ta, x1, cv, ALU.mult)
            S.tt("pool", tb, x2, sv, ALU.mult)
            S.tt("dve", o1, ta, tb, ALU.subtract)
            S.tt("dve", ta, x1, sv, ALU.mult)
            S.tt("pool", tb, x2, cv, ALU.mult)
            S.tt("pool", o2, ta, tb, ALU.add)

        def qk_part2b(X, dst_fm):
            oq = X["outq"]
            for ec in range(8):
                S.tr(psT[:, ec * 128:(ec + 1) * 128], oq[:, ec * 128:(ec + 1) * 128], G.ident_b[:])
            S.copy("act", dst_fm, psT.v(psT.t[:, :].rearrange("p (a b) -> p a b", b=128)))

        def proj(c0):
            for n in range(2):
                for kc in range(8):
                    S.mm(psA[:, n * 512:(n + 1) * 512], hT_t[:, kc, :], Wqkv[:, kc, c0 + n * 512:c0 + (n + 1) * 512], start=(kc == 0), stop=(kc == 7))

        def load_norm(ti):
            r = 2 if ti < 2 else b
            S.dma("sp", xin[:], x2_d.v(x2_d.t[b, ti * 128:(ti + 1) * 128, :]), split=4)
            norm_tile_to_fm(S, G, xin[:], r, G.A1, 0, hT_t[:], wk)

        load_norm(0)
        pend_q = None
        for ti in range(NT):
            lat = ti >= 2
            if lat:
                t0 = (ti - 2) * 128
                S.dma("sp", cs_t[:, 0, :], G.rope_cos_d.v(G.rope_cos_d.t[t0:t0 + 128, :]))
                S.dma("sp", cs_t[:, 1, :], G.rope_sin_d.v(G.rope_sin_d.t[t0:t0 + 128, :]))
            proj(D)
            qk_part1(qs_[0])
            if pend_q is not None:
                qk_part2b(qs_[1], pend_q)
                pend_q = None
            proj(2 * D)
            S.copy("act", V_all[:, ti, :, 0:128], psA.v(psA.t[:, :].rearrange("p (h n) -> p h n", n=128)))
            if lat:
                proj(0)
            qk_part2a(qs_[0], gk_bc, lat)
            qk_part2b(qs_[0], KT_all[:, :, ti * 128:(ti + 1) * 128])
            if lat:
                qk_part1(qs_[1])
            if ti + 1 < NT:
                load_norm(ti + 1)
            if lat:
                qk_part2a(qs_[1], gq_bc, True)
                pend_q = QT_all[:, :, t0:t0 + 128]
        if pend_q is not None:
            qk_part2b(qs_[1], pend_q)
        S.pop_scope()
        S.push_scope()
        Wo = S.sbuf(f"Wo_a{b}", [128, 8, D], BF16)
        S.dma("sp", Wo.v(Wo.t[:, :, :].rearrange("p a b -> p (a b)")), G.wbf["daWo"][:])
        gate_bc = S.sbuf(f"gate_a{b}", [128, D], F32)
        S.dma("sp", gate_bc[:], G.gates_d.v(G.gates_d.t[0, b:b + 1, :].partition_broadcast(128)))
        O_all = S.sbuf(f"O_all{b}", [128, 4, D], F32)
        pT = [S.sbuf(f"pT{i}_{b}", [128, 512], BF16) for i in range(3)]
        rec = S.sbuf(f"rec{b}", [128, 8], F32)
        pre = S.sbuf(f"pre_a{b}", [128, D], BF16)
        preT = S.sbuf(f"preT_a{b}", [128, 8, 128], BF16)
        st8 = S.sbuf(f"st8_{b}", [128, 3, 8], F32)
        aitems = [(qt, h, m, kt) for qt in range(nqt) for h in range(nh) for m in range(2) for kt in range(NKT)]

        def emit_qk(i):
            qt, h, m, kt = aitems[i]
            pr = slice(m * 64, m * 64 + 64)
            ps = psQ[i % 3]
            S.mm(ps[:], KT_all[pr, h, kt * 128:(kt + 1) * 128], QT_all[pr, h, qt * 512:(qt + 1) * 512])
            S.act(pT[i % 3][:], ps[:], AF.Exp, scale=0.125)

        def emit_pv(i):
            qt, h, m, kt = aitems[i]
            for qs in range(4):
                acc = psO[qs // 2][:, (qs % 2) * 256:(qs % 2) * 256 + 129]
                S.mm(acc, pT[i % 3][:, qs * 128:(qs + 1) * 128], V_all[:, kt, h, 0:129],
                     start=(kt == 0 and qs % 2 == 0), stop=(kt == NKT - 1), skip_group_check=True)
            if kt != NKT - 1:
                return
            for qs in range(4):
                c0 = (qs % 2) * 256
                S.recip(rec[:, qs:qs + 1], psO[qs // 2][:, c0 + 128:c0 + 129])
                if m == 0:
                    S.ts("dve", O_all[:, qs, h * 128:(h + 1) * 128], psO[qs // 2][:, c0:c0 + 128], rec[:, qs:qs + 1], None, op0=ALU.mult)
                else:
                    S.tt("dve", rec[:, 4 + qs:5 + qs], rec[:, qs:qs + 1], neglam, ALU.mult)
                    S.stt(O_all[:, qs, h * 128:(h + 1) * 128], psO[qs // 2][:, c0:c0 + 128], rec[:, 4 + qs:5 + qs],
                          O_all[:, qs, h * 128:(h + 1) * 128], ALU.mult, ALU.add)
            if not (h == nh - 1 and m == 1):
                return
            for qs in range(4):
                O3 = O_all.v(O_all.t[:, qs, :].rearrange("p (h n) -> p h n", n=128))
                S.act(junk[:], O_all[:, qs, :], AF.Square)
                S.reduce(st8[:, 0, :], junk.v(junk.t[:, :].rearrange("p (h n) -> p h n", n=128)))
                S.ts("dve", st8[:, 1, :], st8[:, 0, :], 1.0 / 128, EPS, op0=ALU.mult, op1=ALU.add)
                S.act(st8[:, 1, :], st8[:, 1, :], AF.Sqrt)
                S.recip(st8[:, 2, :], st8[:, 1, :])
                S.tt("dve", O3, O3, st8.v(st8.t[:, 2, :].unsqueeze(2).to_broadcast([128, 8, 128])), ALU.mult)
                S.tt("pool", pre.v(pre.t[:, :].rearrange("p (h n) -> p h n", n=128)), O3,
                     sg_bc.v(sg_bc.t[:, :].unsqueeze(1).to_broadcast([128, 8, 128])), ALU.mult)
                for ec in range(8):
                    S.tr(psT[:, ec * 128:(ec + 1) * 128], pre[:, ec * 128:(ec + 1) * 128], G.ident_b[:])
                S.copy("act", preT[:], psT.v(psT.t[:, :].rearrange("p (a b) -> p a b", b=128)))
                for n in range(2):
                    for kc in range(8):
                        S.mm(psA[:, n * 512:(n + 1) * 512], preT[:, kc, :], Wo[:, kc, n * 512:(n + 1) * 512], start=(kc == 0), stop=(kc == 7))
                row = (qt * 4 + qs) * 128
                S.dma("sp", xin[:], x2_d.v(x2_d.t[b, CTX + row:CTX + row + 128, :]))
                S.tt("dve", scrq[:], psA[:], gate_bc[:], ALU.mult)
                S.tt("pool", xin[:], xin[:], scrq[:], ALU.add)
                S.dma("sp", x3_d.v(x3_d.t[b, row:row + 128, :]), xin[:])

        SK = 2
        for i in range(len(aitems) + SK):
            if i < len(aitems):
                emit_qk(i)
            if i >= SK:
                emit_pv(i - SK)
        S.pop_scope()
    S.pop_scope()

def build(cfg):
    nc = bass.Bass("TRN2", target_bir_lowering=False)
    S = Sched(nc)
    G = Ctx()
    setup_common(S, G, need=cfg.get("need", ("rwkv", "da", "moe")))
    outs = []
    dbg = {}
    stop = cfg.get("stop", "end")
    kind_x1 = "ExternalOutput" if stop == "rwkv" else "Internal"
    x1_d = S.dram("x1_d", [NB, TOK, D], F32, kind=kind_x1)
    if cfg.get("dbg_hT"):
        dbg["hT"] = S.dram("dbg_hT", [128, 8, LAT + 2], BF16, kind="ExternalOutput")
    wcast_phase(S, G, cfg.get("need", ("rwkv", "da", "moe")))
    if cfg.get("attn_in_ext"):
        G.x2_ext = S.dram("x2_ext", [NB, TOK, D], F32, kind="ExternalInput")
    if cfg.get("moe_in_ext"):
        G.x1_ext = S.dram("x1_ext", [NB, TOK, D], F32, kind="ExternalInput")
    if not cfg.get("skip_l0"):
        mod_phase(S, G, 0)
    if cfg.get("dbg_mod"):
        dm = S.dram("dbg_modT", [128, 48 * 4], F32, kind="ExternalOutput")
        S.dma("sp", dm[:], G.modT.v(G.modT.t[:, :, :].rearrange("p a b -> p (a b)")))
        dg = S.dram("dbg_gates", [2, 3, D], F32, kind="ExternalOutput")
        S.dma("sp", dg[:], G.gates_d[:])
    if stop == "mod":
        S.barrier()
        S.emit()
        return nc, S
    if not cfg.get("skip_rwkv") and not cfg.get("skip_l0"):
        rwkv_phase(S, G, x1_d, dbg=dbg, nb=cfg.get("nb", NB), **cfg.get("rw", {}))
    if stop == "rwkv":
        S.barrier()
        S.emit()
        return nc, S
    x2_d = S.dram("x2_d", [NB, TOK, D], F32, kind="ExternalOutput" if stop == "moe0" else "Internal")
    if not cfg.get("skip_l0"):
        moe0 = True
    else:
        moe0 = False
    tiles0 = [(b, ti * 128, 2 if ti < 2 else b) for b in range(NB) for ti in range(NT)]
    if cfg.get("dbg_gates"):
        dbg["gates"] = S.dram("dbg_gates32", [12, 128, 32], F32, kind="ExternalOutput")
    if moe0:
      moe_phase(S, G, 0, x1_d if not cfg.get("moe_in_ext") else G.x1_ext, tiles0[:cfg.get("moe_ntiles", len(tiles0))],
                lambda b, row0: x2_d.v(x2_d.t[b, row0:row0 + 128, :]), cfg.get("st0", 12), npairs=cfg.get("npairs", 16), dbg=dbg)
    if stop == "moe0":
        S.barrier()
        S.emit()
        return nc, S
    x3_d = S.dram("x3_d", [NB, LAT, D], F32, kind="ExternalOutput" if stop == "attn" else "Internal")
    mod_phase(S, G, 1)
    attn_phase(S, G, x2_d if not cfg.get("attn_in_ext") else G.x2_ext, x3_d, **cfg.get("at", {}))
    if stop == "attn":
        S.barrier()
        S.emit()
        return nc, S
    out_d = S.dram("out", [NB, LAT, D], F32, kind="ExternalOutput")
    tiles1 = [(b, ti * 128, b) for b in range(NB) for ti in range(LAT // 128)]
    moe_phase(S, G, 1, x3_d, tiles1, lambda b, row0: out_d.v(out_d.t[b, row0:row0 + 128, :]), cfg.get("st1", 8))
    S.barrier()
    S.emit()
    return nc, S


def prep_core_inputs(inputs, core, consts):
    b0 = core * NB
    f = lambda a: np.ascontiguousarray(a, dtype=np.float32)
    m = {}
    m["x"] = f(inputs["x"][b0:b0 + NB])
    m["ctx"] = f(inputs["ctx"][b0:b0 + NB])
    m["c3"] = f(np.concatenate([inputs["c"][b0:b0 + NB], inputs["c_ctx"][None, :]], axis=0))
    pv = np.concatenate([
        inputs["norm1_g"].reshape(16, 128), inputs["norm2_g"].reshape(16, 128),
        inputs["rw_mu"][0].reshape(48, 128), inputs["b_mod"].reshape(96, 128)], axis=0)
    m["pvec"] = f(pv)
    m["w_mod"] = f(inputs["w_mod"])
    m["b_mod"] = f(inputs["b_mod"])
    for k, v in consts.items():
        m[k] = v
    m["rw_w_rkv"] = f(inputs["rw_w_rkv"][0])
    for k in ("rw_w0", "rw_w1", "rw_w2", "rw_a0", "rw_a1", "rw_a2", "rw_g1", "rw_g2", "rw_w_o"):
        m[k] = f(inputs[k][0])
    for k in ("rw_k_k", "rw_k_a", "rw_lnx_g", "rw_lnx_b"):
        m[k] = f(inputs[k][0].reshape(1, D))
    m["rw_r_k"] = f(inputs["rw_r_k"][0].reshape(1, D))
    m["da_w_qkv"] = f(inputs["da_w_qkv"][0])
    m["da_q_norm_g"] = f(inputs["da_q_norm_g"][0].reshape(1, 64))
    m["da_k_norm_g"] = f(inputs["da_k_norm_g"][0].reshape(1, 64))
    m["da_lam"] = f(np.stack([inputs["da_lam_q1"][0], inputs["da_lam_k1"][0], inputs["da_lam_q2"][0], inputs["da_lam_k2"][0]]))
    m["da_subln_g"] = f(inputs["da_subln_g"][0].reshape(1, 128))
    m["da_w_o"] = f(inputs["da_w_o"][0])
    rt = np.concatenate([inputs["moe_router_g"], np.transpose(inputs["moe_router_e"], (0, 2, 1, 3)).reshape(2, D, 32)], axis=2)
    m["moe_router"] = f(rt)
    rb = np.concatenate([inputs["moe_router_g_b"], inputs["moe_router_e_b"].reshape(2, 32)], axis=1).reshape(2, 1, 36)
    m["moe_router_b"] = f(rb)
    m["moe_w_gate"] = f(inputs["moe_w_gate"]).reshape(2, NEXP, D, DFF)
    m["moe_w_up"] = f(inputs["moe_w_up"]).reshape(2, NEXP, D, DFF)
    m["moe_w_down"] = f(inputs["moe_w_down"]).reshape(2, NEXP, DFF, D)
    return m


_CACHE = {}


def kernel(**inputs):
    from concourse.bass_utils import run_bass_kernel_spmd
    n = 8
    if "nc" not in _CACHE:
        _CACHE["nc"] = build({})[0]
        _CACHE["consts"] = host_consts()
    nc = _CACHE["nc"]
    consts = _CACHE["consts"]
    inputs = {k: np.asarray(v) for k, v in inputs.items()}
    in_maps = [prep_core_inputs(inputs, c, consts) for c in range(n)]
    res = run_bass_kernel_spmd(nc, in_maps, core_ids=list(range(n)))
    out = np.concatenate([r["out"] for r in res.results], axis=0)
    return out.astype(np.float32)
```
